# Optimizing a Trainium2 kernel written in Bass

```python
import math
import jax
import jax.numpy as jnp
from jax import lax
import numpy as np

D_MODEL = 1024
BATCH = 8
SEQ = 2048
DEPTH = 2

N_MIXERS = 2
N_CONV_LAYERS = (DEPTH + 1) // 2
N_DELTA_LAYERS = DEPTH // 2

RMS_EPS = 1e-6
LN_EPS = 1e-5

CONV_WIDTH = 31

N_HEADS = 8
HEAD_DIM = D_MODEL // N_HEADS
KEY_DIM = N_HEADS * HEAD_DIM
VAL_DIM = N_HEADS * HEAD_DIM
QKV_DIM = 2 * KEY_DIM + VAL_DIM
N_DIRS = 2
IN_PROJ_DIM = QKV_DIM + VAL_DIM + 2 * N_DIRS * N_HEADS
SHORT_CONV = 5
CHUNK = 64

D_FF = ((8 * D_MODEL // 3 + 127) // 128) * 128
N_EXPERTS = 8
TOP_K = 2
D_FF_EXPERT = 7 * D_MODEL // 2

kernel_name = "hybrid_conformer_gdn_moe_encoder"


def rmsnorm(x, g):
    xf = x.astype(jnp.float32)
    y = xf * lax.rsqrt(jnp.mean(xf * xf, axis=-1, keepdims=True) + RMS_EPS)
    return (y * g.astype(jnp.float32)).astype(x.dtype)


def layernorm(x, g, b):
    xf = x.astype(jnp.float32)
    mu = jnp.mean(xf, axis=-1, keepdims=True)
    xc = xf - mu
    y = xc * lax.rsqrt(jnp.mean(xc * xc, axis=-1, keepdims=True) + LN_EPS)
    return (y * g.astype(jnp.float32) + b.astype(jnp.float32)).astype(x.dtype)


def depthwise_conv(x, w):
    width, chans = w.shape
    return lax.conv_general_dilated(
        x, w[:, None, :].astype(x.dtype), window_strides=(1,),
        padding=[(width // 2, width // 2)],
        dimension_numbers=("NWC", "WIO", "NWC"),
        feature_group_count=chans)


def conformer_conv_mixer(h, pw1_w, pw1_b, dw_w, dw_b, ln_g, ln_b, pw2_w, pw2_b):
    u = h @ pw1_w + pw1_b
    a, b = jnp.split(u, 2, axis=-1)
    u = a * jax.nn.sigmoid(b)
    u = depthwise_conv(u, dw_w) + dw_b
    u = jax.nn.silu(layernorm(u, ln_g, ln_b))
    return u @ pw2_w + pw2_b


def l2norm(t):
    return t * lax.rsqrt(jnp.sum(t * t, axis=-1, keepdims=True) + 1e-6)


def chunk_gated_delta(q, k, v, log_a, beta):
    B, H, S, dk = q.shape
    dv = v.shape[-1]
    C = CHUNK
    N = S // C
    q = q.reshape(B, H, N, C, dk)
    k = k.reshape(B, H, N, C, dk)
    v = v.reshape(B, H, N, C, dv)
    g = jnp.cumsum(log_a.reshape(B, H, N, C), axis=-1)
    bt = beta.reshape(B, H, N, C)[..., None]
    kb = k * bt
    vb = v * bt
    incl = jnp.tril(jnp.ones((C, C), dtype=bool))
    strict = jnp.tril(jnp.ones((C, C), dtype=bool), -1)
    diff = g[..., :, None] - g[..., None, :]
    decay = jnp.where(incl, jnp.exp(jnp.where(incl, diff, 0.0)), 0.0)
    lower = jnp.where(strict, jnp.einsum("bhnid,bhnjd->bhnij", kb, k) * decay, 0.0)
    eg = jnp.exp(g)[..., None]
    rhs = jnp.concatenate([vb, kb * eg], axis=-1)
    sol = lax.linalg.triangular_solve(lower + jnp.eye(C, dtype=lower.dtype), rhs,
                                      left_side=True, lower=True, unit_diagonal=True)
    w_val = sol[..., :dv]
    k_cum = sol[..., dv:]
    p_intra = jnp.einsum("bhnid,bhnjd->bhnij", q, k) * decay
    q_dec = q * eg
    g_last = g[..., -1]
    k_dec = k * jnp.exp(g_last[..., None] - g)[..., None]

    def step(state, xs):
        w_n, kc_n, p_n, qd_n, kd_n, gl_n = xs
        u = w_n - jnp.einsum("bhcd,bhdv->bhcv", kc_n, state)
        o = jnp.einsum("bhcd,bhdv->bhcv", qd_n, state) + jnp.einsum("bhij,bhjv->bhiv", p_n, u)
        state = state * jnp.exp(gl_n)[..., None, None] + jnp.einsum("bhcd,bhcv->bhdv", kd_n, u)
        return state, o

    xs = tuple(jnp.moveaxis(t, 2, 0) for t in (w_val, k_cum, p_intra, q_dec, k_dec, g_last))
    state0 = jnp.zeros((B, H, dk, dv), jnp.float32)
    _, o = lax.scan(step, state0, xs)
    return jnp.moveaxis(o, 0, 2).reshape(B, H, S, dv)


def gated_deltanet_mixer(h, w_in, conv_w, a_log, dt_bias, o_norm, w_out):
    B, S, _ = h.shape
    H, dh = N_HEADS, HEAD_DIM
    proj = h @ w_in
    qkv = proj[..., :QKV_DIM]
    z = proj[..., QKV_DIM:QKV_DIM + VAL_DIM]
    gates = proj[..., QKV_DIM + VAL_DIM:].astype(jnp.float32)
    b_raw = gates[..., :N_DIRS * H].reshape(B, S, N_DIRS, H)
    a_raw = gates[..., N_DIRS * H:].reshape(B, S, N_DIRS, H)
    qkv = jax.nn.silu(depthwise_conv(qkv, conv_w)).astype(jnp.float32)

    def heads(t):
        return t.reshape(B, S, H, dh).transpose(0, 2, 1, 3)

    q = l2norm(heads(qkv[..., :KEY_DIM])) * (dh ** -0.5)
    k = l2norm(heads(qkv[..., KEY_DIM:2 * KEY_DIM]))
    v = heads(qkv[..., 2 * KEY_DIM:])
    beta = jax.nn.sigmoid(b_raw).transpose(2, 0, 3, 1)
    log_a = (-jnp.exp(a_log.astype(jnp.float32))
             * jax.nn.softplus(a_raw + dt_bias.astype(jnp.float32))).transpose(2, 0, 3, 1)

    def flip(t):
        return jnp.flip(t, axis=2)

    o_fwd = chunk_gated_delta(q, k, v, log_a[0], beta[0])
    o_bwd = flip(chunk_gated_delta(flip(q), flip(k), flip(v), flip(log_a[1]), flip(beta[1])))
    o = (o_fwd + o_bwd).transpose(0, 2, 1, 3)
    o = o * lax.rsqrt(jnp.mean(o * o, axis=-1, keepdims=True) + RMS_EPS) * o_norm.astype(jnp.float32)
    o = o * jax.nn.silu(z.astype(jnp.float32).reshape(B, S, H, dh))
    return o.reshape(B, S, VAL_DIM).astype(h.dtype) @ w_out


def swiglu(h, w_gate, w_up, w_down):
    return (jax.nn.silu(h @ w_gate) * (h @ w_up)) @ w_down


def moe_swiglu(h, router, e_gate, e_up, e_down):
    B, S, D = h.shape
    t = h.reshape(B * S, D)
    logits = (t @ router).astype(jnp.float32)
    top_val, top_idx = lax.top_k(logits, TOP_K)
    top_w = jax.nn.softmax(top_val, axis=-1)
    gate = jnp.sum(jax.nn.one_hot(top_idx, N_EXPERTS, dtype=jnp.float32) * top_w[..., None], axis=1)
    out = jnp.zeros((B * S, D), jnp.float32)
    for e in range(N_EXPERTS):
        out = out + gate[:, e:e + 1] * swiglu(t, e_gate[e], e_up[e], e_down[e]).astype(jnp.float32)
    return out.astype(h.dtype).reshape(B, S, D)


def setup_inputs(seed: int = 0) -> dict:
    key = jax.random.key(seed)
    ks = iter(jax.random.split(key, 40))
    D = D_MODEL
    nc, nd = N_CONV_LAYERS, N_DELTA_LAYERS

    def nrm(shape, scale):
        return jax.random.normal(next(ks), shape, jnp.float32) * scale

    x = nrm((BATCH, SEQ, D), 1.0)
    mix_norm = 1.0 + nrm((DEPTH, D), 0.01)
    ffn_norm = 1.0 + nrm((DEPTH, D), 0.01)
    cf_pw1_w = nrm((nc, D, 2 * D), D ** -0.5)
    cf_pw1_b = nrm((nc, 2 * D), 0.01)
    cf_dw_w = nrm((nc, CONV_WIDTH, D), CONV_WIDTH ** -0.5)
    cf_dw_b = nrm((nc, D), 0.01)
    cf_ln_g = 1.0 + nrm((nc, D), 0.01)
    cf_ln_b = nrm((nc, D), 0.01)
    cf_pw2_w = nrm((nc, D, D), D ** -0.5)
    cf_pw2_b = nrm((nc, D), 0.01)
    ffn_w_gate = nrm((nc, D, D_FF), D ** -0.5)
    ffn_w_up = nrm((nc, D, D_FF), D ** -0.5)
    ffn_w_down = nrm((nc, D_FF, D), D_FF ** -0.5)
    gdn_w_in = nrm((nd, D, IN_PROJ_DIM), D ** -0.5)
    gdn_conv_w = nrm((nd, SHORT_CONV, QKV_DIM), SHORT_CONV ** -0.5)
    gdn_a_log = jnp.log(jax.random.uniform(next(ks), (nd, N_DIRS, N_HEADS), jnp.float32, 1.0, 16.0))
    dt = jnp.exp(jax.random.uniform(next(ks), (nd, N_DIRS, N_HEADS), jnp.float32,
                                    math.log(1e-3), math.log(1e-1)))
    gdn_dt_bias = dt + jnp.log(-jnp.expm1(-dt))
    gdn_o_norm = 1.0 + nrm((nd, HEAD_DIM), 0.01)
    gdn_w_out = nrm((nd, VAL_DIM, D), VAL_DIM ** -0.5)
    moe_router = nrm((nd, D, N_EXPERTS), D ** -0.5)
    moe_w_gate = nrm((nd, N_EXPERTS, D, D_FF_EXPERT), D ** -0.5)
    moe_w_up = nrm((nd, N_EXPERTS, D, D_FF_EXPERT), D ** -0.5)
    moe_w_down = nrm((nd, N_EXPERTS, D_FF_EXPERT, D), D_FF_EXPERT ** -0.5)
    final_norm = 1.0 + nrm((D,), 0.01)
    return {
        "x": x, "mix_norm": mix_norm, "ffn_norm": ffn_norm,
        "cf_pw1_w": cf_pw1_w, "cf_pw1_b": cf_pw1_b, "cf_dw_w": cf_dw_w, "cf_dw_b": cf_dw_b,
        "cf_ln_g": cf_ln_g, "cf_ln_b": cf_ln_b, "cf_pw2_w": cf_pw2_w, "cf_pw2_b": cf_pw2_b,
        "ffn_w_gate": ffn_w_gate, "ffn_w_up": ffn_w_up, "ffn_w_down": ffn_w_down,
        "gdn_w_in": gdn_w_in, "gdn_conv_w": gdn_conv_w, "gdn_a_log": gdn_a_log,
        "gdn_dt_bias": gdn_dt_bias, "gdn_o_norm": gdn_o_norm, "gdn_w_out": gdn_w_out,
        "moe_router": moe_router, "moe_w_gate": moe_w_gate, "moe_w_up": moe_w_up,
        "moe_w_down": moe_w_down, "final_norm": final_norm,
    }


def reference(x, mix_norm, ffn_norm, cf_pw1_w, cf_pw1_b, cf_dw_w, cf_dw_b, cf_ln_g, cf_ln_b,
              cf_pw2_w, cf_pw2_b, ffn_w_gate, ffn_w_up, ffn_w_down, gdn_w_in, gdn_conv_w,
              gdn_a_log, gdn_dt_bias, gdn_o_norm, gdn_w_out, moe_router, moe_w_gate, moe_w_up,
              moe_w_down, final_norm):
    for i in range(DEPTH):
        j = i // N_MIXERS
        h = rmsnorm(x, mix_norm[i])
        if i % N_MIXERS == 0:
            x = x + conformer_conv_mixer(h, cf_pw1_w[j], cf_pw1_b[j], cf_dw_w[j], cf_dw_b[j],
                                         cf_ln_g[j], cf_ln_b[j], cf_pw2_w[j], cf_pw2_b[j])
        else:
            x = x + gated_deltanet_mixer(h, gdn_w_in[j], gdn_conv_w[j], gdn_a_log[j],
                                         gdn_dt_bias[j], gdn_o_norm[j], gdn_w_out[j])
        h = rmsnorm(x, ffn_norm[i])
        if i % 2 == 0:
            x = x + swiglu(h, ffn_w_gate[j], ffn_w_up[j], ffn_w_down[j])
        else:
            x = x + moe_swiglu(h, moe_router[j], moe_w_gate[j], moe_w_up[j], moe_w_down[j])
    return rmsnorm(x, final_norm)
```

```python
import contextlib
import numpy as np
import concourse.bass as bass
import concourse.mybir as mybir
from concourse.bass_utils import run_bass_kernel_spmd

F32 = mybir.dt.float32
BF16 = mybir.dt.bfloat16
I32 = mybir.dt.int32
AF = mybir.ActivationFunctionType
ALU = mybir.AluOpType

D = 1024
S = 2048
NCH = D // 128
TT = 512
NTT = S // TT
ENGS = ("sp", "pe", "act", "dve", "pool")


class Tok:
    __slots__ = ("name", "w", "readers")

    def __init__(self, name):
        self.name = name
        self.w = None
        self.readers = []


class Op:
    __slots__ = ("eng", "fn", "deps", "chan", "has_dep", "sig", "name")

    def __init__(self, eng, fn, chan, name):
        self.eng = eng
        self.fn = fn
        self.deps = []
        self.chan = chan
        self.has_dep = False
        self.sig = None
        self.name = name


class Sched:
    def __init__(self):
        self.ops = {e: [] for e in ENGS}
        self.nchan = 0

    def new_chan(self):
        self.nchan += 1
        return self.nchan - 1

    def last_ops(self, engs=("pe", "act", "dve", "pool")):
        return [self.ops[e][-1] for e in engs if self.ops[e]]

    def op(self, eng, fn, reads=(), writes=(), chan=None, name="", after=()):
        o = Op(eng, fn, chan, name)
        for d in after:
            o.deps.append(d)
            if d.chan is None:
                d.has_dep = True
        cand = []
        for t in reads:
            if t.w is not None:
                cand.append((t.w, "raw"))
        for t in writes:
            if t.w is not None:
                cand.append((t.w, "waw"))
            for r in t.readers:
                cand.append((r, "war"))
        seen = set()
        for d, kind in cand:
            if d is o or id(d) in seen:
                continue
            same = (d.eng == eng and d.chan is None and chan is None)
            if same and (eng == "pe" or kind == "war"):
                continue
            seen.add(id(d))
            o.deps.append(d)
            d.has_dep = True
        for t in reads:
            t.readers.append(o)
        for t in writes:
            t.w = o
            t.readers = []
        self.ops[eng].append(o)
        return o

    def barrier(self, engs=("pe", "act", "dve", "pool")):
        last = {e: self.ops[e][-1] for e in engs if self.ops[e]}
        for e in engs:
            o = Op(e, lambda eng: eng.nop(), None, "barrier")
            for e2, l in last.items():
                if e2 != e:
                    o.deps.append(l)
                    if l.chan is None:
                        l.has_dep = True
            self.ops[e].append(o)

    def emit(self, nc, final_wait_ops=()):
        with contextlib.ExitStack() as es:
            EPOCH = 1000
            nsig = {e: sum(1 for o in self.ops[e] if o.chan is None and o.has_dep) for e in ENGS}
            esem = {e: [es.enter_context(nc.semaphore("s_%s%d" % (e, i)))
                        for i in range(nsig[e] // EPOCH + 1)] for e in ENGS}
            for e in ENGS:
                cnt = 0
                for o in self.ops[e]:
                    if o.chan is None and o.has_dep:
                        o.sig = (esem[e][cnt // EPOCH], cnt % EPOCH + 1, 1)
                        cnt += 1
            CEP = 100
            ccnt = [0] * self.nchan
            ntot = [0] * self.nchan
            for e in ENGS:
                for o in self.ops[e]:
                    if o.chan is not None:
                        ntot[o.chan] += 1
            csem = [[es.enter_context(nc.semaphore("c_%d_%d" % (i, k)))
                     for k in range(ntot[i] // CEP + 1)] for i in range(self.nchan)]
            for e in ENGS:
                for o in self.ops[e]:
                    if o.chan is not None:
                        k = ccnt[o.chan]
                        o.sig = (csem[o.chan][k // CEP], (k % CEP + 1) * 16, 16)
                        ccnt[o.chan] += 1
            block = es.enter_context(nc.Block())
            ops = self.ops

            def run(engname, eobj):
                waited = {}
                for o in ops[engname]:
                    need = {}
                    for d in o.deps:
                        sem, val, _ = d.sig
                        k = id(sem)
                        if val > waited.get(k, 0) and val > need.get(k, (None, 0))[1]:
                            need[k] = (sem, val)
                    for k, (sem, val) in need.items():
                        eobj.wait_ge(sem, val)
                        waited[k] = val
                    inst = o.fn(eobj)
                    if o.sig is not None:
                        inst.then_inc(o.sig[0], o.sig[2])

            @block.sync
            def _(e):
                run("sp", e)

            @block.tensor
            def _(e):
                run("pe", e)

            @block.scalar
            def _(e):
                run("act", e)

            @block.vector
            def _(e):
                run("dve", e)

            @block.gpsimd
            def _(e):
                run("pool", e)


VR = dict(mix0=0, mix1=1, ffn0=2, ffn1=3, pw1_ba=4, pw1_bb=5, dw_b=6, ln_g=7, ln_b=8, pw2_b=9,
          dw_w=10, gconv=41)
DFF = 2816
DFFE = 3584
WB = 2048


def build_program(phases=("c0", "f0", "g1", "m1"), dbg=0, heads=tuple(range(8)), two_x=False):
    nc = bass.Bass("TRN2", target_bir_lowering=False, dynamic_dma_scratch_size=2048)

    def din(name, shape):
        return nc.dram_tensor(name, shape, F32, kind="ExternalInput").ap()

    x_d = din("x", [S, D])
    vecs_d = din("vecs", [128, D])
    fng_d = din("fng", [1, D])
    out_d = nc.dram_tensor("out", [S, D], F32, kind="ExternalOutput").ap()
    xacc_d = din("xacc", [S, D]) if two_x else None
    if "c0" in phases:
        w1_d = din("cf_pw1_w", [D, 2 * D])
        w2_d = din("cf_pw2_w", [D, D])
    if "f0" in phases:
        fg_d = din("ffn_w_gate", [D, DFF])
        fu_d = din("ffn_w_up", [D, DFF])
        fd_d = din("ffn_w_down", [DFF, D])

    if "g1" in phases:
        gin_d = din("gdn_w_in", [D, 4128])
        gout_d = din("gdn_w_out", [D, D])
        gsm_d = din("gsmall", [1, 160])
    if "m1" in phases:
        rt_d = din("moe_router", [D, 8])
        mg_d = din("moe_w_gate", [8, D, DFFE])
        mu_d = din("moe_w_up", [8, D, DFFE])
        md_d = din("moe_w_down", [8, DFFE, D])

    sc = Sched()
    with contextlib.ExitStack() as es:
        uniq = [0]

        def sb(name, shape, dt, stack=es):
            uniq[0] += 1
            return stack.enter_context(nc.sbuf_tensor("%s_%d" % (name, uniq[0]), shape, dt))

        xT = sb("xT", [128, NCH, S], F32)
        ident = sb("ident", [128, 128], F32)
        ones_f = sb("ones_f", [128, 128], F32)
        ones_b = sb("ones_b", [128, 128], BF16)
        vecs = sb("vecs_sb", [128, NCH, 64], F32)
        ps = es.enter_context(nc.psum_tensor("ps", [128, 8, 512], F32))
        NST, NWB = 2, 6
        stage = [sb("stage%d" % i, [128, WB], F32) for i in range(NST)]
        wbf = [sb("wbf%d" % i, [128, WB], BF16) for i in range(NWB)]
        cst_d = din("gconst", [128, 2048]) if "g1" in phases else None
        sm = [sb("sm%d" % i, [128, TT], F32) for i in range(4)]
        t_sm = [Tok("sm%d" % i) for i in range(4)]
        smn = [0]

        def next_sm():
            i = smn[0] % 4
            smn[0] += 1
            return i

        t_xT = [[Tok("xT%d_%d" % (c, t)) for t in range(S // 128)] for c in range(NCH)]

        def xtoks(c, t512):
            return [t_xT[c][t512 * 4 + i] for i in range(4)]
        t_ident = Tok("ident")
        t_ones = Tok("ones")
        t_vecs = Tok("vecs")
        t_fng = Tok("fng")
        t_ps = [Tok("ps%d" % i) for i in range(8)]
        t_stage = [Tok("stage%d" % i) for i in range(NST)]
        t_wbf = [Tok("wbf%d" % i) for i in range(NWB)]
        ch_stage = [sc.new_chan() for _ in range(NST)]
        ch_xin = [sc.new_chan(), sc.new_chan()]
        ch_misc = sc.new_chan()
        ch_out = [sc.new_chan(), sc.new_chan()]
        psn = [0]
        stn = [0]
        wbn = [0]

        def next_ps():
            i = psn[0] % 8
            psn[0] += 1
            return i

        def load_block(src, view, cast_eng="pool"):
            a, b = src.shape[1], src.shape[2]
            n = a * b
            s = stn[0] % NST
            stn[0] += 1
            k = wbn[0] % NWB
            wbn[0] += 1
            sc.op("sp", lambda e: e.dma_start(
                out=stage[s][:, 0:n].rearrange("p (a b) -> p a b", a=a), in_=src),
                writes=[t_stage[s]], chan=ch_stage[s])
            sc.op(cast_eng, lambda e: e.tensor_copy(out=wbf[k][:, 0:n], in_=stage[s][:, 0:n]),
                  reads=[t_stage[s]], writes=[t_wbf[k]])
            return wbf[k][:, 0:n].rearrange("p (a b) -> p a b", a=a), t_wbf[k]

        sc.op("pool", lambda e: e.memset(ones_f[:], 1.0), writes=[t_ones])
        sc.op("pool", lambda e: e.memset(ones_b[:], 1.0), writes=[t_ones])
        sc.op("pool", lambda e: e.affine_select(out=ident[:], in_=ones_f[:], pattern=[[-1, 128]],
                                                compare_op=ALU.is_equal, fill=0.0, base=0,
                                                channel_multiplier=1),
              reads=[t_ones], writes=[t_ident])

        def load_x_loop(src_d, xin, t_xin, fence=()):
            for tt in range(S // 128):
                sl = tt % 2
                sc.op("sp", lambda e, tt=tt, sl=sl: e.dma_start(
                    out=xin[sl][:], in_=src_d[tt * 128:(tt + 1) * 128, :]),
                    writes=[t_xin[sl]], chan=ch_xin[sl], after=fence)
                for half in range(2):
                    b = next_ps()

                    def f(e, half=half, b=b, sl=sl):
                        for j in range(4):
                            c = half * 4 + j
                            r = e.transpose(out=ps[:, b, j * 128:(j + 1) * 128],
                                            in_=xin[sl][:, c * 128:(c + 1) * 128],
                                            identity=ident[:])
                        return r
                    sc.op("pe", f, reads=[t_xin[sl], t_ident], writes=[t_ps[b]])
                    eng = "dve" if half == 0 else "act"

                    def g(e, half=half, b=b, tt=tt, eng=eng):
                        o = xT[:, half * 4:half * 4 + 4, tt * 128:(tt + 1) * 128]
                        i = ps[:, b, :].rearrange("p (j r) -> p j r", j=4)
                        if eng == "dve":
                            return e.tensor_copy(out=o, in_=i)
                        return e.activation(out=o, in_=i, func=AF.Copy)
                    sc.op(eng, g, reads=[t_ps[b]],
                          writes=[t_xT[half * 4 + j][tt] for j in range(4)])


        with contextlib.ExitStack() as es1:
            xin = [sb("xin%d" % i, [128, D], F32, es1) for i in range(2)]
            t_xin = [Tok("xin0"), Tok("xin1")]
            sc.op("sp", lambda e: e.dma_start(out=xin[1][:], in_=vecs_d[:, :]), writes=[t_xin[1]],
                  chan=ch_xin[1])
            for half in range(2):
                b = next_ps()

                def f(e, half=half, b=b):
                    for j in range(4):
                        c = half * 4 + j
                        r = e.transpose(out=ps[:, b, j * 128:(j + 1) * 128],
                                        in_=xin[1][:, c * 128:(c + 1) * 128], identity=ident[:])
                    return r
                sc.op("pe", f, reads=[t_xin[1], t_ident], writes=[t_ps[b]])
                sc.op("dve", lambda e, half=half, b=b: e.tensor_copy(
                    out=vecs[:, half * 4:half * 4 + 4, :],
                    in_=ps[:, b, :].rearrange("p (j r) -> p j r", j=4)[:, :, 0:64]),
                    reads=[t_ps[b]], writes=[t_vecs])
            load_x_loop(x_d, xin, t_xin)
        sc.barrier()

        def vcol(c, r):
            return vecs[:, c, r:r + 1]

        def rmsnorm_fm(hT, t_hT, grow, sqtmp, t_sq):
            for t in range(NTT):
                tsl = slice(t * TT, (t + 1) * TT)
                sc.op("act", lambda e, tsl=tsl: e.activation(out=sqtmp[:], in_=xT[:, :, tsl],
                                                             func=AF.Square),
                      reads=[tk for c in range(NCH) for tk in xtoks(c, t)], writes=[t_sq])
                b = next_ps()

                def f(e, b=b):
                    for c in range(NCH):
                        r = e.matmul(ps[:, b, :], lhsT=ones_b[:], rhs=sqtmp[:, c, :],
                                     start=(c == 0), stop=(c == NCH - 1))
                    return r
                sc.op("pe", f, reads=[t_sq, t_ones], writes=[t_ps[b]])
                s0 = next_sm()
                sc.op("act", lambda e, b=b, s0=s0: e.activation(out=sm[s0][:], in_=ps[:, b, :],
                                                                func=AF.Sqrt, scale=1.0 / D,
                                                                bias=1e-6),
                      reads=[t_ps[b]], writes=[t_sm[s0]])
                s1 = next_sm()
                sc.op("dve", lambda e, s0=s0, s1=s1: e.reciprocal(out=sm[s1][:], in_=sm[s0][:]),
                      reads=[t_sm[s0]], writes=[t_sm[s1]])
                for c in range(NCH):
                    sc.op("dve", lambda e, c=c, tsl=tsl, s1=s1: e.scalar_tensor_tensor(
                        out=hT[:, c, tsl], in0=xT[:, c, tsl], scalar=vcol(c, grow), in1=sm[s1][:],
                        op0=ALU.mult, op1=ALU.mult),
                        reads=xtoks(c, t) + [t_vecs, t_sm[s1]], writes=[t_hT[c][t]])

        def dump(src_fn, toks_fn):
            for c in range(NCH):
                for t in range(NTT):
                    tsl = slice(t * TT, (t + 1) * TT)
                    sc.op("dve", lambda e, c=c, tsl=tsl: e.tensor_copy(out=xT[:, c, tsl],
                                                                       in_=src_fn(c, tsl)),
                          reads=toks_fn(c, t), writes=xtoks(c, t))

        def phase_conformer():
            with contextlib.ExitStack() as esp:
                hT = sb("hT", [128, NCH, S], BF16, esp)
                t_hT = [[Tok("hT%d_%d" % (c, t)) for t in range(NTT)] for c in range(NCH)]
                sqtmp = sb("sqtmp", [128, NCH, TT], BF16, esp)
                t_sq = Tok("sqtmp")
                U = sb("ubuf", [128, NCH, S + 30], BF16, esp)
                t_U = [Tok("u%d" % c) for c in range(NCH)]
                diag = sb("diag", [128, 31, 128], BF16, esp)
                t_diag = Tok("diag")
                sig = [sb("sig%d" % i, [128, TT], F32, esp) for i in range(2)]
                t_sig = [Tok("sig0"), Tok("sig1")]
                lnst = [sb("lnst%d" % i, [128, TT], F32, esp) for i in range(3)]
                t_lnst = [Tok("lnst%d" % i) for i in range(3)]
                rmsnorm_fm(hT, t_hT, VR["mix0"], sqtmp, t_sq)
                if dbg == 1:
                    dump(lambda c, tsl: hT[:, c, tsl], lambda c, t: [t_hT[c][t]])
                    return
                sc.op("pool", lambda e: e.memset(U[:], 0.0), writes=t_U)
                sgn = 0
                for h in range(2):
                    wa = [load_block(w1_d[:, h * 512 + q * 256: h * 512 + (q + 1) * 256]
                                     .rearrange("(kc p) n -> p kc n", p=128), None) for q in range(2)]
                    wb_ = [load_block(w1_d[:, D + h * 512 + q * 256: D + h * 512 + (q + 1) * 256]
                                      .rearrange("(kc p) n -> p kc n", p=128), None) for q in range(2)]
                    for jj in range(4):
                        j = h * 4 + jj
                        wA, tA = wa[jj // 2]
                        wB, tB = wb_[jj // 2]
                        co = (jj % 2) * 128
                        for t in range(NTT):
                            tsl = slice(t * TT, (t + 1) * TT)
                            bA = next_ps()
                            bB = next_ps()

                            def f(e, wA=wA, wB=wB, co=co, bA=bA, bB=bB, tsl=tsl):
                                for kc in range(NCH):
                                    e.matmul(ps[:, bA, :], lhsT=wA[:, kc, co:co + 128],
                                             rhs=hT[:, kc, tsl], start=(kc == 0),
                                             stop=(kc == NCH - 1))
                                for kc in range(NCH):
                                    r = e.matmul(ps[:, bB, :], lhsT=wB[:, kc, co:co + 128],
                                                 rhs=hT[:, kc, tsl], start=(kc == 0),
                                                 stop=(kc == NCH - 1))
                                return r
                            sc.op("pe", f, reads=[tA, tB] + [t_hT[c][t] for c in range(NCH)],
                                  writes=[t_ps[bA], t_ps[bB]])
                            sg = sgn % 2
                            sgn += 1
                            sc.op("act", lambda e, bB=bB, sg=sg, j=j: e.activation(
                                out=sig[sg][:], in_=ps[:, bB, :], func=AF.Sigmoid,
                                bias=vcol(j, VR["pw1_bb"])),
                                reads=[t_ps[bB], t_vecs], writes=[t_sig[sg]])
                            sc.op("dve", lambda e, bA=bA, sg=sg, j=j, t=t: e.scalar_tensor_tensor(
                                out=U[:, j, 15 + t * TT:15 + (t + 1) * TT], in0=ps[:, bA, :],
                                scalar=vcol(j, VR["pw1_ba"]), in1=sig[sg][:],
                                op0=ALU.add, op1=ALU.mult),
                                reads=[t_ps[bA], t_sig[sg], t_vecs], writes=[t_U[j]])
                if dbg == 2:
                    dump(lambda c, tsl: U[:, c, 15 + tsl.start:15 + tsl.stop], lambda c, t: [t_U[c]])
                    return
                for j in range(NCH):
                    sc.op("dve", lambda e, j=j: e.tensor_tensor(
                        out=diag[:], in0=ident[:].unsqueeze(1).to_broadcast([128, 31, 128]),
                        in1=vecs[:, j, VR["dw_w"]:VR["dw_w"] + 31].unsqueeze(2)
                        .to_broadcast([128, 31, 128]), op=ALU.mult),
                        reads=[t_ident, t_vecs], writes=[t_diag])
                    for t in range(NTT):
                        b = next_ps()

                        def f(e, j=j, t=t, b=b):
                            for k in range(31):
                                r = e.matmul(ps[:, b, :], lhsT=diag[:, k, :],
                                             rhs=U[:, j, t * TT + k:t * TT + k + TT],
                                             start=(k == 0), stop=(k == 30))
                            return r
                        sc.op("pe", f, reads=[t_diag, t_U[j]], writes=[t_ps[b]])
                        sc.op("act", lambda e, j=j, t=t, b=b: e.activation(
                            out=hT[:, j, t * TT:(t + 1) * TT], in_=ps[:, b, :], func=AF.Identity,
                            bias=vcol(j, VR["dw_b"])),
                            reads=[t_ps[b], t_vecs], writes=[t_hT[j][t]])
                if dbg == 3:
                    dump(lambda c, tsl: hT[:, c, tsl], lambda c, t: [t_hT[c][t]])
                    return
                for t in range(NTT):
                    tsl = slice(t * TT, (t + 1) * TT)
                    sc.op("pool", lambda e, tsl=tsl: e.tensor_tensor(
                        out=sqtmp[:], in0=hT[:, :, tsl], in1=hT[:, :, tsl], op=ALU.mult),
                        reads=[t_hT[c][t] for c in range(NCH)], writes=[t_sq])
                    b1 = next_ps()
                    b2 = next_ps()

                    def f(e, b1=b1, b2=b2, tsl=tsl):
                        for c in range(NCH):
                            e.matmul(ps[:, b1, :], lhsT=ones_b[:], rhs=hT[:, c, tsl],
                                     start=(c == 0), stop=(c == NCH - 1))
                        for c in range(NCH):
                            r = e.matmul(ps[:, b2, :], lhsT=ones_b[:], rhs=sqtmp[:, c, :],
                                         start=(c == 0), stop=(c == NCH - 1))
                        return r
                    sc.op("pe", f, reads=[t_sq, t_ones] + [t_hT[c][t] for c in range(NCH)],
                          writes=[t_ps[b1], t_ps[b2]])
                    m = lnst[0]
                    q = lnst[1]
                    rs = lnst[2]
                    tm, tq, trs = t_lnst
                    sc.op("act", lambda e, m=m, b1=b1: e.activation(
                        out=m[:], in_=ps[:, b1, :], func=AF.Copy, scale=1.0 / D),
                        reads=[t_ps[b1]], writes=[tm])
                    sc.op("dve", lambda e, m=m, q=q: e.tensor_tensor(
                        out=q[:], in0=m[:], in1=m[:], op=ALU.mult),
                        reads=[tm], writes=[tq])
                    sc.op("dve", lambda e, q=q, b2=b2: e.scalar_tensor_tensor(
                        out=q[:], in0=ps[:, b2, :], scalar=1.0 / D, in1=q[:],
                        op0=ALU.mult, op1=ALU.subtract),
                        reads=[t_ps[b2], tq], writes=[tq])
                    sc.op("act", lambda e, q=q: e.activation(
                        out=q[:], in_=q[:], func=AF.Sqrt, bias=1e-5),
                        reads=[tq], writes=[tq])
                    sc.op("dve", lambda e, q=q, rs=rs: e.reciprocal(out=rs[:], in_=q[:]),
                          reads=[tq], writes=[trs])
                    sc.op("dve", lambda e, m=m, rs=rs: e.scalar_tensor_tensor(
                        out=m[:], in0=m[:], scalar=-1.0, in1=rs[:],
                        op0=ALU.mult, op1=ALU.mult),
                        reads=[tm, trs], writes=[tm])
                    for c in range(NCH):
                        w1s = next_sm()
                        sc.op("dve", lambda e, c=c, tsl=tsl, rs=rs, w1s=w1s: e.tensor_tensor(
                            out=sm[w1s][:], in0=hT[:, c, tsl], in1=rs[:], op=ALU.mult),
                            reads=[t_hT[c][t], trs], writes=[t_sm[w1s]])
                        sc.op("pool", lambda e, m=m, w1s=w1s: e.tensor_tensor(
                            out=sm[w1s][:], in0=sm[w1s][:], in1=m[:], op=ALU.add),
                            reads=[t_sm[w1s], tm], writes=[t_sm[w1s]])
                        sc.op("act", lambda e, c=c, tsl=tsl, w1s=w1s: e.activation(
                            out=hT[:, c, tsl], in_=sm[w1s][:], func=AF.Silu,
                            scale=vcol(c, VR["ln_g"]), bias=vcol(c, VR["ln_b"])),
                            reads=[t_sm[w1s], t_vecs], writes=[t_hT[c][t]])
                if dbg == 4:
                    dump(lambda c, tsl: hT[:, c, tsl], lambda c, t: [t_hT[c][t]])
                    return
                for h in range(2):
                    w2 = [load_block(w2_d[:, h * 512 + q * 256: h * 512 + (q + 1) * 256]
                                     .rearrange("(kc p) n -> p kc n", p=128), None) for q in range(2)]
                    for jj in range(4):
                        j = h * 4 + jj
                        wA, tA = w2[jj // 2]
                        co = (jj % 2) * 128
                        for t in range(NTT):
                            tsl = slice(t * TT, (t + 1) * TT)
                            b = next_ps()

                            def f(e, wA=wA, co=co, b=b, tsl=tsl):
                                for kc in range(NCH):
                                    r = e.matmul(ps[:, b, :], lhsT=wA[:, kc, co:co + 128],
                                                 rhs=hT[:, kc, tsl], start=(kc == 0),
                                                 stop=(kc == NCH - 1))
                                return r
                            sc.op("pe", f, reads=[tA] + [t_hT[c][t] for c in range(NCH)],
                                  writes=[t_ps[b]])
                            sc.op("dve", lambda e, b=b, j=j, tsl=tsl: e.scalar_tensor_tensor(
                                out=xT[:, j, tsl], in0=ps[:, b, :], scalar=vcol(j, VR["pw2_b"]),
                                in1=xT[:, j, tsl], op0=ALU.add, op1=ALU.add),
                                reads=[t_ps[b], t_vecs] + xtoks(j, t), writes=xtoks(j, t))

        def swiglu_stream(hT, t_hT, h1, t_h1, sg, t_sg, wg_d, wu_d, wd_d, dff,
                          gate_bc=None, t_gate=None):
            ngrp = (dff + 511) // 512
            sgn = 0
            for g in range(ngrp):
                c0 = g * 512
                ncol = min(512, dff - c0)
                nblk = ncol // 256
                nf = ncol // 128
                wg = [load_block(wg_d[:, c0 + q * 256:c0 + (q + 1) * 256]
                                 .rearrange("(kc p) n -> p kc n", p=128), None) for q in range(nblk)]
                wu = [load_block(wu_d[:, c0 + q * 256:c0 + (q + 1) * 256]
                                 .rearrange("(kc p) n -> p kc n", p=128), None) for q in range(nblk)]
                wd = [load_block(wd_d[c0 + q * 256:c0 + (q + 1) * 256, :]
                                 .rearrange("(fc p) n -> p fc n", p=128), None) for q in range(nblk)]
                for t in range(NTT):
                    tsl = slice(t * TT, (t + 1) * TT)
                    for f_ in range(nf):
                        wG, tG = wg[f_ // 2]
                        wU, tU = wu[f_ // 2]
                        co = (f_ % 2) * 128
                        bG = next_ps()
                        bU = next_ps()

                        def f(e, wG=wG, wU=wU, co=co, bG=bG, bU=bU, tsl=tsl):
                            for kc in range(NCH):
                                e.matmul(ps[:, bG, :], lhsT=wG[:, kc, co:co + 128],
                                         rhs=hT[:, kc, tsl], start=(kc == 0), stop=(kc == NCH - 1))
                            for kc in range(NCH):
                                r = e.matmul(ps[:, bU, :], lhsT=wU[:, kc, co:co + 128],
                                             rhs=hT[:, kc, tsl], start=(kc == 0),
                                             stop=(kc == NCH - 1))
                            return r
                        sc.op("pe", f, reads=[tG, tU] + [t_hT[c][t] for c in range(NCH)],
                              writes=[t_ps[bG], t_ps[bU]])
                        s_ = sgn % 2
                        sgn += 1
                        sc.op("act", lambda e, bG=bG, s_=s_: e.activation(
                            out=sg[s_][:], in_=ps[:, bG, :], func=AF.Silu),
                            reads=[t_ps[bG]], writes=[t_sg[s_]])
                        if gate_bc is None:
                            sc.op("dve", lambda e, bU=bU, s_=s_, f_=f_, tsl=tsl: e.tensor_tensor(
                                out=h1[:, f_, tsl], in0=ps[:, bU, :], in1=sg[s_][:], op=ALU.mult),
                                reads=[t_ps[bU], t_sg[s_]], writes=[t_h1[f_][t]])
                        else:
                            sc.op("pool", lambda e, s_=s_, tsl=tsl: e.tensor_tensor(
                                out=sg[s_][:], in0=sg[s_][:], in1=gate_bc[:, tsl], op=ALU.mult),
                                reads=[t_sg[s_], t_gate], writes=[t_sg[s_]])
                            sc.op("dve", lambda e, bU=bU, s_=s_, f_=f_, tsl=tsl: e.tensor_tensor(
                                out=h1[:, f_, tsl], in0=ps[:, bU, :], in1=sg[s_][:], op=ALU.mult),
                                reads=[t_ps[bU], t_sg[s_]], writes=[t_h1[f_][t]])
                for t in range(NTT):
                    tsl = slice(t * TT, (t + 1) * TT)
                    for j in range(NCH):
                        b = next_ps()

                        def f(e, b=b, j=j, tsl=tsl, wd=wd, nf=nf):
                            for f_ in range(nf):
                                wD, _ = wd[f_ // 2]
                                r = e.matmul(ps[:, b, :],
                                             lhsT=wD[:, f_ % 2, j * 128:(j + 1) * 128],
                                             rhs=h1[:, f_, tsl], start=(f_ == 0),
                                             stop=(f_ == nf - 1))
                            return r
                        sc.op("pe", f, reads=[w[1] for w in wd] + [t_h1[f_][t] for f_ in range(nf)],
                              writes=[t_ps[b]])
                        sc.op("dve", lambda e, b=b, j=j, tsl=tsl: e.tensor_tensor(
                            out=xT[:, j, tsl], in0=ps[:, b, :], in1=xT[:, j, tsl], op=ALU.add),
                            reads=[t_ps[b]] + xtoks(j, t), writes=xtoks(j, t))

        def phase_ffn():
            with contextlib.ExitStack() as esp:
                hT = sb("hT", [128, NCH, S], BF16, esp)
                t_hT = [[Tok("hT%d_%d" % (c, t)) for t in range(NTT)] for c in range(NCH)]
                sqtmp = sb("sqtmp", [128, NCH, TT], BF16, esp)
                t_sq = Tok("sqtmp")
                h1 = sb("h1", [128, 4, S], BF16, esp)
                t_h1 = [[Tok("h1_%d_%d" % (f_, t)) for t in range(NTT)] for f_ in range(4)]
                sg = [sb("sg%d" % i, [128, TT], F32, esp) for i in range(2)]
                t_sg = [Tok("sg0"), Tok("sg1")]
                rmsnorm_fm(hT, t_hT, VR["ffn0"], sqtmp, t_sq)
                swiglu_stream(hT, t_hT, h1, t_h1, sg, t_sg, fg_d, fu_d, fd_d, DFF)


        def phase_gdn():
            NT16 = S // 128
            with contextlib.ExitStack() as esp:
                hT = sb("hT", [128, NCH, S], BF16, esp)
                t_hT = [[Tok("hT%d_%d" % (c, t)) for t in range(NTT)] for c in range(NCH)]
                with contextlib.ExitStack() as esq:
                    sqtmp = sb("sqtmp", [128, NCH, TT], BF16, esq)
                    t_sq = Tok("sqtmp")
                    rmsnorm_fm(hT, t_hT, VR["mix1"], sqtmp, t_sq)
                sc.barrier()
                if two_x:
                    with contextlib.ExitStack() as esx:
                        xin2 = [sb("xin2_%d" % i, [128, D], F32, esx) for i in range(2)]
                        t_xin2 = [Tok("xin2_0"), Tok("xin2_1")]
                        load_x_loop(xacc_d, xin2, t_xin2, fence=sc.last_ops())
                    sc.barrier()
                cst = sb("cst", [128, 2048], F32, esp)
                t_cst = Tok("cst")
                gsm = sb("gsm", [128, 160], F32, esp)
                t_gsm = Tok("gsm")
                graw = sb("graw", [128, NT16, 32], F32, esp)
                beta = sb("beta", [128, NT16, 16], F32, esp)
                la = sb("la", [128, NT16, 16], F32, esp)
                gcol = sb("gcol", [128, NT16, 16], F32, esp)
                glast = sb("glast", [128, NT16, 16], F32, esp)
                beg = sb("beg", [128, NT16, 16], F32, esp)
                kdc = sb("kdc", [128, NT16, 16], F32, esp)
                egl = sb("egl", [128, NT16, 16], F32, esp)
                t_graw, t_beta, t_la, t_gcol, t_glast, t_beg, t_kdc, t_egl = [
                    Tok(n) for n in ("graw", "beta", "la", "gcol", "glast", "beg", "kdc", "egl")]
                pre = sb("pre", [128, S + 4], BF16, esp)
                t_pre = Tok("pre")
                diag5 = sb("diag5", [128, 5, 128], BF16, esp)
                t_diag5 = Tok("diag5")
                QKV = [sb("qkv%d" % i, [128, S], F32, esp) for i in range(3)]
                t_QKV = [[Tok("qkv%d_%d" % (i, t)) for t in range(NTT)] for i in range(3)]
                zw = sb("zw", [128, NCH, 128], BF16, esp)
                t_zw = Tok("zw")
                Oacc = sb("Oacc", [128, NT16, 128], F32, esp)
                t_O = [Tok("O%d" % n) for n in range(NT16)]
                oT = sb("oT", [128, S], BF16, esp)
                t_oT = [Tok("oT%d" % t) for t in range(NTT)]
                Sst = sb("Sst", [128, 128], F32, esp)
                t_S = Tok("S")
                names = ("bV", "Kt", "KD", "lacb", "bcb", "dec", "QDT", "AT", "PT", "Dm", "DTm",
                         "X", "WK", "U", "sz", "yy")
                shp = dict(WK=[128, 256])
                T_ = {n: sb(n, shp.get(n, [128, 128]), F32, esp) for n in names}
                t_T = {n: Tok(n) for n in names}
                ETall = sb("ETall", [128, 7, 128], F32, esp)
                t_ET = Tok("ETall")
                st4 = sb("st4", [128, 4], F32, esp)
                t_st4 = Tok("st4")
                ch_c = sc.new_chan()
                fence = sc.last_ops()
                sc.op("sp", lambda e: e.dma_start(out=cst[:], in_=cst_d[:, :]), writes=[t_cst],
                      chan=ch_c, after=fence)
                sc.op("sp", lambda e: e.dma_start(out=gsm[:],
                                                  in_=gsm_d[0:1, :].partition_broadcast(128)),
                      writes=[t_gsm], chan=ch_c, after=fence)
                triX = [cst[:, 0:128], cst[:, 128:256]]
                MX = [cst[:, 256:256 + 896].rearrange("p (k i) -> p k i", k=7),
                      cst[:, 1152:1152 + 896].rearrange("p (k i) -> p k i", k=7)]
                sc.op("pool", lambda e: e.memset(pre[:], 0.0), writes=[t_pre])
                wgt, t_wgt = load_block(gin_d[:, 4096:4128].rearrange("(kc p) n -> p kc n", p=128),
                                        None)
                for n in range(NT16):
                    b = next_ps()
                    tsl = slice(n * 128, (n + 1) * 128)

                    def f(e, b=b, tsl=tsl):
                        for kc in range(NCH):
                            r = e.matmul(ps[:, b, 0:32], lhsT=hT[:, kc, tsl], rhs=wgt[:, kc, :],
                                         start=(kc == 0), stop=(kc == NCH - 1))
                        return r
                    sc.op("pe", f, reads=[t_wgt] + [t_hT[c][n // 4] for c in range(NCH)],
                          writes=[t_ps[b]])
                    sc.op("dve", lambda e, b=b, n=n: e.tensor_copy(out=graw[:, n, :],
                                                                   in_=ps[:, b, 0:32]),
                          reads=[t_ps[b]], writes=[t_graw])
                sc.op("act", lambda e: e.activation(out=beta[:], in_=graw[:, :, 0:16],
                                                    func=AF.Sigmoid),
                      reads=[t_graw], writes=[t_beta])
                sc.op("dve", lambda e: e.tensor_tensor(
                    out=la[:], in0=graw[:, :, 16:32],
                    in1=gsm[:, 16:32].unsqueeze(1).to_broadcast([128, NT16, 16]), op=ALU.add),
                    reads=[t_graw, t_gsm], writes=[t_la])
                sc.op("act", lambda e: e.activation(out=la[:], in_=la[:], func=AF.Exp),
                      reads=[t_la], writes=[t_la])
                sc.op("act", lambda e: e.activation(out=la[:], in_=la[:], func=AF.Ln, bias=1.0),
                      reads=[t_la], writes=[t_la])
                sc.op("act", lambda e: e.activation(out=gsm[:, 0:16], in_=gsm[:, 0:16],
                                                    func=AF.Exp),
                      reads=[t_gsm], writes=[t_gsm])
                sc.op("dve", lambda e: e.scalar_tensor_tensor(
                    out=la[:], in0=la[:], scalar=-1.0,
                    in1=gsm[:, 0:16].unsqueeze(1).to_broadcast([128, NT16, 16]),
                    op0=ALU.mult, op1=ALU.mult), reads=[t_la, t_gsm], writes=[t_la])
                for n in range(NT16):
                    b = next_ps()

                    def f(e, b=b, n=n):
                        e.matmul(ps[:, b, 0:8], lhsT=triX[0], rhs=la[:, n, 0:8], start=True,
                                 stop=True)
                        e.matmul(ps[:, b, 8:16], lhsT=triX[1], rhs=la[:, n, 8:16], start=True,
                                 stop=True)
                        return e.matmul(ps[:, b, 16:32], lhsT=ones_f[:], rhs=la[:, n, :],
                                        start=True, stop=True)
                    sc.op("pe", f, reads=[t_la, t_cst, t_ones], writes=[t_ps[b]])
                    sc.op("dve", lambda e, b=b, n=n: e.tensor_copy(out=gcol[:, n, :],
                                                                   in_=ps[:, b, 0:16]),
                          reads=[t_ps[b]], writes=[t_gcol])
                    sc.op("dve", lambda e, b=b, n=n: e.tensor_copy(out=glast[:, n, :],
                                                                   in_=ps[:, b, 16:32]),
                          reads=[t_ps[b]], writes=[t_glast])
                sc.op("act", lambda e: e.activation(out=beg[:], in_=gcol[:], func=AF.Exp),
                      reads=[t_gcol], writes=[t_beg])
                sc.op("dve", lambda e: e.tensor_tensor(out=beg[:], in0=beg[:], in1=beta[:],
                                                       op=ALU.mult),
                      reads=[t_beg, t_beta], writes=[t_beg])
                sc.op("dve", lambda e: e.tensor_tensor(out=kdc[:], in0=glast[:], in1=gcol[:],
                                                       op=ALU.subtract),
                      reads=[t_glast, t_gcol], writes=[t_kdc])
                sc.op("act", lambda e: e.activation(out=kdc[:], in_=kdc[:], func=AF.Exp),
                      reads=[t_kdc], writes=[t_kdc])
                sc.op("act", lambda e: e.activation(out=egl[:], in_=glast[:], func=AF.Exp),
                      reads=[t_glast], writes=[t_egl])

                def tt_(n, out, in0, in1, op, eng="dve", rd=(), wr=()):
                    sc.op(eng, lambda e: e.tensor_tensor(out=out, in0=in0, in1=in1, op=op),
                          reads=list(rd), writes=list(wr))

                if dbg == 11:
                    return
                for h in heads:
                    for part in range(3):
                        cidx = part * 8 + h
                        wv, t_wv = load_block(gin_d[:, cidx * 128:(cidx + 1) * 128]
                                              .rearrange("(kc p) n -> p kc n", p=128), None)
                        for k in range(5):
                            sc.op("dve", lambda e, k=k, part=part, h=h: e.tensor_scalar(
                                out=diag5[:, k, :], in0=ident[:],
                                scalar1=vcol(h, VR["gconv"] + k * 3 + part), scalar2=None,
                                op0=ALU.mult), reads=[t_ident, t_vecs], writes=[t_diag5])
                        for t in range(NTT):
                            tsl = slice(t * TT, (t + 1) * TT)
                            b = next_ps()

                            def f(e, b=b, tsl=tsl, wv=wv):
                                for kc in range(NCH):
                                    r = e.matmul(ps[:, b, :], lhsT=wv[:, kc, :], rhs=hT[:, kc, tsl],
                                                 start=(kc == 0), stop=(kc == NCH - 1))
                                return r
                            sc.op("pe", f, reads=[t_wv] + [t_hT[c][t] for c in range(NCH)],
                                  writes=[t_ps[b]])
                            sc.op("act", lambda e, b=b, t=t: e.activation(
                                out=pre[:, 2 + t * TT:2 + (t + 1) * TT], in_=ps[:, b, :],
                                func=AF.Copy), reads=[t_ps[b]], writes=[t_pre])
                        for t in range(NTT):
                            tsl = slice(t * TT, (t + 1) * TT)
                            b = next_ps()

                            def f(e, b=b, t=t):
                                for k in range(5):
                                    r = e.matmul(ps[:, b, :], lhsT=diag5[:, k, :],
                                                 rhs=pre[:, t * TT + k:t * TT + k + TT],
                                                 start=(k == 0), stop=(k == 4))
                                return r
                            sc.op("pe", f, reads=[t_diag5, t_pre], writes=[t_ps[b]])
                            sc.op("act", lambda e, b=b, tsl=tsl, part=part: e.activation(
                                out=QKV[part][:, tsl], in_=ps[:, b, :], func=AF.Silu),
                                reads=[t_ps[b]], writes=[t_QKV[part][t]])
                            if part < 2:
                                la_ = next_sm()
                                lb_ = next_sm()
                                sc.op("pool", lambda e, tsl=tsl, part=part, la_=la_: e.tensor_tensor(
                                    out=sm[la_][:], in0=QKV[part][:, tsl], in1=QKV[part][:, tsl],
                                    op=ALU.mult), reads=[t_QKV[part][t]], writes=[t_sm[la_]])
                                b2 = next_ps()
                                sc.op("pe", lambda e, b2=b2, la_=la_: e.matmul(
                                    ps[:, b2, :], lhsT=ones_f[:], rhs=sm[la_][:], start=True,
                                    stop=True), reads=[t_sm[la_], t_ones], writes=[t_ps[b2]])
                                sc.op("act", lambda e, b2=b2, lb_=lb_: e.activation(
                                    out=sm[lb_][:], in_=ps[:, b2, :], func=AF.Sqrt, bias=1e-6),
                                    reads=[t_ps[b2]], writes=[t_sm[lb_]])
                                sc.op("dve", lambda e, lb_=lb_: e.reciprocal(out=sm[lb_][:],
                                                                             in_=sm[lb_][:]),
                                      reads=[t_sm[lb_]], writes=[t_sm[lb_]])
                                scl = (128.0 ** -0.5) if part == 0 else 1.0
                                sc.op("dve", lambda e, tsl=tsl, part=part, scl=scl, lb_=lb_:
                                      e.scalar_tensor_tensor(
                                          out=QKV[part][:, tsl], in0=QKV[part][:, tsl], scalar=scl,
                                          in1=sm[lb_][:], op0=ALU.mult, op1=ALU.mult),
                                      reads=[t_QKV[part][t], t_sm[lb_]], writes=[t_QKV[part][t]])
                    if dbg == 12:
                        return
                    zwv, t_zwv = load_block(gin_d[:, 3072 + h * 128:3072 + (h + 1) * 128]
                                            .rearrange("(kc p) n -> p kc n", p=128), None)
                    sc.op("pool", lambda e, zwv=zwv: e.tensor_copy(out=zw[:], in_=zwv),
                          reads=[t_zwv], writes=[t_zw])
                    QT, KT, VT = QKV
                    for dr in range(2):
                        sc.op("pool", lambda e: e.memset(Sst[:], 0.0), writes=[t_S])
                        order = range(NT16) if dr == 0 else range(NT16 - 1, -1, -1)
                        dh = dr * 8 + h
                        for n in order:
                            csl = slice(n * 128, (n + 1) * 128)
                            t4 = n // 4
                            rq = [t_QKV[0][t4]]
                            rk = [t_QKV[1][t4]]
                            rv = [t_QKV[2][t4]]
                            bt = next_ps()

                            def f(e, bt=bt, csl=csl):
                                e.transpose(out=ps[:, bt, 0:128], in_=KT[:, csl], identity=ident[:])
                                return e.transpose(out=ps[:, bt, 128:256], in_=VT[:, csl],
                                                   identity=ident[:])
                            sc.op("pe", f, reads=rk + rv + [t_ident], writes=[t_ps[bt]])
                            sc.op("dve", lambda e, bt=bt, n=n, dh=dh: e.tensor_scalar(
                                out=T_["bV"][:], in0=ps[:, bt, 128:256],
                                scalar1=beta[:, n, dh:dh + 1], scalar2=None, op0=ALU.mult),
                                reads=[t_ps[bt], t_beta], writes=[t_T["bV"]])
                            sc.op("dve", lambda e, bt=bt, n=n, dh=dh: e.tensor_scalar(
                                out=T_["Kt"][:], in0=ps[:, bt, 0:128],
                                scalar1=beg[:, n, dh:dh + 1], scalar2=None, op0=ALU.mult),
                                reads=[t_ps[bt], t_beg], writes=[t_T["Kt"]])
                            sc.op("dve", lambda e, bt=bt, n=n, dh=dh: e.tensor_scalar(
                                out=T_["KD"][:], in0=ps[:, bt, 0:128],
                                scalar1=kdc[:, n, dh:dh + 1], scalar2=None, op0=ALU.mult),
                                reads=[t_ps[bt], t_kdc], writes=[t_T["KD"]])
                            sc.op("dve", lambda e, n=n, dh=dh: e.tensor_scalar(
                                out=T_["lacb"][:], in0=ones_f[:], scalar1=la[:, n, dh:dh + 1],
                                scalar2=None, op0=ALU.mult),
                                reads=[t_la, t_ones], writes=[t_T["lacb"]])
                            sc.op("dve", lambda e, n=n, dh=dh: e.tensor_scalar(
                                out=T_["bcb"][:], in0=ones_f[:], scalar1=beta[:, n, dh:dh + 1],
                                scalar2=None, op0=ALU.mult),
                                reads=[t_beta, t_ones], writes=[t_T["bcb"]])
                            bm = next_ps()

                            def f(e, bm=bm, csl=csl, dr=dr):
                                e.matmul(ps[:, bm, 0:128], lhsT=T_["lacb"][:], rhs=triX[dr],
                                         start=True, stop=True)
                                e.matmul(ps[:, bm, 128:256], lhsT=T_["bcb"][:], rhs=ident[:],
                                         start=True, stop=True)
                                e.matmul(ps[:, bm, 256:384], lhsT=KT[:, csl], rhs=KT[:, csl],
                                         start=True, stop=True)
                                return e.matmul(ps[:, bm, 384:512], lhsT=KT[:, csl],
                                                rhs=QT[:, csl], start=True, stop=True)
                            sc.op("pe", f, reads=[t_T["lacb"], t_T["bcb"], t_cst, t_ident] + rk + rq,
                                  writes=[t_ps[bm]])
                            sc.op("dve", lambda e, bm=bm, n=n, dh=dh: e.tensor_scalar(
                                out=T_["dec"][:], in0=ps[:, bm, 0:128],
                                scalar1=gcol[:, n, dh:dh + 1], scalar2=None, op0=ALU.subtract),
                                reads=[t_ps[bm], t_gcol], writes=[t_T["dec"]])
                            sc.op("dve", lambda e: e.tensor_scalar(
                                out=T_["dec"][:], in0=T_["dec"][:], scalar1=0.0, scalar2=None,
                                op0=ALU.min), reads=[t_T["dec"]], writes=[t_T["dec"]])
                            sc.op("act", lambda e: e.activation(out=T_["dec"][:], in_=T_["dec"][:],
                                                                func=AF.Exp),
                                  reads=[t_T["dec"]], writes=[t_T["dec"]])
                            sc.op("act", lambda e, bm=bm: e.activation(
                                out=T_["QDT"][:], in_=ps[:, bm, 0:128], func=AF.Exp),
                                reads=[t_ps[bm]], writes=[t_T["QDT"]])
                            sc.op("pool", lambda e, csl=csl: e.tensor_tensor(
                                out=T_["QDT"][:], in0=T_["QDT"][:], in1=QT[:, csl], op=ALU.mult),
                                reads=[t_T["QDT"]] + rq, writes=[t_T["QDT"]])
                            sc.op("dve", lambda e, bm=bm: e.tensor_tensor(
                                out=T_["AT"][:], in0=ps[:, bm, 256:384], in1=T_["dec"][:],
                                op=ALU.mult), reads=[t_ps[bm], t_T["dec"]], writes=[t_T["AT"]])
                            sc.op("dve", lambda e, bm=bm: e.tensor_tensor(
                                out=T_["AT"][:], in0=ps[:, bm, 128:256], in1=T_["AT"][:],
                                op=ALU.mult), reads=[t_ps[bm], t_T["AT"]], writes=[t_T["AT"]])
                            sc.op("dve", lambda e, bm=bm: e.tensor_tensor(
                                out=T_["PT"][:], in0=ps[:, bm, 384:512], in1=T_["dec"][:],
                                op=ALU.mult), reads=[t_ps[bm], t_T["dec"]], writes=[t_T["PT"]])
                            sc.op("pool", lambda e, dr=dr: e.tensor_tensor(
                                out=T_["PT"][:], in0=T_["PT"][:], in1=triX[dr], op=ALU.mult),
                                reads=[t_T["PT"], t_cst], writes=[t_T["PT"]])
                            for lv in range(7):
                                sc.op("pool" if lv % 2 else "dve", lambda e, dr=dr, lv=lv: e.tensor_tensor(
                                    out=ETall[:, lv, :], in0=T_["AT"][:], in1=MX[dr][:, lv, :],
                                    op=ALU.mult), reads=[t_T["AT"], t_cst], writes=[t_ET])
                            if dbg == 13:
                                return
                            for lv in range(7):
                                Dc = ident if lv == 0 else T_["Dm"]
                                DTc = ident if lv == 0 else T_["DTm"]
                                rD = [t_ident] if lv == 0 else [t_T["Dm"]]
                                rDT = [t_ident] if lv == 0 else [t_T["DTm"]]
                                bx = next_ps()
                                sc.op("pe", lambda e, bx=bx, lv=lv, Dc=Dc: e.matmul(
                                    ps[:, bx, 0:128], lhsT=ETall[:, lv, :], rhs=Dc[:], start=True,
                                    stop=True), reads=[t_ET] + rD, writes=[t_ps[bx]])
                                sc.op("act", lambda e, bx=bx: e.activation(
                                    out=T_["X"][:], in_=ps[:, bx, 0:128], func=AF.Copy),
                                    reads=[t_ps[bx]], writes=[t_T["X"]])
                                by = next_ps()

                                def f(e, by=by, Dc=Dc, DTc=DTc):
                                    e.matmul(ps[:, by, 0:128], lhsT=DTc[:], rhs=T_["X"][:],
                                             start=True, stop=True)
                                    return e.matmul(ps[:, by, 128:256], lhsT=T_["X"][:], rhs=DTc[:],
                                                    start=True, stop=True)
                                sc.op("pe", f, reads=[t_T["X"]] + rDT, writes=[t_ps[by]])
                                sc.op("dve", lambda e, by=by, Dc=Dc: e.tensor_tensor(
                                    out=T_["Dm"][:], in0=Dc[:], in1=ps[:, by, 0:128],
                                    op=ALU.subtract), reads=[t_ps[by]] + rD, writes=[t_T["Dm"]])
                                sc.op("dve", lambda e, by=by, DTc=DTc: e.tensor_tensor(
                                    out=T_["DTm"][:], in0=DTc[:], in1=ps[:, by, 128:256],
                                    op=ALU.subtract), reads=[t_ps[by]] + rDT, writes=[t_T["DTm"]])
                            if dbg == 14:
                                return
                            bw = next_ps()

                            def f(e, bw=bw):
                                e.matmul(ps[:, bw, 0:128], lhsT=T_["DTm"][:], rhs=T_["bV"][:],
                                         start=True, stop=True)
                                return e.matmul(ps[:, bw, 128:256], lhsT=T_["Kt"][:],
                                                rhs=T_["DTm"][:], start=True, stop=True)
                            sc.op("pe", f, reads=[t_T["DTm"], t_T["bV"], t_T["Kt"]],
                                  writes=[t_ps[bw]])
                            sc.op("act", lambda e, bw=bw: e.activation(
                                out=T_["WK"][:], in_=ps[:, bw, 0:256], func=AF.Copy),
                                reads=[t_ps[bw]], writes=[t_T["WK"]])
                            if dbg == 21:
                                return
                            bs = next_ps()
                            sc.op("pe", lambda e, bs=bs: e.matmul(
                                ps[:, bs, 0:128], lhsT=T_["WK"][:, 128:256], rhs=Sst[:], start=True,
                                stop=True), reads=[t_T["WK"], t_S], writes=[t_ps[bs]])
                            sc.op("dve", lambda e, bs=bs: e.tensor_tensor(
                                out=T_["U"][:], in0=T_["WK"][:, 0:128], in1=ps[:, bs, 0:128],
                                op=ALU.subtract), reads=[t_ps[bs], t_T["WK"]], writes=[t_T["U"]])
                            if dbg == 22:
                                return
                            bo = next_ps()

                            def f(e, bo=bo):
                                e.matmul(ps[:, bo, 0:128], lhsT=T_["QDT"][:], rhs=Sst[:], start=True,
                                         stop=False)
                                e.matmul(ps[:, bo, 0:128], lhsT=T_["PT"][:], rhs=T_["U"][:],
                                         start=False, stop=True)
                                return e.matmul(ps[:, bo, 128:256], lhsT=T_["KD"][:], rhs=T_["U"][:],
                                                start=True, stop=True)
                            sc.op("pe", f, reads=[t_T["QDT"], t_S, t_T["PT"], t_T["U"], t_T["KD"]],
                                  writes=[t_ps[bo]])
                            if dbg == 23:
                                return
                            if dr == 0:
                                sc.op("act", lambda e, bo=bo, n=n: e.activation(
                                    out=Oacc[:, n, :], in_=ps[:, bo, 0:128], func=AF.Copy),
                                    reads=[t_ps[bo]], writes=[t_O[n]])
                            else:
                                sc.op("dve", lambda e, bo=bo, n=n: e.tensor_tensor(
                                    out=Oacc[:, n, :], in0=ps[:, bo, 0:128], in1=Oacc[:, n, :],
                                    op=ALU.add), reads=[t_ps[bo], t_O[n]], writes=[t_O[n]])
                            if dbg == 26:
                                return
                            sc.op("dve", lambda e, n=n, dh=dh: e.tensor_scalar(
                                out=Sst[:], in0=Sst[:], scalar1=egl[:, n, dh:dh + 1], scalar2=None,
                                op0=ALU.mult), reads=[t_S, t_egl], writes=[t_S])
                            sc.op("dve", lambda e, bo=bo: e.tensor_tensor(
                                out=Sst[:], in0=ps[:, bo, 128:256], in1=Sst[:], op=ALU.add),
                                reads=[t_ps[bo], t_S], writes=[t_S])
                            if dbg == 24:
                                return
                            if dbg == 25 and n == 1:
                                return
                        if dbg == 15:
                            return
                    if dbg == 16:
                        return
                    for q in range(NTT):
                        bT = next_ps()
                        for i4 in range(4):
                            n = q * 4 + i4
                            csl = slice(n * 128, (n + 1) * 128)
                            sc.op("act", lambda e, n=n: e.activation(
                                out=T_["yy"][:], in_=Oacc[:, n, :], func=AF.Square,
                                accum_out=st4[:, 0:1]), reads=[t_O[n]],
                                writes=[t_T["yy"], t_st4])
                            sc.op("act", lambda e: e.activation(
                                out=st4[:, 1:2], in_=st4[:, 0:1], func=AF.Sqrt, scale=1.0 / 128,
                                bias=1e-6), reads=[t_st4], writes=[t_st4])
                            sc.op("dve", lambda e: e.reciprocal(out=st4[:, 2:3], in_=st4[:, 1:2]),
                                  reads=[t_st4], writes=[t_st4])
                            bz = next_ps()

                            def f(e, bz=bz, csl=csl):
                                for kc in range(NCH):
                                    r = e.matmul(ps[:, bz, 0:128], lhsT=hT[:, kc, csl],
                                                 rhs=zw[:, kc, :], start=(kc == 0),
                                                 stop=(kc == NCH - 1))
                                return r
                            sc.op("pe", f, reads=[t_zw] + [t_hT[c][q] for c in range(NCH)],
                                  writes=[t_ps[bz]])
                            sc.op("act", lambda e, bz=bz: e.activation(
                                out=T_["sz"][:], in_=ps[:, bz, 0:128], func=AF.Silu),
                                reads=[t_ps[bz]], writes=[t_T["sz"]])
                            sc.op("dve", lambda e, n=n: e.scalar_tensor_tensor(
                                out=T_["yy"][:], in0=Oacc[:, n, :], scalar=st4[:, 2:3],
                                in1=gsm[:, 32:160], op0=ALU.mult, op1=ALU.mult),
                                reads=[t_O[n], t_st4, t_gsm], writes=[t_T["yy"]])
                            sc.op("dve", lambda e: e.tensor_tensor(
                                out=T_["yy"][:], in0=T_["yy"][:], in1=T_["sz"][:], op=ALU.mult),
                                reads=[t_T["yy"], t_T["sz"]], writes=[t_T["yy"]])
                            sc.op("pe", lambda e, bT=bT, i4=i4: e.transpose(
                                out=ps[:, bT, i4 * 128:(i4 + 1) * 128], in_=T_["yy"][:],
                                identity=ident[:]), reads=[t_T["yy"], t_ident], writes=[t_ps[bT]])
                        sc.op("act", lambda e, bT=bT, q=q: e.activation(
                            out=oT[:, q * TT:(q + 1) * TT], in_=ps[:, bT, :], func=AF.Copy),
                            reads=[t_ps[bT]], writes=[t_oT[q]])
                    if dbg == 17:
                        return
                    wo, t_wo = load_block(gout_d[h * 128:(h + 1) * 128, :]
                                          .rearrange("p (a n) -> p a n", a=1), None)
                    for t in range(NTT):
                        tsl = slice(t * TT, (t + 1) * TT)
                        for j in range(NCH):
                            b = next_ps()
                            sc.op("pe", lambda e, b=b, j=j, tsl=tsl, wo=wo: e.matmul(
                                ps[:, b, :], lhsT=wo[:, 0, j * 128:(j + 1) * 128], rhs=oT[:, tsl],
                                start=True, stop=True), reads=[t_wo, t_oT[t]], writes=[t_ps[b]])
                            sc.op("dve", lambda e, b=b, j=j, tsl=tsl: e.tensor_tensor(
                                out=xT[:, j, tsl], in0=ps[:, b, :], in1=xT[:, j, tsl], op=ALU.add),
                                reads=[t_ps[b]] + xtoks(j, t), writes=xtoks(j, t))
                    if dbg == 30 + h:
                        return


        def phase_moe():
            NT16 = S // 128
            with contextlib.ExitStack() as esp:
                hT = sb("hT", [128, NCH, S], BF16, esp)
                t_hT = [[Tok("hT%d_%d" % (c, t)) for t in range(NTT)] for c in range(NCH)]
                sqtmp = sb("sqtmp", [128, NCH, TT], BF16, esp)
                t_sq = Tok("sqtmp")
                h1 = sb("h1", [128, 4, S], BF16, esp)
                t_h1 = [[Tok("h1_%d_%d" % (f_, t)) for t in range(NTT)] for f_ in range(4)]
                sg = [sb("sg%d" % i, [128, TT], F32, esp) for i in range(2)]
                t_sg = [Tok("sg0"), Tok("sg1")]
                G = [sb("G%d" % i, [128, S], F32, esp) for i in range(2)]
                t_G = [Tok("G0"), Tok("G1")]
                wr = sb("wr", [128, NCH, 8], F32, esp)
                t_wr = Tok("wr")
                sq32 = [sb("sq32_%d" % i, [128, NCH, 128], F32, esp) for i in range(2)]
                t_sq32 = [Tok("sq32_0"), Tok("sq32_1")]
                rst = sb("rst", [128, NT16, 9], F32, esp)
                t_rst = Tok("rst")
                L = sb("L", [128, NT16, 8], F32, esp)
                v8 = sb("v8", [128, NT16, 8], F32, esp)
                gate = sb("gate", [128, NT16, 8], F32, esp)
                tmpr = sb("tmpr", [128, NT16, 8], F32, esp)
                rs16 = sb("rs16", [128, NT16, 4], F32, esp)
                dg = [sb("dg%d" % i, [128, 128], F32, esp) for i in range(2)]
                t_dg = [Tok("dg0"), Tok("dg1")]
                t_L, t_v8, t_gate, t_tmpr, t_rs16 = (Tok("L"), Tok("v8"), Tok("gate"),
                                                     Tok("tmpr"), Tok("rs16"))
                ch_wr = sc.new_chan()
                rmsnorm_fm(hT, t_hT, VR["ffn1"], sqtmp, t_sq)
                sc.op("sp", lambda e: e.dma_start(
                    out=wr[:], in_=rt_d.rearrange("(c p) e -> p c e", p=128)),
                    writes=[t_wr], chan=ch_wr, after=sc.last_ops())
                for c in range(NCH):
                    sc.op("dve", lambda e, c=c: e.tensor_scalar(
                        out=wr[:, c, :], in0=wr[:, c, :], scalar1=vcol(c, VR["ffn1"]),
                        scalar2=None, op0=ALU.mult), reads=[t_wr, t_vecs], writes=[t_wr])
                for tt in range(NT16):
                    s_ = tt % 2
                    tsl = slice(tt * 128, (tt + 1) * 128)
                    sc.op("act", lambda e, s_=s_, tsl=tsl: e.activation(
                        out=sq32[s_][:], in_=xT[:, :, tsl], func=AF.Square),
                        reads=[t_xT[c][tt] for c in range(NCH)], writes=[t_sq32[s_]])
                    b = next_ps()

                    def f(e, b=b, tsl=tsl, s_=s_):
                        for c in range(NCH):
                            e.matmul(ps[:, b, 0:8], lhsT=xT[:, c, tsl], rhs=wr[:, c, :],
                                     start=(c == 0), stop=(c == NCH - 1))
                        for c in range(NCH):
                            r = e.matmul(ps[:, b, 8:9], lhsT=sq32[s_][:, c, :],
                                         rhs=ones_f[:, 0:1], start=(c == 0), stop=(c == NCH - 1))
                        return r
                    sc.op("pe", f, reads=[t_wr, t_sq32[s_], t_ones] +
                          [t_xT[c][tt] for c in range(NCH)], writes=[t_ps[b]])
                    sc.op("dve", lambda e, b=b, tt=tt: e.tensor_copy(
                        out=rst[:, tt, :], in_=ps[:, b, 0:9]), reads=[t_ps[b]], writes=[t_rst])
                sc.op("act", lambda e: e.activation(
                    out=rs16[:, :, 0:1], in_=rst[:, :, 8:9], func=AF.Sqrt, scale=1.0 / D,
                    bias=1e-6), reads=[t_rst], writes=[t_rs16])
                sc.op("dve", lambda e: e.reciprocal(out=rs16[:, :, 1:2], in_=rs16[:, :, 0:1]),
                      reads=[t_rs16], writes=[t_rs16])
                sc.op("dve", lambda e: e.tensor_tensor(
                    out=L[:], in0=rst[:, :, 0:8],
                    in1=rs16[:, :, 1:2].to_broadcast([128, NT16, 8]), op=ALU.mult),
                    reads=[t_rst, t_rs16], writes=[t_L])
                for tt in range(NT16):
                    sc.op("dve", lambda e, tt=tt: e.max(out=v8[:, tt, :], in_=L[:, tt, :]),
                          reads=[t_L], writes=[t_v8])
                sc.op("dve", lambda e: e.tensor_tensor(
                    out=gate[:], in0=L[:], in1=v8[:, :, 1:2].to_broadcast([128, NT16, 8]),
                    op=ALU.is_ge), reads=[t_L, t_v8], writes=[t_gate])
                sc.op("dve", lambda e: e.tensor_tensor(
                    out=tmpr[:], in0=L[:], in1=v8[:, :, 0:1].to_broadcast([128, NT16, 8]),
                    op=ALU.subtract), reads=[t_L, t_v8], writes=[t_tmpr])
                sc.op("act", lambda e: e.activation(out=tmpr[:], in_=tmpr[:], func=AF.Exp),
                      reads=[t_tmpr], writes=[t_tmpr])
                sc.op("dve", lambda e: e.tensor_tensor(
                    out=rs16[:, :, 2:3], in0=v8[:, :, 1:2], in1=v8[:, :, 0:1], op=ALU.subtract),
                    reads=[t_v8], writes=[t_rs16])
                sc.op("act", lambda e: e.activation(out=rs16[:, :, 2:3], in_=rs16[:, :, 2:3],
                                                    func=AF.Exp),
                      reads=[t_rs16], writes=[t_rs16])
                sc.op("dve", lambda e: e.tensor_scalar(
                    out=rs16[:, :, 2:3], in0=rs16[:, :, 2:3], scalar1=1.0, scalar2=None,
                    op0=ALU.add), reads=[t_rs16], writes=[t_rs16])
                sc.op("dve", lambda e: e.reciprocal(out=rs16[:, :, 3:4], in_=rs16[:, :, 2:3]),
                      reads=[t_rs16], writes=[t_rs16])
                sc.op("dve", lambda e: e.tensor_tensor(
                    out=gate[:], in0=gate[:], in1=tmpr[:], op=ALU.mult),
                    reads=[t_gate, t_tmpr], writes=[t_gate])
                sc.op("dve", lambda e: e.tensor_tensor(
                    out=gate[:], in0=gate[:], in1=rs16[:, :, 3:4].to_broadcast([128, NT16, 8]),
                    op=ALU.mult), reads=[t_gate, t_rs16], writes=[t_gate])
                dgn = 0
                for ex in range(8):
                    gi = ex % 2
                    for q in range(NTT):
                        b = next_ps()
                        for i4 in range(4):
                            tt = q * 4 + i4
                            di = dgn % 2
                            dgn += 1
                            sc.op("dve", lambda e, di=di, tt=tt, ex=ex: e.tensor_scalar(
                                out=dg[di][:], in0=ident[:], scalar1=gate[:, tt, ex:ex + 1],
                                scalar2=None, op0=ALU.mult),
                                reads=[t_ident, t_gate], writes=[t_dg[di]])
                            sc.op("pe", lambda e, b=b, i4=i4, di=di: e.matmul(
                                ps[:, b, i4 * 128:(i4 + 1) * 128], lhsT=ones_f[:], rhs=dg[di][:],
                                start=True, stop=True),
                                reads=[t_dg[di], t_ones], writes=[t_ps[b]])
                        sc.op("act", lambda e, b=b, gi=gi, q=q: e.activation(
                            out=G[gi][:, q * TT:(q + 1) * TT], in_=ps[:, b, :], func=AF.Copy),
                            reads=[t_ps[b]], writes=[t_G[gi]])
                    swiglu_stream(hT, t_hT, h1, t_h1, sg, t_sg, mg_d[ex], mu_d[ex], md_d[ex], DFFE,
                                  gate_bc=G[gi], t_gate=t_G[gi])

        if "c0" in phases:
            phase_conformer()
            sc.barrier()
        if "f0" in phases:
            phase_ffn()
            sc.barrier()
        if "g1" in phases:
            phase_gdn()
            sc.barrier()
        if "m1" in phases:
            phase_moe()
            sc.barrier()

        do_norm = "final" in phases
        with contextlib.ExitStack() as es2:
            xo = [sb("xo%d" % i, [128, D], F32, es2) for i in range(2)]
            fng = sb("fng_sb", [128, D], F32, es2)
            sc.op("sp", lambda e: e.dma_start(out=fng[:],
                                              in_=fng_d[0:1, :].partition_broadcast(128)),
                  writes=[t_fng], chan=ch_misc, after=sc.last_ops())
            yo = [sb("yo%d" % i, [128, D], F32, es2) for i in range(2)]
            sq = sb("sqj", [128, D], F32, es2)
            st = [sb("st%d" % i, [128, 4], F32, es2) for i in range(2)]
            t_xo = [Tok("xo0"), Tok("xo1")]
            t_yo = [Tok("yo0"), Tok("yo1")]
            t_sq2 = Tok("sq")
            t_st = [Tok("st0"), Tok("st1")]
            out_ops = []
            for tt in range(S // 128):
                sl = tt % 2
                for half in range(2):
                    b = next_ps()

                    def f(e, half=half, b=b, tt=tt):
                        for j in range(4):
                            c = half * 4 + j
                            r = e.transpose(out=ps[:, b, j * 128:(j + 1) * 128],
                                            in_=xT[:, c, tt * 128:(tt + 1) * 128],
                                            identity=ident[:])
                        return r
                    sc.op("pe", f, reads=[t_ident] + [t_xT[half * 4 + j][tt] for j in range(4)],
                          writes=[t_ps[b]])
                    dst = xo if do_norm else yo
                    t_dst = t_xo if do_norm else t_yo
                    sc.op("act", lambda e, half=half, b=b, sl=sl, dst=dst: e.activation(
                        out=dst[sl][:, half * 512:(half + 1) * 512], in_=ps[:, b, :], func=AF.Copy),
                        reads=[t_ps[b]], writes=[t_dst[sl]])
                if do_norm:
                    sc.op("act", lambda e, sl=sl: e.activation(
                        out=sq[:], in_=xo[sl][:], func=AF.Square, accum_out=st[sl][:, 0:1]),
                        reads=[t_xo[sl]], writes=[t_sq2, t_st[sl]])
                    sc.op("act", lambda e, sl=sl: e.activation(
                        out=st[sl][:, 1:2], in_=st[sl][:, 0:1], func=AF.Sqrt, scale=1.0 / D,
                        bias=1e-6), reads=[t_st[sl]], writes=[t_st[sl]])
                    sc.op("dve", lambda e, sl=sl: e.reciprocal(out=st[sl][:, 2:3],
                                                               in_=st[sl][:, 1:2]),
                          reads=[t_st[sl]], writes=[t_st[sl]])
                    sc.op("dve", lambda e, sl=sl: e.scalar_tensor_tensor(
                        out=yo[sl][:], in0=xo[sl][:], scalar=st[sl][:, 2:3], in1=fng[:],
                        op0=ALU.mult, op1=ALU.mult),
                        reads=[t_xo[sl], t_st[sl], t_fng], writes=[t_yo[sl]])
                o = sc.op("sp", lambda e, sl=sl, tt=tt: e.dma_start(
                    out=out_d[tt * 128:(tt + 1) * 128, :], in_=yo[sl][:]),
                    reads=[t_yo[sl]], chan=ch_out[sl])
                out_ops.append(o)
            fin = sc.op("sp", lambda e: e.nop())
            fin.deps.extend(out_ops[-2:])
            sc.emit(nc)
    return nc


def pack_vecs(inp):
    rows = np.zeros((128, D), np.float32)
    rows[VR["mix0"]] = inp["mix_norm"][0]
    rows[VR["mix1"]] = inp["mix_norm"][1]
    rows[VR["ffn0"]] = inp["ffn_norm"][0]
    rows[VR["ffn1"]] = inp["ffn_norm"][1]
    rows[VR["pw1_ba"]] = inp["cf_pw1_b"][0, :D]
    rows[VR["pw1_bb"]] = inp["cf_pw1_b"][0, D:]
    rows[VR["dw_b"]] = inp["cf_dw_b"][0]
    rows[VR["ln_g"]] = inp["cf_ln_g"][0]
    rows[VR["ln_b"]] = inp["cf_ln_b"][0]
    rows[VR["pw2_b"]] = inp["cf_pw2_b"][0]
    rows[VR["dw_w"]:VR["dw_w"] + 31] = inp["cf_dw_w"][0]
    gc = inp["gdn_conv_w"][0]
    for k in range(5):
        for part in range(3):
            rows[VR["gconv"] + k * 3 + part] = gc[k, part * D:(part + 1) * D]
    return rows


def gdn_consts():
    i = np.arange(128)
    c = np.zeros((128, 2048), np.float32)
    c[:, 0:128] = (i[:, None] <= i[None, :])
    c[:, 128:256] = (i[:, None] >= i[None, :])
    for k in range(7):
        bsz = 1 << k
        I, J = i[:, None], i[None, :]
        m = ((I // (2 * bsz)) == (J // (2 * bsz))) & (((I // bsz) % 2) == 1) & (((J // bsz) % 2) == 0)
        c[:, 256 + k * 128:256 + (k + 1) * 128] = m.T
        c[:, 1152 + k * 128:1152 + (k + 1) * 128] = m
    return c


def make_in_maps(inp, phases, nb):
    x = np.ascontiguousarray(inp["x"], dtype=np.float32)
    vecs = pack_vecs(inp)
    fng = np.ascontiguousarray(inp["final_norm"], dtype=np.float32).reshape(1, D)
    base = {"vecs": vecs, "fng": fng}
    if "c0" in phases:
        base["cf_pw1_w"] = np.ascontiguousarray(inp["cf_pw1_w"][0])
        base["cf_pw2_w"] = np.ascontiguousarray(inp["cf_pw2_w"][0])
    if "f0" in phases:
        base["ffn_w_gate"] = np.ascontiguousarray(inp["ffn_w_gate"][0])
        base["ffn_w_up"] = np.ascontiguousarray(inp["ffn_w_up"][0])
        base["ffn_w_down"] = np.ascontiguousarray(inp["ffn_w_down"][0])
    if "g1" in phases:
        base["gdn_w_in"] = np.ascontiguousarray(inp["gdn_w_in"][0])
        base["gdn_w_out"] = np.ascontiguousarray(inp["gdn_w_out"][0])
        base["gsmall"] = np.concatenate([inp["gdn_a_log"][0].reshape(-1),
                                         inp["gdn_dt_bias"][0].reshape(-1),
                                         inp["gdn_o_norm"][0].reshape(-1)]).astype(np.float32)[None]
        base["gconst"] = gdn_consts()
    if "m1" in phases:
        base["moe_router"] = np.ascontiguousarray(inp["moe_router"][0])
        base["moe_w_gate"] = np.ascontiguousarray(inp["moe_w_gate"][0])
        base["moe_w_up"] = np.ascontiguousarray(inp["moe_w_up"][0])
        base["moe_w_down"] = np.ascontiguousarray(inp["moe_w_down"][0])
    return [dict(base, x=x[b]) for b in range(nb)]


def _launch(inp, x, phases, nb, heads=tuple(range(8)), xacc=None):
    nc = build_program(phases, heads=heads, two_x=xacc is not None)
    d = dict(inp)
    d["x"] = x
    in_maps = make_in_maps(d, phases, nb)
    if xacc is not None:
        for b_ in range(nb):
            in_maps[b_]["xacc"] = np.ascontiguousarray(xacc[b_])
    res = run_bass_kernel_spmd(nc, in_maps, core_ids=list(range(nb)))
    return np.stack([r["out"] for r in res.results], axis=0)


def kernel(**inp):
    nb = inp["x"].shape[0]
    x0 = np.ascontiguousarray(inp["x"], dtype=np.float32)
    x1 = _launch(inp, x0, ("c0", "f0"), nb)
    acc = x1
    for hs in ((0, 1), (2, 3), (4, 5), (6, 7)):
        acc = _launch(inp, x1, ("g1",), nb, heads=hs, xacc=acc)
    return _launch(inp, acc, ("m1", "final"), nb)
```

```python
import contextlib
import numpy as np
import concourse.bass as bass
import concourse.mybir as mybir
from concourse.bass_utils import run_bass_kernel_spmd

F32 = mybir.dt.float32
BF16 = mybir.dt.bfloat16
I32 = mybir.dt.int32
AF = mybir.ActivationFunctionType
ALU = mybir.AluOpType

D = 1024
S = 2048
NCH = D // 128
TT = 512
NTT = S // TT
ENGS = ("sp", "pe", "act", "dve", "pool")


class Tok:
    __slots__ = ("name", "w", "readers")

    def __init__(self, name):
        self.name = name
        self.w = None
        self.readers = []


class Op:
    __slots__ = ("eng", "fn", "deps", "chan", "has_dep", "sig", "name", "is_nop")

    def __init__(self, eng, fn, chan, name):
        self.eng = eng
        self.fn = fn
        self.deps = []
        self.chan = chan
        self.has_dep = False
        self.sig = None
        self.name = name
        self.is_nop = False


class Sched:
    def __init__(self):
        self.ops = {e: [] for e in ENGS}
        self.nchan = 0

    def new_chan(self):
        self.nchan += 1
        return self.nchan - 1

    def last_real(self, e):
        for o in reversed(self.ops[e]):
            if not o.is_nop:
                return o
        return None

    def last_ops(self, engs=("pe", "act", "dve", "pool")):
        return [o for o in (self.last_real(e) for e in engs) if o is not None]

    def op(self, eng, fn, reads=(), writes=(), chan=None, name="", after=()):
        o = Op(eng, fn, chan, name)
        for d in after:
            o.deps.append(d)
            if d.chan is None:
                d.has_dep = True
        cand = []
        for t in reads:
            if t.w is not None:
                cand.append((t.w, "raw"))
        for t in writes:
            if t.w is not None:
                cand.append((t.w, "waw"))
            for r in t.readers:
                cand.append((r, "war"))
        seen = set()
        for d, kind in cand:
            if d is o or id(d) in seen:
                continue
            same = (d.eng == eng and d.chan is None and chan is None)
            if same and (eng == "pe" or kind == "war"):
                continue
            seen.add(id(d))
            o.deps.append(d)
            d.has_dep = True
        for t in reads:
            t.readers.append(o)
        for t in writes:
            t.w = o
            t.readers = []
        self.ops[eng].append(o)
        return o

    def barrier(self, engs=("pe", "act", "dve", "pool")):
        last = {e: self.last_real(e) for e in engs}
        for e in engs:
            o = Op(e, lambda eng: eng.nop(), None, "barrier")
            o.is_nop = True
            for e2, l in last.items():
                if e2 != e and l is not None:
                    o.deps.append(l)
                    if l.chan is None:
                        l.has_dep = True
            self.ops[e].append(o)

    def emit(self, nc, final_wait_ops=()):
        with contextlib.ExitStack() as es:
            EPOCH = 1000
            nsig = {e: sum(1 for o in self.ops[e] if o.chan is None and o.has_dep) for e in ENGS}
            esem = {e: [es.enter_context(nc.semaphore("s_%s%d" % (e, i)))
                        for i in range(nsig[e] // EPOCH + 1)] for e in ENGS}
            for e in ENGS:
                cnt = 0
                for o in self.ops[e]:
                    if o.chan is None and o.has_dep:
                        o.sig = (esem[e][cnt // EPOCH], cnt % EPOCH + 1, 1)
                        cnt += 1
            CEP = 100
            ccnt = [0] * self.nchan
            ntot = [0] * self.nchan
            for e in ENGS:
                for o in self.ops[e]:
                    if o.chan is not None:
                        ntot[o.chan] += 1
            csem = [[es.enter_context(nc.semaphore("c_%d_%d" % (i, k)))
                     for k in range(ntot[i] // CEP + 1)] for i in range(self.nchan)]
            for e in ENGS:
                for o in self.ops[e]:
                    if o.chan is not None:
                        k = ccnt[o.chan]
                        o.sig = (csem[o.chan][k // CEP], (k % CEP + 1) * 16, 16)
                        ccnt[o.chan] += 1
            block = es.enter_context(nc.Block())
            ops = self.ops

            def run(engname, eobj):
                waited = {}
                for o in ops[engname]:
                    need = {}
                    for d in o.deps:
                        sem, val, _ = d.sig
                        k = id(sem)
                        if val > waited.get(k, 0) and val > need.get(k, (None, 0))[1]:
                            need[k] = (sem, val)
                    for k, (sem, val) in need.items():
                        eobj.wait_ge(sem, val)
                        waited[k] = val
                    inst = o.fn(eobj)
                    if o.sig is not None:
                        inst.then_inc(o.sig[0], o.sig[2])

            @block.sync
            def _(e):
                run("sp", e)

            @block.tensor
            def _(e):
                run("pe", e)

            @block.scalar
            def _(e):
                run("act", e)

            @block.vector
            def _(e):
                run("dve", e)

            @block.gpsimd
            def _(e):
                run("pool", e)


VR = dict(mix0=0, mix1=1, ffn0=2, ffn1=3, pw1_ba=4, pw1_bb=5, dw_b=6, ln_g=7, ln_b=8, pw2_b=9,
          dw_w=10, gconv=41)
DFF = 2816
DFFE = 3584
WB = 2048


def build_program(phases=("c0", "f0", "g1", "m1"), dbg=0, heads=tuple(range(8)), two_x=False):
    nc = bass.Bass("TRN2", target_bir_lowering=False, dynamic_dma_scratch_size=2048)

    def din(name, shape):
        return nc.dram_tensor(name, shape, F32, kind="ExternalInput").ap()

    x_d = din("x", [S, D])
    vecs_d = din("vecs", [128, D])
    fng_d = din("fng", [1, D])
    out_d = nc.dram_tensor("out", [S, D], F32, kind="ExternalOutput").ap()
    xacc_d = din("xacc", [S, D]) if two_x else None
    if "c0" in phases:
        w1_d = din("cf_pw1_w", [D, 2 * D])
        w2_d = din("cf_pw2_w", [D, D])
    if "f0" in phases:
        fg_d = din("ffn_w_gate", [D, DFF])
        fu_d = din("ffn_w_up", [D, DFF])
        fd_d = din("ffn_w_down", [DFF, D])

    if "g1" in phases:
        gin_d = din("gdn_w_in", [D, 4128])
        gout_d = din("gdn_w_out", [D, D])
        gsm_d = din("gsmall", [1, 160])
    if "m1" in phases:
        rt_d = din("moe_router", [D, 8])
        mg_d = din("moe_w_gate", [8, D, DFFE])
        mu_d = din("moe_w_up", [8, D, DFFE])
        md_d = din("moe_w_down", [8, DFFE, D])

    sc = Sched()
    with contextlib.ExitStack() as es:
        uniq = [0]

        def sb(name, shape, dt, stack=es):
            uniq[0] += 1
            return stack.enter_context(nc.sbuf_tensor("%s_%d" % (name, uniq[0]), shape, dt))

        xT = sb("xT", [128, NCH, S], F32)
        ident = sb("ident", [128, 128], F32)
        ones_f = sb("ones_f", [128, 128], F32)
        ones_b = sb("ones_b", [128, 128], BF16)
        vecs = sb("vecs_sb", [128, NCH, 64], F32)
        ps = es.enter_context(nc.psum_tensor("ps", [128, 8, 512], F32))
        NST, NWB = 2, 6
        stage = [sb("stage%d" % i, [128, WB], F32) for i in range(NST)]
        wbf = [sb("wbf%d" % i, [128, WB], BF16) for i in range(NWB)]
        cst_d = din("gconst", [128, 2048]) if "g1" in phases else None
        sm = [sb("sm%d" % i, [128, TT], F32) for i in range(4)]
        t_sm = [Tok("sm%d" % i) for i in range(4)]
        smn = [0]

        def next_sm():
            i = smn[0] % 4
            smn[0] += 1
            return i

        t_xT = [[Tok("xT%d_%d" % (c, t)) for t in range(S // 128)] for c in range(NCH)]

        def xtoks(c, t512):
            return [t_xT[c][t512 * 4 + i] for i in range(4)]
        t_ident = Tok("ident")
        t_ones = Tok("ones")
        t_vecs = Tok("vecs")
        t_fng = Tok("fng")
        t_ps = [Tok("ps%d" % i) for i in range(8)]
        t_stage = [Tok("stage%d" % i) for i in range(NST)]
        t_wbf = [Tok("wbf%d" % i) for i in range(NWB)]
        ch_stage = [sc.new_chan() for _ in range(NST)]
        ch_xin = [sc.new_chan(), sc.new_chan()]
        ch_misc = sc.new_chan()
        ch_out = [sc.new_chan(), sc.new_chan()]
        psn = [0]
        stn = [0]
        wbn = [0]

        def next_ps():
            i = psn[0] % 8
            psn[0] += 1
            return i

        cast_cfg = ["act"]

        def load_block(src, view, cast_eng=None):
            cast_eng = cast_eng or cast_cfg[0]
            a, b = src.shape[1], src.shape[2]
            n = a * b
            s = stn[0] % NST
            stn[0] += 1
            k = wbn[0] % NWB
            wbn[0] += 1
            sc.op("sp", lambda e: e.dma_start(
                out=stage[s][:, 0:n].rearrange("p (a b) -> p a b", a=a), in_=src),
                writes=[t_stage[s]], chan=ch_stage[s])
            if cast_eng == "act":
                sc.op("act", lambda e: e.activation(out=wbf[k][:, 0:n], in_=stage[s][:, 0:n],
                                                    func=AF.Copy),
                      reads=[t_stage[s]], writes=[t_wbf[k]])
            else:
                sc.op(cast_eng, lambda e: e.tensor_copy(out=wbf[k][:, 0:n], in_=stage[s][:, 0:n]),
                      reads=[t_stage[s]], writes=[t_wbf[k]])
            return wbf[k][:, 0:n].rearrange("p (a b) -> p a b", a=a), t_wbf[k]

        sc.op("pool", lambda e: e.memset(ones_f[:], 1.0), writes=[t_ones])
        sc.op("pool", lambda e: e.memset(ones_b[:], 1.0), writes=[t_ones])
        sc.op("pool", lambda e: e.affine_select(out=ident[:], in_=ones_f[:], pattern=[[-1, 128]],
                                                compare_op=ALU.is_equal, fill=0.0, base=0,
                                                channel_multiplier=1),
              reads=[t_ones], writes=[t_ident])

        def load_x_loop(src_d, xin, t_xin, fence=()):
            for tt in range(S // 128):
                sl = tt % 2
                sc.op("sp", lambda e, tt=tt, sl=sl: e.dma_start(
                    out=xin[sl][:], in_=src_d[tt * 128:(tt + 1) * 128, :]),
                    writes=[t_xin[sl]], chan=ch_xin[sl], after=fence)
                for half in range(2):
                    b = next_ps()

                    def f(e, half=half, b=b, sl=sl):
                        for j in range(4):
                            c = half * 4 + j
                            r = e.transpose(out=ps[:, b, j * 128:(j + 1) * 128],
                                            in_=xin[sl][:, c * 128:(c + 1) * 128],
                                            identity=ident[:])
                        return r
                    sc.op("pe", f, reads=[t_xin[sl], t_ident], writes=[t_ps[b]])
                    eng = "dve" if half == 0 else "act"

                    def g(e, half=half, b=b, tt=tt, eng=eng):
                        o = xT[:, half * 4:half * 4 + 4, tt * 128:(tt + 1) * 128]
                        i = ps[:, b, :].rearrange("p (j r) -> p j r", j=4)
                        if eng == "dve":
                            return e.tensor_copy(out=o, in_=i)
                        return e.activation(out=o, in_=i, func=AF.Copy)
                    sc.op(eng, g, reads=[t_ps[b]],
                          writes=[t_xT[half * 4 + j][tt] for j in range(4)])


        with contextlib.ExitStack() as es1:
            xin = [sb("xin%d" % i, [128, D], F32, es1) for i in range(2)]
            t_xin = [Tok("xin0"), Tok("xin1")]
            sc.op("sp", lambda e: e.dma_start(out=xin[1][:], in_=vecs_d[:, :]), writes=[t_xin[1]],
                  chan=ch_xin[1])
            for half in range(2):
                b = next_ps()

                def f(e, half=half, b=b):
                    for j in range(4):
                        c = half * 4 + j
                        r = e.transpose(out=ps[:, b, j * 128:(j + 1) * 128],
                                        in_=xin[1][:, c * 128:(c + 1) * 128], identity=ident[:])
                    return r
                sc.op("pe", f, reads=[t_xin[1], t_ident], writes=[t_ps[b]])
                sc.op("dve", lambda e, half=half, b=b: e.tensor_copy(
                    out=vecs[:, half * 4:half * 4 + 4, :],
                    in_=ps[:, b, :].rearrange("p (j r) -> p j r", j=4)[:, :, 0:64]),
                    reads=[t_ps[b]], writes=[t_vecs])
            load_x_loop(x_d, xin, t_xin)
        sc.barrier()

        def vcol(c, r):
            return vecs[:, c, r:r + 1]

        def rmsnorm_fm(hT, t_hT, grow, sqtmp, t_sq):
            for t in range(NTT):
                tsl = slice(t * TT, (t + 1) * TT)
                sc.op("act", lambda e, tsl=tsl: e.activation(out=sqtmp[:], in_=xT[:, :, tsl],
                                                             func=AF.Square),
                      reads=[tk for c in range(NCH) for tk in xtoks(c, t)], writes=[t_sq])
                b = next_ps()

                def f(e, b=b):
                    for c in range(NCH):
                        r = e.matmul(ps[:, b, :], lhsT=ones_b[:], rhs=sqtmp[:, c, :],
                                     start=(c == 0), stop=(c == NCH - 1))
                    return r
                sc.op("pe", f, reads=[t_sq, t_ones], writes=[t_ps[b]])
                s0 = next_sm()
                sc.op("act", lambda e, b=b, s0=s0: e.activation(out=sm[s0][:], in_=ps[:, b, :],
                                                                func=AF.Sqrt, scale=1.0 / D,
                                                                bias=1e-6),
                      reads=[t_ps[b]], writes=[t_sm[s0]])
                s1 = next_sm()
                sc.op("dve", lambda e, s0=s0, s1=s1: e.reciprocal(out=sm[s1][:], in_=sm[s0][:]),
                      reads=[t_sm[s0]], writes=[t_sm[s1]])
                for c in range(NCH):
                    sc.op("dve", lambda e, c=c, tsl=tsl, s1=s1: e.scalar_tensor_tensor(
                        out=hT[:, c, tsl], in0=xT[:, c, tsl], scalar=vcol(c, grow), in1=sm[s1][:],
                        op0=ALU.mult, op1=ALU.mult),
                        reads=xtoks(c, t) + [t_vecs, t_sm[s1]], writes=[t_hT[c][t]])

        def dump(src_fn, toks_fn):
            for c in range(NCH):
                for t in range(NTT):
                    tsl = slice(t * TT, (t + 1) * TT)
                    sc.op("dve", lambda e, c=c, tsl=tsl: e.tensor_copy(out=xT[:, c, tsl],
                                                                       in_=src_fn(c, tsl)),
                          reads=toks_fn(c, t), writes=xtoks(c, t))

        def phase_conformer():
            with contextlib.ExitStack() as esp:
                hT = sb("hT", [128, NCH, S], BF16, esp)
                t_hT = [[Tok("hT%d_%d" % (c, t)) for t in range(NTT)] for c in range(NCH)]
                sqtmp = sb("sqtmp", [128, NCH, TT], BF16, esp)
                t_sq = Tok("sqtmp")
                U = sb("ubuf", [128, NCH, S + 30], BF16, esp)
                t_U = [Tok("u%d" % c) for c in range(NCH)]
                diag = sb("diag", [128, 31, 128], BF16, esp)
                t_diag = Tok("diag")
                sig = [sb("sig%d" % i, [128, TT], F32, esp) for i in range(2)]
                t_sig = [Tok("sig0"), Tok("sig1")]
                lnst = [sb("lnst%d" % i, [128, TT], F32, esp) for i in range(3)]
                t_lnst = [Tok("lnst%d" % i) for i in range(3)]
                rmsnorm_fm(hT, t_hT, VR["mix0"], sqtmp, t_sq)
                if dbg == 1:
                    dump(lambda c, tsl: hT[:, c, tsl], lambda c, t: [t_hT[c][t]])
                    return
                sc.op("dve", lambda e: e.memset(U[:], 0.0), writes=t_U)
                sgn = 0
                for h in range(2):
                    wa = [load_block(w1_d[:, h * 512 + q * 256: h * 512 + (q + 1) * 256]
                                     .rearrange("(kc p) n -> p kc n", p=128), None) for q in range(2)]
                    wb_ = [load_block(w1_d[:, D + h * 512 + q * 256: D + h * 512 + (q + 1) * 256]
                                      .rearrange("(kc p) n -> p kc n", p=128), None) for q in range(2)]
                    for jj in range(4):
                        j = h * 4 + jj
                        wA, tA = wa[jj // 2]
                        wB, tB = wb_[jj // 2]
                        co = (jj % 2) * 128
                        for t in range(NTT):
                            tsl = slice(t * TT, (t + 1) * TT)
                            bA = next_ps()
                            bB = next_ps()

                            def f(e, wA=wA, wB=wB, co=co, bA=bA, bB=bB, tsl=tsl):
                                for kc in range(NCH):
                                    e.matmul(ps[:, bA, :], lhsT=wA[:, kc, co:co + 128],
                                             rhs=hT[:, kc, tsl], start=(kc == 0),
                                             stop=(kc == NCH - 1))
                                for kc in range(NCH):
                                    r = e.matmul(ps[:, bB, :], lhsT=wB[:, kc, co:co + 128],
                                                 rhs=hT[:, kc, tsl], start=(kc == 0),
                                                 stop=(kc == NCH - 1))
                                return r
                            sc.op("pe", f, reads=[tA, tB] + [t_hT[c][t] for c in range(NCH)],
                                  writes=[t_ps[bA], t_ps[bB]])
                            sg = sgn % 2
                            sgn += 1
                            sc.op("act", lambda e, bB=bB, sg=sg, j=j: e.activation(
                                out=sig[sg][:], in_=ps[:, bB, :], func=AF.Sigmoid,
                                bias=vcol(j, VR["pw1_bb"])),
                                reads=[t_ps[bB], t_vecs], writes=[t_sig[sg]])
                            sc.op("dve", lambda e, bA=bA, sg=sg, j=j, t=t: e.scalar_tensor_tensor(
                                out=U[:, j, 15 + t * TT:15 + (t + 1) * TT], in0=ps[:, bA, :],
                                scalar=vcol(j, VR["pw1_ba"]), in1=sig[sg][:],
                                op0=ALU.add, op1=ALU.mult),
                                reads=[t_ps[bA], t_sig[sg], t_vecs], writes=[t_U[j]])
                if dbg == 2:
                    dump(lambda c, tsl: U[:, c, 15 + tsl.start:15 + tsl.stop], lambda c, t: [t_U[c]])
                    return
                for j in range(NCH):
                    sc.op("dve", lambda e, j=j: e.tensor_tensor(
                        out=diag[:], in0=ident[:].unsqueeze(1).to_broadcast([128, 31, 128]),
                        in1=vecs[:, j, VR["dw_w"]:VR["dw_w"] + 31].unsqueeze(2)
                        .to_broadcast([128, 31, 128]), op=ALU.mult),
                        reads=[t_ident, t_vecs], writes=[t_diag])
                    for t in range(NTT):
                        b = next_ps()

                        def f(e, j=j, t=t, b=b):
                            for k in range(31):
                                r = e.matmul(ps[:, b, :], lhsT=diag[:, k, :],
                                             rhs=U[:, j, t * TT + k:t * TT + k + TT],
                                             start=(k == 0), stop=(k == 30))
                            return r
                        sc.op("pe", f, reads=[t_diag, t_U[j]], writes=[t_ps[b]])
                        sc.op("act", lambda e, j=j, t=t, b=b: e.activation(
                            out=hT[:, j, t * TT:(t + 1) * TT], in_=ps[:, b, :], func=AF.Identity,
                            bias=vcol(j, VR["dw_b"])),
                            reads=[t_ps[b], t_vecs], writes=[t_hT[j][t]])
                if dbg == 3:
                    dump(lambda c, tsl: hT[:, c, tsl], lambda c, t: [t_hT[c][t]])
                    return
                for t in range(NTT):
                    tsl = slice(t * TT, (t + 1) * TT)
                    sc.op("dve", lambda e, tsl=tsl: e.tensor_tensor(
                        out=sqtmp[:], in0=hT[:, :, tsl], in1=hT[:, :, tsl], op=ALU.mult),
                        reads=[t_hT[c][t] for c in range(NCH)], writes=[t_sq])
                    b1 = next_ps()
                    b2 = next_ps()

                    def f(e, b1=b1, b2=b2, tsl=tsl):
                        for c in range(NCH):
                            e.matmul(ps[:, b1, :], lhsT=ones_b[:], rhs=hT[:, c, tsl],
                                     start=(c == 0), stop=(c == NCH - 1))
                        for c in range(NCH):
                            r = e.matmul(ps[:, b2, :], lhsT=ones_b[:], rhs=sqtmp[:, c, :],
                                         start=(c == 0), stop=(c == NCH - 1))
                        return r
                    sc.op("pe", f, reads=[t_sq, t_ones] + [t_hT[c][t] for c in range(NCH)],
                          writes=[t_ps[b1], t_ps[b2]])
                    m = lnst[0]
                    q = lnst[1]
                    rs = lnst[2]
                    tm, tq, trs = t_lnst
                    sc.op("act", lambda e, m=m, b1=b1: e.activation(
                        out=m[:], in_=ps[:, b1, :], func=AF.Copy, scale=1.0 / D),
                        reads=[t_ps[b1]], writes=[tm])
                    sc.op("dve", lambda e, m=m, q=q: e.tensor_tensor(
                        out=q[:], in0=m[:], in1=m[:], op=ALU.mult),
                        reads=[tm], writes=[tq])
                    sc.op("dve", lambda e, q=q, b2=b2: e.scalar_tensor_tensor(
                        out=q[:], in0=ps[:, b2, :], scalar=1.0 / D, in1=q[:],
                        op0=ALU.mult, op1=ALU.subtract),
                        reads=[t_ps[b2], tq], writes=[tq])
                    sc.op("act", lambda e, q=q: e.activation(
                        out=q[:], in_=q[:], func=AF.Sqrt, bias=1e-5),
                        reads=[tq], writes=[tq])
                    sc.op("dve", lambda e, q=q, rs=rs: e.reciprocal(out=rs[:], in_=q[:]),
                          reads=[tq], writes=[trs])
                    sc.op("dve", lambda e, m=m, rs=rs: e.scalar_tensor_tensor(
                        out=m[:], in0=m[:], scalar=-1.0, in1=rs[:],
                        op0=ALU.mult, op1=ALU.mult),
                        reads=[tm, trs], writes=[tm])
                    for c in range(NCH):
                        w1s = next_sm()
                        sc.op("dve", lambda e, c=c, tsl=tsl, rs=rs, w1s=w1s: e.tensor_tensor(
                            out=sm[w1s][:], in0=hT[:, c, tsl], in1=rs[:], op=ALU.mult),
                            reads=[t_hT[c][t], trs], writes=[t_sm[w1s]])
                        sc.op("dve", lambda e, m=m, w1s=w1s: e.tensor_tensor(
                            out=sm[w1s][:], in0=sm[w1s][:], in1=m[:], op=ALU.add),
                            reads=[t_sm[w1s], tm], writes=[t_sm[w1s]])
                        sc.op("act", lambda e, c=c, tsl=tsl, w1s=w1s: e.activation(
                            out=hT[:, c, tsl], in_=sm[w1s][:], func=AF.Silu,
                            scale=vcol(c, VR["ln_g"]), bias=vcol(c, VR["ln_b"])),
                            reads=[t_sm[w1s], t_vecs], writes=[t_hT[c][t]])
                if dbg == 4:
                    dump(lambda c, tsl: hT[:, c, tsl], lambda c, t: [t_hT[c][t]])
                    return
                for h in range(2):
                    w2 = [load_block(w2_d[:, h * 512 + q * 256: h * 512 + (q + 1) * 256]
                                     .rearrange("(kc p) n -> p kc n", p=128), None) for q in range(2)]
                    for jj in range(4):
                        j = h * 4 + jj
                        wA, tA = w2[jj // 2]
                        co = (jj % 2) * 128
                        for t in range(NTT):
                            tsl = slice(t * TT, (t + 1) * TT)
                            b = next_ps()

                            def f(e, wA=wA, co=co, b=b, tsl=tsl):
                                for kc in range(NCH):
                                    r = e.matmul(ps[:, b, :], lhsT=wA[:, kc, co:co + 128],
                                                 rhs=hT[:, kc, tsl], start=(kc == 0),
                                                 stop=(kc == NCH - 1))
                                return r
                            sc.op("pe", f, reads=[tA] + [t_hT[c][t] for c in range(NCH)],
                                  writes=[t_ps[b]])
                            sc.op("dve", lambda e, b=b, j=j, tsl=tsl: e.scalar_tensor_tensor(
                                out=xT[:, j, tsl], in0=ps[:, b, :], scalar=vcol(j, VR["pw2_b"]),
                                in1=xT[:, j, tsl], op0=ALU.add, op1=ALU.add),
                                reads=[t_ps[b], t_vecs] + xtoks(j, t), writes=xtoks(j, t))

        def swiglu_stream(hT, t_hT, h1, t_h1, sg, t_sg, wg_d, wu_d, wd_d, dff,
                          gate_bc=None, t_gate=None):
            ngrp = (dff + 511) // 512
            sgn = 0
            for g in range(ngrp):
                c0 = g * 512
                ncol = min(512, dff - c0)
                nblk = ncol // 256
                nf = ncol // 128
                wg = [load_block(wg_d[:, c0 + q * 256:c0 + (q + 1) * 256]
                                 .rearrange("(kc p) n -> p kc n", p=128), None) for q in range(nblk)]
                wu = [load_block(wu_d[:, c0 + q * 256:c0 + (q + 1) * 256]
                                 .rearrange("(kc p) n -> p kc n", p=128), None) for q in range(nblk)]
                wd = [load_block(wd_d[c0 + q * 256:c0 + (q + 1) * 256, :]
                                 .rearrange("(fc p) n -> p fc n", p=128), None) for q in range(nblk)]
                for t in range(NTT):
                    tsl = slice(t * TT, (t + 1) * TT)
                    for f_ in range(nf):
                        wG, tG = wg[f_ // 2]
                        wU, tU = wu[f_ // 2]
                        co = (f_ % 2) * 128
                        bG = next_ps()
                        bU = next_ps()

                        def f(e, wG=wG, wU=wU, co=co, bG=bG, bU=bU, tsl=tsl):
                            for kc in range(NCH):
                                e.matmul(ps[:, bG, :], lhsT=wG[:, kc, co:co + 128],
                                         rhs=hT[:, kc, tsl], start=(kc == 0), stop=(kc == NCH - 1))
                            for kc in range(NCH):
                                r = e.matmul(ps[:, bU, :], lhsT=wU[:, kc, co:co + 128],
                                             rhs=hT[:, kc, tsl], start=(kc == 0),
                                             stop=(kc == NCH - 1))
                            return r
                        sc.op("pe", f, reads=[tG, tU] + [t_hT[c][t] for c in range(NCH)],
                              writes=[t_ps[bG], t_ps[bU]])
                        s_ = sgn % 2
                        sgn += 1
                        sc.op("act", lambda e, bG=bG, s_=s_: e.activation(
                            out=sg[s_][:], in_=ps[:, bG, :], func=AF.Silu),
                            reads=[t_ps[bG]], writes=[t_sg[s_]])
                        if gate_bc is None:
                            sc.op("dve", lambda e, bU=bU, s_=s_, f_=f_, tsl=tsl: e.tensor_tensor(
                                out=h1[:, f_, tsl], in0=ps[:, bU, :], in1=sg[s_][:], op=ALU.mult),
                                reads=[t_ps[bU], t_sg[s_]], writes=[t_h1[f_][t]])
                        else:
                            sc.op("dve", lambda e, s_=s_, tsl=tsl: e.tensor_tensor(
                                out=sg[s_][:], in0=sg[s_][:], in1=gate_bc[:, tsl], op=ALU.mult),
                                reads=[t_sg[s_], t_gate], writes=[t_sg[s_]])
                            sc.op("dve", lambda e, bU=bU, s_=s_, f_=f_, tsl=tsl: e.tensor_tensor(
                                out=h1[:, f_, tsl], in0=ps[:, bU, :], in1=sg[s_][:], op=ALU.mult),
                                reads=[t_ps[bU], t_sg[s_]], writes=[t_h1[f_][t]])
                for t in range(NTT):
                    tsl = slice(t * TT, (t + 1) * TT)
                    for j in range(NCH):
                        b = next_ps()

                        def f(e, b=b, j=j, tsl=tsl, wd=wd, nf=nf):
                            for f_ in range(nf):
                                wD, _ = wd[f_ // 2]
                                r = e.matmul(ps[:, b, :],
                                             lhsT=wD[:, f_ % 2, j * 128:(j + 1) * 128],
                                             rhs=h1[:, f_, tsl], start=(f_ == 0),
                                             stop=(f_ == nf - 1))
                            return r
                        sc.op("pe", f, reads=[w[1] for w in wd] + [t_h1[f_][t] for f_ in range(nf)],
                              writes=[t_ps[b]])
                        sc.op("dve", lambda e, b=b, j=j, tsl=tsl: e.tensor_tensor(
                            out=xT[:, j, tsl], in0=ps[:, b, :], in1=xT[:, j, tsl], op=ALU.add),
                            reads=[t_ps[b]] + xtoks(j, t), writes=xtoks(j, t))

        def phase_ffn():
            with contextlib.ExitStack() as esp:
                hT = sb("hT", [128, NCH, S], BF16, esp)
                t_hT = [[Tok("hT%d_%d" % (c, t)) for t in range(NTT)] for c in range(NCH)]
                sqtmp = sb("sqtmp", [128, NCH, TT], BF16, esp)
                t_sq = Tok("sqtmp")
                h1 = sb("h1", [128, 4, S], BF16, esp)
                t_h1 = [[Tok("h1_%d_%d" % (f_, t)) for t in range(NTT)] for f_ in range(4)]
                sg = [sb("sg%d" % i, [128, TT], F32, esp) for i in range(2)]
                t_sg = [Tok("sg0"), Tok("sg1")]
                rmsnorm_fm(hT, t_hT, VR["ffn0"], sqtmp, t_sq)
                swiglu_stream(hT, t_hT, h1, t_h1, sg, t_sg, fg_d, fu_d, fd_d, DFF)


        def phase_gdn():
            NT16 = S // 128
            with contextlib.ExitStack() as esp:
                hT = sb("hT", [128, NCH, S], BF16, esp)
                t_hT = [[Tok("hT%d_%d" % (c, t)) for t in range(NTT)] for c in range(NCH)]
                with contextlib.ExitStack() as esq:
                    sqtmp = sb("sqtmp", [128, NCH, TT], BF16, esq)
                    t_sq = Tok("sqtmp")
                    rmsnorm_fm(hT, t_hT, VR["mix1"], sqtmp, t_sq)
                sc.barrier()
                if two_x:
                    with contextlib.ExitStack() as esx:
                        xin2 = [sb("xin2_%d" % i, [128, D], F32, esx) for i in range(2)]
                        t_xin2 = [Tok("xin2_0"), Tok("xin2_1")]
                        load_x_loop(xacc_d, xin2, t_xin2, fence=sc.last_ops())
                    sc.barrier()
                cst = sb("cst", [128, 2048], F32, esp)
                t_cst = Tok("cst")
                gsm = sb("gsm", [128, 160], F32, esp)
                t_gsm = Tok("gsm")
                graw = sb("graw", [128, NT16, 32], F32, esp)
                beta = sb("beta", [128, NT16, 16], F32, esp)
                la = sb("la", [128, NT16, 16], F32, esp)
                gcol = sb("gcol", [128, NT16, 16], F32, esp)
                glast = sb("glast", [128, NT16, 16], F32, esp)
                beg = sb("beg", [128, NT16, 16], F32, esp)
                kdc = sb("kdc", [128, NT16, 16], F32, esp)
                egl = sb("egl", [128, NT16, 16], F32, esp)
                t_graw, t_beta, t_la, t_gcol, t_glast, t_beg, t_kdc, t_egl = [
                    Tok(n) for n in ("graw", "beta", "la", "gcol", "glast", "beg", "kdc", "egl")]
                pre = sb("pre", [128, S + 4], BF16, esp)
                t_pre = Tok("pre")
                diag5 = sb("diag5", [128, 5, 128], BF16, esp)
                t_diag5 = Tok("diag5")
                QKV = [sb("qkv%d" % i, [128, S], F32, esp) for i in range(3)]
                t_QKV = [[Tok("qkv%d_%d" % (i, t)) for t in range(NTT)] for i in range(3)]
                zw = sb("zw", [128, NCH, 128], BF16, esp)
                t_zw = Tok("zw")
                Oacc = sb("Oacc", [128, NT16, 128], F32, esp)
                t_O = [Tok("O%d" % n) for n in range(NT16)]
                oT = sb("oT", [128, S], BF16, esp)
                t_oT = [Tok("oT%d" % t) for t in range(NTT)]
                Sst = sb("Sst", [128, 128], F32, esp)
                t_S = Tok("S")
                names = ("bV", "Kt", "KD", "lacb", "bcb", "dec", "QDT", "AT", "PT", "Dm", "DTm",
                         "X", "WK", "U", "sz", "yy")
                shp = dict(WK=[128, 256])
                T_ = {n: sb(n, shp.get(n, [128, 128]), F32, esp) for n in names}
                t_T = {n: Tok(n) for n in names}
                ETall = sb("ETall", [128, 7, 128], F32, esp)
                t_ET = Tok("ETall")
                st4 = sb("st4", [128, 4], F32, esp)
                t_st4 = Tok("st4")
                ch_c = sc.new_chan()
                fence = sc.last_ops()
                sc.op("sp", lambda e: e.dma_start(out=cst[:], in_=cst_d[:, :]), writes=[t_cst],
                      chan=ch_c, after=fence)
                sc.op("sp", lambda e: e.dma_start(out=gsm[:],
                                                  in_=gsm_d[0:1, :].partition_broadcast(128)),
                      writes=[t_gsm], chan=ch_c, after=fence)
                triX = [cst[:, 0:128], cst[:, 128:256]]
                MX = [cst[:, 256:256 + 896].rearrange("p (k i) -> p k i", k=7),
                      cst[:, 1152:1152 + 896].rearrange("p (k i) -> p k i", k=7)]
                sc.op("dve", lambda e: e.memset(pre[:], 0.0), writes=[t_pre])
                wgt, t_wgt = load_block(gin_d[:, 4096:4128].rearrange("(kc p) n -> p kc n", p=128),
                                        None)
                for n in range(NT16):
                    b = next_ps()
                    tsl = slice(n * 128, (n + 1) * 128)

                    def f(e, b=b, tsl=tsl):
                        for kc in range(NCH):
                            r = e.matmul(ps[:, b, 0:32], lhsT=hT[:, kc, tsl], rhs=wgt[:, kc, :],
                                         start=(kc == 0), stop=(kc == NCH - 1))
                        return r
                    sc.op("pe", f, reads=[t_wgt] + [t_hT[c][n // 4] for c in range(NCH)],
                          writes=[t_ps[b]])
                    sc.op("dve", lambda e, b=b, n=n: e.tensor_copy(out=graw[:, n, :],
                                                                   in_=ps[:, b, 0:32]),
                          reads=[t_ps[b]], writes=[t_graw])
                sc.op("act", lambda e: e.activation(out=beta[:], in_=graw[:, :, 0:16],
                                                    func=AF.Sigmoid),
                      reads=[t_graw], writes=[t_beta])
                sc.op("dve", lambda e: e.tensor_tensor(
                    out=la[:], in0=graw[:, :, 16:32],
                    in1=gsm[:, 16:32].unsqueeze(1).to_broadcast([128, NT16, 16]), op=ALU.add),
                    reads=[t_graw, t_gsm], writes=[t_la])
                sc.op("act", lambda e: e.activation(out=la[:], in_=la[:], func=AF.Exp),
                      reads=[t_la], writes=[t_la])
                sc.op("act", lambda e: e.activation(out=la[:], in_=la[:], func=AF.Ln, bias=1.0),
                      reads=[t_la], writes=[t_la])
                sc.op("act", lambda e: e.activation(out=gsm[:, 0:16], in_=gsm[:, 0:16],
                                                    func=AF.Exp),
                      reads=[t_gsm], writes=[t_gsm])
                sc.op("dve", lambda e: e.scalar_tensor_tensor(
                    out=la[:], in0=la[:], scalar=-1.0,
                    in1=gsm[:, 0:16].unsqueeze(1).to_broadcast([128, NT16, 16]),
                    op0=ALU.mult, op1=ALU.mult), reads=[t_la, t_gsm], writes=[t_la])
                for n in range(NT16):
                    b = next_ps()

                    def f(e, b=b, n=n):
                        e.matmul(ps[:, b, 0:8], lhsT=triX[0], rhs=la[:, n, 0:8], start=True,
                                 stop=True)
                        e.matmul(ps[:, b, 8:16], lhsT=triX[1], rhs=la[:, n, 8:16], start=True,
                                 stop=True)
                        return e.matmul(ps[:, b, 16:32], lhsT=ones_f[:], rhs=la[:, n, :],
                                        start=True, stop=True)
                    sc.op("pe", f, reads=[t_la, t_cst, t_ones], writes=[t_ps[b]])
                    sc.op("dve", lambda e, b=b, n=n: e.tensor_copy(out=gcol[:, n, :],
                                                                   in_=ps[:, b, 0:16]),
                          reads=[t_ps[b]], writes=[t_gcol])
                    sc.op("dve", lambda e, b=b, n=n: e.tensor_copy(out=glast[:, n, :],
                                                                   in_=ps[:, b, 16:32]),
                          reads=[t_ps[b]], writes=[t_glast])
                sc.op("act", lambda e: e.activation(out=beg[:], in_=gcol[:], func=AF.Exp),
                      reads=[t_gcol], writes=[t_beg])
                sc.op("dve", lambda e: e.tensor_tensor(out=beg[:], in0=beg[:], in1=beta[:],
                                                       op=ALU.mult),
                      reads=[t_beg, t_beta], writes=[t_beg])
                sc.op("dve", lambda e: e.tensor_tensor(out=kdc[:], in0=glast[:], in1=gcol[:],
                                                       op=ALU.subtract),
                      reads=[t_glast, t_gcol], writes=[t_kdc])
                sc.op("act", lambda e: e.activation(out=kdc[:], in_=kdc[:], func=AF.Exp),
                      reads=[t_kdc], writes=[t_kdc])
                sc.op("act", lambda e: e.activation(out=egl[:], in_=glast[:], func=AF.Exp),
                      reads=[t_glast], writes=[t_egl])

                def tt_(n, out, in0, in1, op, eng="dve", rd=(), wr=()):
                    sc.op(eng, lambda e: e.tensor_tensor(out=out, in0=in0, in1=in1, op=op),
                          reads=list(rd), writes=list(wr))

                if dbg == 11:
                    return
                for h in heads:
                    for part in range(3):
                        cidx = part * 8 + h
                        wv, t_wv = load_block(gin_d[:, cidx * 128:(cidx + 1) * 128]
                                              .rearrange("(kc p) n -> p kc n", p=128), None)
                        for k in range(5):
                            sc.op("dve", lambda e, k=k, part=part, h=h: e.tensor_scalar(
                                out=diag5[:, k, :], in0=ident[:],
                                scalar1=vcol(h, VR["gconv"] + k * 3 + part), scalar2=None,
                                op0=ALU.mult), reads=[t_ident, t_vecs], writes=[t_diag5])
                        for t in range(NTT):
                            tsl = slice(t * TT, (t + 1) * TT)
                            b = next_ps()

                            def f(e, b=b, tsl=tsl, wv=wv):
                                for kc in range(NCH):
                                    r = e.matmul(ps[:, b, :], lhsT=wv[:, kc, :], rhs=hT[:, kc, tsl],
                                                 start=(kc == 0), stop=(kc == NCH - 1))
                                return r
                            sc.op("pe", f, reads=[t_wv] + [t_hT[c][t] for c in range(NCH)],
                                  writes=[t_ps[b]])
                            sc.op("act", lambda e, b=b, t=t: e.activation(
                                out=pre[:, 2 + t * TT:2 + (t + 1) * TT], in_=ps[:, b, :],
                                func=AF.Copy), reads=[t_ps[b]], writes=[t_pre])
                        for t in range(NTT):
                            tsl = slice(t * TT, (t + 1) * TT)
                            b = next_ps()

                            def f(e, b=b, t=t):
                                for k in range(5):
                                    r = e.matmul(ps[:, b, :], lhsT=diag5[:, k, :],
                                                 rhs=pre[:, t * TT + k:t * TT + k + TT],
                                                 start=(k == 0), stop=(k == 4))
                                return r
                            sc.op("pe", f, reads=[t_diag5, t_pre], writes=[t_ps[b]])
                            sc.op("act", lambda e, b=b, tsl=tsl, part=part: e.activation(
                                out=QKV[part][:, tsl], in_=ps[:, b, :], func=AF.Silu),
                                reads=[t_ps[b]], writes=[t_QKV[part][t]])
                            if part < 2:
                                la_ = next_sm()
                                lb_ = next_sm()
                                sc.op("dve", lambda e, tsl=tsl, part=part, la_=la_: e.tensor_tensor(
                                    out=sm[la_][:], in0=QKV[part][:, tsl], in1=QKV[part][:, tsl],
                                    op=ALU.mult), reads=[t_QKV[part][t]], writes=[t_sm[la_]])
                                b2 = next_ps()
                                sc.op("pe", lambda e, b2=b2, la_=la_: e.matmul(
                                    ps[:, b2, :], lhsT=ones_f[:], rhs=sm[la_][:], start=True,
                                    stop=True), reads=[t_sm[la_], t_ones], writes=[t_ps[b2]])
                                sc.op("act", lambda e, b2=b2, lb_=lb_: e.activation(
                                    out=sm[lb_][:], in_=ps[:, b2, :], func=AF.Sqrt, bias=1e-6),
                                    reads=[t_ps[b2]], writes=[t_sm[lb_]])
                                sc.op("dve", lambda e, lb_=lb_: e.reciprocal(out=sm[lb_][:],
                                                                             in_=sm[lb_][:]),
                                      reads=[t_sm[lb_]], writes=[t_sm[lb_]])
                                scl = (128.0 ** -0.5) if part == 0 else 1.0
                                sc.op("dve", lambda e, tsl=tsl, part=part, scl=scl, lb_=lb_:
                                      e.scalar_tensor_tensor(
                                          out=QKV[part][:, tsl], in0=QKV[part][:, tsl], scalar=scl,
                                          in1=sm[lb_][:], op0=ALU.mult, op1=ALU.mult),
                                      reads=[t_QKV[part][t], t_sm[lb_]], writes=[t_QKV[part][t]])
                    if dbg == 12:
                        return
                    zwv, t_zwv = load_block(gin_d[:, 3072 + h * 128:3072 + (h + 1) * 128]
                                            .rearrange("(kc p) n -> p kc n", p=128), None)
                    sc.op("dve", lambda e, zwv=zwv: e.tensor_copy(out=zw[:], in_=zwv),
                          reads=[t_zwv], writes=[t_zw])
                    QT, KT, VT = QKV
                    for dr in range(2):
                        sc.op("dve", lambda e: e.memset(Sst[:], 0.0), writes=[t_S])
                        order = range(NT16) if dr == 0 else range(NT16 - 1, -1, -1)
                        dh = dr * 8 + h
                        for n in order:
                            csl = slice(n * 128, (n + 1) * 128)
                            t4 = n // 4
                            rq = [t_QKV[0][t4]]
                            rk = [t_QKV[1][t4]]
                            rv = [t_QKV[2][t4]]
                            bt = next_ps()

                            def f(e, bt=bt, csl=csl):
                                e.transpose(out=ps[:, bt, 0:128], in_=KT[:, csl], identity=ident[:])
                                return e.transpose(out=ps[:, bt, 128:256], in_=VT[:, csl],
                                                   identity=ident[:])
                            sc.op("pe", f, reads=rk + rv + [t_ident], writes=[t_ps[bt]])
                            sc.op("dve", lambda e, bt=bt, n=n, dh=dh: e.tensor_scalar(
                                out=T_["bV"][:], in0=ps[:, bt, 128:256],
                                scalar1=beta[:, n, dh:dh + 1], scalar2=None, op0=ALU.mult),
                                reads=[t_ps[bt], t_beta], writes=[t_T["bV"]])
                            sc.op("dve", lambda e, bt=bt, n=n, dh=dh: e.tensor_scalar(
                                out=T_["Kt"][:], in0=ps[:, bt, 0:128],
                                scalar1=beg[:, n, dh:dh + 1], scalar2=None, op0=ALU.mult),
                                reads=[t_ps[bt], t_beg], writes=[t_T["Kt"]])
                            sc.op("dve", lambda e, bt=bt, n=n, dh=dh: e.tensor_scalar(
                                out=T_["KD"][:], in0=ps[:, bt, 0:128],
                                scalar1=kdc[:, n, dh:dh + 1], scalar2=None, op0=ALU.mult),
                                reads=[t_ps[bt], t_kdc], writes=[t_T["KD"]])
                            sc.op("dve", lambda e, n=n, dh=dh: e.tensor_scalar(
                                out=T_["lacb"][:], in0=ones_f[:], scalar1=la[:, n, dh:dh + 1],
                                scalar2=None, op0=ALU.mult),
                                reads=[t_la, t_ones], writes=[t_T["lacb"]])
                            sc.op("dve", lambda e, n=n, dh=dh: e.tensor_scalar(
                                out=T_["bcb"][:], in0=ones_f[:], scalar1=beta[:, n, dh:dh + 1],
                                scalar2=None, op0=ALU.mult),
                                reads=[t_beta, t_ones], writes=[t_T["bcb"]])
                            bm = next_ps()

                            def f(e, bm=bm, csl=csl, dr=dr):
                                e.matmul(ps[:, bm, 0:128], lhsT=T_["lacb"][:], rhs=triX[dr],
                                         start=True, stop=True)
                                e.matmul(ps[:, bm, 128:256], lhsT=T_["bcb"][:], rhs=ident[:],
                                         start=True, stop=True)
                                e.matmul(ps[:, bm, 256:384], lhsT=KT[:, csl], rhs=KT[:, csl],
                                         start=True, stop=True)
                                return e.matmul(ps[:, bm, 384:512], lhsT=KT[:, csl],
                                                rhs=QT[:, csl], start=True, stop=True)
                            sc.op("pe", f, reads=[t_T["lacb"], t_T["bcb"], t_cst, t_ident] + rk + rq,
                                  writes=[t_ps[bm]])
                            sc.op("dve", lambda e, bm=bm, n=n, dh=dh: e.tensor_scalar(
                                out=T_["dec"][:], in0=ps[:, bm, 0:128],
                                scalar1=gcol[:, n, dh:dh + 1], scalar2=None, op0=ALU.subtract),
                                reads=[t_ps[bm], t_gcol], writes=[t_T["dec"]])
                            sc.op("dve", lambda e: e.tensor_scalar(
                                out=T_["dec"][:], in0=T_["dec"][:], scalar1=0.0, scalar2=None,
                                op0=ALU.min), reads=[t_T["dec"]], writes=[t_T["dec"]])
                            sc.op("act", lambda e: e.activation(out=T_["dec"][:], in_=T_["dec"][:],
                                                                func=AF.Exp),
                                  reads=[t_T["dec"]], writes=[t_T["dec"]])
                            sc.op("act", lambda e, bm=bm: e.activation(
                                out=T_["QDT"][:], in_=ps[:, bm, 0:128], func=AF.Exp),
                                reads=[t_ps[bm]], writes=[t_T["QDT"]])
                            sc.op("dve", lambda e, csl=csl: e.tensor_tensor(
                                out=T_["QDT"][:], in0=T_["QDT"][:], in1=QT[:, csl], op=ALU.mult),
                                reads=[t_T["QDT"]] + rq, writes=[t_T["QDT"]])
                            sc.op("dve", lambda e, bm=bm: e.tensor_tensor(
                                out=T_["AT"][:], in0=ps[:, bm, 256:384], in1=T_["dec"][:],
                                op=ALU.mult), reads=[t_ps[bm], t_T["dec"]], writes=[t_T["AT"]])
                            sc.op("dve", lambda e, bm=bm: e.tensor_tensor(
                                out=T_["AT"][:], in0=ps[:, bm, 128:256], in1=T_["AT"][:],
                                op=ALU.mult), reads=[t_ps[bm], t_T["AT"]], writes=[t_T["AT"]])
                            sc.op("dve", lambda e, bm=bm: e.tensor_tensor(
                                out=T_["PT"][:], in0=ps[:, bm, 384:512], in1=T_["dec"][:],
                                op=ALU.mult), reads=[t_ps[bm], t_T["dec"]], writes=[t_T["PT"]])
                            sc.op("dve", lambda e, dr=dr: e.tensor_tensor(
                                out=T_["PT"][:], in0=T_["PT"][:], in1=triX[dr], op=ALU.mult),
                                reads=[t_T["PT"], t_cst], writes=[t_T["PT"]])
                            for lv in range(7):
                                sc.op("dve", lambda e, dr=dr, lv=lv: e.tensor_tensor(
                                    out=ETall[:, lv, :], in0=T_["AT"][:], in1=MX[dr][:, lv, :],
                                    op=ALU.mult), reads=[t_T["AT"], t_cst], writes=[t_ET])
                            if dbg == 13:
                                return
                            for lv in range(7):
                                Dc = ident if lv == 0 else T_["Dm"]
                                DTc = ident if lv == 0 else T_["DTm"]
                                rD = [t_ident] if lv == 0 else [t_T["Dm"]]
                                rDT = [t_ident] if lv == 0 else [t_T["DTm"]]
                                bx = next_ps()
                                sc.op("pe", lambda e, bx=bx, lv=lv, Dc=Dc: e.matmul(
                                    ps[:, bx, 0:128], lhsT=ETall[:, lv, :], rhs=Dc[:], start=True,
                                    stop=True), reads=[t_ET] + rD, writes=[t_ps[bx]])
                                sc.op("act", lambda e, bx=bx: e.activation(
                                    out=T_["X"][:], in_=ps[:, bx, 0:128], func=AF.Copy),
                                    reads=[t_ps[bx]], writes=[t_T["X"]])
                                by = next_ps()

                                def f(e, by=by, Dc=Dc, DTc=DTc):
                                    e.matmul(ps[:, by, 0:128], lhsT=DTc[:], rhs=T_["X"][:],
                                             start=True, stop=True)
                                    return e.matmul(ps[:, by, 128:256], lhsT=T_["X"][:], rhs=DTc[:],
                                                    start=True, stop=True)
                                sc.op("pe", f, reads=[t_T["X"]] + rDT, writes=[t_ps[by]])
                                sc.op("dve", lambda e, by=by, Dc=Dc: e.tensor_tensor(
                                    out=T_["Dm"][:], in0=Dc[:], in1=ps[:, by, 0:128],
                                    op=ALU.subtract), reads=[t_ps[by]] + rD, writes=[t_T["Dm"]])
                                sc.op("dve", lambda e, by=by, DTc=DTc: e.tensor_tensor(
                                    out=T_["DTm"][:], in0=DTc[:], in1=ps[:, by, 128:256],
                                    op=ALU.subtract), reads=[t_ps[by]] + rDT, writes=[t_T["DTm"]])
                            if dbg == 14:
                                return
                            bw = next_ps()

                            def f(e, bw=bw):
                                e.matmul(ps[:, bw, 0:128], lhsT=T_["DTm"][:], rhs=T_["bV"][:],
                                         start=True, stop=True)
                                return e.matmul(ps[:, bw, 128:256], lhsT=T_["Kt"][:],
                                                rhs=T_["DTm"][:], start=True, stop=True)
                            sc.op("pe", f, reads=[t_T["DTm"], t_T["bV"], t_T["Kt"]],
                                  writes=[t_ps[bw]])
                            sc.op("act", lambda e, bw=bw: e.activation(
                                out=T_["WK"][:], in_=ps[:, bw, 0:256], func=AF.Copy),
                                reads=[t_ps[bw]], writes=[t_T["WK"]])
                            if dbg == 21:
                                return
                            bs = next_ps()
                            sc.op("pe", lambda e, bs=bs: e.matmul(
                                ps[:, bs, 0:128], lhsT=T_["WK"][:, 128:256], rhs=Sst[:], start=True,
                                stop=True), reads=[t_T["WK"], t_S], writes=[t_ps[bs]])
                            sc.op("dve", lambda e, bs=bs: e.tensor_tensor(
                                out=T_["U"][:], in0=T_["WK"][:, 0:128], in1=ps[:, bs, 0:128],
                                op=ALU.subtract), reads=[t_ps[bs], t_T["WK"]], writes=[t_T["U"]])
                            if dbg == 22:
                                return
                            bo = next_ps()

                            def f(e, bo=bo):
                                e.matmul(ps[:, bo, 0:128], lhsT=T_["QDT"][:], rhs=Sst[:], start=True,
                                         stop=False)
                                e.matmul(ps[:, bo, 0:128], lhsT=T_["PT"][:], rhs=T_["U"][:],
                                         start=False, stop=True)
                                return e.matmul(ps[:, bo, 128:256], lhsT=T_["KD"][:], rhs=T_["U"][:],
                                                start=True, stop=True)
                            sc.op("pe", f, reads=[t_T["QDT"], t_S, t_T["PT"], t_T["U"], t_T["KD"]],
                                  writes=[t_ps[bo]])
                            if dbg == 23:
                                return
                            if dr == 0:
                                sc.op("act", lambda e, bo=bo, n=n: e.activation(
                                    out=Oacc[:, n, :], in_=ps[:, bo, 0:128], func=AF.Copy),
                                    reads=[t_ps[bo]], writes=[t_O[n]])
                            else:
                                sc.op("dve", lambda e, bo=bo, n=n: e.tensor_tensor(
                                    out=Oacc[:, n, :], in0=ps[:, bo, 0:128], in1=Oacc[:, n, :],
                                    op=ALU.add), reads=[t_ps[bo], t_O[n]], writes=[t_O[n]])
                            if dbg == 26:
                                return
                            sc.op("dve", lambda e, n=n, dh=dh: e.tensor_scalar(
                                out=Sst[:], in0=Sst[:], scalar1=egl[:, n, dh:dh + 1], scalar2=None,
                                op0=ALU.mult), reads=[t_S, t_egl], writes=[t_S])
                            sc.op("dve", lambda e, bo=bo: e.tensor_tensor(
                                out=Sst[:], in0=ps[:, bo, 128:256], in1=Sst[:], op=ALU.add),
                                reads=[t_ps[bo], t_S], writes=[t_S])
                            if dbg == 24:
                                return
                            if dbg == 25 and n == 1:
                                return
                        if dbg == 15:
                            return
                    if dbg == 16:
                        return
                    for q in range(NTT):
                        bT = next_ps()
                        for i4 in range(4):
                            n = q * 4 + i4
                            csl = slice(n * 128, (n + 1) * 128)
                            sc.op("act", lambda e, n=n: e.activation(
                                out=T_["yy"][:], in_=Oacc[:, n, :], func=AF.Square,
                                accum_out=st4[:, 0:1]), reads=[t_O[n]],
                                writes=[t_T["yy"], t_st4])
                            sc.op("act", lambda e: e.activation(
                                out=st4[:, 1:2], in_=st4[:, 0:1], func=AF.Sqrt, scale=1.0 / 128,
                                bias=1e-6), reads=[t_st4], writes=[t_st4])
                            sc.op("dve", lambda e: e.reciprocal(out=st4[:, 2:3], in_=st4[:, 1:2]),
                                  reads=[t_st4], writes=[t_st4])
                            bz = next_ps()

                            def f(e, bz=bz, csl=csl):
                                for kc in range(NCH):
                                    r = e.matmul(ps[:, bz, 0:128], lhsT=hT[:, kc, csl],
                                                 rhs=zw[:, kc, :], start=(kc == 0),
                                                 stop=(kc == NCH - 1))
                                return r
                            sc.op("pe", f, reads=[t_zw] + [t_hT[c][q] for c in range(NCH)],
                                  writes=[t_ps[bz]])
                            sc.op("act", lambda e, bz=bz: e.activation(
                                out=T_["sz"][:], in_=ps[:, bz, 0:128], func=AF.Silu),
                                reads=[t_ps[bz]], writes=[t_T["sz"]])
                            sc.op("dve", lambda e, n=n: e.scalar_tensor_tensor(
                                out=T_["yy"][:], in0=Oacc[:, n, :], scalar=st4[:, 2:3],
                                in1=gsm[:, 32:160], op0=ALU.mult, op1=ALU.mult),
                                reads=[t_O[n], t_st4, t_gsm], writes=[t_T["yy"]])
                            sc.op("dve", lambda e: e.tensor_tensor(
                                out=T_["yy"][:], in0=T_["yy"][:], in1=T_["sz"][:], op=ALU.mult),
                                reads=[t_T["yy"], t_T["sz"]], writes=[t_T["yy"]])
                            sc.op("pe", lambda e, bT=bT, i4=i4: e.transpose(
                                out=ps[:, bT, i4 * 128:(i4 + 1) * 128], in_=T_["yy"][:],
                                identity=ident[:]), reads=[t_T["yy"], t_ident], writes=[t_ps[bT]])
                        sc.op("act", lambda e, bT=bT, q=q: e.activation(
                            out=oT[:, q * TT:(q + 1) * TT], in_=ps[:, bT, :], func=AF.Copy),
                            reads=[t_ps[bT]], writes=[t_oT[q]])
                    if dbg == 17:
                        return
                    wo, t_wo = load_block(gout_d[h * 128:(h + 1) * 128, :]
                                          .rearrange("p (a n) -> p a n", a=1), None)
                    for t in range(NTT):
                        tsl = slice(t * TT, (t + 1) * TT)
                        for j in range(NCH):
                            b = next_ps()
                            sc.op("pe", lambda e, b=b, j=j, tsl=tsl, wo=wo: e.matmul(
                                ps[:, b, :], lhsT=wo[:, 0, j * 128:(j + 1) * 128], rhs=oT[:, tsl],
                                start=True, stop=True), reads=[t_wo, t_oT[t]], writes=[t_ps[b]])
                            sc.op("dve", lambda e, b=b, j=j, tsl=tsl: e.tensor_tensor(
                                out=xT[:, j, tsl], in0=ps[:, b, :], in1=xT[:, j, tsl], op=ALU.add),
                                reads=[t_ps[b]] + xtoks(j, t), writes=xtoks(j, t))
                    if dbg == 30 + h:
                        return


        def phase_moe():
            NT16 = S // 128
            with contextlib.ExitStack() as esp:
                hT = sb("hT", [128, NCH, S], BF16, esp)
                t_hT = [[Tok("hT%d_%d" % (c, t)) for t in range(NTT)] for c in range(NCH)]
                sqtmp = sb("sqtmp", [128, NCH, TT], BF16, esp)
                t_sq = Tok("sqtmp")
                h1 = sb("h1", [128, 4, S], BF16, esp)
                t_h1 = [[Tok("h1_%d_%d" % (f_, t)) for t in range(NTT)] for f_ in range(4)]
                sg = [sb("sg%d" % i, [128, TT], F32, esp) for i in range(2)]
                t_sg = [Tok("sg0"), Tok("sg1")]
                G = [sb("G%d" % i, [128, S], F32, esp) for i in range(2)]
                t_G = [Tok("G0"), Tok("G1")]
                wr = sb("wr", [128, NCH, 8], F32, esp)
                t_wr = Tok("wr")
                sq32 = [sb("sq32_%d" % i, [128, NCH, 128], F32, esp) for i in range(2)]
                t_sq32 = [Tok("sq32_0"), Tok("sq32_1")]
                rst = sb("rst", [128, NT16, 9], F32, esp)
                t_rst = Tok("rst")
                L = sb("L", [128, NT16, 8], F32, esp)
                v8 = sb("v8", [128, NT16, 8], F32, esp)
                gate = sb("gate", [128, NT16, 8], F32, esp)
                tmpr = sb("tmpr", [128, NT16, 8], F32, esp)
                rs16 = sb("rs16", [128, NT16, 4], F32, esp)
                dg = [sb("dg%d" % i, [128, 128], F32, esp) for i in range(2)]
                t_dg = [Tok("dg0"), Tok("dg1")]
                t_L, t_v8, t_gate, t_tmpr, t_rs16 = (Tok("L"), Tok("v8"), Tok("gate"),
                                                     Tok("tmpr"), Tok("rs16"))
                ch_wr = sc.new_chan()
                rmsnorm_fm(hT, t_hT, VR["ffn1"], sqtmp, t_sq)
                sc.op("sp", lambda e: e.dma_start(
                    out=wr[:], in_=rt_d.rearrange("(c p) e -> p c e", p=128)),
                    writes=[t_wr], chan=ch_wr, after=sc.last_ops())
                for c in range(NCH):
                    sc.op("dve", lambda e, c=c: e.tensor_scalar(
                        out=wr[:, c, :], in0=wr[:, c, :], scalar1=vcol(c, VR["ffn1"]),
                        scalar2=None, op0=ALU.mult), reads=[t_wr, t_vecs], writes=[t_wr])
                for tt in range(NT16):
                    s_ = tt % 2
                    tsl = slice(tt * 128, (tt + 1) * 128)
                    sc.op("act", lambda e, s_=s_, tsl=tsl: e.activation(
                        out=sq32[s_][:], in_=xT[:, :, tsl], func=AF.Square),
                        reads=[t_xT[c][tt] for c in range(NCH)], writes=[t_sq32[s_]])
                    b = next_ps()

                    def f(e, b=b, tsl=tsl, s_=s_):
                        for c in range(NCH):
                            e.matmul(ps[:, b, 0:8], lhsT=xT[:, c, tsl], rhs=wr[:, c, :],
                                     start=(c == 0), stop=(c == NCH - 1))
                        for c in range(NCH):
                            r = e.matmul(ps[:, b, 8:9], lhsT=sq32[s_][:, c, :],
                                         rhs=ones_f[:, 0:1], start=(c == 0), stop=(c == NCH - 1))
                        return r
                    sc.op("pe", f, reads=[t_wr, t_sq32[s_], t_ones] +
                          [t_xT[c][tt] for c in range(NCH)], writes=[t_ps[b]])
                    sc.op("dve", lambda e, b=b, tt=tt: e.tensor_copy(
                        out=rst[:, tt, :], in_=ps[:, b, 0:9]), reads=[t_ps[b]], writes=[t_rst])
                sc.op("act", lambda e: e.activation(
                    out=rs16[:, :, 0:1], in_=rst[:, :, 8:9], func=AF.Sqrt, scale=1.0 / D,
                    bias=1e-6), reads=[t_rst], writes=[t_rs16])
                sc.op("dve", lambda e: e.reciprocal(out=rs16[:, :, 1:2], in_=rs16[:, :, 0:1]),
                      reads=[t_rs16], writes=[t_rs16])
                sc.op("dve", lambda e: e.tensor_tensor(
                    out=L[:], in0=rst[:, :, 0:8],
                    in1=rs16[:, :, 1:2].to_broadcast([128, NT16, 8]), op=ALU.mult),
                    reads=[t_rst, t_rs16], writes=[t_L])
                for tt in range(NT16):
                    sc.op("dve", lambda e, tt=tt: e.max(out=v8[:, tt, :], in_=L[:, tt, :]),
                          reads=[t_L], writes=[t_v8])
                sc.op("dve", lambda e: e.tensor_tensor(
                    out=gate[:], in0=L[:], in1=v8[:, :, 1:2].to_broadcast([128, NT16, 8]),
                    op=ALU.is_ge), reads=[t_L, t_v8], writes=[t_gate])
                sc.op("dve", lambda e: e.tensor_tensor(
                    out=tmpr[:], in0=L[:], in1=v8[:, :, 0:1].to_broadcast([128, NT16, 8]),
                    op=ALU.subtract), reads=[t_L, t_v8], writes=[t_tmpr])
                sc.op("act", lambda e: e.activation(out=tmpr[:], in_=tmpr[:], func=AF.Exp),
                      reads=[t_tmpr], writes=[t_tmpr])
                sc.op("dve", lambda e: e.tensor_tensor(
                    out=rs16[:, :, 2:3], in0=v8[:, :, 1:2], in1=v8[:, :, 0:1], op=ALU.subtract),
                    reads=[t_v8], writes=[t_rs16])
                sc.op("act", lambda e: e.activation(out=rs16[:, :, 2:3], in_=rs16[:, :, 2:3],
                                                    func=AF.Exp),
                      reads=[t_rs16], writes=[t_rs16])
                sc.op("dve", lambda e: e.tensor_scalar(
                    out=rs16[:, :, 2:3], in0=rs16[:, :, 2:3], scalar1=1.0, scalar2=None,
                    op0=ALU.add), reads=[t_rs16], writes=[t_rs16])
                sc.op("dve", lambda e: e.reciprocal(out=rs16[:, :, 3:4], in_=rs16[:, :, 2:3]),
                      reads=[t_rs16], writes=[t_rs16])
                sc.op("dve", lambda e: e.tensor_tensor(
                    out=gate[:], in0=gate[:], in1=tmpr[:], op=ALU.mult),
                    reads=[t_gate, t_tmpr], writes=[t_gate])
                sc.op("dve", lambda e: e.tensor_tensor(
                    out=gate[:], in0=gate[:], in1=rs16[:, :, 3:4].to_broadcast([128, NT16, 8]),
                    op=ALU.mult), reads=[t_gate, t_rs16], writes=[t_gate])
                dgn = 0
                for ex in range(8):
                    gi = ex % 2
                    for q in range(NTT):
                        b = next_ps()
                        for i4 in range(4):
                            tt = q * 4 + i4
                            di = dgn % 2
                            dgn += 1
                            sc.op("dve", lambda e, di=di, tt=tt, ex=ex: e.tensor_tensor(
                                out=dg[di][:], in0=ident[:],
                                in1=gate[:, tt, ex:ex + 1].to_broadcast([128, 128]), op=ALU.mult),
                                reads=[t_ident, t_gate], writes=[t_dg[di]])
                            sc.op("pe", lambda e, b=b, i4=i4, di=di: e.matmul(
                                ps[:, b, i4 * 128:(i4 + 1) * 128], lhsT=ones_f[:], rhs=dg[di][:],
                                start=True, stop=True),
                                reads=[t_dg[di], t_ones], writes=[t_ps[b]])
                        sc.op("act", lambda e, b=b, gi=gi, q=q: e.activation(
                            out=G[gi][:, q * TT:(q + 1) * TT], in_=ps[:, b, :], func=AF.Copy),
                            reads=[t_ps[b]], writes=[t_G[gi]])
                    swiglu_stream(hT, t_hT, h1, t_h1, sg, t_sg, mg_d[ex], mu_d[ex], md_d[ex], DFFE,
                                  gate_bc=G[gi], t_gate=t_G[gi])

        if "c0" in phases:
            phase_conformer()
            sc.barrier()
        if "f0" in phases:
            phase_ffn()
            sc.barrier()
        if "g1" in phases:
            phase_gdn()
            sc.barrier()
        if "m1" in phases:
            phase_moe()
            sc.barrier()

        do_norm = "final" in phases
        with contextlib.ExitStack() as es2:
            xo = [sb("xo%d" % i, [128, D], F32, es2) for i in range(2)]
            fng = sb("fng_sb", [128, D], F32, es2)
            sc.op("sp", lambda e: e.dma_start(out=fng[:],
                                              in_=fng_d[0:1, :].partition_broadcast(128)),
                  writes=[t_fng], chan=ch_misc, after=sc.last_ops())
            yo = [sb("yo%d" % i, [128, D], F32, es2) for i in range(2)]
            sq = sb("sqj", [128, D], F32, es2)
            st = [sb("st%d" % i, [128, 4], F32, es2) for i in range(2)]
            t_xo = [Tok("xo0"), Tok("xo1")]
            t_yo = [Tok("yo0"), Tok("yo1")]
            t_sq2 = Tok("sq")
            t_st = [Tok("st0"), Tok("st1")]
            out_ops = []
            for tt in range(S // 128):
                sl = tt % 2
                for half in range(2):
                    b = next_ps()

                    def f(e, half=half, b=b, tt=tt):
                        for j in range(4):
                            c = half * 4 + j
                            r = e.transpose(out=ps[:, b, j * 128:(j + 1) * 128],
                                            in_=xT[:, c, tt * 128:(tt + 1) * 128],
                                            identity=ident[:])
                        return r
                    sc.op("pe", f, reads=[t_ident] + [t_xT[half * 4 + j][tt] for j in range(4)],
                          writes=[t_ps[b]])
                    dst = xo if do_norm else yo
                    t_dst = t_xo if do_norm else t_yo
                    sc.op("act", lambda e, half=half, b=b, sl=sl, dst=dst: e.activation(
                        out=dst[sl][:, half * 512:(half + 1) * 512], in_=ps[:, b, :], func=AF.Copy),
                        reads=[t_ps[b]], writes=[t_dst[sl]])
                if do_norm:
                    sc.op("act", lambda e, sl=sl: e.activation(
                        out=sq[:], in_=xo[sl][:], func=AF.Square, accum_out=st[sl][:, 0:1]),
                        reads=[t_xo[sl]], writes=[t_sq2, t_st[sl]])
                    sc.op("act", lambda e, sl=sl: e.activation(
                        out=st[sl][:, 1:2], in_=st[sl][:, 0:1], func=AF.Sqrt, scale=1.0 / D,
                        bias=1e-6), reads=[t_st[sl]], writes=[t_st[sl]])
                    sc.op("dve", lambda e, sl=sl: e.reciprocal(out=st[sl][:, 2:3],
                                                               in_=st[sl][:, 1:2]),
                          reads=[t_st[sl]], writes=[t_st[sl]])
                    sc.op("dve", lambda e, sl=sl: e.scalar_tensor_tensor(
                        out=yo[sl][:], in0=xo[sl][:], scalar=st[sl][:, 2:3], in1=fng[:],
                        op0=ALU.mult, op1=ALU.mult),
                        reads=[t_xo[sl], t_st[sl], t_fng], writes=[t_yo[sl]])
                o = sc.op("sp", lambda e, sl=sl, tt=tt: e.dma_start(
                    out=out_d[tt * 128:(tt + 1) * 128, :], in_=yo[sl][:]),
                    reads=[t_yo[sl]], chan=ch_out[sl])
                out_ops.append(o)
            fin = sc.op("sp", lambda e: e.nop())
            fin.is_nop = True
            fin.deps.extend(out_ops[-2:])
            sc.emit(nc)
    return nc


def pack_vecs(inp):
    rows = np.zeros((128, D), np.float32)
    rows[VR["mix0"]] = inp["mix_norm"][0]
    rows[VR["mix1"]] = inp["mix_norm"][1]
    rows[VR["ffn0"]] = inp["ffn_norm"][0]
    rows[VR["ffn1"]] = inp["ffn_norm"][1]
    rows[VR["pw1_ba"]] = inp["cf_pw1_b"][0, :D]
    rows[VR["pw1_bb"]] = inp["cf_pw1_b"][0, D:]
    rows[VR["dw_b"]] = inp["cf_dw_b"][0]
    rows[VR["ln_g"]] = inp["cf_ln_g"][0]
    rows[VR["ln_b"]] = inp["cf_ln_b"][0]
    rows[VR["pw2_b"]] = inp["cf_pw2_b"][0]
    rows[VR["dw_w"]:VR["dw_w"] + 31] = inp["cf_dw_w"][0]
    gc = inp["gdn_conv_w"][0]
    for k in range(5):
        for part in range(3):
            rows[VR["gconv"] + k * 3 + part] = gc[k, part * D:(part + 1) * D]
    return rows


def gdn_consts():
    i = np.arange(128)
    c = np.zeros((128, 2048), np.float32)
    c[:, 0:128] = (i[:, None] <= i[None, :])
    c[:, 128:256] = (i[:, None] >= i[None, :])
    for k in range(7):
        bsz = 1 << k
        I, J = i[:, None], i[None, :]
        m = ((I // (2 * bsz)) == (J // (2 * bsz))) & (((I // bsz) % 2) == 1) & (((J // bsz) % 2) == 0)
        c[:, 256 + k * 128:256 + (k + 1) * 128] = m.T
        c[:, 1152 + k * 128:1152 + (k + 1) * 128] = m
    return c


def make_in_maps(inp, phases, nb):
    x = np.ascontiguousarray(inp["x"], dtype=np.float32)
    vecs = pack_vecs(inp)
    fng = np.ascontiguousarray(inp["final_norm"], dtype=np.float32).reshape(1, D)
    base = {"vecs": vecs, "fng": fng}
    if "c0" in phases:
        base["cf_pw1_w"] = np.ascontiguousarray(inp["cf_pw1_w"][0])
        base["cf_pw2_w"] = np.ascontiguousarray(inp["cf_pw2_w"][0])
    if "f0" in phases:
        base["ffn_w_gate"] = np.ascontiguousarray(inp["ffn_w_gate"][0])
        base["ffn_w_up"] = np.ascontiguousarray(inp["ffn_w_up"][0])
        base["ffn_w_down"] = np.ascontiguousarray(inp["ffn_w_down"][0])
    if "g1" in phases:
        base["gdn_w_in"] = np.ascontiguousarray(inp["gdn_w_in"][0])
        base["gdn_w_out"] = np.ascontiguousarray(inp["gdn_w_out"][0])
        base["gsmall"] = np.concatenate([inp["gdn_a_log"][0].reshape(-1),
                                         inp["gdn_dt_bias"][0].reshape(-1),
                                         inp["gdn_o_norm"][0].reshape(-1)]).astype(np.float32)[None]
        base["gconst"] = gdn_consts()
    if "m1" in phases:
        base["moe_router"] = np.ascontiguousarray(inp["moe_router"][0])
        base["moe_w_gate"] = np.ascontiguousarray(inp["moe_w_gate"][0])
        base["moe_w_up"] = np.ascontiguousarray(inp["moe_w_up"][0])
        base["moe_w_down"] = np.ascontiguousarray(inp["moe_w_down"][0])
    return [dict(base, x=x[b]) for b in range(nb)]


def kernel(**inp):
    phases = ("c0", "f0", "g1", "m1", "final")
    nb = inp["x"].shape[0]
    nc = build_program(phases)
    in_maps = make_in_maps(inp, phases, nb)
    res = run_bass_kernel_spmd(nc, in_maps, core_ids=list(range(nb)))
    return np.stack([r["out"] for r in res.results], axis=0)
```

```python
import contextlib
import numpy as np
import concourse.bass as bass
import concourse.mybir as mybir
from concourse.bass_utils import run_bass_kernel_spmd

F32 = mybir.dt.float32
BF16 = mybir.dt.bfloat16
I32 = mybir.dt.int32
AF = mybir.ActivationFunctionType
ALU = mybir.AluOpType

D = 1024
S = 2048
NCH = D // 128
TT = 512
NTT = S // TT
ENGS = ("sp", "pe", "act", "dve", "pool")


class Tok:
    __slots__ = ("name", "w", "readers")

    def __init__(self, name):
        self.name = name
        self.w = None
        self.readers = []


class Op:
    __slots__ = ("eng", "fn", "deps", "chan", "has_dep", "sig", "name", "is_nop")

    def __init__(self, eng, fn, chan, name):
        self.eng = eng
        self.fn = fn
        self.deps = []
        self.chan = chan
        self.has_dep = False
        self.sig = None
        self.name = name
        self.is_nop = False


class Rec:
    def __init__(self):
        self.items = []

    def op(self, *a, **k):
        self.items.append((a, k))


class Sched:
    def __init__(self):
        self.ops = {e: [] for e in ENGS}
        self.nchan = 0

    def new_chan(self):
        self.nchan += 1
        return self.nchan - 1

    def last_real(self, e):
        for o in reversed(self.ops[e]):
            if not o.is_nop:
                return o
        return None

    def last_ops(self, engs=("pe", "act", "dve", "pool")):
        return [o for o in (self.last_real(e) for e in engs) if o is not None]

    def op(self, eng, fn, reads=(), writes=(), chan=None, name="", after=()):
        o = Op(eng, fn, chan, name)
        for d in after:
            o.deps.append(d)
            if d.chan is None:
                d.has_dep = True
        cand = []
        for t in reads:
            if t.w is not None:
                cand.append((t.w, "raw"))
        for t in writes:
            if t.w is not None:
                cand.append((t.w, "waw"))
            for r in t.readers:
                cand.append((r, "war"))
        seen = set()
        for d, kind in cand:
            if d is o or id(d) in seen:
                continue
            same = (d.eng == eng and d.chan is None and chan is None)
            if same and (eng == "pe" or kind == "war"):
                continue
            seen.add(id(d))
            o.deps.append(d)
            d.has_dep = True
        for t in reads:
            t.readers.append(o)
        for t in writes:
            t.w = o
            t.readers = []
        self.ops[eng].append(o)
        return o

    def barrier(self, engs=("pe", "act", "dve", "pool")):
        last = {e: self.last_real(e) for e in engs}
        for e in engs:
            o = Op(e, lambda eng: eng.nop(), None, "barrier")
            o.is_nop = True
            for e2, l in last.items():
                if e2 != e and l is not None:
                    o.deps.append(l)
                    if l.chan is None:
                        l.has_dep = True
            self.ops[e].append(o)

    def emit(self, nc, final_wait_ops=()):
        with contextlib.ExitStack() as es:
            EPOCH = 1000
            nsig = {e: sum(1 for o in self.ops[e] if o.chan is None and o.has_dep) for e in ENGS}
            esem = {e: [es.enter_context(nc.semaphore("s_%s%d" % (e, i)))
                        for i in range(nsig[e] // EPOCH + 1)] for e in ENGS}
            for e in ENGS:
                cnt = 0
                for o in self.ops[e]:
                    if o.chan is None and o.has_dep:
                        o.sig = (esem[e][cnt // EPOCH], cnt % EPOCH + 1, 1)
                        cnt += 1
            CEP = 100
            ccnt = [0] * self.nchan
            ntot = [0] * self.nchan
            for e in ENGS:
                for o in self.ops[e]:
                    if o.chan is not None:
                        ntot[o.chan] += 1
            csem = [[es.enter_context(nc.semaphore("c_%d_%d" % (i, k)))
                     for k in range(ntot[i] // CEP + 1)] for i in range(self.nchan)]
            for e in ENGS:
                for o in self.ops[e]:
                    if o.chan is not None:
                        k = ccnt[o.chan]
                        o.sig = (csem[o.chan][k // CEP], (k % CEP + 1) * 16, 16)
                        ccnt[o.chan] += 1
            block = es.enter_context(nc.Block())
            ops = self.ops

            def run(engname, eobj):
                waited = {}
                for o in ops[engname]:
                    need = {}
                    for d in o.deps:
                        sem, val, _ = d.sig
                        k = id(sem)
                        if val > waited.get(k, 0) and val > need.get(k, (None, 0))[1]:
                            need[k] = (sem, val)
                    for k, (sem, val) in need.items():
                        eobj.wait_ge(sem, val)
                        waited[k] = val
                    inst = o.fn(eobj)
                    if o.sig is not None:
                        inst.then_inc(o.sig[0], o.sig[2])

            @block.sync
            def _(e):
                run("sp", e)

            @block.tensor
            def _(e):
                run("pe", e)

            @block.scalar
            def _(e):
                run("act", e)

            @block.vector
            def _(e):
                run("dve", e)

            @block.gpsimd
            def _(e):
                run("pool", e)


VR = dict(mix0=0, mix1=1, ffn0=2, ffn1=3, pw1_ba=4, pw1_bb=5, dw_b=6, ln_g=7, ln_b=8, pw2_b=9,
          dw_w=10, gconv=41)
DFF = 2816
DFFE = 3584
WB = 2048


def build_program(phases=("c0", "f0", "g1", "m1"), dbg=0, heads=tuple(range(8)), two_x=False):
    nc = bass.Bass("TRN2", target_bir_lowering=False, dynamic_dma_scratch_size=2048)

    def din(name, shape):
        return nc.dram_tensor(name, shape, F32, kind="ExternalInput").ap()

    x_d = din("x", [S, D])
    vecs_d = din("vecs", [128, D])
    fng_d = din("fng", [1, D])
    out_d = nc.dram_tensor("out", [S, D], F32, kind="ExternalOutput").ap()
    xacc_d = din("xacc", [S, D]) if two_x else None
    if "c0" in phases:
        w1_d = din("cf_pw1_w", [D, 2 * D])
        w2_d = din("cf_pw2_w", [D, D])
    if "f0" in phases:
        fg_d = din("ffn_w_gate", [D, DFF])
        fu_d = din("ffn_w_up", [D, DFF])
        fd_d = din("ffn_w_down", [DFF, D])

    if "g1" in phases:
        gin_d = din("gdn_w_in", [D, 4128])
        gout_d = din("gdn_w_out", [D, D])
        gsm_d = din("gsmall", [1, 160])
    if "m1" in phases:
        rt_d = din("moe_router", [D, 8])
        mg_d = din("moe_w_gate", [8, D, DFFE])
        mu_d = din("moe_w_up", [8, D, DFFE])
        md_d = din("moe_w_down", [8, DFFE, D])

    sc = Sched()
    with contextlib.ExitStack() as es:
        uniq = [0]

        def sb(name, shape, dt, stack=es):
            uniq[0] += 1
            return stack.enter_context(nc.sbuf_tensor("%s_%d" % (name, uniq[0]), shape, dt))

        xT = sb("xT", [128, NCH, S], F32)
        ident = sb("ident", [128, 128], F32)
        ones_f = sb("ones_f", [128, 128], F32)
        ones_b = sb("ones_b", [128, 128], BF16)
        vecs = sb("vecs_sb", [128, NCH, 64], F32)
        ps = es.enter_context(nc.psum_tensor("ps", [128, 8, 512], F32))
        stage, wbf, t_stage, t_wbf = [], [], [], []
        pool_fence = [[], 0]

        def set_pools(stack, nst, nwb, elems):
            stage[:] = [sb("stage%d" % i, [128, elems], F32, stack) for i in range(nst)]
            wbf[:] = [sb("wbf%d" % i, [128, elems], BF16, stack) for i in range(nwb)]
            t_stage[:] = [Tok("stage%d" % i) for i in range(nst)]
            t_wbf[:] = [Tok("wbf%d" % i) for i in range(nwb)]
            pool_fence[0] = sc.last_ops()
            pool_fence[1] = nst
        cst_d = din("gconst", [128, 2048]) if "g1" in phases else None
        sm = [sb("sm%d" % i, [128, TT], F32) for i in range(4)]
        t_sm = [Tok("sm%d" % i) for i in range(4)]
        smn = [0]

        def next_sm():
            i = smn[0] % 4
            smn[0] += 1
            return i

        t_xT = [[Tok("xT%d_%d" % (c, t)) for t in range(S // 128)] for c in range(NCH)]

        def xtoks(c, t512):
            return [t_xT[c][t512 * 4 + i] for i in range(4)]
        t_ident = Tok("ident")
        t_ones = Tok("ones")
        t_vecs = Tok("vecs")
        t_fng = Tok("fng")
        t_ps = [Tok("ps%d" % i) for i in range(8)]
        ch_stage = [sc.new_chan() for _ in range(2)]
        ch_xin = [sc.new_chan(), sc.new_chan()]
        ch_misc = sc.new_chan()
        ch_out = [sc.new_chan(), sc.new_chan()]
        psn = [0]
        stn = [0]
        wbn = [0]

        def next_ps():
            i = psn[0] % 8
            psn[0] += 1
            return i

        cast_cfg = ["act"]

        def load_block(src, view, cast_eng=None):
            cast_eng = cast_eng or cast_cfg[0]
            a, b = src.shape[1], src.shape[2]
            n = a * b
            s = stn[0] % len(stage)
            stn[0] += 1
            k = wbn[0] % len(wbf)
            wbn[0] += 1
            aft = ()
            if pool_fence[1] > 0:
                aft = pool_fence[0]
                pool_fence[1] -= 1
            st_, wb_, ts_, tw_ = stage[s], wbf[k], t_stage[s], t_wbf[k]
            sc.op("sp", lambda e: e.dma_start(
                out=st_[:, 0:n].rearrange("p (a b) -> p a b", a=a), in_=src),
                writes=[ts_], chan=ch_stage[s], after=aft)
            if cast_eng == "act":
                sc.op("act", lambda e: e.activation(out=wb_[:, 0:n], in_=st_[:, 0:n],
                                                    func=AF.Copy),
                      reads=[ts_], writes=[tw_])
            else:
                sc.op(cast_eng, lambda e: e.tensor_copy(out=wb_[:, 0:n], in_=st_[:, 0:n]),
                      reads=[ts_], writes=[tw_])
            return wb_[:, 0:n].rearrange("p (a b) -> p a b", a=a), tw_

        sc.op("pool", lambda e: e.memset(ones_f[:], 1.0), writes=[t_ones])
        sc.op("pool", lambda e: e.memset(ones_b[:], 1.0), writes=[t_ones])
        sc.op("pool", lambda e: e.affine_select(out=ident[:], in_=ones_f[:], pattern=[[-1, 128]],
                                                compare_op=ALU.is_equal, fill=0.0, base=0,
                                                channel_multiplier=1),
              reads=[t_ones], writes=[t_ident])

        def load_x_loop(src_d, xin, t_xin, fence=()):
            for tt in range(S // 128):
                sl = tt % 2
                sc.op("sp", lambda e, tt=tt, sl=sl: e.dma_start(
                    out=xin[sl][:], in_=src_d[tt * 128:(tt + 1) * 128, :]),
                    writes=[t_xin[sl]], chan=ch_xin[sl], after=fence)
                for half in range(2):
                    b = next_ps()

                    def f(e, half=half, b=b, sl=sl):
                        for j in range(4):
                            c = half * 4 + j
                            r = e.transpose(out=ps[:, b, j * 128:(j + 1) * 128],
                                            in_=xin[sl][:, c * 128:(c + 1) * 128],
                                            identity=ident[:])
                        return r
                    sc.op("pe", f, reads=[t_xin[sl], t_ident], writes=[t_ps[b]])
                    eng = "dve" if half == 0 else "act"

                    def g(e, half=half, b=b, tt=tt, eng=eng):
                        o = xT[:, half * 4:half * 4 + 4, tt * 128:(tt + 1) * 128]
                        i = ps[:, b, :].rearrange("p (j r) -> p j r", j=4)
                        if eng == "dve":
                            return e.tensor_copy(out=o, in_=i)
                        return e.activation(out=o, in_=i, func=AF.Copy)
                    sc.op(eng, g, reads=[t_ps[b]],
                          writes=[t_xT[half * 4 + j][tt] for j in range(4)])


        with contextlib.ExitStack() as es1:
            xin = [sb("xin%d" % i, [128, D], F32, es1) for i in range(2)]
            t_xin = [Tok("xin0"), Tok("xin1")]
            sc.op("sp", lambda e: e.dma_start(out=xin[1][:], in_=vecs_d[:, :]), writes=[t_xin[1]],
                  chan=ch_xin[1])
            for half in range(2):
                b = next_ps()

                def f(e, half=half, b=b):
                    for j in range(4):
                        c = half * 4 + j
                        r = e.transpose(out=ps[:, b, j * 128:(j + 1) * 128],
                                        in_=xin[1][:, c * 128:(c + 1) * 128], identity=ident[:])
                    return r
                sc.op("pe", f, reads=[t_xin[1], t_ident], writes=[t_ps[b]])
                sc.op("dve", lambda e, half=half, b=b: e.tensor_copy(
                    out=vecs[:, half * 4:half * 4 + 4, :],
                    in_=ps[:, b, :].rearrange("p (j r) -> p j r", j=4)[:, :, 0:64]),
                    reads=[t_ps[b]], writes=[t_vecs])
            load_x_loop(x_d, xin, t_xin)
        sc.barrier()

        def vcol(c, r):
            return vecs[:, c, r:r + 1]

        def rmsnorm_fm(hT, t_hT, grow, sqtmp, t_sq):
            for t in range(NTT):
                tsl = slice(t * TT, (t + 1) * TT)
                sc.op("act", lambda e, tsl=tsl: e.activation(out=sqtmp[:], in_=xT[:, :, tsl],
                                                             func=AF.Square),
                      reads=[tk for c in range(NCH) for tk in xtoks(c, t)], writes=[t_sq])
                b = next_ps()

                def f(e, b=b):
                    for c in range(NCH):
                        r = e.matmul(ps[:, b, :], lhsT=ones_b[:], rhs=sqtmp[:, c, :],
                                     start=(c == 0), stop=(c == NCH - 1))
                    return r
                sc.op("pe", f, reads=[t_sq, t_ones], writes=[t_ps[b]])
                s0 = next_sm()
                sc.op("act", lambda e, b=b, s0=s0: e.activation(out=sm[s0][:], in_=ps[:, b, :],
                                                                func=AF.Sqrt, scale=1.0 / D,
                                                                bias=1e-6),
                      reads=[t_ps[b]], writes=[t_sm[s0]])
                s1 = next_sm()
                sc.op("dve", lambda e, s0=s0, s1=s1: e.reciprocal(out=sm[s1][:], in_=sm[s0][:]),
                      reads=[t_sm[s0]], writes=[t_sm[s1]])
                for c in range(NCH):
                    sc.op("dve", lambda e, c=c, tsl=tsl, s1=s1: e.scalar_tensor_tensor(
                        out=hT[:, c, tsl], in0=xT[:, c, tsl], scalar=vcol(c, grow), in1=sm[s1][:],
                        op0=ALU.mult, op1=ALU.mult),
                        reads=xtoks(c, t) + [t_vecs, t_sm[s1]], writes=[t_hT[c][t]])

        def dump(src_fn, toks_fn):
            for c in range(NCH):
                for t in range(NTT):
                    tsl = slice(t * TT, (t + 1) * TT)
                    sc.op("dve", lambda e, c=c, tsl=tsl: e.tensor_copy(out=xT[:, c, tsl],
                                                                       in_=src_fn(c, tsl)),
                          reads=toks_fn(c, t), writes=xtoks(c, t))

        def phase_conformer():
            with contextlib.ExitStack() as esp:
                set_pools(esp, 2, 6, WB)
                hT = sb("hT", [128, NCH, S], BF16, esp)
                t_hT = [[Tok("hT%d_%d" % (c, t)) for t in range(NTT)] for c in range(NCH)]
                sqtmp = sb("sqtmp", [128, NCH, TT], BF16, esp)
                t_sq = Tok("sqtmp")
                U = sb("ubuf", [128, NCH, S + 30], BF16, esp)
                t_U = [Tok("u%d" % c) for c in range(NCH)]
                diag = sb("diag", [128, 31, 128], BF16, esp)
                t_diag = Tok("diag")
                sig = [sb("sig%d" % i, [128, TT], F32, esp) for i in range(2)]
                t_sig = [Tok("sig0"), Tok("sig1")]
                lnst = [sb("lnst%d" % i, [128, TT], F32, esp) for i in range(3)]
                t_lnst = [Tok("lnst%d" % i) for i in range(3)]
                rmsnorm_fm(hT, t_hT, VR["mix0"], sqtmp, t_sq)
                if dbg == 1:
                    dump(lambda c, tsl: hT[:, c, tsl], lambda c, t: [t_hT[c][t]])
                    return
                sc.op("dve", lambda e: e.memset(U[:], 0.0), writes=t_U)
                sgn = 0
                for h in range(2):
                    wa = [load_block(w1_d[:, h * 512 + q * 256: h * 512 + (q + 1) * 256]
                                     .rearrange("(kc p) n -> p kc n", p=128), None) for q in range(2)]
                    wb_ = [load_block(w1_d[:, D + h * 512 + q * 256: D + h * 512 + (q + 1) * 256]
                                      .rearrange("(kc p) n -> p kc n", p=128), None) for q in range(2)]
                    for jj in range(4):
                        j = h * 4 + jj
                        wA, tA = wa[jj // 2]
                        wB, tB = wb_[jj // 2]
                        co = (jj % 2) * 128
                        for t in range(NTT):
                            tsl = slice(t * TT, (t + 1) * TT)
                            bA = next_ps()
                            bB = next_ps()

                            def f(e, wA=wA, wB=wB, co=co, bA=bA, bB=bB, tsl=tsl):
                                for kc in range(NCH):
                                    e.matmul(ps[:, bA, :], lhsT=wA[:, kc, co:co + 128],
                                             rhs=hT[:, kc, tsl], start=(kc == 0),
                                             stop=(kc == NCH - 1))
                                for kc in range(NCH):
                                    r = e.matmul(ps[:, bB, :], lhsT=wB[:, kc, co:co + 128],
                                                 rhs=hT[:, kc, tsl], start=(kc == 0),
                                                 stop=(kc == NCH - 1))
                                return r
                            sc.op("pe", f, reads=[tA, tB] + [t_hT[c][t] for c in range(NCH)],
                                  writes=[t_ps[bA], t_ps[bB]])
                            sg = sgn % 2
                            sgn += 1
                            sc.op("act", lambda e, bB=bB, sg=sg, j=j: e.activation(
                                out=sig[sg][:], in_=ps[:, bB, :], func=AF.Sigmoid,
                                bias=vcol(j, VR["pw1_bb"])),
                                reads=[t_ps[bB], t_vecs], writes=[t_sig[sg]])
                            sc.op("dve", lambda e, bA=bA, sg=sg, j=j, t=t: e.scalar_tensor_tensor(
                                out=U[:, j, 15 + t * TT:15 + (t + 1) * TT], in0=ps[:, bA, :],
                                scalar=vcol(j, VR["pw1_ba"]), in1=sig[sg][:],
                                op0=ALU.add, op1=ALU.mult),
                                reads=[t_ps[bA], t_sig[sg], t_vecs], writes=[t_U[j]])
                if dbg == 2:
                    dump(lambda c, tsl: U[:, c, 15 + tsl.start:15 + tsl.stop], lambda c, t: [t_U[c]])
                    return
                for j in range(NCH):
                    sc.op("dve", lambda e, j=j: e.tensor_tensor(
                        out=diag[:], in0=ident[:].unsqueeze(1).to_broadcast([128, 31, 128]),
                        in1=vecs[:, j, VR["dw_w"]:VR["dw_w"] + 31].unsqueeze(2)
                        .to_broadcast([128, 31, 128]), op=ALU.mult),
                        reads=[t_ident, t_vecs], writes=[t_diag])
                    for t in range(NTT):
                        b = next_ps()

                        def f(e, j=j, t=t, b=b):
                            for k in range(31):
                                r = e.matmul(ps[:, b, :], lhsT=diag[:, k, :],
                                             rhs=U[:, j, t * TT + k:t * TT + k + TT],
                                             start=(k == 0), stop=(k == 30))
                            return r
                        sc.op("pe", f, reads=[t_diag, t_U[j]], writes=[t_ps[b]])
                        sc.op("act", lambda e, j=j, t=t, b=b: e.activation(
                            out=hT[:, j, t * TT:(t + 1) * TT], in_=ps[:, b, :], func=AF.Identity,
                            bias=vcol(j, VR["dw_b"])),
                            reads=[t_ps[b], t_vecs], writes=[t_hT[j][t]])
                if dbg == 3:
                    dump(lambda c, tsl: hT[:, c, tsl], lambda c, t: [t_hT[c][t]])
                    return
                for t in range(NTT):
                    tsl = slice(t * TT, (t + 1) * TT)
                    sc.op("dve", lambda e, tsl=tsl: e.tensor_tensor(
                        out=sqtmp[:], in0=hT[:, :, tsl], in1=hT[:, :, tsl], op=ALU.mult),
                        reads=[t_hT[c][t] for c in range(NCH)], writes=[t_sq])
                    b1 = next_ps()
                    b2 = next_ps()

                    def f(e, b1=b1, b2=b2, tsl=tsl):
                        for c in range(NCH):
                            e.matmul(ps[:, b1, :], lhsT=ones_b[:], rhs=hT[:, c, tsl],
                                     start=(c == 0), stop=(c == NCH - 1))
                        for c in range(NCH):
                            r = e.matmul(ps[:, b2, :], lhsT=ones_b[:], rhs=sqtmp[:, c, :],
                                         start=(c == 0), stop=(c == NCH - 1))
                        return r
                    sc.op("pe", f, reads=[t_sq, t_ones] + [t_hT[c][t] for c in range(NCH)],
                          writes=[t_ps[b1], t_ps[b2]])
                    m = lnst[0]
                    q = lnst[1]
                    rs = lnst[2]
                    tm, tq, trs = t_lnst
                    sc.op("act", lambda e, m=m, b1=b1: e.activation(
                        out=m[:], in_=ps[:, b1, :], func=AF.Copy, scale=1.0 / D),
                        reads=[t_ps[b1]], writes=[tm])
                    sc.op("dve", lambda e, m=m, q=q: e.tensor_tensor(
                        out=q[:], in0=m[:], in1=m[:], op=ALU.mult),
                        reads=[tm], writes=[tq])
                    sc.op("dve", lambda e, q=q, b2=b2: e.scalar_tensor_tensor(
                        out=q[:], in0=ps[:, b2, :], scalar=1.0 / D, in1=q[:],
                        op0=ALU.mult, op1=ALU.subtract),
                        reads=[t_ps[b2], tq], writes=[tq])
                    sc.op("act", lambda e, q=q: e.activation(
                        out=q[:], in_=q[:], func=AF.Sqrt, bias=1e-5),
                        reads=[tq], writes=[tq])
                    sc.op("dve", lambda e, q=q, rs=rs: e.reciprocal(out=rs[:], in_=q[:]),
                          reads=[tq], writes=[trs])
                    sc.op("dve", lambda e, m=m, rs=rs: e.scalar_tensor_tensor(
                        out=m[:], in0=m[:], scalar=-1.0, in1=rs[:],
                        op0=ALU.mult, op1=ALU.mult),
                        reads=[tm, trs], writes=[tm])
                    for c in range(NCH):
                        w1s = next_sm()
                        sc.op("dve", lambda e, c=c, tsl=tsl, rs=rs, w1s=w1s: e.tensor_tensor(
                            out=sm[w1s][:], in0=hT[:, c, tsl], in1=rs[:], op=ALU.mult),
                            reads=[t_hT[c][t], trs], writes=[t_sm[w1s]])
                        sc.op("dve", lambda e, m=m, w1s=w1s: e.tensor_tensor(
                            out=sm[w1s][:], in0=sm[w1s][:], in1=m[:], op=ALU.add),
                            reads=[t_sm[w1s], tm], writes=[t_sm[w1s]])
                        sc.op("act", lambda e, c=c, tsl=tsl, w1s=w1s: e.activation(
                            out=hT[:, c, tsl], in_=sm[w1s][:], func=AF.Silu,
                            scale=vcol(c, VR["ln_g"]), bias=vcol(c, VR["ln_b"])),
                            reads=[t_sm[w1s], t_vecs], writes=[t_hT[c][t]])
                if dbg == 4:
                    dump(lambda c, tsl: hT[:, c, tsl], lambda c, t: [t_hT[c][t]])
                    return
                for h in range(2):
                    w2 = [load_block(w2_d[:, h * 512 + q * 256: h * 512 + (q + 1) * 256]
                                     .rearrange("(kc p) n -> p kc n", p=128), None) for q in range(2)]
                    for jj in range(4):
                        j = h * 4 + jj
                        wA, tA = w2[jj // 2]
                        co = (jj % 2) * 128
                        for t in range(NTT):
                            tsl = slice(t * TT, (t + 1) * TT)
                            b = next_ps()

                            def f(e, wA=wA, co=co, b=b, tsl=tsl):
                                for kc in range(NCH):
                                    r = e.matmul(ps[:, b, :], lhsT=wA[:, kc, co:co + 128],
                                                 rhs=hT[:, kc, tsl], start=(kc == 0),
                                                 stop=(kc == NCH - 1))
                                return r
                            sc.op("pe", f, reads=[tA] + [t_hT[c][t] for c in range(NCH)],
                                  writes=[t_ps[b]])
                            sc.op("dve", lambda e, b=b, j=j, tsl=tsl: e.scalar_tensor_tensor(
                                out=xT[:, j, tsl], in0=ps[:, b, :], scalar=vcol(j, VR["pw2_b"]),
                                in1=xT[:, j, tsl], op0=ALU.add, op1=ALU.add),
                                reads=[t_ps[b], t_vecs] + xtoks(j, t), writes=xtoks(j, t))

        def swiglu_stream(hT, t_hT, h1, t_h1, sg, t_sg, wg_d, wu_d, wd_d, dff,
                          gate_bc=None, t_gate=None):
            ngrp = (dff + 511) // 512
            sgn = 0
            for g in range(ngrp):
                c0 = g * 512
                ncol = min(512, dff - c0)
                nblk = ncol // 256
                nf = ncol // 128
                wg = [load_block(wg_d[:, c0 + q * 256:c0 + (q + 1) * 256]
                                 .rearrange("(kc p) n -> p kc n", p=128), None) for q in range(nblk)]
                wu = [load_block(wu_d[:, c0 + q * 256:c0 + (q + 1) * 256]
                                 .rearrange("(kc p) n -> p kc n", p=128), None) for q in range(nblk)]
                wd = [load_block(wd_d[c0 + q * 256:c0 + (q + 1) * 256, :]
                                 .rearrange("(fc p) n -> p fc n", p=128), None) for q in range(nblk)]
                for t in range(NTT):
                    tsl = slice(t * TT, (t + 1) * TT)
                    for f_ in range(nf):
                        wG, tG = wg[f_ // 2]
                        wU, tU = wu[f_ // 2]
                        co = (f_ % 2) * 128
                        bG = next_ps()
                        bU = next_ps()

                        def f(e, wG=wG, wU=wU, co=co, bG=bG, bU=bU, tsl=tsl):
                            for kc in range(NCH):
                                e.matmul(ps[:, bG, :], lhsT=wG[:, kc, co:co + 128],
                                         rhs=hT[:, kc, tsl], start=(kc == 0), stop=(kc == NCH - 1))
                            for kc in range(NCH):
                                r = e.matmul(ps[:, bU, :], lhsT=wU[:, kc, co:co + 128],
                                             rhs=hT[:, kc, tsl], start=(kc == 0),
                                             stop=(kc == NCH - 1))
                            return r
                        sc.op("pe", f, reads=[tG, tU] + [t_hT[c][t] for c in range(NCH)],
                              writes=[t_ps[bG], t_ps[bU]])
                        s_ = sgn % 2
                        sgn += 1
                        sc.op("act", lambda e, bG=bG, s_=s_: e.activation(
                            out=sg[s_][:], in_=ps[:, bG, :], func=AF.Silu),
                            reads=[t_ps[bG]], writes=[t_sg[s_]])
                        if gate_bc is None:
                            sc.op("dve", lambda e, bU=bU, s_=s_, f_=f_, tsl=tsl: e.tensor_tensor(
                                out=h1[:, f_, tsl], in0=ps[:, bU, :], in1=sg[s_][:], op=ALU.mult),
                                reads=[t_ps[bU], t_sg[s_]], writes=[t_h1[f_][t]])
                        else:
                            sc.op("dve", lambda e, s_=s_, tsl=tsl: e.tensor_tensor(
                                out=sg[s_][:], in0=sg[s_][:], in1=gate_bc[:, tsl], op=ALU.mult),
                                reads=[t_sg[s_], t_gate], writes=[t_sg[s_]])
                            sc.op("dve", lambda e, bU=bU, s_=s_, f_=f_, tsl=tsl: e.tensor_tensor(
                                out=h1[:, f_, tsl], in0=ps[:, bU, :], in1=sg[s_][:], op=ALU.mult),
                                reads=[t_ps[bU], t_sg[s_]], writes=[t_h1[f_][t]])
                for t in range(NTT):
                    tsl = slice(t * TT, (t + 1) * TT)
                    for j in range(NCH):
                        b = next_ps()

                        def f(e, b=b, j=j, tsl=tsl, wd=wd, nf=nf):
                            for f_ in range(nf):
                                wD, _ = wd[f_ // 2]
                                r = e.matmul(ps[:, b, :],
                                             lhsT=wD[:, f_ % 2, j * 128:(j + 1) * 128],
                                             rhs=h1[:, f_, tsl], start=(f_ == 0),
                                             stop=(f_ == nf - 1))
                            return r
                        sc.op("pe", f, reads=[w[1] for w in wd] + [t_h1[f_][t] for f_ in range(nf)],
                              writes=[t_ps[b]])
                        sc.op("dve", lambda e, b=b, j=j, tsl=tsl: e.tensor_tensor(
                            out=xT[:, j, tsl], in0=ps[:, b, :], in1=xT[:, j, tsl], op=ALU.add),
                            reads=[t_ps[b]] + xtoks(j, t), writes=xtoks(j, t))

        def phase_ffn():
            with contextlib.ExitStack() as esp:
                set_pools(esp, 2, 6, WB)
                hT = sb("hT", [128, NCH, S], BF16, esp)
                t_hT = [[Tok("hT%d_%d" % (c, t)) for t in range(NTT)] for c in range(NCH)]
                sqtmp = sb("sqtmp", [128, NCH, TT], BF16, esp)
                t_sq = Tok("sqtmp")
                h1 = sb("h1", [128, 4, S], BF16, esp)
                t_h1 = [[Tok("h1_%d_%d" % (f_, t)) for t in range(NTT)] for f_ in range(4)]
                sg = [sb("sg%d" % i, [128, TT], F32, esp) for i in range(2)]
                t_sg = [Tok("sg0"), Tok("sg1")]
                rmsnorm_fm(hT, t_hT, VR["ffn0"], sqtmp, t_sq)
                swiglu_stream(hT, t_hT, h1, t_h1, sg, t_sg, fg_d, fu_d, fd_d, DFF)


        def phase_gdn():
            NT16 = S // 128
            with contextlib.ExitStack() as esp:
                set_pools(esp, 2, 4, 1024)
                hT = sb("hT", [128, NCH, S], BF16, esp)
                t_hT = [[Tok("hT%d_%d" % (c, t)) for t in range(NTT)] for c in range(NCH)]
                with contextlib.ExitStack() as esq:
                    sqtmp = sb("sqtmp", [128, NCH, TT], BF16, esq)
                    t_sq = Tok("sqtmp")
                    rmsnorm_fm(hT, t_hT, VR["mix1"], sqtmp, t_sq)
                sc.barrier()
                if two_x:
                    with contextlib.ExitStack() as esx:
                        xin2 = [sb("xin2_%d" % i, [128, D], F32, esx) for i in range(2)]
                        t_xin2 = [Tok("xin2_0"), Tok("xin2_1")]
                        load_x_loop(xacc_d, xin2, t_xin2, fence=sc.last_ops())
                    sc.barrier()
                cst = sb("cst", [128, 2048], F32, esp)
                t_cst = Tok("cst")
                gsm = sb("gsm", [128, 160], F32, esp)
                t_gsm = Tok("gsm")
                graw = sb("graw", [128, NT16, 32], F32, esp)
                beta = sb("beta", [128, NT16, 16], F32, esp)
                la = sb("la", [128, NT16, 16], F32, esp)
                gcol = sb("gcol", [128, NT16, 16], F32, esp)
                glast = sb("glast", [128, NT16, 16], F32, esp)
                beg = sb("beg", [128, NT16, 16], F32, esp)
                kdc = sb("kdc", [128, NT16, 16], F32, esp)
                egl = sb("egl", [128, NT16, 16], F32, esp)
                t_graw, t_beta, t_la, t_gcol, t_glast, t_beg, t_kdc, t_egl = [
                    Tok(n) for n in ("graw", "beta", "la", "gcol", "glast", "beg", "kdc", "egl")]
                pre = sb("pre", [128, S + 4], BF16, esp)
                t_pre = Tok("pre")
                diag5 = sb("diag5", [128, 5, 128], BF16, esp)
                t_diag5 = Tok("diag5")
                QKV = [sb("qkv%d" % i, [128, S], F32, esp) for i in range(3)]
                t_QKV = [[Tok("qkv%d_%d" % (i, t)) for t in range(NTT)] for i in range(3)]
                zw = sb("zw", [128, NCH, 128], BF16, esp)
                t_zw = Tok("zw")
                Oacc = sb("Oacc", [128, NT16, 128], F32, esp)
                t_O = [Tok("O%d" % n) for n in range(NT16)]
                oT = sb("oT", [128, S], BF16, esp)
                t_oT = [Tok("oT%d" % t) for t in range(NTT)]
                names = ("bV", "Kt", "KD", "lacb", "bcb", "dec", "QDT", "AT", "PT", "Dm", "DTm",
                         "X", "WK", "U")
                shp = dict(WK=[128, 256])
                TS = [{n: sb("%s_c%d" % (n, c_), shp.get(n, [128, 128]), F32, esp) for n in names}
                      for c_ in range(2)]
                t_TS = [{n: Tok("%s_c%d" % (n, c_)) for n in names} for c_ in range(2)]
                ETs = [sb("ETall%d" % c_, [128, 7, 128], F32, esp) for c_ in range(2)]
                t_ETs = [Tok("ET0"), Tok("ET1")]
                Ss = [sb("Sst%d" % c_, [128, 128], F32, esp) for c_ in range(2)]
                t_Ss = [Tok("S0"), Tok("S1")]
                T_ = {n: sb(n, [128, 128], F32, esp) for n in ("sz", "yy")}
                t_T = {n: Tok(n) for n in ("sz", "yy")}
                bpn = [0, 0]

                def bank_pool(c_):
                    def nb_():
                        i = c_ * 4 + bpn[c_] % 4
                        bpn[c_] += 1
                        return i
                    return nb_
                st4 = sb("st4", [128, 4], F32, esp)
                t_st4 = Tok("st4")
                ch_c = sc.new_chan()
                fence = sc.last_ops()
                sc.op("sp", lambda e: e.dma_start(out=cst[:], in_=cst_d[:, :]), writes=[t_cst],
                      chan=ch_c, after=fence)
                sc.op("sp", lambda e: e.dma_start(out=gsm[:],
                                                  in_=gsm_d[0:1, :].partition_broadcast(128)),
                      writes=[t_gsm], chan=ch_c, after=fence)
                triX = [cst[:, 0:128], cst[:, 128:256]]
                MX = [cst[:, 256:256 + 896].rearrange("p (k i) -> p k i", k=7),
                      cst[:, 1152:1152 + 896].rearrange("p (k i) -> p k i", k=7)]
                sc.op("dve", lambda e: e.memset(pre[:], 0.0), writes=[t_pre])
                wgt, t_wgt = load_block(gin_d[:, 4096:4128].rearrange("(kc p) n -> p kc n", p=128),
                                        None)
                for n in range(NT16):
                    b = next_ps()
                    tsl = slice(n * 128, (n + 1) * 128)

                    def f(e, b=b, tsl=tsl):
                        for kc in range(NCH):
                            r = e.matmul(ps[:, b, 0:32], lhsT=hT[:, kc, tsl], rhs=wgt[:, kc, :],
                                         start=(kc == 0), stop=(kc == NCH - 1))
                        return r
                    sc.op("pe", f, reads=[t_wgt] + [t_hT[c][n // 4] for c in range(NCH)],
                          writes=[t_ps[b]])
                    sc.op("dve", lambda e, b=b, n=n: e.tensor_copy(out=graw[:, n, :],
                                                                   in_=ps[:, b, 0:32]),
                          reads=[t_ps[b]], writes=[t_graw])
                sc.op("act", lambda e: e.activation(out=beta[:], in_=graw[:, :, 0:16],
                                                    func=AF.Sigmoid),
                      reads=[t_graw], writes=[t_beta])
                sc.op("dve", lambda e: e.tensor_tensor(
                    out=la[:], in0=graw[:, :, 16:32],
                    in1=gsm[:, 16:32].unsqueeze(1).to_broadcast([128, NT16, 16]), op=ALU.add),
                    reads=[t_graw, t_gsm], writes=[t_la])
                sc.op("act", lambda e: e.activation(out=la[:], in_=la[:], func=AF.Exp),
                      reads=[t_la], writes=[t_la])
                sc.op("act", lambda e: e.activation(out=la[:], in_=la[:], func=AF.Ln, bias=1.0),
                      reads=[t_la], writes=[t_la])
                sc.op("act", lambda e: e.activation(out=gsm[:, 0:16], in_=gsm[:, 0:16],
                                                    func=AF.Exp),
                      reads=[t_gsm], writes=[t_gsm])
                sc.op("dve", lambda e: e.scalar_tensor_tensor(
                    out=la[:], in0=la[:], scalar=-1.0,
                    in1=gsm[:, 0:16].unsqueeze(1).to_broadcast([128, NT16, 16]),
                    op0=ALU.mult, op1=ALU.mult), reads=[t_la, t_gsm], writes=[t_la])
                for n in range(NT16):
                    b = next_ps()

                    def f(e, b=b, n=n):
                        e.matmul(ps[:, b, 0:8], lhsT=triX[0], rhs=la[:, n, 0:8], start=True,
                                 stop=True)
                        e.matmul(ps[:, b, 8:16], lhsT=triX[1], rhs=la[:, n, 8:16], start=True,
                                 stop=True)
                        return e.matmul(ps[:, b, 16:32], lhsT=ones_f[:], rhs=la[:, n, :],
                                        start=True, stop=True)
                    sc.op("pe", f, reads=[t_la, t_cst, t_ones], writes=[t_ps[b]])
                    sc.op("dve", lambda e, b=b, n=n: e.tensor_copy(out=gcol[:, n, :],
                                                                   in_=ps[:, b, 0:16]),
                          reads=[t_ps[b]], writes=[t_gcol])
                    sc.op("dve", lambda e, b=b, n=n: e.tensor_copy(out=glast[:, n, :],
                                                                   in_=ps[:, b, 16:32]),
                          reads=[t_ps[b]], writes=[t_glast])
                sc.op("act", lambda e: e.activation(out=beg[:], in_=gcol[:], func=AF.Exp),
                      reads=[t_gcol], writes=[t_beg])
                sc.op("dve", lambda e: e.tensor_tensor(out=beg[:], in0=beg[:], in1=beta[:],
                                                       op=ALU.mult),
                      reads=[t_beg, t_beta], writes=[t_beg])
                sc.op("dve", lambda e: e.tensor_tensor(out=kdc[:], in0=glast[:], in1=gcol[:],
                                                       op=ALU.subtract),
                      reads=[t_glast, t_gcol], writes=[t_kdc])
                sc.op("act", lambda e: e.activation(out=kdc[:], in_=kdc[:], func=AF.Exp),
                      reads=[t_kdc], writes=[t_kdc])
                sc.op("act", lambda e: e.activation(out=egl[:], in_=glast[:], func=AF.Exp),
                      reads=[t_glast], writes=[t_egl])

                def tt_(n, out, in0, in1, op, eng="dve", rd=(), wr=()):
                    sc.op(eng, lambda e: e.tensor_tensor(out=out, in0=in0, in1=in1, op=op),
                          reads=list(rd), writes=list(wr))

                if dbg == 11:
                    return
                for h in heads:
                    for part in range(3):
                        cidx = part * 8 + h
                        wv, t_wv = load_block(gin_d[:, cidx * 128:(cidx + 1) * 128]
                                              .rearrange("(kc p) n -> p kc n", p=128), None)
                        for k in range(5):
                            sc.op("dve", lambda e, k=k, part=part, h=h: e.tensor_scalar(
                                out=diag5[:, k, :], in0=ident[:],
                                scalar1=vcol(h, VR["gconv"] + k * 3 + part), scalar2=None,
                                op0=ALU.mult), reads=[t_ident, t_vecs], writes=[t_diag5])
                        for t in range(NTT):
                            tsl = slice(t * TT, (t + 1) * TT)
                            b = next_ps()

                            def f(e, b=b, tsl=tsl, wv=wv):
                                for kc in range(NCH):
                                    r = e.matmul(ps[:, b, :], lhsT=wv[:, kc, :], rhs=hT[:, kc, tsl],
                                                 start=(kc == 0), stop=(kc == NCH - 1))
                                return r
                            sc.op("pe", f, reads=[t_wv] + [t_hT[c][t] for c in range(NCH)],
                                  writes=[t_ps[b]])
                            sc.op("act", lambda e, b=b, t=t: e.activation(
                                out=pre[:, 2 + t * TT:2 + (t + 1) * TT], in_=ps[:, b, :],
                                func=AF.Copy), reads=[t_ps[b]], writes=[t_pre])
                        for t in range(NTT):
                            tsl = slice(t * TT, (t + 1) * TT)
                            b = next_ps()

                            def f(e, b=b, t=t):
                                for k in range(5):
                                    r = e.matmul(ps[:, b, :], lhsT=diag5[:, k, :],
                                                 rhs=pre[:, t * TT + k:t * TT + k + TT],
                                                 start=(k == 0), stop=(k == 4))
                                return r
                            sc.op("pe", f, reads=[t_diag5, t_pre], writes=[t_ps[b]])
                            sc.op("act", lambda e, b=b, tsl=tsl, part=part: e.activation(
                                out=QKV[part][:, tsl], in_=ps[:, b, :], func=AF.Silu),
                                reads=[t_ps[b]], writes=[t_QKV[part][t]])
                            if part < 2:
                                la_ = next_sm()
                                lb_ = next_sm()
                                sc.op("dve", lambda e, tsl=tsl, part=part, la_=la_: e.tensor_tensor(
                                    out=sm[la_][:], in0=QKV[part][:, tsl], in1=QKV[part][:, tsl],
                                    op=ALU.mult), reads=[t_QKV[part][t]], writes=[t_sm[la_]])
                                b2 = next_ps()
                                sc.op("pe", lambda e, b2=b2, la_=la_: e.matmul(
                                    ps[:, b2, :], lhsT=ones_f[:], rhs=sm[la_][:], start=True,
                                    stop=True), reads=[t_sm[la_], t_ones], writes=[t_ps[b2]])
                                sc.op("act", lambda e, b2=b2, lb_=lb_: e.activation(
                                    out=sm[lb_][:], in_=ps[:, b2, :], func=AF.Sqrt, bias=1e-6),
                                    reads=[t_ps[b2]], writes=[t_sm[lb_]])
                                sc.op("dve", lambda e, lb_=lb_: e.reciprocal(out=sm[lb_][:],
                                                                             in_=sm[lb_][:]),
                                      reads=[t_sm[lb_]], writes=[t_sm[lb_]])
                                scl = (128.0 ** -0.5) if part == 0 else 1.0
                                sc.op("dve", lambda e, tsl=tsl, part=part, scl=scl, lb_=lb_:
                                      e.scalar_tensor_tensor(
                                          out=QKV[part][:, tsl], in0=QKV[part][:, tsl], scalar=scl,
                                          in1=sm[lb_][:], op0=ALU.mult, op1=ALU.mult),
                                      reads=[t_QKV[part][t], t_sm[lb_]], writes=[t_QKV[part][t]])
                    if dbg == 12:
                        return
                    zwv, t_zwv = load_block(gin_d[:, 3072 + h * 128:3072 + (h + 1) * 128]
                                            .rearrange("(kc p) n -> p kc n", p=128), None)
                    sc.op("dve", lambda e, zwv=zwv: e.tensor_copy(out=zw[:], in_=zwv),
                          reads=[t_zwv], writes=[t_zw])
                    QT, KT, VT = QKV
                    def chain(dr, T_, t_T, ETall, t_ET, Sst, t_S, next_ps, sc):
                        sc.op("dve", lambda e: e.memset(Sst[:], 0.0), writes=[t_S])
                        order = range(NT16) if dr == 0 else range(NT16 - 1, -1, -1)
                        dh = dr * 8 + h
                        for n in order:
                            csl = slice(n * 128, (n + 1) * 128)
                            t4 = n // 4
                            rq = [t_QKV[0][t4]]
                            rk = [t_QKV[1][t4]]
                            rv = [t_QKV[2][t4]]
                            bt = next_ps()

                            def f(e, bt=bt, csl=csl):
                                e.transpose(out=ps[:, bt, 0:128], in_=KT[:, csl], identity=ident[:])
                                return e.transpose(out=ps[:, bt, 128:256], in_=VT[:, csl],
                                                   identity=ident[:])
                            sc.op("pe", f, reads=rk + rv + [t_ident], writes=[t_ps[bt]])
                            sc.op("dve", lambda e, bt=bt, n=n, dh=dh: e.tensor_scalar(
                                out=T_["bV"][:], in0=ps[:, bt, 128:256],
                                scalar1=beta[:, n, dh:dh + 1], scalar2=None, op0=ALU.mult),
                                reads=[t_ps[bt], t_beta], writes=[t_T["bV"]])
                            sc.op("dve", lambda e, bt=bt, n=n, dh=dh: e.tensor_scalar(
                                out=T_["Kt"][:], in0=ps[:, bt, 0:128],
                                scalar1=beg[:, n, dh:dh + 1], scalar2=None, op0=ALU.mult),
                                reads=[t_ps[bt], t_beg], writes=[t_T["Kt"]])
                            sc.op("dve", lambda e, bt=bt, n=n, dh=dh: e.tensor_scalar(
                                out=T_["KD"][:], in0=ps[:, bt, 0:128],
                                scalar1=kdc[:, n, dh:dh + 1], scalar2=None, op0=ALU.mult),
                                reads=[t_ps[bt], t_kdc], writes=[t_T["KD"]])
                            sc.op("dve", lambda e, n=n, dh=dh: e.tensor_scalar(
                                out=T_["lacb"][:], in0=ones_f[:], scalar1=la[:, n, dh:dh + 1],
                                scalar2=None, op0=ALU.mult),
                                reads=[t_la, t_ones], writes=[t_T["lacb"]])
                            sc.op("dve", lambda e, n=n, dh=dh: e.tensor_scalar(
                                out=T_["bcb"][:], in0=ones_f[:], scalar1=beta[:, n, dh:dh + 1],
                                scalar2=None, op0=ALU.mult),
                                reads=[t_beta, t_ones], writes=[t_T["bcb"]])
                            bm = next_ps()

                            def f(e, bm=bm, csl=csl, dr=dr):
                                e.matmul(ps[:, bm, 0:128], lhsT=T_["lacb"][:], rhs=triX[dr],
                                         start=True, stop=True)
                                e.matmul(ps[:, bm, 128:256], lhsT=T_["bcb"][:], rhs=ident[:],
                                         start=True, stop=True)
                                e.matmul(ps[:, bm, 256:384], lhsT=KT[:, csl], rhs=KT[:, csl],
                                         start=True, stop=True)
                                return e.matmul(ps[:, bm, 384:512], lhsT=KT[:, csl],
                                                rhs=QT[:, csl], start=True, stop=True)
                            sc.op("pe", f, reads=[t_T["lacb"], t_T["bcb"], t_cst, t_ident] + rk + rq,
                                  writes=[t_ps[bm]])
                            sc.op("dve", lambda e, bm=bm, n=n, dh=dh: e.tensor_scalar(
                                out=T_["dec"][:], in0=ps[:, bm, 0:128],
                                scalar1=gcol[:, n, dh:dh + 1], scalar2=None, op0=ALU.subtract),
                                reads=[t_ps[bm], t_gcol], writes=[t_T["dec"]])
                            sc.op("dve", lambda e: e.tensor_scalar(
                                out=T_["dec"][:], in0=T_["dec"][:], scalar1=0.0, scalar2=None,
                                op0=ALU.min), reads=[t_T["dec"]], writes=[t_T["dec"]])
                            sc.op("act", lambda e: e.activation(out=T_["dec"][:], in_=T_["dec"][:],
                                                                func=AF.Exp),
                                  reads=[t_T["dec"]], writes=[t_T["dec"]])
                            sc.op("act", lambda e, bm=bm: e.activation(
                                out=T_["QDT"][:], in_=ps[:, bm, 0:128], func=AF.Exp),
                                reads=[t_ps[bm]], writes=[t_T["QDT"]])
                            sc.op("dve", lambda e, csl=csl: e.tensor_tensor(
                                out=T_["QDT"][:], in0=T_["QDT"][:], in1=QT[:, csl], op=ALU.mult),
                                reads=[t_T["QDT"]] + rq, writes=[t_T["QDT"]])
                            sc.op("dve", lambda e, bm=bm: e.tensor_tensor(
                                out=T_["AT"][:], in0=ps[:, bm, 256:384], in1=T_["dec"][:],
                                op=ALU.mult), reads=[t_ps[bm], t_T["dec"]], writes=[t_T["AT"]])
                            sc.op("dve", lambda e, bm=bm: e.tensor_tensor(
                                out=T_["AT"][:], in0=ps[:, bm, 128:256], in1=T_["AT"][:],
                                op=ALU.mult), reads=[t_ps[bm], t_T["AT"]], writes=[t_T["AT"]])
                            sc.op("dve", lambda e, bm=bm: e.tensor_tensor(
                                out=T_["PT"][:], in0=ps[:, bm, 384:512], in1=T_["dec"][:],
                                op=ALU.mult), reads=[t_ps[bm], t_T["dec"]], writes=[t_T["PT"]])
                            sc.op("dve", lambda e, dr=dr: e.tensor_tensor(
                                out=T_["PT"][:], in0=T_["PT"][:], in1=triX[dr], op=ALU.mult),
                                reads=[t_T["PT"], t_cst], writes=[t_T["PT"]])
                            for lv in range(7):
                                sc.op("dve", lambda e, dr=dr, lv=lv: e.tensor_tensor(
                                    out=ETall[:, lv, :], in0=T_["AT"][:], in1=MX[dr][:, lv, :],
                                    op=ALU.mult), reads=[t_T["AT"], t_cst], writes=[t_ET])
                            if dbg == 13:
                                return
                            for lv in range(7):
                                Dc = ident if lv == 0 else T_["Dm"]
                                DTc = ident if lv == 0 else T_["DTm"]
                                rD = [t_ident] if lv == 0 else [t_T["Dm"]]
                                rDT = [t_ident] if lv == 0 else [t_T["DTm"]]
                                bx = next_ps()
                                sc.op("pe", lambda e, bx=bx, lv=lv, Dc=Dc: e.matmul(
                                    ps[:, bx, 0:128], lhsT=ETall[:, lv, :], rhs=Dc[:], start=True,
                                    stop=True), reads=[t_ET] + rD, writes=[t_ps[bx]])
                                sc.op("act", lambda e, bx=bx: e.activation(
                                    out=T_["X"][:], in_=ps[:, bx, 0:128], func=AF.Copy),
                                    reads=[t_ps[bx]], writes=[t_T["X"]])
                                by = next_ps()

                                def f(e, by=by, Dc=Dc, DTc=DTc):
                                    e.matmul(ps[:, by, 0:128], lhsT=DTc[:], rhs=T_["X"][:],
                                             start=True, stop=True)
                                    return e.matmul(ps[:, by, 128:256], lhsT=T_["X"][:], rhs=DTc[:],
                                                    start=True, stop=True)
                                sc.op("pe", f, reads=[t_T["X"]] + rDT, writes=[t_ps[by]])
                                sc.op("dve", lambda e, by=by, Dc=Dc: e.tensor_tensor(
                                    out=T_["Dm"][:], in0=Dc[:], in1=ps[:, by, 0:128],
                                    op=ALU.subtract), reads=[t_ps[by]] + rD, writes=[t_T["Dm"]])
                                sc.op("dve", lambda e, by=by, DTc=DTc: e.tensor_tensor(
                                    out=T_["DTm"][:], in0=DTc[:], in1=ps[:, by, 128:256],
                                    op=ALU.subtract), reads=[t_ps[by]] + rDT, writes=[t_T["DTm"]])
                            if dbg == 14:
                                return
                            bw = next_ps()

                            def f(e, bw=bw):
                                e.matmul(ps[:, bw, 0:128], lhsT=T_["DTm"][:], rhs=T_["bV"][:],
                                         start=True, stop=True)
                                return e.matmul(ps[:, bw, 128:256], lhsT=T_["Kt"][:],
                                                rhs=T_["DTm"][:], start=True, stop=True)
                            sc.op("pe", f, reads=[t_T["DTm"], t_T["bV"], t_T["Kt"]],
                                  writes=[t_ps[bw]])
                            sc.op("act", lambda e, bw=bw: e.activation(
                                out=T_["WK"][:], in_=ps[:, bw, 0:256], func=AF.Copy),
                                reads=[t_ps[bw]], writes=[t_T["WK"]])
                            if dbg == 21:
                                return
                            bs = next_ps()
                            sc.op("pe", lambda e, bs=bs: e.matmul(
                                ps[:, bs, 0:128], lhsT=T_["WK"][:, 128:256], rhs=Sst[:], start=True,
                                stop=True), reads=[t_T["WK"], t_S], writes=[t_ps[bs]])
                            sc.op("dve", lambda e, bs=bs: e.tensor_tensor(
                                out=T_["U"][:], in0=T_["WK"][:, 0:128], in1=ps[:, bs, 0:128],
                                op=ALU.subtract), reads=[t_ps[bs], t_T["WK"]], writes=[t_T["U"]])
                            if dbg == 22:
                                return
                            bo = next_ps()

                            def f(e, bo=bo):
                                e.matmul(ps[:, bo, 0:128], lhsT=T_["QDT"][:], rhs=Sst[:], start=True,
                                         stop=False)
                                e.matmul(ps[:, bo, 0:128], lhsT=T_["PT"][:], rhs=T_["U"][:],
                                         start=False, stop=True)
                                return e.matmul(ps[:, bo, 128:256], lhsT=T_["KD"][:], rhs=T_["U"][:],
                                                start=True, stop=True)
                            sc.op("pe", f, reads=[t_T["QDT"], t_S, t_T["PT"], t_T["U"], t_T["KD"]],
                                  writes=[t_ps[bo]])
                            if dbg == 23:
                                return
                            sc.op("dve", lambda e, bo=bo, n=n: e.tensor_tensor(
                                out=Oacc[:, n, :], in0=ps[:, bo, 0:128], in1=Oacc[:, n, :],
                                op=ALU.add), reads=[t_ps[bo], t_O[n]], writes=[t_O[n]])
                            sc.op("dve", lambda e, n=n, dh=dh: e.tensor_scalar(
                                out=Sst[:], in0=Sst[:], scalar1=egl[:, n, dh:dh + 1], scalar2=None,
                                op0=ALU.mult), reads=[t_S, t_egl], writes=[t_S])
                            sc.op("dve", lambda e, bo=bo: e.tensor_tensor(
                                out=Sst[:], in0=ps[:, bo, 128:256], in1=Sst[:], op=ALU.add),
                                reads=[t_ps[bo], t_S], writes=[t_S])
                    sc.op("dve", lambda e: e.memset(Oacc[:], 0.0), writes=t_O)
                    recs = [Rec(), Rec()]
                    for c_ in range(2):
                        chain(c_, TS[c_], t_TS[c_], ETs[c_], t_ETs[c_], Ss[c_], t_Ss[c_],
                              bank_pool(c_), recs[c_])
                    for i_ in range(max(len(r.items) for r in recs)):
                        for r in recs:
                            if i_ < len(r.items):
                                a_, k_ = r.items[i_]
                                sc.op(*a_, **k_)
                    for q in range(NTT):
                        bT = next_ps()
                        for i4 in range(4):
                            n = q * 4 + i4
                            csl = slice(n * 128, (n + 1) * 128)
                            sc.op("act", lambda e, n=n: e.activation(
                                out=T_["yy"][:], in_=Oacc[:, n, :], func=AF.Square,
                                accum_out=st4[:, 0:1]), reads=[t_O[n]],
                                writes=[t_T["yy"], t_st4])
                            sc.op("act", lambda e: e.activation(
                                out=st4[:, 1:2], in_=st4[:, 0:1], func=AF.Sqrt, scale=1.0 / 128,
                                bias=1e-6), reads=[t_st4], writes=[t_st4])
                            sc.op("dve", lambda e: e.reciprocal(out=st4[:, 2:3], in_=st4[:, 1:2]),
                                  reads=[t_st4], writes=[t_st4])
                            bz = next_ps()

                            def f(e, bz=bz, csl=csl):
                                for kc in range(NCH):
                                    r = e.matmul(ps[:, bz, 0:128], lhsT=hT[:, kc, csl],
                                                 rhs=zw[:, kc, :], start=(kc == 0),
                                                 stop=(kc == NCH - 1))
                                return r
                            sc.op("pe", f, reads=[t_zw] + [t_hT[c][q] for c in range(NCH)],
                                  writes=[t_ps[bz]])
                            sc.op("act", lambda e, bz=bz: e.activation(
                                out=T_["sz"][:], in_=ps[:, bz, 0:128], func=AF.Silu),
                                reads=[t_ps[bz]], writes=[t_T["sz"]])
                            sc.op("dve", lambda e, n=n: e.scalar_tensor_tensor(
                                out=T_["yy"][:], in0=Oacc[:, n, :], scalar=st4[:, 2:3],
                                in1=gsm[:, 32:160], op0=ALU.mult, op1=ALU.mult),
                                reads=[t_O[n], t_st4, t_gsm], writes=[t_T["yy"]])
                            sc.op("dve", lambda e: e.tensor_tensor(
                                out=T_["yy"][:], in0=T_["yy"][:], in1=T_["sz"][:], op=ALU.mult),
                                reads=[t_T["yy"], t_T["sz"]], writes=[t_T["yy"]])
                            sc.op("pe", lambda e, bT=bT, i4=i4: e.transpose(
                                out=ps[:, bT, i4 * 128:(i4 + 1) * 128], in_=T_["yy"][:],
                                identity=ident[:]), reads=[t_T["yy"], t_ident], writes=[t_ps[bT]])
                        sc.op("act", lambda e, bT=bT, q=q: e.activation(
                            out=oT[:, q * TT:(q + 1) * TT], in_=ps[:, bT, :], func=AF.Copy),
                            reads=[t_ps[bT]], writes=[t_oT[q]])
                    if dbg == 17:
                        return
                    wo, t_wo = load_block(gout_d[h * 128:(h + 1) * 128, :]
                                          .rearrange("p (a n) -> p a n", a=1), None)
                    for t in range(NTT):
                        tsl = slice(t * TT, (t + 1) * TT)
                        for j in range(NCH):
                            b = next_ps()
                            sc.op("pe", lambda e, b=b, j=j, tsl=tsl, wo=wo: e.matmul(
                                ps[:, b, :], lhsT=wo[:, 0, j * 128:(j + 1) * 128], rhs=oT[:, tsl],
                                start=True, stop=True), reads=[t_wo, t_oT[t]], writes=[t_ps[b]])
                            sc.op("dve", lambda e, b=b, j=j, tsl=tsl: e.tensor_tensor(
                                out=xT[:, j, tsl], in0=ps[:, b, :], in1=xT[:, j, tsl], op=ALU.add),
                                reads=[t_ps[b]] + xtoks(j, t), writes=xtoks(j, t))
                    if dbg == 30 + h:
                        return


        def phase_moe():
            NT16 = S // 128
            with contextlib.ExitStack() as esp:
                set_pools(esp, 2, 6, WB)
                hT = sb("hT", [128, NCH, S], BF16, esp)
                t_hT = [[Tok("hT%d_%d" % (c, t)) for t in range(NTT)] for c in range(NCH)]
                sqtmp = sb("sqtmp", [128, NCH, TT], BF16, esp)
                t_sq = Tok("sqtmp")
                h1 = sb("h1", [128, 4, S], BF16, esp)
                t_h1 = [[Tok("h1_%d_%d" % (f_, t)) for t in range(NTT)] for f_ in range(4)]
                sg = [sb("sg%d" % i, [128, TT], F32, esp) for i in range(2)]
                t_sg = [Tok("sg0"), Tok("sg1")]
                G = [sb("G%d" % i, [128, S], F32, esp) for i in range(2)]
                t_G = [Tok("G0"), Tok("G1")]
                wr = sb("wr", [128, NCH, 8], F32, esp)
                t_wr = Tok("wr")
                sq32 = [sb("sq32_%d" % i, [128, NCH, 128], F32, esp) for i in range(2)]
                t_sq32 = [Tok("sq32_0"), Tok("sq32_1")]
                rst = sb("rst", [128, NT16, 9], F32, esp)
                t_rst = Tok("rst")
                L = sb("L", [128, NT16, 8], F32, esp)
                v8 = sb("v8", [128, NT16, 8], F32, esp)
                gate = sb("gate", [128, NT16, 8], F32, esp)
                tmpr = sb("tmpr", [128, NT16, 8], F32, esp)
                rs16 = sb("rs16", [128, NT16, 4], F32, esp)
                dg = [sb("dg%d" % i, [128, 128], F32, esp) for i in range(2)]
                t_dg = [Tok("dg0"), Tok("dg1")]
                t_L, t_v8, t_gate, t_tmpr, t_rs16 = (Tok("L"), Tok("v8"), Tok("gate"),
                                                     Tok("tmpr"), Tok("rs16"))
                ch_wr = sc.new_chan()
                rmsnorm_fm(hT, t_hT, VR["ffn1"], sqtmp, t_sq)
                sc.op("sp", lambda e: e.dma_start(
                    out=wr[:], in_=rt_d.rearrange("(c p) e -> p c e", p=128)),
                    writes=[t_wr], chan=ch_wr, after=sc.last_ops())
                for c in range(NCH):
                    sc.op("dve", lambda e, c=c: e.tensor_scalar(
                        out=wr[:, c, :], in0=wr[:, c, :], scalar1=vcol(c, VR["ffn1"]),
                        scalar2=None, op0=ALU.mult), reads=[t_wr, t_vecs], writes=[t_wr])
                for tt in range(NT16):
                    s_ = tt % 2
                    tsl = slice(tt * 128, (tt + 1) * 128)
                    sc.op("act", lambda e, s_=s_, tsl=tsl: e.activation(
                        out=sq32[s_][:], in_=xT[:, :, tsl], func=AF.Square),
                        reads=[t_xT[c][tt] for c in range(NCH)], writes=[t_sq32[s_]])
                    b = next_ps()

                    def f(e, b=b, tsl=tsl, s_=s_):
                        for c in range(NCH):
                            e.matmul(ps[:, b, 0:8], lhsT=xT[:, c, tsl], rhs=wr[:, c, :],
                                     start=(c == 0), stop=(c == NCH - 1))
                        for c in range(NCH):
                            r = e.matmul(ps[:, b, 8:9], lhsT=sq32[s_][:, c, :],
                                         rhs=ones_f[:, 0:1], start=(c == 0), stop=(c == NCH - 1))
                        return r
                    sc.op("pe", f, reads=[t_wr, t_sq32[s_], t_ones] +
                          [t_xT[c][tt] for c in range(NCH)], writes=[t_ps[b]])
                    sc.op("dve", lambda e, b=b, tt=tt: e.tensor_copy(
                        out=rst[:, tt, :], in_=ps[:, b, 0:9]), reads=[t_ps[b]], writes=[t_rst])
                sc.op("act", lambda e: e.activation(
                    out=rs16[:, :, 0:1], in_=rst[:, :, 8:9], func=AF.Sqrt, scale=1.0 / D,
                    bias=1e-6), reads=[t_rst], writes=[t_rs16])
                sc.op("dve", lambda e: e.reciprocal(out=rs16[:, :, 1:2], in_=rs16[:, :, 0:1]),
                      reads=[t_rs16], writes=[t_rs16])
                sc.op("dve", lambda e: e.tensor_tensor(
                    out=L[:], in0=rst[:, :, 0:8],
                    in1=rs16[:, :, 1:2].to_broadcast([128, NT16, 8]), op=ALU.mult),
                    reads=[t_rst, t_rs16], writes=[t_L])
                for tt in range(NT16):
                    sc.op("dve", lambda e, tt=tt: e.max(out=v8[:, tt, :], in_=L[:, tt, :]),
                          reads=[t_L], writes=[t_v8])
                sc.op("dve", lambda e: e.tensor_tensor(
                    out=gate[:], in0=L[:], in1=v8[:, :, 1:2].to_broadcast([128, NT16, 8]),
                    op=ALU.is_ge), reads=[t_L, t_v8], writes=[t_gate])
                sc.op("dve", lambda e: e.tensor_tensor(
                    out=tmpr[:], in0=L[:], in1=v8[:, :, 0:1].to_broadcast([128, NT16, 8]),
                    op=ALU.subtract), reads=[t_L, t_v8], writes=[t_tmpr])
                sc.op("act", lambda e: e.activation(out=tmpr[:], in_=tmpr[:], func=AF.Exp),
                      reads=[t_tmpr], writes=[t_tmpr])
                sc.op("dve", lambda e: e.tensor_tensor(
                    out=rs16[:, :, 2:3], in0=v8[:, :, 1:2], in1=v8[:, :, 0:1], op=ALU.subtract),
                    reads=[t_v8], writes=[t_rs16])
                sc.op("act", lambda e: e.activation(out=rs16[:, :, 2:3], in_=rs16[:, :, 2:3],
                                                    func=AF.Exp),
                      reads=[t_rs16], writes=[t_rs16])
                sc.op("dve", lambda e: e.tensor_scalar(
                    out=rs16[:, :, 2:3], in0=rs16[:, :, 2:3], scalar1=1.0, scalar2=None,
                    op0=ALU.add), reads=[t_rs16], writes=[t_rs16])
                sc.op("dve", lambda e: e.reciprocal(out=rs16[:, :, 3:4], in_=rs16[:, :, 2:3]),
                      reads=[t_rs16], writes=[t_rs16])
                sc.op("dve", lambda e: e.tensor_tensor(
                    out=gate[:], in0=gate[:], in1=tmpr[:], op=ALU.mult),
                    reads=[t_gate, t_tmpr], writes=[t_gate])
                sc.op("dve", lambda e: e.tensor_tensor(
                    out=gate[:], in0=gate[:], in1=rs16[:, :, 3:4].to_broadcast([128, NT16, 8]),
                    op=ALU.mult), reads=[t_gate, t_rs16], writes=[t_gate])
                dgn = 0
                for ex in range(8):
                    gi = ex % 2
                    for q in range(NTT):
                        b = next_ps()
                        for i4 in range(4):
                            tt = q * 4 + i4
                            di = dgn % 2
                            dgn += 1
                            sc.op("dve", lambda e, di=di, tt=tt, ex=ex: e.tensor_tensor(
                                out=dg[di][:], in0=ident[:],
                                in1=gate[:, tt, ex:ex + 1].to_broadcast([128, 128]), op=ALU.mult),
                                reads=[t_ident, t_gate], writes=[t_dg[di]])
                            sc.op("pe", lambda e, b=b, i4=i4, di=di: e.matmul(
                                ps[:, b, i4 * 128:(i4 + 1) * 128], lhsT=ones_f[:], rhs=dg[di][:],
                                start=True, stop=True),
                                reads=[t_dg[di], t_ones], writes=[t_ps[b]])
                        sc.op("act", lambda e, b=b, gi=gi, q=q: e.activation(
                            out=G[gi][:, q * TT:(q + 1) * TT], in_=ps[:, b, :], func=AF.Copy),
                            reads=[t_ps[b]], writes=[t_G[gi]])
                    swiglu_stream(hT, t_hT, h1, t_h1, sg, t_sg, mg_d[ex], mu_d[ex], md_d[ex], DFFE,
                                  gate_bc=G[gi], t_gate=t_G[gi])

        if "c0" in phases:
            phase_conformer()
            sc.barrier()
        if "f0" in phases:
            phase_ffn()
            sc.barrier()
        if "g1" in phases:
            phase_gdn()
            sc.barrier()
        if "m1" in phases:
            phase_moe()
            sc.barrier()

        do_norm = "final" in phases
        with contextlib.ExitStack() as es2:
            xo = [sb("xo%d" % i, [128, D], F32, es2) for i in range(2)]
            fng = sb("fng_sb", [128, D], F32, es2)
            sc.op("sp", lambda e: e.dma_start(out=fng[:],
                                              in_=fng_d[0:1, :].partition_broadcast(128)),
                  writes=[t_fng], chan=ch_misc, after=sc.last_ops())
            yo = [sb("yo%d" % i, [128, D], F32, es2) for i in range(2)]
            sq = sb("sqj", [128, D], F32, es2)
            st = [sb("st%d" % i, [128, 4], F32, es2) for i in range(2)]
            t_xo = [Tok("xo0"), Tok("xo1")]
            t_yo = [Tok("yo0"), Tok("yo1")]
            t_sq2 = Tok("sq")
            t_st = [Tok("st0"), Tok("st1")]
            out_ops = []
            for tt in range(S // 128):
                sl = tt % 2
                for half in range(2):
                    b = next_ps()

                    def f(e, half=half, b=b, tt=tt):
                        for j in range(4):
                            c = half * 4 + j
                            r = e.transpose(out=ps[:, b, j * 128:(j + 1) * 128],
                                            in_=xT[:, c, tt * 128:(tt + 1) * 128],
                                            identity=ident[:])
                        return r
                    sc.op("pe", f, reads=[t_ident] + [t_xT[half * 4 + j][tt] for j in range(4)],
                          writes=[t_ps[b]])
                    dst = xo if do_norm else yo
                    t_dst = t_xo if do_norm else t_yo
                    sc.op("act", lambda e, half=half, b=b, sl=sl, dst=dst: e.activation(
                        out=dst[sl][:, half * 512:(half + 1) * 512], in_=ps[:, b, :], func=AF.Copy),
                        reads=[t_ps[b]], writes=[t_dst[sl]])
                if do_norm:
                    sc.op("act", lambda e, sl=sl: e.activation(
                        out=sq[:], in_=xo[sl][:], func=AF.Square, accum_out=st[sl][:, 0:1]),
                        reads=[t_xo[sl]], writes=[t_sq2, t_st[sl]])
                    sc.op("act", lambda e, sl=sl: e.activation(
                        out=st[sl][:, 1:2], in_=st[sl][:, 0:1], func=AF.Sqrt, scale=1.0 / D,
                        bias=1e-6), reads=[t_st[sl]], writes=[t_st[sl]])
                    sc.op("dve", lambda e, sl=sl: e.reciprocal(out=st[sl][:, 2:3],
                                                               in_=st[sl][:, 1:2]),
                          reads=[t_st[sl]], writes=[t_st[sl]])
                    sc.op("dve", lambda e, sl=sl: e.scalar_tensor_tensor(
                        out=yo[sl][:], in0=xo[sl][:], scalar=st[sl][:, 2:3], in1=fng[:],
                        op0=ALU.mult, op1=ALU.mult),
                        reads=[t_xo[sl], t_st[sl], t_fng], writes=[t_yo[sl]])
                o = sc.op("sp", lambda e, sl=sl, tt=tt: e.dma_start(
                    out=out_d[tt * 128:(tt + 1) * 128, :], in_=yo[sl][:]),
                    reads=[t_yo[sl]], chan=ch_out[sl])
                out_ops.append(o)
            fin = sc.op("sp", lambda e: e.nop())
            fin.is_nop = True
            fin.deps.extend(out_ops[-2:])
            sc.emit(nc)
    return nc


def pack_vecs(inp):
    rows = np.zeros((128, D), np.float32)
    rows[VR["mix0"]] = inp["mix_norm"][0]
    rows[VR["mix1"]] = inp["mix_norm"][1]
    rows[VR["ffn0"]] = inp["ffn_norm"][0]
    rows[VR["ffn1"]] = inp["ffn_norm"][1]
    rows[VR["pw1_ba"]] = inp["cf_pw1_b"][0, :D]
    rows[VR["pw1_bb"]] = inp["cf_pw1_b"][0, D:]
    rows[VR["dw_b"]] = inp["cf_dw_b"][0]
    rows[VR["ln_g"]] = inp["cf_ln_g"][0]
    rows[VR["ln_b"]] = inp["cf_ln_b"][0]
    rows[VR["pw2_b"]] = inp["cf_pw2_b"][0]
    rows[VR["dw_w"]:VR["dw_w"] + 31] = inp["cf_dw_w"][0]
    gc = inp["gdn_conv_w"][0]
    for k in range(5):
        for part in range(3):
            rows[VR["gconv"] + k * 3 + part] = gc[k, part * D:(part + 1) * D]
    return rows


def gdn_consts():
    i = np.arange(128)
    c = np.zeros((128, 2048), np.float32)
    c[:, 0:128] = (i[:, None] <= i[None, :])
    c[:, 128:256] = (i[:, None] >= i[None, :])
    for k in range(7):
        bsz = 1 << k
        I, J = i[:, None], i[None, :]
        m = ((I // (2 * bsz)) == (J // (2 * bsz))) & (((I // bsz) % 2) == 1) & (((J // bsz) % 2) == 0)
        c[:, 256 + k * 128:256 + (k + 1) * 128] = m.T
        c[:, 1152 + k * 128:1152 + (k + 1) * 128] = m
    return c


def make_in_maps(inp, phases, nb):
    x = np.ascontiguousarray(inp["x"], dtype=np.float32)
    vecs = pack_vecs(inp)
    fng = np.ascontiguousarray(inp["final_norm"], dtype=np.float32).reshape(1, D)
    base = {"vecs": vecs, "fng": fng}
    if "c0" in phases:
        base["cf_pw1_w"] = np.ascontiguousarray(inp["cf_pw1_w"][0])
        base["cf_pw2_w"] = np.ascontiguousarray(inp["cf_pw2_w"][0])
    if "f0" in phases:
        base["ffn_w_gate"] = np.ascontiguousarray(inp["ffn_w_gate"][0])
        base["ffn_w_up"] = np.ascontiguousarray(inp["ffn_w_up"][0])
        base["ffn_w_down"] = np.ascontiguousarray(inp["ffn_w_down"][0])
    if "g1" in phases:
        base["gdn_w_in"] = np.ascontiguousarray(inp["gdn_w_in"][0])
        base["gdn_w_out"] = np.ascontiguousarray(inp["gdn_w_out"][0])
        base["gsmall"] = np.concatenate([inp["gdn_a_log"][0].reshape(-1),
                                         inp["gdn_dt_bias"][0].reshape(-1),
                                         inp["gdn_o_norm"][0].reshape(-1)]).astype(np.float32)[None]
        base["gconst"] = gdn_consts()
    if "m1" in phases:
        base["moe_router"] = np.ascontiguousarray(inp["moe_router"][0])
        base["moe_w_gate"] = np.ascontiguousarray(inp["moe_w_gate"][0])
        base["moe_w_up"] = np.ascontiguousarray(inp["moe_w_up"][0])
        base["moe_w_down"] = np.ascontiguousarray(inp["moe_w_down"][0])
    return [dict(base, x=x[b]) for b in range(nb)]


def kernel(**inp):
    phases = ("c0", "f0", "g1", "m1", "final")
    nb = inp["x"].shape[0]
    nc = build_program(phases)
    in_maps = make_in_maps(inp, phases, nb)
    res = run_bass_kernel_spmd(nc, in_maps, core_ids=list(range(nb)))
    return np.stack([r["out"] for r in res.results], axis=0)
```

```python
import contextlib
import numpy as np
import concourse.bass as bass
import concourse.mybir as mybir
from concourse.bass_utils import run_bass_kernel_spmd

F32 = mybir.dt.float32
BF16 = mybir.dt.bfloat16
I32 = mybir.dt.int32
AF = mybir.ActivationFunctionType
ALU = mybir.AluOpType

D = 1024
S = 2048
NCH = D // 128
TT = 512
NTT = S // TT
ENGS = ("sp", "pe", "act", "dve", "pool")


class Tok:
    __slots__ = ("name", "w", "readers")

    def __init__(self, name):
        self.name = name
        self.w = None
        self.readers = []


class Op:
    __slots__ = ("eng", "fn", "deps", "chan", "has_dep", "sig", "name", "is_nop")

    def __init__(self, eng, fn, chan, name):
        self.eng = eng
        self.fn = fn
        self.deps = []
        self.chan = chan
        self.has_dep = False
        self.sig = None
        self.name = name
        self.is_nop = False


class Rec:
    def __init__(self):
        self.items = []

    def op(self, *a, **k):
        self.items.append((a, k))


class Sched:
    def __init__(self):
        self.ops = {e: [] for e in ENGS}
        self.nchan = 0

    def new_chan(self):
        self.nchan += 1
        return self.nchan - 1

    def last_real(self, e):
        for o in reversed(self.ops[e]):
            if not o.is_nop:
                return o
        return None

    def last_ops(self, engs=("pe", "act", "dve", "pool")):
        return [o for o in (self.last_real(e) for e in engs) if o is not None]

    def op(self, eng, fn, reads=(), writes=(), chan=None, name="", after=()):
        o = Op(eng, fn, chan, name)
        for d in after:
            o.deps.append(d)
            if d.chan is None:
                d.has_dep = True
        cand = []
        for t in reads:
            if t.w is not None:
                cand.append((t.w, "raw"))
        for t in writes:
            if t.w is not None:
                cand.append((t.w, "waw"))
            for r in t.readers:
                cand.append((r, "war"))
        seen = set()
        for d, kind in cand:
            if d is o or id(d) in seen:
                continue
            same = (d.eng == eng and d.chan is None and chan is None)
            if same and (eng == "pe" or kind == "war"):
                continue
            seen.add(id(d))
            o.deps.append(d)
            d.has_dep = True
        for t in reads:
            t.readers.append(o)
        for t in writes:
            t.w = o
            t.readers = []
        self.ops[eng].append(o)
        return o

    def barrier(self, engs=("pe", "act", "dve", "pool")):
        last = {e: self.last_real(e) for e in engs}
        for e in engs:
            o = Op(e, lambda eng: eng.nop(), None, "barrier")
            o.is_nop = True
            for e2, l in last.items():
                if e2 != e and l is not None:
                    o.deps.append(l)
                    if l.chan is None:
                        l.has_dep = True
            self.ops[e].append(o)

    def emit(self, nc, final_wait_ops=()):
        with contextlib.ExitStack() as es:
            EPOCH = 1000
            nsig = {e: sum(1 for o in self.ops[e] if o.chan is None and o.has_dep) for e in ENGS}
            esem = {e: [es.enter_context(nc.semaphore("s_%s%d" % (e, i)))
                        for i in range(nsig[e] // EPOCH + 1)] for e in ENGS}
            for e in ENGS:
                cnt = 0
                for o in self.ops[e]:
                    if o.chan is None and o.has_dep:
                        o.sig = (esem[e][cnt // EPOCH], cnt % EPOCH + 1, 1)
                        cnt += 1
            CEP = 100
            ccnt = [0] * self.nchan
            ntot = [0] * self.nchan
            for e in ENGS:
                for o in self.ops[e]:
                    if o.chan is not None:
                        ntot[o.chan] += 1
            csem = [[es.enter_context(nc.semaphore("c_%d_%d" % (i, k)))
                     for k in range(ntot[i] // CEP + 1)] for i in range(self.nchan)]
            for e in ENGS:
                for o in self.ops[e]:
                    if o.chan is not None:
                        k = ccnt[o.chan]
                        o.sig = (csem[o.chan][k // CEP], (k % CEP + 1) * 16, 16)
                        ccnt[o.chan] += 1
            block = es.enter_context(nc.Block())
            ops = self.ops

            def run(engname, eobj):
                waited = {}
                for o in ops[engname]:
                    need = {}
                    for d in o.deps:
                        sem, val, _ = d.sig
                        k = id(sem)
                        if val > waited.get(k, 0) and val > need.get(k, (None, 0))[1]:
                            need[k] = (sem, val)
                    for k, (sem, val) in need.items():
                        eobj.wait_ge(sem, val)
                        waited[k] = val
                    inst = o.fn(eobj)
                    if o.sig is not None:
                        inst.then_inc(o.sig[0], o.sig[2])

            @block.sync
            def _(e):
                run("sp", e)

            @block.tensor
            def _(e):
                run("pe", e)

            @block.scalar
            def _(e):
                run("act", e)

            @block.vector
            def _(e):
                run("dve", e)

            @block.gpsimd
            def _(e):
                run("pool", e)


VR = dict(mix0=0, mix1=1, ffn0=2, ffn1=3, pw1_ba=4, pw1_bb=5, dw_b=6, ln_g=7, ln_b=8, pw2_b=9,
          dw_w=10, gconv=41)
DFF = 2816
DFFE = 3584
USE_F32R = False
WB = 2048


def build_program(phases=("c0", "f0", "g1", "m1"), dbg=0, heads=tuple(range(8)), two_x=False):
    nc = bass.Bass("TRN2", target_bir_lowering=False, dynamic_dma_scratch_size=2048)

    def din(name, shape):
        return nc.dram_tensor(name, shape, F32, kind="ExternalInput").ap()

    x_d = din("x", [S, D])
    vecs_d = din("vecs", [128, D])
    fng_d = din("fng", [1, D])
    out_d = nc.dram_tensor("out", [S, D], F32, kind="ExternalOutput").ap()
    xacc_d = din("xacc", [S, D]) if two_x else None
    if "c0" in phases:
        w1_d = din("cf_pw1_w", [D, 2 * D])
        w2_d = din("cf_pw2_w", [D, D])
    if "f0" in phases:
        fg_d = din("ffn_w_gate", [D, DFF])
        fu_d = din("ffn_w_up", [D, DFF])
        fd_d = din("ffn_w_down", [DFF, D])

    if "g1" in phases:
        gin_d = din("gdn_w_in", [D, 4128])
        gout_d = din("gdn_w_out", [D, D])
        gsm_d = din("gsmall", [1, 160])
    if "m1" in phases:
        rt_d = din("moe_router", [D, 8])
        mg_d = din("moe_w_gate", [8, D, DFFE])
        mu_d = din("moe_w_up", [8, D, DFFE])
        md_d = din("moe_w_down", [8, DFFE, D])

    sc = Sched()
    with contextlib.ExitStack() as es:
        uniq = [0]

        def sb(name, shape, dt, stack=es):
            uniq[0] += 1
            return stack.enter_context(nc.sbuf_tensor("%s_%d" % (name, uniq[0]), shape, dt))

        xT = sb("xT", [128, NCH, S], F32)
        ident = sb("ident", [128, 128], F32)
        ones_f = sb("ones_f", [128, 128], F32)
        ones_b = sb("ones_b", [128, 128], BF16)
        vecs = sb("vecs_sb", [128, NCH, 64], F32)
        ps = es.enter_context(nc.psum_tensor("ps", [128, 8, 512], F32))
        stage, wbf, t_stage, t_wbf = [], [], [], []
        pool_fence = [[], 0]

        def set_pools(stack, nst, nwb, elems):
            stage[:] = [sb("stage%d" % i, [128, elems], F32, stack) for i in range(nst)]
            wbf[:] = [sb("wbf%d" % i, [128, elems], BF16, stack) for i in range(nwb)]
            t_stage[:] = [Tok("stage%d" % i) for i in range(nst)]
            t_wbf[:] = [Tok("wbf%d" % i) for i in range(nwb)]
            pool_fence[0] = sc.last_ops()
            pool_fence[1] = nst
        cst_d = din("gconst", [128, 2048]) if "g1" in phases else None
        sm = [sb("sm%d" % i, [128, TT], F32) for i in range(4)]
        t_sm = [Tok("sm%d" % i) for i in range(4)]
        smn = [0]

        def next_sm():
            i = smn[0] % 4
            smn[0] += 1
            return i

        t_xT = [[Tok("xT%d_%d" % (c, t)) for t in range(S // 128)] for c in range(NCH)]

        def xtoks(c, t512):
            return [t_xT[c][t512 * 4 + i] for i in range(4)]
        t_ident = Tok("ident")
        t_ones = Tok("ones")
        t_vecs = Tok("vecs")
        t_fng = Tok("fng")
        t_ps = [Tok("ps%d" % i) for i in range(8)]
        ch_stage = [sc.new_chan() for _ in range(2)]
        ch_xin = [sc.new_chan(), sc.new_chan()]
        ch_misc = sc.new_chan()
        ch_out = [sc.new_chan(), sc.new_chan()]
        psn = [0]
        stn = [0]
        wbn = [0]

        def next_ps():
            i = psn[0] % 8
            psn[0] += 1
            return i

        cast_cfg = ["act"]

        def load_block(src, view, cast_eng=None):
            cast_eng = cast_eng or cast_cfg[0]
            a, b = src.shape[1], src.shape[2]
            n = a * b
            s = stn[0] % len(stage)
            stn[0] += 1
            k = wbn[0] % len(wbf)
            wbn[0] += 1
            aft = ()
            if pool_fence[1] > 0:
                aft = pool_fence[0]
                pool_fence[1] -= 1
            st_, wb_, ts_, tw_ = stage[s], wbf[k], t_stage[s], t_wbf[k]
            sc.op("sp", lambda e: e.dma_start(
                out=st_[:, 0:n].rearrange("p (a b) -> p a b", a=a), in_=src),
                writes=[ts_], chan=ch_stage[s], after=aft)
            if cast_eng == "act":
                sc.op("act", lambda e: e.activation(out=wb_[:, 0:n], in_=st_[:, 0:n],
                                                    func=AF.Copy),
                      reads=[ts_], writes=[tw_])
            else:
                sc.op(cast_eng, lambda e: e.tensor_copy(out=wb_[:, 0:n], in_=st_[:, 0:n]),
                      reads=[ts_], writes=[tw_])
            return wb_[:, 0:n].rearrange("p (a b) -> p a b", a=a), tw_

        sc.op("pool", lambda e: e.memset(ones_f[:], 1.0), writes=[t_ones])
        sc.op("pool", lambda e: e.memset(ones_b[:], 1.0), writes=[t_ones])
        sc.op("pool", lambda e: e.affine_select(out=ident[:], in_=ones_f[:], pattern=[[-1, 128]],
                                                compare_op=ALU.is_equal, fill=0.0, base=0,
                                                channel_multiplier=1),
              reads=[t_ones], writes=[t_ident])

        def load_x_loop(src_d, xin, t_xin, fence=()):
            for tt in range(S // 128):
                sl = tt % 2
                sc.op("sp", lambda e, tt=tt, sl=sl: e.dma_start(
                    out=xin[sl][:], in_=src_d[tt * 128:(tt + 1) * 128, :]),
                    writes=[t_xin[sl]], chan=ch_xin[sl], after=fence)
                for half in range(2):
                    b = next_ps()

                    def f(e, half=half, b=b, sl=sl):
                        for j in range(4):
                            c = half * 4 + j
                            r = e.transpose(out=ps[:, b, j * 128:(j + 1) * 128],
                                            in_=xin[sl][:, c * 128:(c + 1) * 128],
                                            identity=ident[:])
                        return r
                    sc.op("pe", f, reads=[t_xin[sl], t_ident], writes=[t_ps[b]])
                    eng = "dve" if half == 0 else "act"

                    def g(e, half=half, b=b, tt=tt, eng=eng):
                        o = xT[:, half * 4:half * 4 + 4, tt * 128:(tt + 1) * 128]
                        i = ps[:, b, :].rearrange("p (j r) -> p j r", j=4)
                        if eng == "dve":
                            return e.tensor_copy(out=o, in_=i)
                        return e.activation(out=o, in_=i, func=AF.Copy)
                    sc.op(eng, g, reads=[t_ps[b]],
                          writes=[t_xT[half * 4 + j][tt] for j in range(4)])


        with contextlib.ExitStack() as es1:
            xin = [sb("xin%d" % i, [128, D], F32, es1) for i in range(2)]
            t_xin = [Tok("xin0"), Tok("xin1")]
            sc.op("sp", lambda e: e.dma_start(out=xin[1][:], in_=vecs_d[:, :]), writes=[t_xin[1]],
                  chan=ch_xin[1])
            for half in range(2):
                b = next_ps()

                def f(e, half=half, b=b):
                    for j in range(4):
                        c = half * 4 + j
                        r = e.transpose(out=ps[:, b, j * 128:(j + 1) * 128],
                                        in_=xin[1][:, c * 128:(c + 1) * 128], identity=ident[:])
                    return r
                sc.op("pe", f, reads=[t_xin[1], t_ident], writes=[t_ps[b]])
                sc.op("dve", lambda e, half=half, b=b: e.tensor_copy(
                    out=vecs[:, half * 4:half * 4 + 4, :],
                    in_=ps[:, b, :].rearrange("p (j r) -> p j r", j=4)[:, :, 0:64]),
                    reads=[t_ps[b]], writes=[t_vecs])
            load_x_loop(x_d, xin, t_xin)
        sc.barrier()

        def vcol(c, r):
            return vecs[:, c, r:r + 1]

        def rmsnorm_fm(hT, t_hT, grow, sqtmp, t_sq):
            for t in range(NTT):
                tsl = slice(t * TT, (t + 1) * TT)
                sc.op("act", lambda e, tsl=tsl: e.activation(out=sqtmp[:], in_=xT[:, :, tsl],
                                                             func=AF.Square),
                      reads=[tk for c in range(NCH) for tk in xtoks(c, t)], writes=[t_sq])
                b = next_ps()

                def f(e, b=b):
                    for c in range(NCH):
                        r = e.matmul(ps[:, b, :], lhsT=ones_b[:], rhs=sqtmp[:, c, :],
                                     start=(c == 0), stop=(c == NCH - 1))
                    return r
                sc.op("pe", f, reads=[t_sq, t_ones], writes=[t_ps[b]])
                s0 = next_sm()
                sc.op("act", lambda e, b=b, s0=s0: e.activation(out=sm[s0][:], in_=ps[:, b, :],
                                                                func=AF.Sqrt, scale=1.0 / D,
                                                                bias=1e-6),
                      reads=[t_ps[b]], writes=[t_sm[s0]])
                s1 = next_sm()
                sc.op("dve", lambda e, s0=s0, s1=s1: e.reciprocal(out=sm[s1][:], in_=sm[s0][:]),
                      reads=[t_sm[s0]], writes=[t_sm[s1]])
                for c in range(NCH):
                    sc.op("dve", lambda e, c=c, tsl=tsl, s1=s1: e.scalar_tensor_tensor(
                        out=hT[:, c, tsl], in0=xT[:, c, tsl], scalar=vcol(c, grow), in1=sm[s1][:],
                        op0=ALU.mult, op1=ALU.mult),
                        reads=xtoks(c, t) + [t_vecs, t_sm[s1]], writes=[t_hT[c][t]])

        def dump(src_fn, toks_fn):
            for c in range(NCH):
                for t in range(NTT):
                    tsl = slice(t * TT, (t + 1) * TT)
                    sc.op("dve", lambda e, c=c, tsl=tsl: e.tensor_copy(out=xT[:, c, tsl],
                                                                       in_=src_fn(c, tsl)),
                          reads=toks_fn(c, t), writes=xtoks(c, t))

        def phase_conformer():
            with contextlib.ExitStack() as esp:
                set_pools(esp, 2, 6, WB)
                hT = sb("hT", [128, NCH, S], BF16, esp)
                t_hT = [[Tok("hT%d_%d" % (c, t)) for t in range(NTT)] for c in range(NCH)]
                sqtmp = sb("sqtmp", [128, NCH, TT], BF16, esp)
                t_sq = Tok("sqtmp")
                U = sb("ubuf", [128, NCH, S + 30], BF16, esp)
                t_U = [Tok("u%d" % c) for c in range(NCH)]
                diag = sb("diag", [128, 31, 128], BF16, esp)
                t_diag = Tok("diag")
                sig = [sb("sig%d" % i, [128, TT], F32, esp) for i in range(2)]
                t_sig = [Tok("sig0"), Tok("sig1")]
                lnst = [sb("lnst%d" % i, [128, TT], F32, esp) for i in range(3)]
                t_lnst = [Tok("lnst%d" % i) for i in range(3)]
                rmsnorm_fm(hT, t_hT, VR["mix0"], sqtmp, t_sq)
                if dbg == 1:
                    dump(lambda c, tsl: hT[:, c, tsl], lambda c, t: [t_hT[c][t]])
                    return
                sc.op("dve", lambda e: e.memset(U[:], 0.0), writes=t_U)
                sgn = 0
                for h in range(2):
                    wa = [load_block(w1_d[:, h * 512 + q * 256: h * 512 + (q + 1) * 256]
                                     .rearrange("(kc p) n -> p kc n", p=128), None) for q in range(2)]
                    wb_ = [load_block(w1_d[:, D + h * 512 + q * 256: D + h * 512 + (q + 1) * 256]
                                      .rearrange("(kc p) n -> p kc n", p=128), None) for q in range(2)]
                    for jj in range(4):
                        j = h * 4 + jj
                        wA, tA = wa[jj // 2]
                        wB, tB = wb_[jj // 2]
                        co = (jj % 2) * 128
                        for t in range(NTT):
                            tsl = slice(t * TT, (t + 1) * TT)
                            bA = next_ps()
                            bB = next_ps()

                            def f(e, wA=wA, wB=wB, co=co, bA=bA, bB=bB, tsl=tsl):
                                for kc in range(NCH):
                                    e.matmul(ps[:, bA, :], lhsT=wA[:, kc, co:co + 128],
                                             rhs=hT[:, kc, tsl], start=(kc == 0),
                                             stop=(kc == NCH - 1))
                                for kc in range(NCH):
                                    r = e.matmul(ps[:, bB, :], lhsT=wB[:, kc, co:co + 128],
                                                 rhs=hT[:, kc, tsl], start=(kc == 0),
                                                 stop=(kc == NCH - 1))
                                return r
                            sc.op("pe", f, reads=[tA, tB] + [t_hT[c][t] for c in range(NCH)],
                                  writes=[t_ps[bA], t_ps[bB]])
                            sg = sgn % 2
                            sgn += 1
                            sc.op("act", lambda e, bB=bB, sg=sg, j=j: e.activation(
                                out=sig[sg][:], in_=ps[:, bB, :], func=AF.Sigmoid,
                                bias=vcol(j, VR["pw1_bb"])),
                                reads=[t_ps[bB], t_vecs], writes=[t_sig[sg]])
                            sc.op("dve", lambda e, bA=bA, sg=sg, j=j, t=t: e.scalar_tensor_tensor(
                                out=U[:, j, 15 + t * TT:15 + (t + 1) * TT], in0=ps[:, bA, :],
                                scalar=vcol(j, VR["pw1_ba"]), in1=sig[sg][:],
                                op0=ALU.add, op1=ALU.mult),
                                reads=[t_ps[bA], t_sig[sg], t_vecs], writes=[t_U[j]])
                if dbg == 2:
                    dump(lambda c, tsl: U[:, c, 15 + tsl.start:15 + tsl.stop], lambda c, t: [t_U[c]])
                    return
                for j in range(NCH):
                    sc.op("dve", lambda e, j=j: e.tensor_tensor(
                        out=diag[:], in0=ident[:].unsqueeze(1).to_broadcast([128, 31, 128]),
                        in1=vecs[:, j, VR["dw_w"]:VR["dw_w"] + 31].unsqueeze(2)
                        .to_broadcast([128, 31, 128]), op=ALU.mult),
                        reads=[t_ident, t_vecs], writes=[t_diag])
                    for t in range(NTT):
                        b = next_ps()

                        def f(e, j=j, t=t, b=b):
                            for k in range(31):
                                r = e.matmul(ps[:, b, :], lhsT=diag[:, k, :],
                                             rhs=U[:, j, t * TT + k:t * TT + k + TT],
                                             start=(k == 0), stop=(k == 30))
                            return r
                        sc.op("pe", f, reads=[t_diag, t_U[j]], writes=[t_ps[b]])
                        sc.op("act", lambda e, j=j, t=t, b=b: e.activation(
                            out=hT[:, j, t * TT:(t + 1) * TT], in_=ps[:, b, :], func=AF.Identity,
                            bias=vcol(j, VR["dw_b"])),
                            reads=[t_ps[b], t_vecs], writes=[t_hT[j][t]])
                if dbg == 3:
                    dump(lambda c, tsl: hT[:, c, tsl], lambda c, t: [t_hT[c][t]])
                    return
                for t in range(NTT):
                    tsl = slice(t * TT, (t + 1) * TT)
                    sc.op("dve", lambda e, tsl=tsl: e.tensor_tensor(
                        out=sqtmp[:], in0=hT[:, :, tsl], in1=hT[:, :, tsl], op=ALU.mult),
                        reads=[t_hT[c][t] for c in range(NCH)], writes=[t_sq])
                    b1 = next_ps()
                    b2 = next_ps()

                    def f(e, b1=b1, b2=b2, tsl=tsl):
                        for c in range(NCH):
                            e.matmul(ps[:, b1, :], lhsT=ones_b[:], rhs=hT[:, c, tsl],
                                     start=(c == 0), stop=(c == NCH - 1))
                        for c in range(NCH):
                            r = e.matmul(ps[:, b2, :], lhsT=ones_b[:], rhs=sqtmp[:, c, :],
                                         start=(c == 0), stop=(c == NCH - 1))
                        return r
                    sc.op("pe", f, reads=[t_sq, t_ones] + [t_hT[c][t] for c in range(NCH)],
                          writes=[t_ps[b1], t_ps[b2]])
                    m = lnst[0]
                    q = lnst[1]
                    rs = lnst[2]
                    tm, tq, trs = t_lnst
                    sc.op("act", lambda e, m=m, b1=b1: e.activation(
                        out=m[:], in_=ps[:, b1, :], func=AF.Copy, scale=1.0 / D),
                        reads=[t_ps[b1]], writes=[tm])
                    sc.op("dve", lambda e, m=m, q=q: e.tensor_tensor(
                        out=q[:], in0=m[:], in1=m[:], op=ALU.mult),
                        reads=[tm], writes=[tq])
                    sc.op("dve", lambda e, q=q, b2=b2: e.scalar_tensor_tensor(
                        out=q[:], in0=ps[:, b2, :], scalar=1.0 / D, in1=q[:],
                        op0=ALU.mult, op1=ALU.subtract),
                        reads=[t_ps[b2], tq], writes=[tq])
                    sc.op("act", lambda e, q=q: e.activation(
                        out=q[:], in_=q[:], func=AF.Sqrt, bias=1e-5),
                        reads=[tq], writes=[tq])
                    sc.op("dve", lambda e, q=q, rs=rs: e.reciprocal(out=rs[:], in_=q[:]),
                          reads=[tq], writes=[trs])
                    sc.op("dve", lambda e, m=m, rs=rs: e.scalar_tensor_tensor(
                        out=m[:], in0=m[:], scalar=-1.0, in1=rs[:],
                        op0=ALU.mult, op1=ALU.mult),
                        reads=[tm, trs], writes=[tm])
                    for c in range(NCH):
                        w1s = next_sm()
                        sc.op("dve", lambda e, c=c, tsl=tsl, rs=rs, w1s=w1s: e.tensor_tensor(
                            out=sm[w1s][:], in0=hT[:, c, tsl], in1=rs[:], op=ALU.mult),
                            reads=[t_hT[c][t], trs], writes=[t_sm[w1s]])
                        sc.op("dve", lambda e, m=m, w1s=w1s: e.tensor_tensor(
                            out=sm[w1s][:], in0=sm[w1s][:], in1=m[:], op=ALU.add),
                            reads=[t_sm[w1s], tm], writes=[t_sm[w1s]])
                        sc.op("act", lambda e, c=c, tsl=tsl, w1s=w1s: e.activation(
                            out=hT[:, c, tsl], in_=sm[w1s][:], func=AF.Silu,
                            scale=vcol(c, VR["ln_g"]), bias=vcol(c, VR["ln_b"])),
                            reads=[t_sm[w1s], t_vecs], writes=[t_hT[c][t]])
                if dbg == 4:
                    dump(lambda c, tsl: hT[:, c, tsl], lambda c, t: [t_hT[c][t]])
                    return
                for h in range(2):
                    w2 = [load_block(w2_d[:, h * 512 + q * 256: h * 512 + (q + 1) * 256]
                                     .rearrange("(kc p) n -> p kc n", p=128), None) for q in range(2)]
                    for jj in range(4):
                        j = h * 4 + jj
                        wA, tA = w2[jj // 2]
                        co = (jj % 2) * 128
                        for t in range(NTT):
                            tsl = slice(t * TT, (t + 1) * TT)
                            b = next_ps()

                            def f(e, wA=wA, co=co, b=b, tsl=tsl):
                                for kc in range(NCH):
                                    r = e.matmul(ps[:, b, :], lhsT=wA[:, kc, co:co + 128],
                                                 rhs=hT[:, kc, tsl], start=(kc == 0),
                                                 stop=(kc == NCH - 1))
                                return r
                            sc.op("pe", f, reads=[tA] + [t_hT[c][t] for c in range(NCH)],
                                  writes=[t_ps[b]])
                            sc.op("dve", lambda e, b=b, j=j, tsl=tsl: e.scalar_tensor_tensor(
                                out=xT[:, j, tsl], in0=ps[:, b, :], scalar=vcol(j, VR["pw2_b"]),
                                in1=xT[:, j, tsl], op0=ALU.add, op1=ALU.add),
                                reads=[t_ps[b], t_vecs] + xtoks(j, t), writes=xtoks(j, t))

        def swiglu_stream(hT, t_hT, h1, t_h1, sg, t_sg, wg_d, wu_d, wd_d, dff,
                          gate_bc=None, t_gate=None):
            ngrp = (dff + 511) // 512
            sgn = 0
            for g in range(ngrp):
                c0 = g * 512
                ncol = min(512, dff - c0)
                nblk = ncol // 256
                nf = ncol // 128
                wg = [load_block(wg_d[:, c0 + q * 256:c0 + (q + 1) * 256]
                                 .rearrange("(kc p) n -> p kc n", p=128), None) for q in range(nblk)]
                wu = [load_block(wu_d[:, c0 + q * 256:c0 + (q + 1) * 256]
                                 .rearrange("(kc p) n -> p kc n", p=128), None) for q in range(nblk)]
                wd = [load_block(wd_d[c0 + q * 256:c0 + (q + 1) * 256, :]
                                 .rearrange("(fc p) n -> p fc n", p=128), None) for q in range(nblk)]
                for t in range(NTT):
                    tsl = slice(t * TT, (t + 1) * TT)
                    for f_ in range(nf):
                        wG, tG = wg[f_ // 2]
                        wU, tU = wu[f_ // 2]
                        co = (f_ % 2) * 128
                        bG = next_ps()
                        bU = next_ps()

                        def f(e, wG=wG, wU=wU, co=co, bG=bG, bU=bU, tsl=tsl):
                            for kc in range(NCH):
                                e.matmul(ps[:, bG, :], lhsT=wG[:, kc, co:co + 128],
                                         rhs=hT[:, kc, tsl], start=(kc == 0), stop=(kc == NCH - 1))
                            for kc in range(NCH):
                                r = e.matmul(ps[:, bU, :], lhsT=wU[:, kc, co:co + 128],
                                             rhs=hT[:, kc, tsl], start=(kc == 0),
                                             stop=(kc == NCH - 1))
                            return r
                        sc.op("pe", f, reads=[tG, tU] + [t_hT[c][t] for c in range(NCH)],
                              writes=[t_ps[bG], t_ps[bU]])
                        s_ = sgn % 2
                        sgn += 1
                        sc.op("act", lambda e, bG=bG, s_=s_: e.activation(
                            out=sg[s_][:], in_=ps[:, bG, :], func=AF.Silu),
                            reads=[t_ps[bG]], writes=[t_sg[s_]])
                        if gate_bc is None:
                            sc.op("dve", lambda e, bU=bU, s_=s_, f_=f_, tsl=tsl: e.tensor_tensor(
                                out=h1[:, f_, tsl], in0=ps[:, bU, :], in1=sg[s_][:], op=ALU.mult),
                                reads=[t_ps[bU], t_sg[s_]], writes=[t_h1[f_][t]])
                        else:
                            sc.op("dve", lambda e, s_=s_, tsl=tsl: e.tensor_tensor(
                                out=sg[s_][:], in0=sg[s_][:], in1=gate_bc[:, tsl], op=ALU.mult),
                                reads=[t_sg[s_], t_gate], writes=[t_sg[s_]])
                            sc.op("dve", lambda e, bU=bU, s_=s_, f_=f_, tsl=tsl: e.tensor_tensor(
                                out=h1[:, f_, tsl], in0=ps[:, bU, :], in1=sg[s_][:], op=ALU.mult),
                                reads=[t_ps[bU], t_sg[s_]], writes=[t_h1[f_][t]])
                for t in range(NTT):
                    tsl = slice(t * TT, (t + 1) * TT)
                    for j in range(NCH):
                        b = next_ps()

                        def f(e, b=b, j=j, tsl=tsl, wd=wd, nf=nf):
                            for f_ in range(nf):
                                wD, _ = wd[f_ // 2]
                                r = e.matmul(ps[:, b, :],
                                             lhsT=wD[:, f_ % 2, j * 128:(j + 1) * 128],
                                             rhs=h1[:, f_, tsl], start=(f_ == 0),
                                             stop=(f_ == nf - 1))
                            return r
                        sc.op("pe", f, reads=[w[1] for w in wd] + [t_h1[f_][t] for f_ in range(nf)],
                              writes=[t_ps[b]])
                        sc.op("dve", lambda e, b=b, j=j, tsl=tsl: e.tensor_tensor(
                            out=xT[:, j, tsl], in0=ps[:, b, :], in1=xT[:, j, tsl], op=ALU.add),
                            reads=[t_ps[b]] + xtoks(j, t), writes=xtoks(j, t))

        def phase_ffn():
            with contextlib.ExitStack() as esp:
                set_pools(esp, 2, 6, WB)
                hT = sb("hT", [128, NCH, S], BF16, esp)
                t_hT = [[Tok("hT%d_%d" % (c, t)) for t in range(NTT)] for c in range(NCH)]
                sqtmp = sb("sqtmp", [128, NCH, TT], BF16, esp)
                t_sq = Tok("sqtmp")
                h1 = sb("h1", [128, 4, S], BF16, esp)
                t_h1 = [[Tok("h1_%d_%d" % (f_, t)) for t in range(NTT)] for f_ in range(4)]
                sg = [sb("sg%d" % i, [128, TT], F32, esp) for i in range(2)]
                t_sg = [Tok("sg0"), Tok("sg1")]
                rmsnorm_fm(hT, t_hT, VR["ffn0"], sqtmp, t_sq)
                swiglu_stream(hT, t_hT, h1, t_h1, sg, t_sg, fg_d, fu_d, fd_d, DFF)


        F32R = mybir.dt.float32r

        def fr(ap):
            return ap.bitcast(F32R) if USE_F32R else ap

        def phase_gdn():
            NT16 = S // 128
            with contextlib.ExitStack() as esp:
                set_pools(esp, 2, 4, 1024)
                hT = sb("hT", [128, NCH, S], BF16, esp)
                t_hT = [[Tok("hT%d_%d" % (c, t)) for t in range(NTT)] for c in range(NCH)]
                with contextlib.ExitStack() as esq:
                    sqtmp = sb("sqtmp", [128, NCH, TT], BF16, esq)
                    t_sq = Tok("sqtmp")
                    rmsnorm_fm(hT, t_hT, VR["mix1"], sqtmp, t_sq)
                sc.barrier()
                if two_x:
                    with contextlib.ExitStack() as esx:
                        xin2 = [sb("xin2_%d" % i, [128, D], F32, esx) for i in range(2)]
                        t_xin2 = [Tok("xin2_0"), Tok("xin2_1")]
                        load_x_loop(xacc_d, xin2, t_xin2, fence=sc.last_ops())
                    sc.barrier()
                cst = sb("cst", [128, 2048], F32, esp)
                t_cst = Tok("cst")
                gsm = sb("gsm", [128, 160], F32, esp)
                t_gsm = Tok("gsm")
                graw = sb("graw", [128, NT16, 32], F32, esp)
                beta = sb("beta", [128, NT16, 16], F32, esp)
                la = sb("la", [128, NT16, 16], F32, esp)
                gcol = sb("gcol", [128, NT16, 16], F32, esp)
                glast = sb("glast", [128, NT16, 16], F32, esp)
                beg = sb("beg", [128, NT16, 16], F32, esp)
                kdc = sb("kdc", [128, NT16, 16], F32, esp)
                egl = sb("egl", [128, NT16, 16], F32, esp)
                t_graw, t_beta, t_la, t_gcol, t_glast, t_beg, t_kdc, t_egl = [
                    Tok(n) for n in ("graw", "beta", "la", "gcol", "glast", "beg", "kdc", "egl")]
                pre = sb("pre", [128, S + 4], BF16, esp)
                t_pre = Tok("pre")
                diag5 = sb("diag5", [128, 5, 128], BF16, esp)
                t_diag5 = Tok("diag5")
                QKV = [sb("qkv%d" % i, [128, S], F32, esp) for i in range(3)]
                t_QKV = [[Tok("qkv%d_%d" % (i, t)) for t in range(NTT)] for i in range(3)]
                zw = sb("zw", [128, NCH, 128], BF16, esp)
                t_zw = Tok("zw")
                Oacc = sb("Oacc", [128, NT16, 128], F32, esp)
                t_O = [Tok("O%d" % n) for n in range(NT16)]
                oT = sb("oT", [128, S], BF16, esp)
                t_oT = [Tok("oT%d" % t) for t in range(NTT)]
                names = ("bV", "Kt", "KD", "dec", "QDT", "AT", "PT", "Dm", "DTm", "X", "WK", "U")
                shp = dict(WK=[128, 256])
                TS = [{n: sb("%s_c%d" % (n, c_), shp.get(n, [128, 128]), F32, esp) for n in names}
                      for c_ in range(4)]
                t_TS = [{n: Tok("%s_c%d" % (n, c_)) for n in names} for c_ in range(4)]
                ETs = [sb("ETall%d" % c_, [128, 2, 128], F32, esp) for c_ in range(4)]
                t_ETs = [[Tok("ET%d_0" % c_), Tok("ET%d_1" % c_)] for c_ in range(4)]
                Ss = [sb("Sst%d" % c_, [128, 128], F32, esp) for c_ in range(2)]
                t_Ss = [Tok("S0"), Tok("S1")]
                T_ = {n: sb(n, [128, 128], F32, esp) for n in ("sz", "yy")}
                t_T = {n: Tok(n) for n in ("sz", "yy")}
                bpn = [0, 0, 0, 0]

                def bank_pool(c_):
                    def nb_():
                        i = c_ * 2 + bpn[c_] % 2
                        bpn[c_] += 1
                        return i
                    return nb_
                st4 = sb("st4", [128, 4], F32, esp)
                t_st4 = Tok("st4")
                ch_c = sc.new_chan()
                fence = sc.last_ops()
                sc.op("sp", lambda e: e.dma_start(out=cst[:], in_=cst_d[:, :]), writes=[t_cst],
                      chan=ch_c, after=fence)
                sc.op("sp", lambda e: e.dma_start(out=gsm[:],
                                                  in_=gsm_d[0:1, :].partition_broadcast(128)),
                      writes=[t_gsm], chan=ch_c, after=fence)
                triX = [cst[:, 0:128], cst[:, 128:256]]
                MX = [cst[:, 256:256 + 896].rearrange("p (k i) -> p k i", k=7),
                      cst[:, 1152:1152 + 896].rearrange("p (k i) -> p k i", k=7)]
                sc.op("dve", lambda e: e.memset(pre[:], 0.0), writes=[t_pre])
                wgt, t_wgt = load_block(gin_d[:, 4096:4128].rearrange("(kc p) n -> p kc n", p=128),
                                        None)
                for n in range(NT16):
                    b = next_ps()
                    tsl = slice(n * 128, (n + 1) * 128)

                    def f(e, b=b, tsl=tsl):
                        for kc in range(NCH):
                            r = e.matmul(ps[:, b, 0:32], lhsT=hT[:, kc, tsl], rhs=wgt[:, kc, :],
                                         start=(kc == 0), stop=(kc == NCH - 1))
                        return r
                    sc.op("pe", f, reads=[t_wgt] + [t_hT[c][n // 4] for c in range(NCH)],
                          writes=[t_ps[b]])
                    sc.op("dve", lambda e, b=b, n=n: e.tensor_copy(out=graw[:, n, :],
                                                                   in_=ps[:, b, 0:32]),
                          reads=[t_ps[b]], writes=[t_graw])
                sc.op("act", lambda e: e.activation(out=beta[:], in_=graw[:, :, 0:16],
                                                    func=AF.Sigmoid),
                      reads=[t_graw], writes=[t_beta])
                sc.op("dve", lambda e: e.tensor_tensor(
                    out=la[:], in0=graw[:, :, 16:32],
                    in1=gsm[:, 16:32].unsqueeze(1).to_broadcast([128, NT16, 16]), op=ALU.add),
                    reads=[t_graw, t_gsm], writes=[t_la])
                sc.op("act", lambda e: e.activation(out=la[:], in_=la[:], func=AF.Exp),
                      reads=[t_la], writes=[t_la])
                sc.op("act", lambda e: e.activation(out=la[:], in_=la[:], func=AF.Ln, bias=1.0),
                      reads=[t_la], writes=[t_la])
                sc.op("act", lambda e: e.activation(out=gsm[:, 0:16], in_=gsm[:, 0:16],
                                                    func=AF.Exp),
                      reads=[t_gsm], writes=[t_gsm])
                sc.op("dve", lambda e: e.scalar_tensor_tensor(
                    out=la[:], in0=la[:], scalar=-1.0,
                    in1=gsm[:, 0:16].unsqueeze(1).to_broadcast([128, NT16, 16]),
                    op0=ALU.mult, op1=ALU.mult), reads=[t_la, t_gsm], writes=[t_la])
                for n in range(NT16):
                    b = next_ps()

                    def f(e, b=b, n=n):
                        e.matmul(ps[:, b, 0:8], lhsT=triX[0], rhs=la[:, n, 0:8], start=True,
                                 stop=True)
                        e.matmul(ps[:, b, 8:16], lhsT=triX[1], rhs=la[:, n, 8:16], start=True,
                                 stop=True)
                        return e.matmul(ps[:, b, 16:32], lhsT=ones_f[:], rhs=la[:, n, :],
                                        start=True, stop=True)
                    sc.op("pe", f, reads=[t_la, t_cst, t_ones], writes=[t_ps[b]])
                    sc.op("dve", lambda e, b=b, n=n: e.tensor_copy(out=gcol[:, n, :],
                                                                   in_=ps[:, b, 0:16]),
                          reads=[t_ps[b]], writes=[t_gcol])
                    sc.op("dve", lambda e, b=b, n=n: e.tensor_copy(out=glast[:, n, :],
                                                                   in_=ps[:, b, 16:32]),
                          reads=[t_ps[b]], writes=[t_glast])
                sc.op("act", lambda e: e.activation(out=beg[:], in_=gcol[:], func=AF.Exp),
                      reads=[t_gcol], writes=[t_beg])
                sc.op("dve", lambda e: e.tensor_tensor(out=beg[:], in0=beg[:], in1=beta[:],
                                                       op=ALU.mult),
                      reads=[t_beg, t_beta], writes=[t_beg])
                sc.op("dve", lambda e: e.tensor_tensor(out=kdc[:], in0=glast[:], in1=gcol[:],
                                                       op=ALU.subtract),
                      reads=[t_glast, t_gcol], writes=[t_kdc])
                sc.op("act", lambda e: e.activation(out=kdc[:], in_=kdc[:], func=AF.Exp),
                      reads=[t_kdc], writes=[t_kdc])
                sc.op("act", lambda e: e.activation(out=egl[:], in_=glast[:], func=AF.Exp),
                      reads=[t_glast], writes=[t_egl])

                def tt_(n, out, in0, in1, op, eng="dve", rd=(), wr=()):
                    sc.op(eng, lambda e: e.tensor_tensor(out=out, in0=in0, in1=in1, op=op),
                          reads=list(rd), writes=list(wr))

                if dbg == 11:
                    return
                for h in heads:
                    for part in range(3):
                        cidx = part * 8 + h
                        wv, t_wv = load_block(gin_d[:, cidx * 128:(cidx + 1) * 128]
                                              .rearrange("(kc p) n -> p kc n", p=128), None)
                        for k in range(5):
                            sc.op("dve", lambda e, k=k, part=part, h=h: e.tensor_scalar(
                                out=diag5[:, k, :], in0=ident[:],
                                scalar1=vcol(h, VR["gconv"] + k * 3 + part), scalar2=None,
                                op0=ALU.mult), reads=[t_ident, t_vecs], writes=[t_diag5])
                        for t in range(NTT):
                            tsl = slice(t * TT, (t + 1) * TT)
                            b = next_ps()

                            def f(e, b=b, tsl=tsl, wv=wv):
                                for kc in range(NCH):
                                    r = e.matmul(ps[:, b, :], lhsT=wv[:, kc, :], rhs=hT[:, kc, tsl],
                                                 start=(kc == 0), stop=(kc == NCH - 1))
                                return r
                            sc.op("pe", f, reads=[t_wv] + [t_hT[c][t] for c in range(NCH)],
                                  writes=[t_ps[b]])
                            sc.op("act", lambda e, b=b, t=t: e.activation(
                                out=pre[:, 2 + t * TT:2 + (t + 1) * TT], in_=ps[:, b, :],
                                func=AF.Copy), reads=[t_ps[b]], writes=[t_pre])
                        for t in range(NTT):
                            tsl = slice(t * TT, (t + 1) * TT)
                            b = next_ps()

                            def f(e, b=b, t=t):
                                for k in range(5):
                                    r = e.matmul(ps[:, b, :], lhsT=diag5[:, k, :],
                                                 rhs=pre[:, t * TT + k:t * TT + k + TT],
                                                 start=(k == 0), stop=(k == 4))
                                return r
                            sc.op("pe", f, reads=[t_diag5, t_pre], writes=[t_ps[b]])
                            sc.op("act", lambda e, b=b, tsl=tsl, part=part: e.activation(
                                out=QKV[part][:, tsl], in_=ps[:, b, :], func=AF.Silu),
                                reads=[t_ps[b]], writes=[t_QKV[part][t]])
                            if part < 2:
                                la_ = next_sm()
                                lb_ = next_sm()
                                sc.op("dve", lambda e, tsl=tsl, part=part, la_=la_: e.tensor_tensor(
                                    out=sm[la_][:], in0=QKV[part][:, tsl], in1=QKV[part][:, tsl],
                                    op=ALU.mult), reads=[t_QKV[part][t]], writes=[t_sm[la_]])
                                b2 = next_ps()
                                sc.op("pe", lambda e, b2=b2, la_=la_: e.matmul(
                                    ps[:, b2, :], lhsT=ones_f[:], rhs=sm[la_][:], start=True,
                                    stop=True), reads=[t_sm[la_], t_ones], writes=[t_ps[b2]])
                                sc.op("act", lambda e, b2=b2, lb_=lb_: e.activation(
                                    out=sm[lb_][:], in_=ps[:, b2, :], func=AF.Sqrt, bias=1e-6),
                                    reads=[t_ps[b2]], writes=[t_sm[lb_]])
                                sc.op("dve", lambda e, lb_=lb_: e.reciprocal(out=sm[lb_][:],
                                                                             in_=sm[lb_][:]),
                                      reads=[t_sm[lb_]], writes=[t_sm[lb_]])
                                scl = (128.0 ** -0.5) if part == 0 else 1.0
                                sc.op("dve", lambda e, tsl=tsl, part=part, scl=scl, lb_=lb_:
                                      e.scalar_tensor_tensor(
                                          out=QKV[part][:, tsl], in0=QKV[part][:, tsl], scalar=scl,
                                          in1=sm[lb_][:], op0=ALU.mult, op1=ALU.mult),
                                      reads=[t_QKV[part][t], t_sm[lb_]], writes=[t_QKV[part][t]])
                    if dbg == 12:
                        return
                    zwv, t_zwv = load_block(gin_d[:, 3072 + h * 128:3072 + (h + 1) * 128]
                                            .rearrange("(kc p) n -> p kc n", p=128), None)
                    sc.op("dve", lambda e, zwv=zwv: e.tensor_copy(out=zw[:], in_=zwv),
                          reads=[t_zwv], writes=[t_zw])
                    QT, KT, VT = QKV
                    def chain(dr, T_, t_T, ETall, t_ET, Sst, t_S, next_ps, sc, chunks, do_memset):
                        if do_memset:
                            sc.op("dve", lambda e: e.memset(Sst[:], 0.0), writes=[t_S])
                        order = chunks
                        dh = dr * 8 + h
                        for n in order:
                            csl = slice(n * 128, (n + 1) * 128)
                            t4 = n // 4
                            rq = [t_QKV[0][t4]]
                            rk = [t_QKV[1][t4]]
                            rv = [t_QKV[2][t4]]
                            bt = next_ps()

                            def f(e, bt=bt, csl=csl):
                                e.transpose(out=ps[:, bt, 0:128], in_=KT[:, csl], identity=ident[:])
                                return e.transpose(out=ps[:, bt, 128:256], in_=VT[:, csl],
                                                   identity=ident[:])
                            sc.op("pe", f, reads=rk + rv + [t_ident], writes=[t_ps[bt]])
                            sc.op("act", lambda e, bt=bt, n=n, dh=dh: e.activation(
                                out=T_["bV"][:], in_=ps[:, bt, 128:256], func=AF.Copy,
                                scale=beta[:, n, dh:dh + 1]),
                                reads=[t_ps[bt], t_beta], writes=[t_T["bV"]])
                            sc.op("act", lambda e, bt=bt, n=n, dh=dh: e.activation(
                                out=T_["Kt"][:], in_=ps[:, bt, 0:128], func=AF.Copy,
                                scale=beg[:, n, dh:dh + 1]),
                                reads=[t_ps[bt], t_beg], writes=[t_T["Kt"]])
                            sc.op("act", lambda e, bt=bt, n=n, dh=dh: e.activation(
                                out=T_["KD"][:], in_=ps[:, bt, 0:128], func=AF.Copy,
                                scale=kdc[:, n, dh:dh + 1]),
                                reads=[t_ps[bt], t_kdc], writes=[t_T["KD"]])
                            sc.op("act", lambda e, n=n, dh=dh: e.activation(
                                out=T_["X"][:], in_=ones_f[:], func=AF.Copy,
                                scale=la[:, n, dh:dh + 1]),
                                reads=[t_la, t_ones], writes=[t_T["X"]])
                            sc.op("act", lambda e, n=n, dh=dh: e.activation(
                                out=T_["U"][:], in_=ones_f[:], func=AF.Copy,
                                scale=beta[:, n, dh:dh + 1]),
                                reads=[t_beta, t_ones], writes=[t_T["U"]])
                            bm = next_ps()

                            def f(e, bm=bm, csl=csl, dr=dr):
                                e.matmul(ps[:, bm, 0:128], lhsT=fr(T_["X"][:]), rhs=fr(triX[dr]),
                                         start=True, stop=True)
                                e.matmul(ps[:, bm, 128:256], lhsT=fr(T_["U"][:]), rhs=fr(ident[:]),
                                         start=True, stop=True)
                                e.matmul(ps[:, bm, 256:384], lhsT=fr(KT[:, csl]), rhs=fr(KT[:, csl]),
                                         start=True, stop=True)
                                return e.matmul(ps[:, bm, 384:512], lhsT=fr(KT[:, csl]),
                                                rhs=fr(QT[:, csl]), start=True, stop=True)
                            sc.op("pe", f, reads=[t_T["X"], t_T["U"], t_cst, t_ident] + rk + rq,
                                  writes=[t_ps[bm]])
                            sc.op("dve", lambda e, bm=bm, n=n, dh=dh: e.tensor_scalar(
                                out=T_["dec"][:], in0=ps[:, bm, 0:128],
                                scalar1=gcol[:, n, dh:dh + 1], scalar2=0.0, op0=ALU.subtract,
                                op1=ALU.min), reads=[t_ps[bm], t_gcol], writes=[t_T["dec"]])
                            sc.op("act", lambda e: e.activation(out=T_["dec"][:], in_=T_["dec"][:],
                                                                func=AF.Exp),
                                  reads=[t_T["dec"]], writes=[t_T["dec"]])
                            sc.op("act", lambda e, bm=bm: e.activation(
                                out=T_["QDT"][:], in_=ps[:, bm, 0:128], func=AF.Exp),
                                reads=[t_ps[bm]], writes=[t_T["QDT"]])
                            sc.op("dve", lambda e, csl=csl: e.tensor_tensor(
                                out=T_["QDT"][:], in0=T_["QDT"][:], in1=QT[:, csl], op=ALU.mult),
                                reads=[t_T["QDT"]] + rq, writes=[t_T["QDT"]])
                            sc.op("dve", lambda e, bm=bm: e.tensor_tensor(
                                out=T_["AT"][:], in0=ps[:, bm, 256:384], in1=T_["dec"][:],
                                op=ALU.mult), reads=[t_ps[bm], t_T["dec"]], writes=[t_T["AT"]])
                            sc.op("dve", lambda e, bm=bm: e.tensor_tensor(
                                out=T_["AT"][:], in0=ps[:, bm, 128:256], in1=T_["AT"][:],
                                op=ALU.mult), reads=[t_ps[bm], t_T["AT"]], writes=[t_T["AT"]])
                            sc.op("dve", lambda e, bm=bm: e.tensor_tensor(
                                out=T_["PT"][:], in0=ps[:, bm, 384:512], in1=T_["dec"][:],
                                op=ALU.mult), reads=[t_ps[bm], t_T["dec"]], writes=[t_T["PT"]])
                            sc.op("dve", lambda e, dr=dr: e.tensor_tensor(
                                out=T_["PT"][:], in0=T_["PT"][:], in1=triX[dr], op=ALU.mult),
                                reads=[t_T["PT"], t_cst], writes=[t_T["PT"]])
                            if dbg == 13:
                                return
                            for lv in range(7):
                                Dc = ident if lv == 0 else T_["Dm"]
                                DTc = ident if lv == 0 else T_["DTm"]
                                rD = [t_ident] if lv == 0 else [t_T["Dm"]]
                                rDT = [t_ident] if lv == 0 else [t_T["DTm"]]
                                bx = next_ps()
                                if lv == 0:
                                    sc.op("dve", lambda e, dr=dr: e.tensor_tensor(
                                        out=ETall[:, 0, :], in0=T_["AT"][:], in1=MX[dr][:, 0, :],
                                        op=ALU.mult), reads=[t_T["AT"], t_cst], writes=[t_ET[0]])
                                sc.op("pe", lambda e, bx=bx, lv=lv, Dc=Dc: e.matmul(
                                    ps[:, bx, 0:128], lhsT=fr(ETall[:, lv % 2, :]), rhs=fr(Dc[:]), start=True,
                                    stop=True), reads=[t_ET[lv % 2]] + rD, writes=[t_ps[bx]])
                                if lv < 6:
                                    sc.op("dve", lambda e, dr=dr, lv=lv: e.tensor_tensor(
                                        out=ETall[:, (lv + 1) % 2, :], in0=T_["AT"][:],
                                        in1=MX[dr][:, lv + 1, :], op=ALU.mult),
                                        reads=[t_T["AT"], t_cst], writes=[t_ET[(lv + 1) % 2]])
                                sc.op("act", lambda e, bx=bx: e.activation(
                                    out=T_["X"][:], in_=ps[:, bx, 0:128], func=AF.Copy),
                                    reads=[t_ps[bx]], writes=[t_T["X"]])
                                by = next_ps()

                                def f(e, by=by, Dc=Dc, DTc=DTc):
                                    e.matmul(ps[:, by, 0:128], lhsT=fr(DTc[:]), rhs=fr(T_["X"][:]),
                                             start=True, stop=True)
                                    return e.matmul(ps[:, by, 128:256], lhsT=fr(T_["X"][:]), rhs=fr(DTc[:]),
                                                    start=True, stop=True)
                                sc.op("pe", f, reads=[t_T["X"]] + rDT, writes=[t_ps[by]])
                                sc.op("dve", lambda e, by=by, Dc=Dc: e.tensor_tensor(
                                    out=T_["Dm"][:], in0=Dc[:], in1=ps[:, by, 0:128],
                                    op=ALU.subtract), reads=[t_ps[by]] + rD, writes=[t_T["Dm"]])
                                sc.op("dve", lambda e, by=by, DTc=DTc: e.tensor_tensor(
                                    out=T_["DTm"][:], in0=DTc[:], in1=ps[:, by, 128:256],
                                    op=ALU.subtract), reads=[t_ps[by]] + rDT, writes=[t_T["DTm"]])
                            if dbg == 14:
                                return
                            bw = next_ps()

                            def f(e, bw=bw):
                                e.matmul(ps[:, bw, 0:128], lhsT=fr(T_["DTm"][:]), rhs=fr(T_["bV"][:]),
                                         start=True, stop=True)
                                return e.matmul(ps[:, bw, 128:256], lhsT=fr(T_["Kt"][:]),
                                                rhs=fr(T_["DTm"][:]), start=True, stop=True)
                            sc.op("pe", f, reads=[t_T["DTm"], t_T["bV"], t_T["Kt"]],
                                  writes=[t_ps[bw]])
                            sc.op("act", lambda e, bw=bw: e.activation(
                                out=T_["WK"][:], in_=ps[:, bw, 0:256], func=AF.Copy),
                                reads=[t_ps[bw]], writes=[t_T["WK"]])
                            if dbg == 21:
                                return
                            bs = next_ps()
                            sc.op("pe", lambda e, bs=bs: e.matmul(
                                ps[:, bs, 0:128], lhsT=fr(T_["WK"][:, 128:256]), rhs=fr(Sst[:]), start=True,
                                stop=True), reads=[t_T["WK"], t_S], writes=[t_ps[bs]])
                            sc.op("dve", lambda e, bs=bs: e.tensor_tensor(
                                out=T_["U"][:], in0=T_["WK"][:, 0:128], in1=ps[:, bs, 0:128],
                                op=ALU.subtract), reads=[t_ps[bs], t_T["WK"]], writes=[t_T["U"]])
                            if dbg == 22:
                                return
                            bo = next_ps()

                            def f(e, bo=bo):
                                e.matmul(ps[:, bo, 0:128], lhsT=fr(T_["QDT"][:]), rhs=fr(Sst[:]), start=True,
                                         stop=False)
                                e.matmul(ps[:, bo, 0:128], lhsT=fr(T_["PT"][:]), rhs=fr(T_["U"][:]),
                                         start=False, stop=True)
                                return e.matmul(ps[:, bo, 128:256], lhsT=fr(T_["KD"][:]), rhs=fr(T_["U"][:]),
                                                start=True, stop=True)
                            sc.op("pe", f, reads=[t_T["QDT"], t_S, t_T["PT"], t_T["U"], t_T["KD"]],
                                  writes=[t_ps[bo]])
                            if dbg == 23:
                                return
                            sc.op("dve", lambda e, bo=bo, n=n: e.tensor_tensor(
                                out=Oacc[:, n, :], in0=ps[:, bo, 0:128], in1=Oacc[:, n, :],
                                op=ALU.add), reads=[t_ps[bo], t_O[n]], writes=[t_O[n]])
                            sc.op("act", lambda e, n=n, dh=dh: e.activation(
                                out=Sst[:], in_=Sst[:], func=AF.Copy, scale=egl[:, n, dh:dh + 1]),
                                reads=[t_S, t_egl], writes=[t_S])
                            sc.op("dve", lambda e, bo=bo: e.tensor_tensor(
                                out=Sst[:], in0=ps[:, bo, 128:256], in1=Sst[:], op=ALU.add),
                                reads=[t_ps[bo], t_S], writes=[t_S])
                    sc.op("dve", lambda e: e.memset(Oacc[:], 0.0), writes=t_O)
                    recs = [Rec() for _ in range(4)]
                    offs = []
                    for w_ in range(4):
                        dr_, par_ = w_ % 2, w_ // 2
                        full = list(range(NT16)) if dr_ == 0 else list(range(NT16 - 1, -1, -1))
                        chain(dr_, TS[w_], t_TS[w_], ETs[w_], t_ETs[w_], Ss[dr_], t_Ss[dr_],
                              bank_pool(w_), recs[w_], full[par_::2], par_ == 0)
                    per_inst = len(recs[2].items) // (NT16 // 2)
                    offs = [0, 0, per_inst // 2 + 1, per_inst // 2 + 1]
                    tot = max(len(r.items) + o_ for r, o_ in zip(recs, offs))
                    for i_ in range(tot):
                        for r, o_ in zip(recs, offs):
                            j_ = i_ - o_
                            if 0 <= j_ < len(r.items):
                                a_, k_ = r.items[j_]
                                sc.op(*a_, **k_)
                    for q in range(NTT):
                        bT = next_ps()
                        for i4 in range(4):
                            n = q * 4 + i4
                            csl = slice(n * 128, (n + 1) * 128)
                            sc.op("act", lambda e, n=n: e.activation(
                                out=T_["yy"][:], in_=Oacc[:, n, :], func=AF.Square,
                                accum_out=st4[:, 0:1]), reads=[t_O[n]],
                                writes=[t_T["yy"], t_st4])
                            sc.op("act", lambda e: e.activation(
                                out=st4[:, 1:2], in_=st4[:, 0:1], func=AF.Sqrt, scale=1.0 / 128,
                                bias=1e-6), reads=[t_st4], writes=[t_st4])
                            sc.op("dve", lambda e: e.reciprocal(out=st4[:, 2:3], in_=st4[:, 1:2]),
                                  reads=[t_st4], writes=[t_st4])
                            bz = next_ps()

                            def f(e, bz=bz, csl=csl):
                                for kc in range(NCH):
                                    r = e.matmul(ps[:, bz, 0:128], lhsT=hT[:, kc, csl],
                                                 rhs=zw[:, kc, :], start=(kc == 0),
                                                 stop=(kc == NCH - 1))
                                return r
                            sc.op("pe", f, reads=[t_zw] + [t_hT[c][q] for c in range(NCH)],
                                  writes=[t_ps[bz]])
                            sc.op("act", lambda e, bz=bz: e.activation(
                                out=T_["sz"][:], in_=ps[:, bz, 0:128], func=AF.Silu),
                                reads=[t_ps[bz]], writes=[t_T["sz"]])
                            sc.op("dve", lambda e, n=n: e.scalar_tensor_tensor(
                                out=T_["yy"][:], in0=Oacc[:, n, :], scalar=st4[:, 2:3],
                                in1=gsm[:, 32:160], op0=ALU.mult, op1=ALU.mult),
                                reads=[t_O[n], t_st4, t_gsm], writes=[t_T["yy"]])
                            sc.op("dve", lambda e: e.tensor_tensor(
                                out=T_["yy"][:], in0=T_["yy"][:], in1=T_["sz"][:], op=ALU.mult),
                                reads=[t_T["yy"], t_T["sz"]], writes=[t_T["yy"]])
                            sc.op("pe", lambda e, bT=bT, i4=i4: e.transpose(
                                out=ps[:, bT, i4 * 128:(i4 + 1) * 128], in_=T_["yy"][:],
                                identity=ident[:]), reads=[t_T["yy"], t_ident], writes=[t_ps[bT]])
                        sc.op("act", lambda e, bT=bT, q=q: e.activation(
                            out=oT[:, q * TT:(q + 1) * TT], in_=ps[:, bT, :], func=AF.Copy),
                            reads=[t_ps[bT]], writes=[t_oT[q]])
                    if dbg == 17:
                        return
                    wo, t_wo = load_block(gout_d[h * 128:(h + 1) * 128, :]
                                          .rearrange("p (a n) -> p a n", a=1), None)
                    for t in range(NTT):
                        tsl = slice(t * TT, (t + 1) * TT)
                        for j in range(NCH):
                            b = next_ps()
                            sc.op("pe", lambda e, b=b, j=j, tsl=tsl, wo=wo: e.matmul(
                                ps[:, b, :], lhsT=wo[:, 0, j * 128:(j + 1) * 128], rhs=oT[:, tsl],
                                start=True, stop=True), reads=[t_wo, t_oT[t]], writes=[t_ps[b]])
                            sc.op("dve", lambda e, b=b, j=j, tsl=tsl: e.tensor_tensor(
                                out=xT[:, j, tsl], in0=ps[:, b, :], in1=xT[:, j, tsl], op=ALU.add),
                                reads=[t_ps[b]] + xtoks(j, t), writes=xtoks(j, t))
                    if dbg == 30 + h:
                        return


        def phase_moe():
            NT16 = S // 128
            with contextlib.ExitStack() as esp:
                set_pools(esp, 2, 6, WB)
                hT = sb("hT", [128, NCH, S], BF16, esp)
                t_hT = [[Tok("hT%d_%d" % (c, t)) for t in range(NTT)] for c in range(NCH)]
                sqtmp = sb("sqtmp", [128, NCH, TT], BF16, esp)
                t_sq = Tok("sqtmp")
                h1 = sb("h1", [128, 4, S], BF16, esp)
                t_h1 = [[Tok("h1_%d_%d" % (f_, t)) for t in range(NTT)] for f_ in range(4)]
                sg = [sb("sg%d" % i, [128, TT], F32, esp) for i in range(2)]
                t_sg = [Tok("sg0"), Tok("sg1")]
                G = [sb("G%d" % i, [128, S], F32, esp) for i in range(2)]
                t_G = [Tok("G0"), Tok("G1")]
                wr = sb("wr", [128, NCH, 8], F32, esp)
                t_wr = Tok("wr")
                sq32 = [sb("sq32_%d" % i, [128, NCH, 128], F32, esp) for i in range(2)]
                t_sq32 = [Tok("sq32_0"), Tok("sq32_1")]
                rst = sb("rst", [128, NT16, 9], F32, esp)
                t_rst = Tok("rst")
                L = sb("L", [128, NT16, 8], F32, esp)
                v8 = sb("v8", [128, NT16, 8], F32, esp)
                gate = sb("gate", [128, NT16, 8], F32, esp)
                tmpr = sb("tmpr", [128, NT16, 8], F32, esp)
                rs16 = sb("rs16", [128, NT16, 4], F32, esp)
                dg = [sb("dg%d" % i, [128, 128], F32, esp) for i in range(2)]
                t_dg = [Tok("dg0"), Tok("dg1")]
                t_L, t_v8, t_gate, t_tmpr, t_rs16 = (Tok("L"), Tok("v8"), Tok("gate"),
                                                     Tok("tmpr"), Tok("rs16"))
                ch_wr = sc.new_chan()
                rmsnorm_fm(hT, t_hT, VR["ffn1"], sqtmp, t_sq)
                sc.op("sp", lambda e: e.dma_start(
                    out=wr[:], in_=rt_d.rearrange("(c p) e -> p c e", p=128)),
                    writes=[t_wr], chan=ch_wr, after=sc.last_ops())
                for c in range(NCH):
                    sc.op("dve", lambda e, c=c: e.tensor_scalar(
                        out=wr[:, c, :], in0=wr[:, c, :], scalar1=vcol(c, VR["ffn1"]),
                        scalar2=None, op0=ALU.mult), reads=[t_wr, t_vecs], writes=[t_wr])
                for tt in range(NT16):
                    s_ = tt % 2
                    tsl = slice(tt * 128, (tt + 1) * 128)
                    sc.op("act", lambda e, s_=s_, tsl=tsl: e.activation(
                        out=sq32[s_][:], in_=xT[:, :, tsl], func=AF.Square),
                        reads=[t_xT[c][tt] for c in range(NCH)], writes=[t_sq32[s_]])
                    b = next_ps()

                    def f(e, b=b, tsl=tsl, s_=s_):
                        for c in range(NCH):
                            e.matmul(ps[:, b, 0:8], lhsT=xT[:, c, tsl], rhs=wr[:, c, :],
                                     start=(c == 0), stop=(c == NCH - 1))
                        for c in range(NCH):
                            r = e.matmul(ps[:, b, 8:9], lhsT=sq32[s_][:, c, :],
                                         rhs=ones_f[:, 0:1], start=(c == 0), stop=(c == NCH - 1))
                        return r
                    sc.op("pe", f, reads=[t_wr, t_sq32[s_], t_ones] +
                          [t_xT[c][tt] for c in range(NCH)], writes=[t_ps[b]])
                    sc.op("dve", lambda e, b=b, tt=tt: e.tensor_copy(
                        out=rst[:, tt, :], in_=ps[:, b, 0:9]), reads=[t_ps[b]], writes=[t_rst])
                sc.op("act", lambda e: e.activation(
                    out=rs16[:, :, 0:1], in_=rst[:, :, 8:9], func=AF.Sqrt, scale=1.0 / D,
                    bias=1e-6), reads=[t_rst], writes=[t_rs16])
                sc.op("dve", lambda e: e.reciprocal(out=rs16[:, :, 1:2], in_=rs16[:, :, 0:1]),
                      reads=[t_rs16], writes=[t_rs16])
                sc.op("dve", lambda e: e.tensor_tensor(
                    out=L[:], in0=rst[:, :, 0:8],
                    in1=rs16[:, :, 1:2].to_broadcast([128, NT16, 8]), op=ALU.mult),
                    reads=[t_rst, t_rs16], writes=[t_L])
                for tt in range(NT16):
                    sc.op("dve", lambda e, tt=tt: e.max(out=v8[:, tt, :], in_=L[:, tt, :]),
                          reads=[t_L], writes=[t_v8])
                sc.op("dve", lambda e: e.tensor_tensor(
                    out=gate[:], in0=L[:], in1=v8[:, :, 1:2].to_broadcast([128, NT16, 8]),
                    op=ALU.is_ge), reads=[t_L, t_v8], writes=[t_gate])
                sc.op("dve", lambda e: e.tensor_tensor(
                    out=tmpr[:], in0=L[:], in1=v8[:, :, 0:1].to_broadcast([128, NT16, 8]),
                    op=ALU.subtract), reads=[t_L, t_v8], writes=[t_tmpr])
                sc.op("act", lambda e: e.activation(out=tmpr[:], in_=tmpr[:], func=AF.Exp),
                      reads=[t_tmpr], writes=[t_tmpr])
                sc.op("dve", lambda e: e.tensor_tensor(
                    out=rs16[:, :, 2:3], in0=v8[:, :, 1:2], in1=v8[:, :, 0:1], op=ALU.subtract),
                    reads=[t_v8], writes=[t_rs16])
                sc.op("act", lambda e: e.activation(out=rs16[:, :, 2:3], in_=rs16[:, :, 2:3],
                                                    func=AF.Exp),
                      reads=[t_rs16], writes=[t_rs16])
                sc.op("dve", lambda e: e.tensor_scalar(
                    out=rs16[:, :, 2:3], in0=rs16[:, :, 2:3], scalar1=1.0, scalar2=None,
                    op0=ALU.add), reads=[t_rs16], writes=[t_rs16])
                sc.op("dve", lambda e: e.reciprocal(out=rs16[:, :, 3:4], in_=rs16[:, :, 2:3]),
                      reads=[t_rs16], writes=[t_rs16])
                sc.op("dve", lambda e: e.tensor_tensor(
                    out=gate[:], in0=gate[:], in1=tmpr[:], op=ALU.mult),
                    reads=[t_gate, t_tmpr], writes=[t_gate])
                sc.op("dve", lambda e: e.tensor_tensor(
                    out=gate[:], in0=gate[:], in1=rs16[:, :, 3:4].to_broadcast([128, NT16, 8]),
                    op=ALU.mult), reads=[t_gate, t_rs16], writes=[t_gate])
                dgn = 0
                for ex in range(8):
                    gi = ex % 2
                    for q in range(NTT):
                        b = next_ps()
                        for i4 in range(4):
                            tt = q * 4 + i4
                            di = dgn % 2
                            dgn += 1
                            sc.op("dve", lambda e, di=di, tt=tt, ex=ex: e.tensor_tensor(
                                out=dg[di][:], in0=ident[:],
                                in1=gate[:, tt, ex:ex + 1].to_broadcast([128, 128]), op=ALU.mult),
                                reads=[t_ident, t_gate], writes=[t_dg[di]])
                            sc.op("pe", lambda e, b=b, i4=i4, di=di: e.matmul(
                                ps[:, b, i4 * 128:(i4 + 1) * 128], lhsT=ones_f[:], rhs=dg[di][:],
                                start=True, stop=True),
                                reads=[t_dg[di], t_ones], writes=[t_ps[b]])
                        sc.op("act", lambda e, b=b, gi=gi, q=q: e.activation(
                            out=G[gi][:, q * TT:(q + 1) * TT], in_=ps[:, b, :], func=AF.Copy),
                            reads=[t_ps[b]], writes=[t_G[gi]])
                    swiglu_stream(hT, t_hT, h1, t_h1, sg, t_sg, mg_d[ex], mu_d[ex], md_d[ex], DFFE,
                                  gate_bc=G[gi], t_gate=t_G[gi])

        if "c0" in phases:
            phase_conformer()
            sc.barrier()
        if "f0" in phases:
            phase_ffn()
            sc.barrier()
        if "g1" in phases:
            phase_gdn()
            sc.barrier()
        if "m1" in phases:
            phase_moe()
            sc.barrier()

        do_norm = "final" in phases
        with contextlib.ExitStack() as es2:
            xo = [sb("xo%d" % i, [128, D], F32, es2) for i in range(2)]
            fng = sb("fng_sb", [128, D], F32, es2)
            sc.op("sp", lambda e: e.dma_start(out=fng[:],
                                              in_=fng_d[0:1, :].partition_broadcast(128)),
                  writes=[t_fng], chan=ch_misc, after=sc.last_ops())
            yo = [sb("yo%d" % i, [128, D], F32, es2) for i in range(2)]
            sq = sb("sqj", [128, D], F32, es2)
            st = [sb("st%d" % i, [128, 4], F32, es2) for i in range(2)]
            t_xo = [Tok("xo0"), Tok("xo1")]
            t_yo = [Tok("yo0"), Tok("yo1")]
            t_sq2 = Tok("sq")
            t_st = [Tok("st0"), Tok("st1")]
            out_ops = []
            for tt in range(S // 128):
                sl = tt % 2
                for half in range(2):
                    b = next_ps()

                    def f(e, half=half, b=b, tt=tt):
                        for j in range(4):
                            c = half * 4 + j
                            r = e.transpose(out=ps[:, b, j * 128:(j + 1) * 128],
                                            in_=xT[:, c, tt * 128:(tt + 1) * 128],
                                            identity=ident[:])
                        return r
                    sc.op("pe", f, reads=[t_ident] + [t_xT[half * 4 + j][tt] for j in range(4)],
                          writes=[t_ps[b]])
                    dst = xo if do_norm else yo
                    t_dst = t_xo if do_norm else t_yo
                    sc.op("act", lambda e, half=half, b=b, sl=sl, dst=dst: e.activation(
                        out=dst[sl][:, half * 512:(half + 1) * 512], in_=ps[:, b, :], func=AF.Copy),
                        reads=[t_ps[b]], writes=[t_dst[sl]])
                if do_norm:
                    sc.op("act", lambda e, sl=sl: e.activation(
                        out=sq[:], in_=xo[sl][:], func=AF.Square, accum_out=st[sl][:, 0:1]),
                        reads=[t_xo[sl]], writes=[t_sq2, t_st[sl]])
                    sc.op("act", lambda e, sl=sl: e.activation(
                        out=st[sl][:, 1:2], in_=st[sl][:, 0:1], func=AF.Sqrt, scale=1.0 / D,
                        bias=1e-6), reads=[t_st[sl]], writes=[t_st[sl]])
                    sc.op("dve", lambda e, sl=sl: e.reciprocal(out=st[sl][:, 2:3],
                                                               in_=st[sl][:, 1:2]),
                          reads=[t_st[sl]], writes=[t_st[sl]])
                    sc.op("dve", lambda e, sl=sl: e.scalar_tensor_tensor(
                        out=yo[sl][:], in0=xo[sl][:], scalar=st[sl][:, 2:3], in1=fng[:],
                        op0=ALU.mult, op1=ALU.mult),
                        reads=[t_xo[sl], t_st[sl], t_fng], writes=[t_yo[sl]])
                o = sc.op("sp", lambda e, sl=sl, tt=tt: e.dma_start(
                    out=out_d[tt * 128:(tt + 1) * 128, :], in_=yo[sl][:]),
                    reads=[t_yo[sl]], chan=ch_out[sl])
                out_ops.append(o)
            fin = sc.op("sp", lambda e: e.nop())
            fin.is_nop = True
            fin.deps.extend(out_ops[-2:])
            sc.emit(nc)
    return nc


def pack_vecs(inp):
    rows = np.zeros((128, D), np.float32)
    rows[VR["mix0"]] = inp["mix_norm"][0]
    rows[VR["mix1"]] = inp["mix_norm"][1]
    rows[VR["ffn0"]] = inp["ffn_norm"][0]
    rows[VR["ffn1"]] = inp["ffn_norm"][1]
    rows[VR["pw1_ba"]] = inp["cf_pw1_b"][0, :D]
    rows[VR["pw1_bb"]] = inp["cf_pw1_b"][0, D:]
    rows[VR["dw_b"]] = inp["cf_dw_b"][0]
    rows[VR["ln_g"]] = inp["cf_ln_g"][0]
    rows[VR["ln_b"]] = inp["cf_ln_b"][0]
    rows[VR["pw2_b"]] = inp["cf_pw2_b"][0]
    rows[VR["dw_w"]:VR["dw_w"] + 31] = inp["cf_dw_w"][0]
    gc = inp["gdn_conv_w"][0]
    for k in range(5):
        for part in range(3):
            rows[VR["gconv"] + k * 3 + part] = gc[k, part * D:(part + 1) * D]
    return rows


def gdn_consts():
    i = np.arange(128)
    c = np.zeros((128, 2048), np.float32)
    c[:, 0:128] = (i[:, None] <= i[None, :])
    c[:, 128:256] = (i[:, None] >= i[None, :])
    for k in range(7):
        bsz = 1 << k
        I, J = i[:, None], i[None, :]
        m = ((I // (2 * bsz)) == (J // (2 * bsz))) & (((I // bsz) % 2) == 1) & (((J // bsz) % 2) == 0)
        c[:, 256 + k * 128:256 + (k + 1) * 128] = m.T
        c[:, 1152 + k * 128:1152 + (k + 1) * 128] = m
    return c


def make_in_maps(inp, phases, nb):
    x = np.ascontiguousarray(inp["x"], dtype=np.float32)
    vecs = pack_vecs(inp)
    fng = np.ascontiguousarray(inp["final_norm"], dtype=np.float32).reshape(1, D)
    base = {"vecs": vecs, "fng": fng}
    if "c0" in phases:
        base["cf_pw1_w"] = np.ascontiguousarray(inp["cf_pw1_w"][0])
        base["cf_pw2_w"] = np.ascontiguousarray(inp["cf_pw2_w"][0])
    if "f0" in phases:
        base["ffn_w_gate"] = np.ascontiguousarray(inp["ffn_w_gate"][0])
        base["ffn_w_up"] = np.ascontiguousarray(inp["ffn_w_up"][0])
        base["ffn_w_down"] = np.ascontiguousarray(inp["ffn_w_down"][0])
    if "g1" in phases:
        base["gdn_w_in"] = np.ascontiguousarray(inp["gdn_w_in"][0])
        base["gdn_w_out"] = np.ascontiguousarray(inp["gdn_w_out"][0])
        base["gsmall"] = np.concatenate([inp["gdn_a_log"][0].reshape(-1),
                                         inp["gdn_dt_bias"][0].reshape(-1),
                                         inp["gdn_o_norm"][0].reshape(-1)]).astype(np.float32)[None]
        base["gconst"] = gdn_consts()
    if "m1" in phases:
        base["moe_router"] = np.ascontiguousarray(inp["moe_router"][0])
        base["moe_w_gate"] = np.ascontiguousarray(inp["moe_w_gate"][0])
        base["moe_w_up"] = np.ascontiguousarray(inp["moe_w_up"][0])
        base["moe_w_down"] = np.ascontiguousarray(inp["moe_w_down"][0])
    return [dict(base, x=x[b]) for b in range(nb)]


def kernel(**inp):
    phases = ("c0", "f0", "g1", "m1", "final")
    nb = inp["x"].shape[0]
    nc = build_program(phases)
    in_maps = make_in_maps(inp, phases, nb)
    res = run_bass_kernel_spmd(nc, in_maps, core_ids=list(range(nb)))
    return np.stack([r["out"] for r in res.results], axis=0)
```

```python
import contextlib
import numpy as np
import concourse.bass as bass
import concourse.mybir as mybir
from concourse.bass_utils import run_bass_kernel_spmd

F32 = mybir.dt.float32
BF16 = mybir.dt.bfloat16
I32 = mybir.dt.int32
AF = mybir.ActivationFunctionType
ALU = mybir.AluOpType

D = 1024
S = 2048
NCH = D // 128
TT = 512
NTT = S // TT
ENGS = ("sp", "pe", "act", "dve", "pool")


class Tok:
    __slots__ = ("name", "w", "readers")

    def __init__(self, name):
        self.name = name
        self.w = None
        self.readers = []


class Op:
    __slots__ = ("eng", "fn", "deps", "chan", "has_dep", "sig", "name", "is_nop")

    def __init__(self, eng, fn, chan, name):
        self.eng = eng
        self.fn = fn
        self.deps = []
        self.chan = chan
        self.has_dep = False
        self.sig = None
        self.name = name
        self.is_nop = False


class Rec:
    def __init__(self):
        self.items = []

    def op(self, *a, **k):
        self.items.append((a, k))


class Sched:
    def __init__(self):
        self.ops = {e: [] for e in ENGS}
        self.nchan = 0

    def new_chan(self):
        self.nchan += 1
        return self.nchan - 1

    def last_real(self, e):
        for o in reversed(self.ops[e]):
            if not o.is_nop:
                return o
        return None

    def last_ops(self, engs=("pe", "act", "dve", "pool")):
        return [o for o in (self.last_real(e) for e in engs) if o is not None]

    def op(self, eng, fn, reads=(), writes=(), chan=None, name="", after=()):
        o = Op(eng, fn, chan, name)
        for d in after:
            o.deps.append(d)
            if d.chan is None:
                d.has_dep = True
        cand = []
        for t in reads:
            if t.w is not None:
                cand.append((t.w, "raw"))
        for t in writes:
            if t.w is not None:
                cand.append((t.w, "waw"))
            for r in t.readers:
                cand.append((r, "war"))
        seen = set()
        for d, kind in cand:
            if d is o or id(d) in seen:
                continue
            same = (d.eng == eng and d.chan is None and chan is None)
            if same and (eng == "pe" or kind == "war"):
                continue
            seen.add(id(d))
            o.deps.append(d)
            d.has_dep = True
        for t in reads:
            t.readers.append(o)
        for t in writes:
            t.w = o
            t.readers = []
        self.ops[eng].append(o)
        return o

    def barrier(self, engs=("pe", "act", "dve", "pool")):
        last = {e: self.last_real(e) for e in engs}
        for e in engs:
            o = Op(e, lambda eng: eng.nop(), None, "barrier")
            o.is_nop = True
            for e2, l in last.items():
                if e2 != e and l is not None:
                    o.deps.append(l)
                    if l.chan is None:
                        l.has_dep = True
            self.ops[e].append(o)

    def emit(self, nc, final_wait_ops=()):
        with contextlib.ExitStack() as es:
            EPOCH = 1000
            nsig = {e: sum(1 for o in self.ops[e] if o.chan is None and o.has_dep) for e in ENGS}
            esem = {e: [es.enter_context(nc.semaphore("s_%s%d" % (e, i)))
                        for i in range(nsig[e] // EPOCH + 1)] for e in ENGS}
            for e in ENGS:
                cnt = 0
                for o in self.ops[e]:
                    if o.chan is None and o.has_dep:
                        o.sig = (esem[e][cnt // EPOCH], cnt % EPOCH + 1, 1)
                        cnt += 1
            CEP = 100
            ccnt = [0] * self.nchan
            ntot = [0] * self.nchan
            for e in ENGS:
                for o in self.ops[e]:
                    if o.chan is not None:
                        ntot[o.chan] += 1
            csem = [[es.enter_context(nc.semaphore("c_%d_%d" % (i, k)))
                     for k in range(ntot[i] // CEP + 1)] for i in range(self.nchan)]
            for e in ENGS:
                for o in self.ops[e]:
                    if o.chan is not None:
                        k = ccnt[o.chan]
                        o.sig = (csem[o.chan][k // CEP], (k % CEP + 1) * 16, 16)
                        ccnt[o.chan] += 1
            block = es.enter_context(nc.Block())
            ops = self.ops

            def run(engname, eobj):
                waited = {}
                for o in ops[engname]:
                    need = {}
                    for d in o.deps:
                        sem, val, _ = d.sig
                        k = id(sem)
                        if val > waited.get(k, 0) and val > need.get(k, (None, 0))[1]:
                            need[k] = (sem, val)
                    for k, (sem, val) in need.items():
                        eobj.wait_ge(sem, val)
                        waited[k] = val
                    inst = o.fn(eobj)
                    if o.sig is not None:
                        inst.then_inc(o.sig[0], o.sig[2])

            @block.sync
            def _(e):
                run("sp", e)

            @block.tensor
            def _(e):
                run("pe", e)

            @block.scalar
            def _(e):
                run("act", e)

            @block.vector
            def _(e):
                run("dve", e)

            @block.gpsimd
            def _(e):
                run("pool", e)


VR = dict(mix0=0, mix1=1, ffn0=2, ffn1=3, pw1_ba=4, pw1_bb=5, dw_b=6, ln_g=7, ln_b=8, pw2_b=9,
          dw_w=10, gconv=41)
DFF = 2816
DFFE = 3584
USE_F32R = True
WB = 2048


def build_program(phases=("c0", "f0", "g1", "m1"), dbg=0, heads=tuple(range(8)), two_x=False):
    nc = bass.Bass("TRN2", target_bir_lowering=False, dynamic_dma_scratch_size=2048)

    def din(name, shape):
        return nc.dram_tensor(name, shape, F32, kind="ExternalInput").ap()

    x_d = din("x", [S, D])
    vecs_d = din("vecs", [128, D])
    fng_d = din("fng", [1, D])
    out_d = nc.dram_tensor("out", [S, D], F32, kind="ExternalOutput").ap()
    xacc_d = din("xacc", [S, D]) if two_x else None
    if "c0" in phases:
        w1_d = din("cf_pw1_w", [D, 2 * D])
        w2_d = din("cf_pw2_w", [D, D])
    if "f0" in phases:
        fg_d = din("ffn_w_gate", [D, DFF])
        fu_d = din("ffn_w_up", [D, DFF])
        fd_d = din("ffn_w_down", [DFF, D])

    if "g1" in phases:
        gin_d = din("gdn_w_in", [D, 4128])
        gout_d = din("gdn_w_out", [D, D])
        gsm_d = din("gsmall", [1, 160])
    if "m1" in phases:
        rt_d = din("moe_router", [D, 8])
        mg_d = din("moe_w_gate", [8, D, DFFE])
        mu_d = din("moe_w_up", [8, D, DFFE])
        md_d = din("moe_w_down", [8, DFFE, D])

    sc = Sched()
    with contextlib.ExitStack() as es:
        uniq = [0]

        def sb(name, shape, dt, stack=es):
            uniq[0] += 1
            return stack.enter_context(nc.sbuf_tensor("%s_%d" % (name, uniq[0]), shape, dt))

        xT = sb("xT", [128, NCH, S], F32)
        ident = sb("ident", [128, 128], F32)
        ones_f = sb("ones_f", [128, 128], F32)
        ones_b = sb("ones_b", [128, 128], BF16)
        vecs = sb("vecs_sb", [128, NCH, 64], F32)
        ps = es.enter_context(nc.psum_tensor("ps", [128, 8, 512], F32))
        stage, wbf, t_stage, t_wbf = [], [], [], []
        pool_fence = [[], 0]

        def set_pools(stack, nst, nwb, elems):
            stage[:] = [sb("stage%d" % i, [128, elems], F32, stack) for i in range(nst)]
            wbf[:] = [sb("wbf%d" % i, [128, elems], BF16, stack) for i in range(nwb)]
            t_stage[:] = [Tok("stage%d" % i) for i in range(nst)]
            t_wbf[:] = [Tok("wbf%d" % i) for i in range(nwb)]
            pool_fence[0] = sc.last_ops()
            pool_fence[1] = nst
        cst_d = din("gconst", [128, 2048]) if "g1" in phases else None
        sm = [sb("sm%d" % i, [128, TT], F32) for i in range(4)]
        t_sm = [Tok("sm%d" % i) for i in range(4)]
        smn = [0]

        def next_sm():
            i = smn[0] % 4
            smn[0] += 1
            return i

        t_xT = [[Tok("xT%d_%d" % (c, t)) for t in range(S // 128)] for c in range(NCH)]

        def xtoks(c, t512):
            return [t_xT[c][t512 * 4 + i] for i in range(4)]
        t_ident = Tok("ident")
        t_ones = Tok("ones")
        t_vecs = Tok("vecs")
        t_fng = Tok("fng")
        t_ps = [Tok("ps%d" % i) for i in range(8)]
        ch_stage = [sc.new_chan() for _ in range(2)]
        ch_xin = [sc.new_chan(), sc.new_chan()]
        ch_misc = sc.new_chan()
        ch_out = [sc.new_chan(), sc.new_chan()]
        psn = [0]
        stn = [0]
        wbn = [0]

        def next_ps():
            i = psn[0] % 8
            psn[0] += 1
            return i

        cast_cfg = ["act"]

        def load_block(src, view, cast_eng=None):
            cast_eng = cast_eng or cast_cfg[0]
            a, b = src.shape[1], src.shape[2]
            n = a * b
            s = stn[0] % len(stage)
            stn[0] += 1
            k = wbn[0] % len(wbf)
            wbn[0] += 1
            aft = ()
            if pool_fence[1] > 0:
                aft = pool_fence[0]
                pool_fence[1] -= 1
            st_, wb_, ts_, tw_ = stage[s], wbf[k], t_stage[s], t_wbf[k]
            sc.op("sp", lambda e: e.dma_start(
                out=st_[:, 0:n].rearrange("p (a b) -> p a b", a=a), in_=src),
                writes=[ts_], chan=ch_stage[s], after=aft)
            if cast_eng == "act":
                sc.op("act", lambda e: e.activation(out=wb_[:, 0:n], in_=st_[:, 0:n],
                                                    func=AF.Copy),
                      reads=[ts_], writes=[tw_])
            else:
                sc.op(cast_eng, lambda e: e.tensor_copy(out=wb_[:, 0:n], in_=st_[:, 0:n]),
                      reads=[ts_], writes=[tw_])
            return wb_[:, 0:n].rearrange("p (a b) -> p a b", a=a), tw_

        def fr0(ap):
            return ap.bitcast(mybir.dt.float32r) if USE_F32R else ap

        sc.op("pool", lambda e: e.memset(ones_f[:], 1.0), writes=[t_ones])
        sc.op("pool", lambda e: e.memset(ones_b[:], 1.0), writes=[t_ones])
        sc.op("pool", lambda e: e.affine_select(out=fr0(ident[:]), in_=ones_f[:], pattern=[[-1, 128]],
                                                compare_op=ALU.is_equal, fill=0.0, base=0,
                                                channel_multiplier=1),
              reads=[t_ones], writes=[t_ident])

        def load_x_loop(src_d, xin, t_xin, fence=()):
            for tt in range(S // 128):
                sl = tt % 2
                sc.op("sp", lambda e, tt=tt, sl=sl: e.dma_start(
                    out=xin[sl][:], in_=src_d[tt * 128:(tt + 1) * 128, :]),
                    writes=[t_xin[sl]], chan=ch_xin[sl], after=fence)
                for half in range(2):
                    b = next_ps()

                    def f(e, half=half, b=b, sl=sl):
                        for j in range(4):
                            c = half * 4 + j
                            r = e.transpose(out=ps[:, b, j * 128:(j + 1) * 128],
                                            in_=xin[sl][:, c * 128:(c + 1) * 128],
                                            identity=ident[:])
                        return r
                    sc.op("pe", f, reads=[t_xin[sl], t_ident], writes=[t_ps[b]])
                    eng = "dve" if half == 0 else "act"

                    def g(e, half=half, b=b, tt=tt, eng=eng):
                        o = xT[:, half * 4:half * 4 + 4, tt * 128:(tt + 1) * 128]
                        i = ps[:, b, :].rearrange("p (j r) -> p j r", j=4)
                        if eng == "dve":
                            return e.tensor_copy(out=o, in_=i)
                        return e.activation(out=o, in_=i, func=AF.Copy)
                    sc.op(eng, g, reads=[t_ps[b]],
                          writes=[t_xT[half * 4 + j][tt] for j in range(4)])


        with contextlib.ExitStack() as es1:
            xin = [sb("xin%d" % i, [128, D], F32, es1) for i in range(2)]
            t_xin = [Tok("xin0"), Tok("xin1")]
            sc.op("sp", lambda e: e.dma_start(out=xin[1][:], in_=vecs_d[:, :]), writes=[t_xin[1]],
                  chan=ch_xin[1])
            for half in range(2):
                b = next_ps()

                def f(e, half=half, b=b):
                    for j in range(4):
                        c = half * 4 + j
                        r = e.transpose(out=ps[:, b, j * 128:(j + 1) * 128],
                                        in_=xin[1][:, c * 128:(c + 1) * 128], identity=ident[:])
                    return r
                sc.op("pe", f, reads=[t_xin[1], t_ident], writes=[t_ps[b]])
                sc.op("dve", lambda e, half=half, b=b: e.tensor_copy(
                    out=vecs[:, half * 4:half * 4 + 4, :],
                    in_=ps[:, b, :].rearrange("p (j r) -> p j r", j=4)[:, :, 0:64]),
                    reads=[t_ps[b]], writes=[t_vecs])
            load_x_loop(x_d, xin, t_xin)
        sc.barrier()

        def vcol(c, r):
            return vecs[:, c, r:r + 1]

        def rmsnorm_fm(hT, t_hT, grow, sqtmp, t_sq):
            for t in range(NTT):
                tsl = slice(t * TT, (t + 1) * TT)
                sc.op("act", lambda e, tsl=tsl: e.activation(out=sqtmp[:], in_=xT[:, :, tsl],
                                                             func=AF.Square),
                      reads=[tk for c in range(NCH) for tk in xtoks(c, t)], writes=[t_sq])
                b = next_ps()

                def f(e, b=b):
                    for c in range(NCH):
                        r = e.matmul(ps[:, b, :], lhsT=ones_b[:], rhs=sqtmp[:, c, :],
                                     start=(c == 0), stop=(c == NCH - 1))
                    return r
                sc.op("pe", f, reads=[t_sq, t_ones], writes=[t_ps[b]])
                s0 = next_sm()
                sc.op("act", lambda e, b=b, s0=s0: e.activation(out=sm[s0][:], in_=ps[:, b, :],
                                                                func=AF.Sqrt, scale=1.0 / D,
                                                                bias=1e-6),
                      reads=[t_ps[b]], writes=[t_sm[s0]])
                s1 = next_sm()
                sc.op("dve", lambda e, s0=s0, s1=s1: e.reciprocal(out=sm[s1][:], in_=sm[s0][:]),
                      reads=[t_sm[s0]], writes=[t_sm[s1]])
                for c in range(NCH):
                    sc.op("dve", lambda e, c=c, tsl=tsl, s1=s1: e.scalar_tensor_tensor(
                        out=hT[:, c, tsl], in0=xT[:, c, tsl], scalar=vcol(c, grow), in1=sm[s1][:],
                        op0=ALU.mult, op1=ALU.mult),
                        reads=xtoks(c, t) + [t_vecs, t_sm[s1]], writes=[t_hT[c][t]])

        def dump(src_fn, toks_fn):
            for c in range(NCH):
                for t in range(NTT):
                    tsl = slice(t * TT, (t + 1) * TT)
                    sc.op("dve", lambda e, c=c, tsl=tsl: e.tensor_copy(out=xT[:, c, tsl],
                                                                       in_=src_fn(c, tsl)),
                          reads=toks_fn(c, t), writes=xtoks(c, t))

        def phase_conformer():
            with contextlib.ExitStack() as esp:
                set_pools(esp, 2, 6, WB)
                hT = sb("hT", [128, NCH, S], BF16, esp)
                t_hT = [[Tok("hT%d_%d" % (c, t)) for t in range(NTT)] for c in range(NCH)]
                sqtmp = sb("sqtmp", [128, NCH, TT], BF16, esp)
                t_sq = Tok("sqtmp")
                U = sb("ubuf", [128, NCH, S + 30], BF16, esp)
                t_U = [Tok("u%d" % c) for c in range(NCH)]
                diag = sb("diag", [128, 31, 128], BF16, esp)
                t_diag = Tok("diag")
                sig = [sb("sig%d" % i, [128, TT], F32, esp) for i in range(2)]
                t_sig = [Tok("sig0"), Tok("sig1")]
                lnst = [sb("lnst%d" % i, [128, TT], F32, esp) for i in range(3)]
                t_lnst = [Tok("lnst%d" % i) for i in range(3)]
                rmsnorm_fm(hT, t_hT, VR["mix0"], sqtmp, t_sq)
                if dbg == 1:
                    dump(lambda c, tsl: hT[:, c, tsl], lambda c, t: [t_hT[c][t]])
                    return
                sc.op("dve", lambda e: e.memset(U[:], 0.0), writes=t_U)
                sgn = 0
                for h in range(2):
                    wa = [load_block(w1_d[:, h * 512 + q * 256: h * 512 + (q + 1) * 256]
                                     .rearrange("(kc p) n -> p kc n", p=128), None) for q in range(2)]
                    wb_ = [load_block(w1_d[:, D + h * 512 + q * 256: D + h * 512 + (q + 1) * 256]
                                      .rearrange("(kc p) n -> p kc n", p=128), None) for q in range(2)]
                    for jj in range(4):
                        j = h * 4 + jj
                        wA, tA = wa[jj // 2]
                        wB, tB = wb_[jj // 2]
                        co = (jj % 2) * 128
                        for t in range(NTT):
                            tsl = slice(t * TT, (t + 1) * TT)
                            bA = next_ps()
                            bB = next_ps()

                            def f(e, wA=wA, wB=wB, co=co, bA=bA, bB=bB, tsl=tsl):
                                for kc in range(NCH):
                                    e.matmul(ps[:, bA, :], lhsT=wA[:, kc, co:co + 128],
                                             rhs=hT[:, kc, tsl], start=(kc == 0),
                                             stop=(kc == NCH - 1))
                                for kc in range(NCH):
                                    r = e.matmul(ps[:, bB, :], lhsT=wB[:, kc, co:co + 128],
                                                 rhs=hT[:, kc, tsl], start=(kc == 0),
                                                 stop=(kc == NCH - 1))
                                return r
                            sc.op("pe", f, reads=[tA, tB] + [t_hT[c][t] for c in range(NCH)],
                                  writes=[t_ps[bA], t_ps[bB]])
                            sg = sgn % 2
                            sgn += 1
                            sc.op("act", lambda e, bB=bB, sg=sg, j=j: e.activation(
                                out=sig[sg][:], in_=ps[:, bB, :], func=AF.Sigmoid,
                                bias=vcol(j, VR["pw1_bb"])),
                                reads=[t_ps[bB], t_vecs], writes=[t_sig[sg]])
                            sc.op("dve", lambda e, bA=bA, sg=sg, j=j, t=t: e.scalar_tensor_tensor(
                                out=U[:, j, 15 + t * TT:15 + (t + 1) * TT], in0=ps[:, bA, :],
                                scalar=vcol(j, VR["pw1_ba"]), in1=sig[sg][:],
                                op0=ALU.add, op1=ALU.mult),
                                reads=[t_ps[bA], t_sig[sg], t_vecs], writes=[t_U[j]])
                if dbg == 2:
                    dump(lambda c, tsl: U[:, c, 15 + tsl.start:15 + tsl.stop], lambda c, t: [t_U[c]])
                    return
                for j in range(NCH):
                    sc.op("dve", lambda e, j=j: e.tensor_tensor(
                        out=diag[:], in0=ident[:].unsqueeze(1).to_broadcast([128, 31, 128]),
                        in1=vecs[:, j, VR["dw_w"]:VR["dw_w"] + 31].unsqueeze(2)
                        .to_broadcast([128, 31, 128]), op=ALU.mult),
                        reads=[t_ident, t_vecs], writes=[t_diag])
                    for t in range(NTT):
                        b = next_ps()

                        def f(e, j=j, t=t, b=b):
                            for k in range(31):
                                r = e.matmul(ps[:, b, :], lhsT=diag[:, k, :],
                                             rhs=U[:, j, t * TT + k:t * TT + k + TT],
                                             start=(k == 0), stop=(k == 30))
                            return r
                        sc.op("pe", f, reads=[t_diag, t_U[j]], writes=[t_ps[b]])
                        sc.op("act", lambda e, j=j, t=t, b=b: e.activation(
                            out=hT[:, j, t * TT:(t + 1) * TT], in_=ps[:, b, :], func=AF.Identity,
                            bias=vcol(j, VR["dw_b"])),
                            reads=[t_ps[b], t_vecs], writes=[t_hT[j][t]])
                if dbg == 3:
                    dump(lambda c, tsl: hT[:, c, tsl], lambda c, t: [t_hT[c][t]])
                    return
                for t in range(NTT):
                    tsl = slice(t * TT, (t + 1) * TT)
                    sc.op("dve", lambda e, tsl=tsl: e.tensor_tensor(
                        out=sqtmp[:], in0=hT[:, :, tsl], in1=hT[:, :, tsl], op=ALU.mult),
                        reads=[t_hT[c][t] for c in range(NCH)], writes=[t_sq])
                    b1 = next_ps()
                    b2 = next_ps()

                    def f(e, b1=b1, b2=b2, tsl=tsl):
                        for c in range(NCH):
                            e.matmul(ps[:, b1, :], lhsT=ones_b[:], rhs=hT[:, c, tsl],
                                     start=(c == 0), stop=(c == NCH - 1))
                        for c in range(NCH):
                            r = e.matmul(ps[:, b2, :], lhsT=ones_b[:], rhs=sqtmp[:, c, :],
                                         start=(c == 0), stop=(c == NCH - 1))
                        return r
                    sc.op("pe", f, reads=[t_sq, t_ones] + [t_hT[c][t] for c in range(NCH)],
                          writes=[t_ps[b1], t_ps[b2]])
                    m = lnst[0]
                    q = lnst[1]
                    rs = lnst[2]
                    tm, tq, trs = t_lnst
                    sc.op("act", lambda e, m=m, b1=b1: e.activation(
                        out=m[:], in_=ps[:, b1, :], func=AF.Copy, scale=1.0 / D),
                        reads=[t_ps[b1]], writes=[tm])
                    sc.op("dve", lambda e, m=m, q=q: e.tensor_tensor(
                        out=q[:], in0=m[:], in1=m[:], op=ALU.mult),
                        reads=[tm], writes=[tq])
                    sc.op("dve", lambda e, q=q, b2=b2: e.scalar_tensor_tensor(
                        out=q[:], in0=ps[:, b2, :], scalar=1.0 / D, in1=q[:],
                        op0=ALU.mult, op1=ALU.subtract),
                        reads=[t_ps[b2], tq], writes=[tq])
                    sc.op("act", lambda e, q=q: e.activation(
                        out=q[:], in_=q[:], func=AF.Sqrt, bias=1e-5),
                        reads=[tq], writes=[tq])
                    sc.op("dve", lambda e, q=q, rs=rs: e.reciprocal(out=rs[:], in_=q[:]),
                          reads=[tq], writes=[trs])
                    sc.op("dve", lambda e, m=m, rs=rs: e.scalar_tensor_tensor(
                        out=m[:], in0=m[:], scalar=-1.0, in1=rs[:],
                        op0=ALU.mult, op1=ALU.mult),
                        reads=[tm, trs], writes=[tm])
                    for c in range(NCH):
                        w1s = next_sm()
                        sc.op("dve", lambda e, c=c, tsl=tsl, rs=rs, w1s=w1s: e.tensor_tensor(
                            out=sm[w1s][:], in0=hT[:, c, tsl], in1=rs[:], op=ALU.mult),
                            reads=[t_hT[c][t], trs], writes=[t_sm[w1s]])
                        sc.op("dve", lambda e, m=m, w1s=w1s: e.tensor_tensor(
                            out=sm[w1s][:], in0=sm[w1s][:], in1=m[:], op=ALU.add),
                            reads=[t_sm[w1s], tm], writes=[t_sm[w1s]])
                        sc.op("act", lambda e, c=c, tsl=tsl, w1s=w1s: e.activation(
                            out=hT[:, c, tsl], in_=sm[w1s][:], func=AF.Silu,
                            scale=vcol(c, VR["ln_g"]), bias=vcol(c, VR["ln_b"])),
                            reads=[t_sm[w1s], t_vecs], writes=[t_hT[c][t]])
                if dbg == 4:
                    dump(lambda c, tsl: hT[:, c, tsl], lambda c, t: [t_hT[c][t]])
                    return
                for h in range(2):
                    w2 = [load_block(w2_d[:, h * 512 + q * 256: h * 512 + (q + 1) * 256]
                                     .rearrange("(kc p) n -> p kc n", p=128), None) for q in range(2)]
                    for jj in range(4):
                        j = h * 4 + jj
                        wA, tA = w2[jj // 2]
                        co = (jj % 2) * 128
                        for t in range(NTT):
                            tsl = slice(t * TT, (t + 1) * TT)
                            b = next_ps()

                            def f(e, wA=wA, co=co, b=b, tsl=tsl):
                                for kc in range(NCH):
                                    r = e.matmul(ps[:, b, :], lhsT=wA[:, kc, co:co + 128],
                                                 rhs=hT[:, kc, tsl], start=(kc == 0),
                                                 stop=(kc == NCH - 1))
                                return r
                            sc.op("pe", f, reads=[tA] + [t_hT[c][t] for c in range(NCH)],
                                  writes=[t_ps[b]])
                            sc.op("dve", lambda e, b=b, j=j, tsl=tsl: e.scalar_tensor_tensor(
                                out=xT[:, j, tsl], in0=ps[:, b, :], scalar=vcol(j, VR["pw2_b"]),
                                in1=xT[:, j, tsl], op0=ALU.add, op1=ALU.add),
                                reads=[t_ps[b], t_vecs] + xtoks(j, t), writes=xtoks(j, t))

        def swiglu_stream(hT, t_hT, h1, t_h1, sg, t_sg, wg_d, wu_d, wd_d, dff,
                          gate_bc=None, t_gate=None):
            ngrp = (dff + 511) // 512
            sgn = 0
            for g in range(ngrp):
                c0 = g * 512
                ncol = min(512, dff - c0)
                nblk = ncol // 256
                nf = ncol // 128
                wg = [load_block(wg_d[:, c0 + q * 256:c0 + (q + 1) * 256]
                                 .rearrange("(kc p) n -> p kc n", p=128), None) for q in range(nblk)]
                wu = [load_block(wu_d[:, c0 + q * 256:c0 + (q + 1) * 256]
                                 .rearrange("(kc p) n -> p kc n", p=128), None) for q in range(nblk)]
                wd = [load_block(wd_d[c0 + q * 256:c0 + (q + 1) * 256, :]
                                 .rearrange("(fc p) n -> p fc n", p=128), None) for q in range(nblk)]
                for t in range(NTT):
                    tsl = slice(t * TT, (t + 1) * TT)
                    for f_ in range(nf):
                        wG, tG = wg[f_ // 2]
                        wU, tU = wu[f_ // 2]
                        co = (f_ % 2) * 128
                        bG = next_ps()
                        bU = next_ps()

                        def f(e, wG=wG, wU=wU, co=co, bG=bG, bU=bU, tsl=tsl):
                            for kc in range(NCH):
                                e.matmul(ps[:, bG, :], lhsT=wG[:, kc, co:co + 128],
                                         rhs=hT[:, kc, tsl], start=(kc == 0), stop=(kc == NCH - 1))
                            for kc in range(NCH):
                                r = e.matmul(ps[:, bU, :], lhsT=wU[:, kc, co:co + 128],
                                             rhs=hT[:, kc, tsl], start=(kc == 0),
                                             stop=(kc == NCH - 1))
                            return r
                        sc.op("pe", f, reads=[tG, tU] + [t_hT[c][t] for c in range(NCH)],
                              writes=[t_ps[bG], t_ps[bU]])
                        s_ = sgn % 2
                        sgn += 1
                        sc.op("act", lambda e, bG=bG, s_=s_: e.activation(
                            out=sg[s_][:], in_=ps[:, bG, :], func=AF.Silu),
                            reads=[t_ps[bG]], writes=[t_sg[s_]])
                        if gate_bc is None:
                            sc.op("dve", lambda e, bU=bU, s_=s_, f_=f_, tsl=tsl: e.tensor_tensor(
                                out=h1[:, f_, tsl], in0=ps[:, bU, :], in1=sg[s_][:], op=ALU.mult),
                                reads=[t_ps[bU], t_sg[s_]], writes=[t_h1[f_][t]])
                        else:
                            sc.op("dve", lambda e, s_=s_, tsl=tsl: e.tensor_tensor(
                                out=sg[s_][:], in0=sg[s_][:], in1=gate_bc[:, tsl], op=ALU.mult),
                                reads=[t_sg[s_], t_gate], writes=[t_sg[s_]])
                            sc.op("dve", lambda e, bU=bU, s_=s_, f_=f_, tsl=tsl: e.tensor_tensor(
                                out=h1[:, f_, tsl], in0=ps[:, bU, :], in1=sg[s_][:], op=ALU.mult),
                                reads=[t_ps[bU], t_sg[s_]], writes=[t_h1[f_][t]])
                for t in range(NTT):
                    tsl = slice(t * TT, (t + 1) * TT)
                    for j in range(NCH):
                        b = next_ps()

                        def f(e, b=b, j=j, tsl=tsl, wd=wd, nf=nf):
                            for f_ in range(nf):
                                wD, _ = wd[f_ // 2]
                                r = e.matmul(ps[:, b, :],
                                             lhsT=wD[:, f_ % 2, j * 128:(j + 1) * 128],
                                             rhs=h1[:, f_, tsl], start=(f_ == 0),
                                             stop=(f_ == nf - 1))
                            return r
                        sc.op("pe", f, reads=[w[1] for w in wd] + [t_h1[f_][t] for f_ in range(nf)],
                              writes=[t_ps[b]])
                        sc.op("dve", lambda e, b=b, j=j, tsl=tsl: e.tensor_tensor(
                            out=xT[:, j, tsl], in0=ps[:, b, :], in1=xT[:, j, tsl], op=ALU.add),
                            reads=[t_ps[b]] + xtoks(j, t), writes=xtoks(j, t))

        def phase_ffn():
            with contextlib.ExitStack() as esp:
                set_pools(esp, 2, 6, WB)
                hT = sb("hT", [128, NCH, S], BF16, esp)
                t_hT = [[Tok("hT%d_%d" % (c, t)) for t in range(NTT)] for c in range(NCH)]
                sqtmp = sb("sqtmp", [128, NCH, TT], BF16, esp)
                t_sq = Tok("sqtmp")
                h1 = sb("h1", [128, 4, S], BF16, esp)
                t_h1 = [[Tok("h1_%d_%d" % (f_, t)) for t in range(NTT)] for f_ in range(4)]
                sg = [sb("sg%d" % i, [128, TT], F32, esp) for i in range(2)]
                t_sg = [Tok("sg0"), Tok("sg1")]
                rmsnorm_fm(hT, t_hT, VR["ffn0"], sqtmp, t_sq)
                swiglu_stream(hT, t_hT, h1, t_h1, sg, t_sg, fg_d, fu_d, fd_d, DFF)


        F32R = mybir.dt.float32r

        def fr(ap):
            return ap.bitcast(F32R) if USE_F32R else ap

        def phase_gdn():
            NT16 = S // 128
            with contextlib.ExitStack() as esp:
                set_pools(esp, 2, 4, 1024)
                hT = sb("hT", [128, NCH, S], BF16, esp)
                t_hT = [[Tok("hT%d_%d" % (c, t)) for t in range(NTT)] for c in range(NCH)]
                with contextlib.ExitStack() as esq:
                    sqtmp = sb("sqtmp", [128, NCH, TT], BF16, esq)
                    t_sq = Tok("sqtmp")
                    rmsnorm_fm(hT, t_hT, VR["mix1"], sqtmp, t_sq)
                sc.barrier()
                if two_x:
                    with contextlib.ExitStack() as esx:
                        xin2 = [sb("xin2_%d" % i, [128, D], F32, esx) for i in range(2)]
                        t_xin2 = [Tok("xin2_0"), Tok("xin2_1")]
                        load_x_loop(xacc_d, xin2, t_xin2, fence=sc.last_ops())
                    sc.barrier()
                cst = sb("cst", [128, 2048], F32, esp)
                t_cst = Tok("cst")
                gsm = sb("gsm", [128, 160], F32, esp)
                t_gsm = Tok("gsm")
                graw = sb("graw", [128, NT16, 32], F32, esp)
                beta = sb("beta", [128, NT16, 16], F32, esp)
                la = sb("la", [128, NT16, 16], F32, esp)
                gcol = sb("gcol", [128, NT16, 16], F32, esp)
                glast = sb("glast", [128, NT16, 16], F32, esp)
                beg = sb("beg", [128, NT16, 16], F32, esp)
                kdc = sb("kdc", [128, NT16, 16], F32, esp)
                egl = sb("egl", [128, NT16, 16], F32, esp)
                t_graw, t_beta, t_la, t_gcol, t_glast, t_beg, t_kdc, t_egl = [
                    Tok(n) for n in ("graw", "beta", "la", "gcol", "glast", "beg", "kdc", "egl")]
                pre = sb("pre", [128, S + 4], BF16, esp)
                t_pre = Tok("pre")
                diag5 = sb("diag5", [128, 5, 128], BF16, esp)
                t_diag5 = Tok("diag5")
                QKV = [sb("qkv%d" % i, [128, S], F32, esp) for i in range(3)]
                t_QKV = [[Tok("qkv%d_%d" % (i, t)) for t in range(NTT)] for i in range(3)]
                zw = sb("zw", [128, NCH, 128], BF16, esp)
                t_zw = Tok("zw")
                Oacc = sb("Oacc", [128, NT16, 128], F32, esp)
                t_O = [Tok("O%d" % n) for n in range(NT16)]
                oT = sb("oT", [128, S], BF16, esp)
                t_oT = [Tok("oT%d" % t) for t in range(NTT)]
                names = ("bV", "Kt", "KD", "dec", "QDT", "AT", "PT", "Dm", "DTm", "X", "WK", "U")
                shp = dict(WK=[128, 256])
                TS = [{n: sb("%s_c%d" % (n, c_), shp.get(n, [128, 128]), F32, esp) for n in names}
                      for c_ in range(4)]
                t_TS = [{n: Tok("%s_c%d" % (n, c_)) for n in names} for c_ in range(4)]
                ETs = [sb("ETall%d" % c_, [128, 2, 128], F32, esp) for c_ in range(4)]
                t_ETs = [[Tok("ET%d_0" % c_), Tok("ET%d_1" % c_)] for c_ in range(4)]
                Ss = [sb("Sst%d" % c_, [128, 128], F32, esp) for c_ in range(2)]
                t_Ss = [Tok("S0"), Tok("S1")]
                T_ = {n: sb(n, [128, 128], F32, esp) for n in ("sz", "yy")}
                t_T = {n: Tok(n) for n in ("sz", "yy")}
                bpn = [0, 0, 0, 0]

                def bank_pool(c_):
                    def nb_():
                        i = c_ * 2 + bpn[c_] % 2
                        bpn[c_] += 1
                        return i
                    return nb_
                st4 = sb("st4", [128, 4], F32, esp)
                t_st4 = Tok("st4")
                ch_c = sc.new_chan()
                fence = sc.last_ops()
                sc.op("sp", lambda e: e.dma_start(out=cst[:], in_=cst_d[:, :]), writes=[t_cst],
                      chan=ch_c, after=fence)
                sc.op("sp", lambda e: e.dma_start(out=gsm[:],
                                                  in_=gsm_d[0:1, :].partition_broadcast(128)),
                      writes=[t_gsm], chan=ch_c, after=fence)
                cstr = sb("cstr", [128, 256], F32, esp)
                sc.op("act", lambda e: e.activation(out=fr(cstr[:]), in_=cst[:, 0:256], func=AF.Copy),
                      reads=[t_cst], writes=[t_cst])
                triX = [cstr[:, 0:128], cstr[:, 128:256]]
                MX = [cst[:, 256:256 + 896].rearrange("p (k i) -> p k i", k=7),
                      cst[:, 1152:1152 + 896].rearrange("p (k i) -> p k i", k=7)]
                sc.op("dve", lambda e: e.memset(pre[:], 0.0), writes=[t_pre])
                wgt, t_wgt = load_block(gin_d[:, 4096:4128].rearrange("(kc p) n -> p kc n", p=128),
                                        None)
                for n in range(NT16):
                    b = next_ps()
                    tsl = slice(n * 128, (n + 1) * 128)

                    def f(e, b=b, tsl=tsl):
                        for kc in range(NCH):
                            r = e.matmul(ps[:, b, 0:32], lhsT=hT[:, kc, tsl], rhs=wgt[:, kc, :],
                                         start=(kc == 0), stop=(kc == NCH - 1))
                        return r
                    sc.op("pe", f, reads=[t_wgt] + [t_hT[c][n // 4] for c in range(NCH)],
                          writes=[t_ps[b]])
                    sc.op("dve", lambda e, b=b, n=n: e.tensor_copy(out=graw[:, n, :],
                                                                   in_=ps[:, b, 0:32]),
                          reads=[t_ps[b]], writes=[t_graw])
                sc.op("act", lambda e: e.activation(out=beta[:], in_=graw[:, :, 0:16],
                                                    func=AF.Sigmoid),
                      reads=[t_graw], writes=[t_beta])
                sc.op("dve", lambda e: e.tensor_tensor(
                    out=la[:], in0=graw[:, :, 16:32],
                    in1=gsm[:, 16:32].unsqueeze(1).to_broadcast([128, NT16, 16]), op=ALU.add),
                    reads=[t_graw, t_gsm], writes=[t_la])
                sc.op("act", lambda e: e.activation(out=la[:], in_=la[:], func=AF.Exp),
                      reads=[t_la], writes=[t_la])
                sc.op("act", lambda e: e.activation(out=la[:], in_=la[:], func=AF.Ln, bias=1.0),
                      reads=[t_la], writes=[t_la])
                sc.op("act", lambda e: e.activation(out=gsm[:, 0:16], in_=gsm[:, 0:16],
                                                    func=AF.Exp),
                      reads=[t_gsm], writes=[t_gsm])
                sc.op("dve", lambda e: e.scalar_tensor_tensor(
                    out=la[:], in0=la[:], scalar=-1.0,
                    in1=gsm[:, 0:16].unsqueeze(1).to_broadcast([128, NT16, 16]),
                    op0=ALU.mult, op1=ALU.mult), reads=[t_la, t_gsm], writes=[t_la])
                for n in range(NT16):
                    b = next_ps()

                    def f(e, b=b, n=n):
                        e.matmul(ps[:, b, 0:8], lhsT=triX[0], rhs=la[:, n, 0:8], start=True,
                                 stop=True)
                        e.matmul(ps[:, b, 8:16], lhsT=triX[1], rhs=la[:, n, 8:16], start=True,
                                 stop=True)
                        return e.matmul(ps[:, b, 16:32], lhsT=ones_f[:], rhs=la[:, n, :],
                                        start=True, stop=True)
                    sc.op("pe", f, reads=[t_la, t_cst, t_ones], writes=[t_ps[b]])
                    sc.op("dve", lambda e, b=b, n=n: e.tensor_copy(out=gcol[:, n, :],
                                                                   in_=ps[:, b, 0:16]),
                          reads=[t_ps[b]], writes=[t_gcol])
                    sc.op("dve", lambda e, b=b, n=n: e.tensor_copy(out=glast[:, n, :],
                                                                   in_=ps[:, b, 16:32]),
                          reads=[t_ps[b]], writes=[t_glast])
                sc.op("act", lambda e: e.activation(out=beg[:], in_=gcol[:], func=AF.Exp),
                      reads=[t_gcol], writes=[t_beg])
                sc.op("dve", lambda e: e.tensor_tensor(out=beg[:], in0=beg[:], in1=beta[:],
                                                       op=ALU.mult),
                      reads=[t_beg, t_beta], writes=[t_beg])
                sc.op("dve", lambda e: e.tensor_tensor(out=kdc[:], in0=glast[:], in1=gcol[:],
                                                       op=ALU.subtract),
                      reads=[t_glast, t_gcol], writes=[t_kdc])
                sc.op("act", lambda e: e.activation(out=kdc[:], in_=kdc[:], func=AF.Exp),
                      reads=[t_kdc], writes=[t_kdc])
                sc.op("act", lambda e: e.activation(out=egl[:], in_=glast[:], func=AF.Exp),
                      reads=[t_glast], writes=[t_egl])

                def tt_(n, out, in0, in1, op, eng="dve", rd=(), wr=()):
                    sc.op(eng, lambda e: e.tensor_tensor(out=out, in0=in0, in1=in1, op=op),
                          reads=list(rd), writes=list(wr))

                if dbg == 11:
                    return
                for h in heads:
                    for part in range(3):
                        cidx = part * 8 + h
                        wv, t_wv = load_block(gin_d[:, cidx * 128:(cidx + 1) * 128]
                                              .rearrange("(kc p) n -> p kc n", p=128), None)
                        for k in range(5):
                            sc.op("dve", lambda e, k=k, part=part, h=h: e.tensor_scalar(
                                out=diag5[:, k, :], in0=ident[:],
                                scalar1=vcol(h, VR["gconv"] + k * 3 + part), scalar2=None,
                                op0=ALU.mult), reads=[t_ident, t_vecs], writes=[t_diag5])
                        for t in range(NTT):
                            tsl = slice(t * TT, (t + 1) * TT)
                            b = next_ps()

                            def f(e, b=b, tsl=tsl, wv=wv):
                                for kc in range(NCH):
                                    r = e.matmul(ps[:, b, :], lhsT=wv[:, kc, :], rhs=hT[:, kc, tsl],
                                                 start=(kc == 0), stop=(kc == NCH - 1))
                                return r
                            sc.op("pe", f, reads=[t_wv] + [t_hT[c][t] for c in range(NCH)],
                                  writes=[t_ps[b]])
                            sc.op("act", lambda e, b=b, t=t: e.activation(
                                out=pre[:, 2 + t * TT:2 + (t + 1) * TT], in_=ps[:, b, :],
                                func=AF.Copy), reads=[t_ps[b]], writes=[t_pre])
                        for t in range(NTT):
                            tsl = slice(t * TT, (t + 1) * TT)
                            b = next_ps()

                            def f(e, b=b, t=t):
                                for k in range(5):
                                    r = e.matmul(ps[:, b, :], lhsT=diag5[:, k, :],
                                                 rhs=pre[:, t * TT + k:t * TT + k + TT],
                                                 start=(k == 0), stop=(k == 4))
                                return r
                            sc.op("pe", f, reads=[t_diag5, t_pre], writes=[t_ps[b]])
                            sc.op("act", lambda e, b=b, tsl=tsl, part=part: e.activation(
                                out=fr(QKV[part][:, tsl]), in_=ps[:, b, :], func=AF.Silu),
                                reads=[t_ps[b]], writes=[t_QKV[part][t]])
                            if part < 2:
                                la_ = next_sm()
                                lb_ = next_sm()
                                sc.op("dve", lambda e, tsl=tsl, part=part, la_=la_: e.tensor_tensor(
                                    out=sm[la_][:], in0=QKV[part][:, tsl], in1=QKV[part][:, tsl],
                                    op=ALU.mult), reads=[t_QKV[part][t]], writes=[t_sm[la_]])
                                b2 = next_ps()
                                sc.op("pe", lambda e, b2=b2, la_=la_: e.matmul(
                                    ps[:, b2, :], lhsT=ones_f[:], rhs=sm[la_][:], start=True,
                                    stop=True), reads=[t_sm[la_], t_ones], writes=[t_ps[b2]])
                                sc.op("act", lambda e, b2=b2, lb_=lb_: e.activation(
                                    out=sm[lb_][:], in_=ps[:, b2, :], func=AF.Sqrt, bias=1e-6),
                                    reads=[t_ps[b2]], writes=[t_sm[lb_]])
                                sc.op("dve", lambda e, lb_=lb_: e.reciprocal(out=sm[lb_][:],
                                                                             in_=sm[lb_][:]),
                                      reads=[t_sm[lb_]], writes=[t_sm[lb_]])
                                scl = (128.0 ** -0.5) if part == 0 else 1.0
                                sc.op("dve", lambda e, tsl=tsl, part=part, scl=scl, lb_=lb_:
                                      e.scalar_tensor_tensor(
                                          out=fr(QKV[part][:, tsl]), in0=QKV[part][:, tsl], scalar=scl,
                                          in1=sm[lb_][:], op0=ALU.mult, op1=ALU.mult),
                                      reads=[t_QKV[part][t], t_sm[lb_]], writes=[t_QKV[part][t]])
                    if dbg == 12:
                        return
                    zwv, t_zwv = load_block(gin_d[:, 3072 + h * 128:3072 + (h + 1) * 128]
                                            .rearrange("(kc p) n -> p kc n", p=128), None)
                    sc.op("dve", lambda e, zwv=zwv: e.tensor_copy(out=zw[:], in_=zwv),
                          reads=[t_zwv], writes=[t_zw])
                    QT, KT, VT = QKV
                    def chain(dr, T_, t_T, ETall, t_ET, Sst, t_S, next_ps, sc, chunks, do_memset):
                        if do_memset:
                            sc.op("act", lambda e: e.activation(out=fr(Sst[:]), in_=ones_f[:], func=AF.Copy,
                                                                scale=0.0),
                                  reads=[t_ones], writes=[t_S])
                        order = chunks
                        dh = dr * 8 + h
                        for n in order:
                            csl = slice(n * 128, (n + 1) * 128)
                            t4 = n // 4
                            rq = [t_QKV[0][t4]]
                            rk = [t_QKV[1][t4]]
                            rv = [t_QKV[2][t4]]
                            bt = next_ps()

                            def f(e, bt=bt, csl=csl):
                                e.transpose(out=ps[:, bt, 0:128], in_=KT[:, csl], identity=ident[:])
                                return e.transpose(out=ps[:, bt, 128:256], in_=VT[:, csl],
                                                   identity=ident[:])
                            sc.op("pe", f, reads=rk + rv + [t_ident], writes=[t_ps[bt]])
                            sc.op("act", lambda e, bt=bt, n=n, dh=dh: e.activation(
                                out=fr(T_["bV"][:]), in_=ps[:, bt, 128:256], func=AF.Copy,
                                scale=beta[:, n, dh:dh + 1]),
                                reads=[t_ps[bt], t_beta], writes=[t_T["bV"]])
                            sc.op("act", lambda e, bt=bt, n=n, dh=dh: e.activation(
                                out=fr(T_["Kt"][:]), in_=ps[:, bt, 0:128], func=AF.Copy,
                                scale=beg[:, n, dh:dh + 1]),
                                reads=[t_ps[bt], t_beg], writes=[t_T["Kt"]])
                            sc.op("act", lambda e, bt=bt, n=n, dh=dh: e.activation(
                                out=fr(T_["KD"][:]), in_=ps[:, bt, 0:128], func=AF.Copy,
                                scale=kdc[:, n, dh:dh + 1]),
                                reads=[t_ps[bt], t_kdc], writes=[t_T["KD"]])
                            sc.op("act", lambda e, n=n, dh=dh: e.activation(
                                out=fr(T_["X"][:]), in_=ones_f[:], func=AF.Copy,
                                scale=la[:, n, dh:dh + 1]),
                                reads=[t_la, t_ones], writes=[t_T["X"]])
                            sc.op("act", lambda e, n=n, dh=dh: e.activation(
                                out=fr(T_["U"][:]), in_=ones_f[:], func=AF.Copy,
                                scale=beta[:, n, dh:dh + 1]),
                                reads=[t_beta, t_ones], writes=[t_T["U"]])
                            bm = next_ps()

                            def f(e, bm=bm, csl=csl, dr=dr):
                                e.matmul(ps[:, bm, 0:128], lhsT=fr(T_["X"][:]), rhs=fr(triX[dr]),
                                         start=True, stop=True)
                                e.matmul(ps[:, bm, 128:256], lhsT=fr(T_["U"][:]), rhs=fr(ident[:]),
                                         start=True, stop=True)
                                e.matmul(ps[:, bm, 256:384], lhsT=fr(KT[:, csl]), rhs=fr(KT[:, csl]),
                                         start=True, stop=True)
                                return e.matmul(ps[:, bm, 384:512], lhsT=fr(KT[:, csl]),
                                                rhs=fr(QT[:, csl]), start=True, stop=True)
                            sc.op("pe", f, reads=[t_T["X"], t_T["U"], t_cst, t_ident] + rk + rq,
                                  writes=[t_ps[bm]])
                            sc.op("dve", lambda e, bm=bm, n=n, dh=dh: e.tensor_scalar(
                                out=T_["dec"][:], in0=ps[:, bm, 0:128],
                                scalar1=gcol[:, n, dh:dh + 1], scalar2=0.0, op0=ALU.subtract,
                                op1=ALU.min), reads=[t_ps[bm], t_gcol], writes=[t_T["dec"]])
                            sc.op("act", lambda e: e.activation(out=T_["dec"][:], in_=T_["dec"][:],
                                                                func=AF.Exp),
                                  reads=[t_T["dec"]], writes=[t_T["dec"]])
                            sc.op("act", lambda e, bm=bm: e.activation(
                                out=fr(T_["QDT"][:]), in_=ps[:, bm, 0:128], func=AF.Exp),
                                reads=[t_ps[bm]], writes=[t_T["QDT"]])
                            sc.op("dve", lambda e, csl=csl: e.tensor_tensor(
                                out=fr(T_["QDT"][:]), in0=T_["QDT"][:], in1=QT[:, csl], op=ALU.mult),
                                reads=[t_T["QDT"]] + rq, writes=[t_T["QDT"]])
                            sc.op("dve", lambda e, bm=bm: e.tensor_tensor(
                                out=T_["AT"][:], in0=ps[:, bm, 256:384], in1=T_["dec"][:],
                                op=ALU.mult), reads=[t_ps[bm], t_T["dec"]], writes=[t_T["AT"]])
                            sc.op("dve", lambda e, bm=bm: e.tensor_tensor(
                                out=T_["AT"][:], in0=ps[:, bm, 128:256], in1=T_["AT"][:],
                                op=ALU.mult), reads=[t_ps[bm], t_T["AT"]], writes=[t_T["AT"]])
                            sc.op("dve", lambda e, bm=bm: e.tensor_tensor(
                                out=fr(T_["PT"][:]), in0=ps[:, bm, 384:512], in1=T_["dec"][:],
                                op=ALU.mult), reads=[t_ps[bm], t_T["dec"]], writes=[t_T["PT"]])
                            sc.op("dve", lambda e, dr=dr: e.tensor_tensor(
                                out=fr(T_["PT"][:]), in0=T_["PT"][:], in1=triX[dr], op=ALU.mult),
                                reads=[t_T["PT"], t_cst], writes=[t_T["PT"]])
                            if dbg == 13:
                                return
                            for lv in range(7):
                                Dc = ident if lv == 0 else T_["Dm"]
                                DTc = ident if lv == 0 else T_["DTm"]
                                rD = [t_ident] if lv == 0 else [t_T["Dm"]]
                                rDT = [t_ident] if lv == 0 else [t_T["DTm"]]
                                bx = next_ps()
                                if lv == 0:
                                    sc.op("dve", lambda e, dr=dr: e.tensor_tensor(
                                        out=fr(ETall[:, 0, :]), in0=T_["AT"][:], in1=MX[dr][:, 0, :],
                                        op=ALU.mult), reads=[t_T["AT"], t_cst], writes=[t_ET[0]])
                                sc.op("pe", lambda e, bx=bx, lv=lv, Dc=Dc: e.matmul(
                                    ps[:, bx, 0:128], lhsT=fr(ETall[:, lv % 2, :]), rhs=fr(Dc[:]), start=True,
                                    stop=True), reads=[t_ET[lv % 2]] + rD, writes=[t_ps[bx]])
                                if lv < 6:
                                    sc.op("dve", lambda e, dr=dr, lv=lv: e.tensor_tensor(
                                        out=fr(ETall[:, (lv + 1) % 2, :]), in0=T_["AT"][:],
                                        in1=MX[dr][:, lv + 1, :], op=ALU.mult),
                                        reads=[t_T["AT"], t_cst], writes=[t_ET[(lv + 1) % 2]])
                                sc.op("act", lambda e, bx=bx: e.activation(
                                    out=fr(T_["X"][:]), in_=ps[:, bx, 0:128], func=AF.Copy),
                                    reads=[t_ps[bx]], writes=[t_T["X"]])
                                by = next_ps()

                                def f(e, by=by, Dc=Dc, DTc=DTc):
                                    e.matmul(ps[:, by, 0:128], lhsT=fr(DTc[:]), rhs=fr(T_["X"][:]),
                                             start=True, stop=True)
                                    return e.matmul(ps[:, by, 128:256], lhsT=fr(T_["X"][:]), rhs=fr(DTc[:]),
                                                    start=True, stop=True)
                                sc.op("pe", f, reads=[t_T["X"]] + rDT, writes=[t_ps[by]])
                                sc.op("dve", lambda e, by=by, Dc=Dc: e.tensor_tensor(
                                    out=fr(T_["Dm"][:]), in0=Dc[:], in1=ps[:, by, 0:128],
                                    op=ALU.subtract), reads=[t_ps[by]] + rD, writes=[t_T["Dm"]])
                                sc.op("dve", lambda e, by=by, DTc=DTc: e.tensor_tensor(
                                    out=fr(T_["DTm"][:]), in0=DTc[:], in1=ps[:, by, 128:256],
                                    op=ALU.subtract), reads=[t_ps[by]] + rDT, writes=[t_T["DTm"]])
                            if dbg == 14:
                                return
                            bw = next_ps()

                            def f(e, bw=bw):
                                e.matmul(ps[:, bw, 0:128], lhsT=fr(T_["DTm"][:]), rhs=fr(T_["bV"][:]),
                                         start=True, stop=True)
                                return e.matmul(ps[:, bw, 128:256], lhsT=fr(T_["Kt"][:]),
                                                rhs=fr(T_["DTm"][:]), start=True, stop=True)
                            sc.op("pe", f, reads=[t_T["DTm"], t_T["bV"], t_T["Kt"]],
                                  writes=[t_ps[bw]])
                            sc.op("act", lambda e, bw=bw: e.activation(
                                out=fr(T_["WK"][:]), in_=ps[:, bw, 0:256], func=AF.Copy),
                                reads=[t_ps[bw]], writes=[t_T["WK"]])
                            if dbg == 21:
                                return
                            bs = next_ps()
                            sc.op("pe", lambda e, bs=bs: e.matmul(
                                ps[:, bs, 0:128], lhsT=fr(T_["WK"][:, 128:256]), rhs=fr(Sst[:]), start=True,
                                stop=True), reads=[t_T["WK"], t_S], writes=[t_ps[bs]])
                            sc.op("dve", lambda e, bs=bs: e.tensor_tensor(
                                out=fr(T_["U"][:]), in0=T_["WK"][:, 0:128], in1=ps[:, bs, 0:128],
                                op=ALU.subtract), reads=[t_ps[bs], t_T["WK"]], writes=[t_T["U"]])
                            if dbg == 22:
                                return
                            bo = next_ps()

                            def f(e, bo=bo):
                                e.matmul(ps[:, bo, 0:128], lhsT=fr(T_["QDT"][:]), rhs=fr(Sst[:]), start=True,
                                         stop=False)
                                e.matmul(ps[:, bo, 0:128], lhsT=fr(T_["PT"][:]), rhs=fr(T_["U"][:]),
                                         start=False, stop=True)
                                return e.matmul(ps[:, bo, 128:256], lhsT=fr(T_["KD"][:]), rhs=fr(T_["U"][:]),
                                                start=True, stop=True)
                            sc.op("pe", f, reads=[t_T["QDT"], t_S, t_T["PT"], t_T["U"], t_T["KD"]],
                                  writes=[t_ps[bo]])
                            if dbg == 23:
                                return
                            sc.op("dve", lambda e, bo=bo, n=n: e.tensor_tensor(
                                out=Oacc[:, n, :], in0=ps[:, bo, 0:128], in1=Oacc[:, n, :],
                                op=ALU.add), reads=[t_ps[bo], t_O[n]], writes=[t_O[n]])
                            sc.op("act", lambda e, n=n, dh=dh: e.activation(
                                out=fr(Sst[:]), in_=Sst[:], func=AF.Copy, scale=egl[:, n, dh:dh + 1]),
                                reads=[t_S, t_egl], writes=[t_S])
                            sc.op("dve", lambda e, bo=bo: e.tensor_tensor(
                                out=fr(Sst[:]), in0=ps[:, bo, 128:256], in1=Sst[:], op=ALU.add),
                                reads=[t_ps[bo], t_S], writes=[t_S])
                    sc.op("dve", lambda e: e.memset(Oacc[:], 0.0), writes=t_O)
                    recs = [Rec() for _ in range(4)]
                    offs = []
                    for w_ in range(4):
                        dr_, par_ = w_ % 2, w_ // 2
                        full = list(range(NT16)) if dr_ == 0 else list(range(NT16 - 1, -1, -1))
                        chain(dr_, TS[w_], t_TS[w_], ETs[w_], t_ETs[w_], Ss[dr_], t_Ss[dr_],
                              bank_pool(w_), recs[w_], full[par_::2], par_ == 0)
                    per_inst = len(recs[2].items) // (NT16 // 2)
                    offs = [0, 0, per_inst // 2 + 1, per_inst // 2 + 1]
                    tot = max(len(r.items) + o_ for r, o_ in zip(recs, offs))
                    for i_ in range(tot):
                        for r, o_ in zip(recs, offs):
                            j_ = i_ - o_
                            if 0 <= j_ < len(r.items):
                                a_, k_ = r.items[j_]
                                sc.op(*a_, **k_)
                    for q in range(NTT):
                        bT = next_ps()
                        for i4 in range(4):
                            n = q * 4 + i4
                            csl = slice(n * 128, (n + 1) * 128)
                            sc.op("act", lambda e, n=n: e.activation(
                                out=T_["yy"][:], in_=Oacc[:, n, :], func=AF.Square,
                                accum_out=st4[:, 0:1]), reads=[t_O[n]],
                                writes=[t_T["yy"], t_st4])
                            sc.op("act", lambda e: e.activation(
                                out=st4[:, 1:2], in_=st4[:, 0:1], func=AF.Sqrt, scale=1.0 / 128,
                                bias=1e-6), reads=[t_st4], writes=[t_st4])
                            sc.op("dve", lambda e: e.reciprocal(out=st4[:, 2:3], in_=st4[:, 1:2]),
                                  reads=[t_st4], writes=[t_st4])
                            bz = next_ps()

                            def f(e, bz=bz, csl=csl):
                                for kc in range(NCH):
                                    r = e.matmul(ps[:, bz, 0:128], lhsT=hT[:, kc, csl],
                                                 rhs=zw[:, kc, :], start=(kc == 0),
                                                 stop=(kc == NCH - 1))
                                return r
                            sc.op("pe", f, reads=[t_zw] + [t_hT[c][q] for c in range(NCH)],
                                  writes=[t_ps[bz]])
                            sc.op("act", lambda e, bz=bz: e.activation(
                                out=T_["sz"][:], in_=ps[:, bz, 0:128], func=AF.Silu),
                                reads=[t_ps[bz]], writes=[t_T["sz"]])
                            sc.op("dve", lambda e, n=n: e.scalar_tensor_tensor(
                                out=T_["yy"][:], in0=Oacc[:, n, :], scalar=st4[:, 2:3],
                                in1=gsm[:, 32:160], op0=ALU.mult, op1=ALU.mult),
                                reads=[t_O[n], t_st4, t_gsm], writes=[t_T["yy"]])
                            sc.op("dve", lambda e: e.tensor_tensor(
                                out=T_["yy"][:], in0=T_["yy"][:], in1=T_["sz"][:], op=ALU.mult),
                                reads=[t_T["yy"], t_T["sz"]], writes=[t_T["yy"]])
                            sc.op("pe", lambda e, bT=bT, i4=i4: e.transpose(
                                out=ps[:, bT, i4 * 128:(i4 + 1) * 128], in_=T_["yy"][:],
                                identity=ident[:]), reads=[t_T["yy"], t_ident], writes=[t_ps[bT]])
                        sc.op("act", lambda e, bT=bT, q=q: e.activation(
                            out=oT[:, q * TT:(q + 1) * TT], in_=ps[:, bT, :], func=AF.Copy),
                            reads=[t_ps[bT]], writes=[t_oT[q]])
                    if dbg == 17:
                        return
                    wo, t_wo = load_block(gout_d[h * 128:(h + 1) * 128, :]
                                          .rearrange("p (a n) -> p a n", a=1), None)
                    for t in range(NTT):
                        tsl = slice(t * TT, (t + 1) * TT)
                        for j in range(NCH):
                            b = next_ps()
                            sc.op("pe", lambda e, b=b, j=j, tsl=tsl, wo=wo: e.matmul(
                                ps[:, b, :], lhsT=wo[:, 0, j * 128:(j + 1) * 128], rhs=oT[:, tsl],
                                start=True, stop=True), reads=[t_wo, t_oT[t]], writes=[t_ps[b]])
                            sc.op("dve", lambda e, b=b, j=j, tsl=tsl: e.tensor_tensor(
                                out=xT[:, j, tsl], in0=ps[:, b, :], in1=xT[:, j, tsl], op=ALU.add),
                                reads=[t_ps[b]] + xtoks(j, t), writes=xtoks(j, t))
                    if dbg == 30 + h:
                        return


        def phase_moe():
            NT16 = S // 128
            with contextlib.ExitStack() as esp:
                set_pools(esp, 2, 6, WB)
                hT = sb("hT", [128, NCH, S], BF16, esp)
                t_hT = [[Tok("hT%d_%d" % (c, t)) for t in range(NTT)] for c in range(NCH)]
                sqtmp = sb("sqtmp", [128, NCH, TT], BF16, esp)
                t_sq = Tok("sqtmp")
                h1 = sb("h1", [128, 4, S], BF16, esp)
                t_h1 = [[Tok("h1_%d_%d" % (f_, t)) for t in range(NTT)] for f_ in range(4)]
                sg = [sb("sg%d" % i, [128, TT], F32, esp) for i in range(2)]
                t_sg = [Tok("sg0"), Tok("sg1")]
                G = [sb("G%d" % i, [128, S], F32, esp) for i in range(2)]
                t_G = [Tok("G0"), Tok("G1")]
                wr = sb("wr", [128, NCH, 8], F32, esp)
                t_wr = Tok("wr")
                sq32 = [sb("sq32_%d" % i, [128, NCH, 128], F32, esp) for i in range(2)]
                t_sq32 = [Tok("sq32_0"), Tok("sq32_1")]
                rst = sb("rst", [128, NT16, 9], F32, esp)
                t_rst = Tok("rst")
                L = sb("L", [128, NT16, 8], F32, esp)
                v8 = sb("v8", [128, NT16, 8], F32, esp)
                gate = sb("gate", [128, NT16, 8], F32, esp)
                tmpr = sb("tmpr", [128, NT16, 8], F32, esp)
                rs16 = sb("rs16", [128, NT16, 4], F32, esp)
                dg = [sb("dg%d" % i, [128, 128], F32, esp) for i in range(2)]
                t_dg = [Tok("dg0"), Tok("dg1")]
                t_L, t_v8, t_gate, t_tmpr, t_rs16 = (Tok("L"), Tok("v8"), Tok("gate"),
                                                     Tok("tmpr"), Tok("rs16"))
                ch_wr = sc.new_chan()
                rmsnorm_fm(hT, t_hT, VR["ffn1"], sqtmp, t_sq)
                sc.op("sp", lambda e: e.dma_start(
                    out=wr[:], in_=rt_d.rearrange("(c p) e -> p c e", p=128)),
                    writes=[t_wr], chan=ch_wr, after=sc.last_ops())
                for c in range(NCH):
                    sc.op("dve", lambda e, c=c: e.tensor_scalar(
                        out=wr[:, c, :], in0=wr[:, c, :], scalar1=vcol(c, VR["ffn1"]),
                        scalar2=None, op0=ALU.mult), reads=[t_wr, t_vecs], writes=[t_wr])
                for tt in range(NT16):
                    s_ = tt % 2
                    tsl = slice(tt * 128, (tt + 1) * 128)
                    sc.op("act", lambda e, s_=s_, tsl=tsl: e.activation(
                        out=sq32[s_][:], in_=xT[:, :, tsl], func=AF.Square),
                        reads=[t_xT[c][tt] for c in range(NCH)], writes=[t_sq32[s_]])
                    b = next_ps()

                    def f(e, b=b, tsl=tsl, s_=s_):
                        for c in range(NCH):
                            e.matmul(ps[:, b, 0:8], lhsT=xT[:, c, tsl], rhs=wr[:, c, :],
                                     start=(c == 0), stop=(c == NCH - 1))
                        for c in range(NCH):
                            r = e.matmul(ps[:, b, 8:9], lhsT=sq32[s_][:, c, :],
                                         rhs=ones_f[:, 0:1], start=(c == 0), stop=(c == NCH - 1))
                        return r
                    sc.op("pe", f, reads=[t_wr, t_sq32[s_], t_ones] +
                          [t_xT[c][tt] for c in range(NCH)], writes=[t_ps[b]])
                    sc.op("dve", lambda e, b=b, tt=tt: e.tensor_copy(
                        out=rst[:, tt, :], in_=ps[:, b, 0:9]), reads=[t_ps[b]], writes=[t_rst])
                sc.op("act", lambda e: e.activation(
                    out=rs16[:, :, 0:1], in_=rst[:, :, 8:9], func=AF.Sqrt, scale=1.0 / D,
                    bias=1e-6), reads=[t_rst], writes=[t_rs16])
                sc.op("dve", lambda e: e.reciprocal(out=rs16[:, :, 1:2], in_=rs16[:, :, 0:1]),
                      reads=[t_rs16], writes=[t_rs16])
                sc.op("dve", lambda e: e.tensor_tensor(
                    out=L[:], in0=rst[:, :, 0:8],
                    in1=rs16[:, :, 1:2].to_broadcast([128, NT16, 8]), op=ALU.mult),
                    reads=[t_rst, t_rs16], writes=[t_L])
                for tt in range(NT16):
                    sc.op("dve", lambda e, tt=tt: e.max(out=v8[:, tt, :], in_=L[:, tt, :]),
                          reads=[t_L], writes=[t_v8])
                sc.op("dve", lambda e: e.tensor_tensor(
                    out=gate[:], in0=L[:], in1=v8[:, :, 1:2].to_broadcast([128, NT16, 8]),
                    op=ALU.is_ge), reads=[t_L, t_v8], writes=[t_gate])
                sc.op("dve", lambda e: e.tensor_tensor(
                    out=tmpr[:], in0=L[:], in1=v8[:, :, 0:1].to_broadcast([128, NT16, 8]),
                    op=ALU.subtract), reads=[t_L, t_v8], writes=[t_tmpr])
                sc.op("act", lambda e: e.activation(out=tmpr[:], in_=tmpr[:], func=AF.Exp),
                      reads=[t_tmpr], writes=[t_tmpr])
                sc.op("dve", lambda e: e.tensor_tensor(
                    out=rs16[:, :, 2:3], in0=v8[:, :, 1:2], in1=v8[:, :, 0:1], op=ALU.subtract),
                    reads=[t_v8], writes=[t_rs16])
                sc.op("act", lambda e: e.activation(out=rs16[:, :, 2:3], in_=rs16[:, :, 2:3],
                                                    func=AF.Exp),
                      reads=[t_rs16], writes=[t_rs16])
                sc.op("dve", lambda e: e.tensor_scalar(
                    out=rs16[:, :, 2:3], in0=rs16[:, :, 2:3], scalar1=1.0, scalar2=None,
                    op0=ALU.add), reads=[t_rs16], writes=[t_rs16])
                sc.op("dve", lambda e: e.reciprocal(out=rs16[:, :, 3:4], in_=rs16[:, :, 2:3]),
                      reads=[t_rs16], writes=[t_rs16])
                sc.op("dve", lambda e: e.tensor_tensor(
                    out=gate[:], in0=gate[:], in1=tmpr[:], op=ALU.mult),
                    reads=[t_gate, t_tmpr], writes=[t_gate])
                sc.op("dve", lambda e: e.tensor_tensor(
                    out=gate[:], in0=gate[:], in1=rs16[:, :, 3:4].to_broadcast([128, NT16, 8]),
                    op=ALU.mult), reads=[t_gate, t_rs16], writes=[t_gate])
                dgn = 0
                for ex in range(8):
                    gi = ex % 2
                    for q in range(NTT):
                        b = next_ps()
                        for i4 in range(4):
                            tt = q * 4 + i4
                            di = dgn % 2
                            dgn += 1
                            sc.op("dve", lambda e, di=di, tt=tt, ex=ex: e.tensor_tensor(
                                out=dg[di][:], in0=ident[:],
                                in1=gate[:, tt, ex:ex + 1].to_broadcast([128, 128]), op=ALU.mult),
                                reads=[t_ident, t_gate], writes=[t_dg[di]])
                            sc.op("pe", lambda e, b=b, i4=i4, di=di: e.matmul(
                                ps[:, b, i4 * 128:(i4 + 1) * 128], lhsT=ones_f[:], rhs=dg[di][:],
                                start=True, stop=True),
                                reads=[t_dg[di], t_ones], writes=[t_ps[b]])
                        sc.op("act", lambda e, b=b, gi=gi, q=q: e.activation(
                            out=G[gi][:, q * TT:(q + 1) * TT], in_=ps[:, b, :], func=AF.Copy),
                            reads=[t_ps[b]], writes=[t_G[gi]])
                    swiglu_stream(hT, t_hT, h1, t_h1, sg, t_sg, mg_d[ex], mu_d[ex], md_d[ex], DFFE,
                                  gate_bc=G[gi], t_gate=t_G[gi])

        if "c0" in phases:
            phase_conformer()
            sc.barrier()
        if "f0" in phases:
            phase_ffn()
            sc.barrier()
        if "g1" in phases:
            phase_gdn()
            sc.barrier()
        if "m1" in phases:
            phase_moe()
            sc.barrier()

        do_norm = "final" in phases
        with contextlib.ExitStack() as es2:
            xo = [sb("xo%d" % i, [128, D], F32, es2) for i in range(2)]
            fng = sb("fng_sb", [128, D], F32, es2)
            sc.op("sp", lambda e: e.dma_start(out=fng[:],
                                              in_=fng_d[0:1, :].partition_broadcast(128)),
                  writes=[t_fng], chan=ch_misc, after=sc.last_ops())
            yo = [sb("yo%d" % i, [128, D], F32, es2) for i in range(2)]
            sq = sb("sqj", [128, D], F32, es2)
            st = [sb("st%d" % i, [128, 4], F32, es2) for i in range(2)]
            t_xo = [Tok("xo0"), Tok("xo1")]
            t_yo = [Tok("yo0"), Tok("yo1")]
            t_sq2 = Tok("sq")
            t_st = [Tok("st0"), Tok("st1")]
            out_ops = []
            for tt in range(S // 128):
                sl = tt % 2
                for half in range(2):
                    b = next_ps()

                    def f(e, half=half, b=b, tt=tt):
                        for j in range(4):
                            c = half * 4 + j
                            r = e.transpose(out=ps[:, b, j * 128:(j + 1) * 128],
                                            in_=xT[:, c, tt * 128:(tt + 1) * 128],
                                            identity=ident[:])
                        return r
                    sc.op("pe", f, reads=[t_ident] + [t_xT[half * 4 + j][tt] for j in range(4)],
                          writes=[t_ps[b]])
                    dst = xo if do_norm else yo
                    t_dst = t_xo if do_norm else t_yo
                    sc.op("act", lambda e, half=half, b=b, sl=sl, dst=dst: e.activation(
                        out=dst[sl][:, half * 512:(half + 1) * 512], in_=ps[:, b, :], func=AF.Copy),
                        reads=[t_ps[b]], writes=[t_dst[sl]])
                if do_norm:
                    sc.op("act", lambda e, sl=sl: e.activation(
                        out=sq[:], in_=xo[sl][:], func=AF.Square, accum_out=st[sl][:, 0:1]),
                        reads=[t_xo[sl]], writes=[t_sq2, t_st[sl]])
                    sc.op("act", lambda e, sl=sl: e.activation(
                        out=st[sl][:, 1:2], in_=st[sl][:, 0:1], func=AF.Sqrt, scale=1.0 / D,
                        bias=1e-6), reads=[t_st[sl]], writes=[t_st[sl]])
                    sc.op("dve", lambda e, sl=sl: e.reciprocal(out=st[sl][:, 2:3],
                                                               in_=st[sl][:, 1:2]),
                          reads=[t_st[sl]], writes=[t_st[sl]])
                    sc.op("dve", lambda e, sl=sl: e.scalar_tensor_tensor(
                        out=yo[sl][:], in0=xo[sl][:], scalar=st[sl][:, 2:3], in1=fng[:],
                        op0=ALU.mult, op1=ALU.mult),
                        reads=[t_xo[sl], t_st[sl], t_fng], writes=[t_yo[sl]])
                o = sc.op("sp", lambda e, sl=sl, tt=tt: e.dma_start(
                    out=out_d[tt * 128:(tt + 1) * 128, :], in_=yo[sl][:]),
                    reads=[t_yo[sl]], chan=ch_out[sl])
                out_ops.append(o)
            fin = sc.op("sp", lambda e: e.nop())
            fin.is_nop = True
            fin.deps.extend(out_ops[-2:])
            sc.emit(nc)
    return nc


def pack_vecs(inp):
    rows = np.zeros((128, D), np.float32)
    rows[VR["mix0"]] = inp["mix_norm"][0]
    rows[VR["mix1"]] = inp["mix_norm"][1]
    rows[VR["ffn0"]] = inp["ffn_norm"][0]
    rows[VR["ffn1"]] = inp["ffn_norm"][1]
    rows[VR["pw1_ba"]] = inp["cf_pw1_b"][0, :D]
    rows[VR["pw1_bb"]] = inp["cf_pw1_b"][0, D:]
    rows[VR["dw_b"]] = inp["cf_dw_b"][0]
    rows[VR["ln_g"]] = inp["cf_ln_g"][0]
    rows[VR["ln_b"]] = inp["cf_ln_b"][0]
    rows[VR["pw2_b"]] = inp["cf_pw2_b"][0]
    rows[VR["dw_w"]:VR["dw_w"] + 31] = inp["cf_dw_w"][0]
    gc = inp["gdn_conv_w"][0]
    for k in range(5):
        for part in range(3):
            rows[VR["gconv"] + k * 3 + part] = gc[k, part * D:(part + 1) * D]
    return rows


def gdn_consts():
    i = np.arange(128)
    c = np.zeros((128, 2048), np.float32)
    c[:, 0:128] = (i[:, None] <= i[None, :])
    c[:, 128:256] = (i[:, None] >= i[None, :])
    for k in range(7):
        bsz = 1 << k
        I, J = i[:, None], i[None, :]
        m = ((I // (2 * bsz)) == (J // (2 * bsz))) & (((I // bsz) % 2) == 1) & (((J // bsz) % 2) == 0)
        c[:, 256 + k * 128:256 + (k + 1) * 128] = m.T
        c[:, 1152 + k * 128:1152 + (k + 1) * 128] = m
    return c


def make_in_maps(inp, phases, nb):
    x = np.ascontiguousarray(inp["x"], dtype=np.float32)
    vecs = pack_vecs(inp)
    fng = np.ascontiguousarray(inp["final_norm"], dtype=np.float32).reshape(1, D)
    base = {"vecs": vecs, "fng": fng}
    if "c0" in phases:
        base["cf_pw1_w"] = np.ascontiguousarray(inp["cf_pw1_w"][0])
        base["cf_pw2_w"] = np.ascontiguousarray(inp["cf_pw2_w"][0])
    if "f0" in phases:
        base["ffn_w_gate"] = np.ascontiguousarray(inp["ffn_w_gate"][0])
        base["ffn_w_up"] = np.ascontiguousarray(inp["ffn_w_up"][0])
        base["ffn_w_down"] = np.ascontiguousarray(inp["ffn_w_down"][0])
    if "g1" in phases:
        base["gdn_w_in"] = np.ascontiguousarray(inp["gdn_w_in"][0])
        base["gdn_w_out"] = np.ascontiguousarray(inp["gdn_w_out"][0])
        base["gsmall"] = np.concatenate([inp["gdn_a_log"][0].reshape(-1),
                                         inp["gdn_dt_bias"][0].reshape(-1),
                                         inp["gdn_o_norm"][0].reshape(-1)]).astype(np.float32)[None]
        base["gconst"] = gdn_consts()
    if "m1" in phases:
        base["moe_router"] = np.ascontiguousarray(inp["moe_router"][0])
        base["moe_w_gate"] = np.ascontiguousarray(inp["moe_w_gate"][0])
        base["moe_w_up"] = np.ascontiguousarray(inp["moe_w_up"][0])
        base["moe_w_down"] = np.ascontiguousarray(inp["moe_w_down"][0])
    return [dict(base, x=x[b]) for b in range(nb)]


def kernel(**inp):
    phases = ("c0", "f0", "g1", "m1", "final")
    nb = inp["x"].shape[0]
    nc = build_program(phases)
    in_maps = make_in_maps(inp, phases, nb)
    res = run_bass_kernel_spmd(nc, in_maps, core_ids=list(range(nb)))
    return np.stack([r["out"] for r in res.results], axis=0)
```

```python
import contextlib
import numpy as np
import concourse.bass as bass
import concourse.mybir as mybir
from concourse.bass_utils import run_bass_kernel_spmd

F32 = mybir.dt.float32
BF16 = mybir.dt.bfloat16
I32 = mybir.dt.int32
AF = mybir.ActivationFunctionType
ALU = mybir.AluOpType

D = 1024
S = 2048
NCH = D // 128
TT = 512
NTT = S // TT
ENGS = ("sp", "pe", "act", "dve", "pool")


class Tok:
    __slots__ = ("name", "w", "readers")

    def __init__(self, name):
        self.name = name
        self.w = None
        self.readers = []


class Op:
    __slots__ = ("eng", "fn", "deps", "chan", "has_dep", "sig", "name", "is_nop")

    def __init__(self, eng, fn, chan, name):
        self.eng = eng
        self.fn = fn
        self.deps = []
        self.chan = chan
        self.has_dep = False
        self.sig = None
        self.name = name
        self.is_nop = False


class Rec:
    def __init__(self):
        self.items = []

    def op(self, *a, **k):
        self.items.append((a, k))


class Sched:
    def __init__(self):
        self.ops = {e: [] for e in ENGS}
        self.nchan = 0

    def new_chan(self):
        self.nchan += 1
        return self.nchan - 1

    def last_real(self, e):
        for o in reversed(self.ops[e]):
            if not o.is_nop:
                return o
        return None

    def last_ops(self, engs=("pe", "act", "dve", "pool")):
        return [o for o in (self.last_real(e) for e in engs) if o is not None]

    def op(self, eng, fn, reads=(), writes=(), chan=None, name="", after=()):
        o = Op(eng, fn, chan, name)
        for d in after:
            o.deps.append(d)
            if d.chan is None:
                d.has_dep = True
        cand = []
        for t in reads:
            if t.w is not None:
                cand.append((t.w, "raw"))
        for t in writes:
            if t.w is not None:
                cand.append((t.w, "waw"))
            for r in t.readers:
                cand.append((r, "war"))
        seen = set()
        for d, kind in cand:
            if d is o or id(d) in seen:
                continue
            same = (d.eng == eng and d.chan is None and chan is None)
            if same and (eng == "pe" or kind == "war"):
                continue
            seen.add(id(d))
            o.deps.append(d)
            d.has_dep = True
        for t in reads:
            t.readers.append(o)
        for t in writes:
            t.w = o
            t.readers = []
        self.ops[eng].append(o)
        return o

    def barrier(self, engs=("pe", "act", "dve", "pool")):
        last = {e: self.last_real(e) for e in engs}
        for e in engs:
            o = Op(e, lambda eng: eng.nop(), None, "barrier")
            o.is_nop = True
            for e2, l in last.items():
                if e2 != e and l is not None:
                    o.deps.append(l)
                    if l.chan is None:
                        l.has_dep = True
            self.ops[e].append(o)

    def emit(self, nc, final_wait_ops=()):
        with contextlib.ExitStack() as es:
            EPOCH = 1000
            nsig = {e: sum(1 for o in self.ops[e] if o.chan is None and o.has_dep) for e in ENGS}
            esem = {e: [es.enter_context(nc.semaphore("s_%s%d" % (e, i)))
                        for i in range(nsig[e] // EPOCH + 1)] for e in ENGS}
            for e in ENGS:
                cnt = 0
                for o in self.ops[e]:
                    if o.chan is None and o.has_dep:
                        o.sig = (esem[e][cnt // EPOCH], cnt % EPOCH + 1, 1)
                        cnt += 1
            CEP = 100
            ccnt = [0] * self.nchan
            ntot = [0] * self.nchan
            for e in ENGS:
                for o in self.ops[e]:
                    if o.chan is not None:
                        ntot[o.chan] += 1
            csem = [[es.enter_context(nc.semaphore("c_%d_%d" % (i, k)))
                     for k in range(ntot[i] // CEP + 1)] for i in range(self.nchan)]
            for e in ENGS:
                for o in self.ops[e]:
                    if o.chan is not None:
                        k = ccnt[o.chan]
                        o.sig = (csem[o.chan][k // CEP], (k % CEP + 1) * 16, 16)
                        ccnt[o.chan] += 1
            block = es.enter_context(nc.Block())
            ops = self.ops

            def run(engname, eobj):
                waited = {}
                for o in ops[engname]:
                    need = {}
                    for d in o.deps:
                        sem, val, _ = d.sig
                        k = id(sem)
                        if val > waited.get(k, 0) and val > need.get(k, (None, 0))[1]:
                            need[k] = (sem, val)
                    for k, (sem, val) in need.items():
                        eobj.wait_ge(sem, val)
                        waited[k] = val
                    inst = o.fn(eobj)
                    if o.sig is not None:
                        inst.then_inc(o.sig[0], o.sig[2])

            @block.sync
            def _(e):
                run("sp", e)

            @block.tensor
            def _(e):
                run("pe", e)

            @block.scalar
            def _(e):
                run("act", e)

            @block.vector
            def _(e):
                run("dve", e)

            @block.gpsimd
            def _(e):
                run("pool", e)


VR = dict(mix0=0, mix1=1, ffn0=2, ffn1=3, pw1_ba=4, pw1_bb=5, dw_b=6, ln_g=7, ln_b=8, pw2_b=9,
          dw_w=10, gconv=41)
DFF = 2816
DFFE = 3584
USE_F32R = True
WB = 2048


def build_program(phases=("c0", "f0", "g1", "m1"), dbg=0, heads=tuple(range(8)), two_x=False):
    nc = bass.Bass("TRN2", target_bir_lowering=False, dynamic_dma_scratch_size=2048)

    def din(name, shape):
        return nc.dram_tensor(name, shape, F32, kind="ExternalInput").ap()

    x_d = din("x", [S, D])
    vecs_d = din("vecs", [128, D])
    fng_d = din("fng", [1, D])
    out_d = nc.dram_tensor("out", [S, D], F32, kind="ExternalOutput").ap()
    xacc_d = din("xacc", [S, D]) if two_x else None
    if "c0" in phases:
        w1_d = din("cf_pw1_w", [D, 2 * D])
        w2_d = din("cf_pw2_w", [D, D])
    if "f0" in phases:
        fg_d = din("ffn_w_gate", [D, DFF])
        fu_d = din("ffn_w_up", [D, DFF])
        fd_d = din("ffn_w_down", [DFF, D])

    if "g1" in phases:
        gin_d = din("gdn_w_in", [D, 4128])
        gout_d = din("gdn_w_out", [D, D])
        gsm_d = din("gsmall", [1, 160])
    if "m1" in phases:
        rt_d = din("moe_router", [D, 8])
        mg_d = din("moe_w_gate", [8, D, DFFE])
        mu_d = din("moe_w_up", [8, D, DFFE])
        md_d = din("moe_w_down", [8, DFFE, D])

    sc = Sched()
    with contextlib.ExitStack() as es:
        uniq = [0]

        def sb(name, shape, dt, stack=es):
            uniq[0] += 1
            return stack.enter_context(nc.sbuf_tensor("%s_%d" % (name, uniq[0]), shape, dt))

        xT = sb("xT", [128, NCH, S], F32)
        ident = sb("ident", [128, 128], F32)
        ones_f = sb("ones_f", [128, 128], F32)
        ones_b = sb("ones_b", [128, 128], BF16)
        vecs = sb("vecs_sb", [128, NCH, 64], F32)
        ps = es.enter_context(nc.psum_tensor("ps", [128, 8, 512], F32))
        stage, wbf, t_stage, t_wbf = [], [], [], []
        pool_fence = [[], 0]

        def set_pools(stack, nst, nwb, elems):
            stage[:] = [sb("stage%d" % i, [128, elems], F32, stack) for i in range(nst)]
            wbf[:] = [sb("wbf%d" % i, [128, elems], BF16, stack) for i in range(nwb)]
            t_stage[:] = [Tok("stage%d" % i) for i in range(nst)]
            t_wbf[:] = [Tok("wbf%d" % i) for i in range(nwb)]
            pool_fence[0] = sc.last_ops()
            pool_fence[1] = nst
        cst_d = din("gconst", [128, 2048]) if "g1" in phases else None
        sm = [sb("sm%d" % i, [128, TT], F32) for i in range(4)]
        t_sm = [Tok("sm%d" % i) for i in range(4)]
        smn = [0]

        def next_sm():
            i = smn[0] % 4
            smn[0] += 1
            return i

        t_xT = [[Tok("xT%d_%d" % (c, t)) for t in range(S // 128)] for c in range(NCH)]

        def xtoks(c, t512):
            return [t_xT[c][t512 * 4 + i] for i in range(4)]
        t_ident = Tok("ident")
        t_ones = Tok("ones")
        t_vecs = Tok("vecs")
        t_fng = Tok("fng")
        t_ps = [Tok("ps%d" % i) for i in range(8)]
        ch_stage = [sc.new_chan() for _ in range(2)]
        ch_xin = [sc.new_chan(), sc.new_chan()]
        ch_misc = sc.new_chan()
        ch_out = [sc.new_chan(), sc.new_chan()]
        psn = [0]
        stn = [0]
        wbn = [0]

        def next_ps():
            i = psn[0] % 8
            psn[0] += 1
            return i

        cast_cfg = ["act"]

        def load_block(src, view, cast_eng=None):
            cast_eng = cast_eng or cast_cfg[0]
            a, b = src.shape[1], src.shape[2]
            n = a * b
            s = stn[0] % len(stage)
            stn[0] += 1
            k = wbn[0] % len(wbf)
            wbn[0] += 1
            aft = ()
            if pool_fence[1] > 0:
                aft = pool_fence[0]
                pool_fence[1] -= 1
            st_, wb_, ts_, tw_ = stage[s], wbf[k], t_stage[s], t_wbf[k]
            sc.op("sp", lambda e: e.dma_start(
                out=st_[:, 0:n].rearrange("p (a b) -> p a b", a=a), in_=src),
                writes=[ts_], chan=ch_stage[s], after=aft)
            if cast_eng == "act":
                sc.op("act", lambda e: e.activation(out=wb_[:, 0:n], in_=st_[:, 0:n],
                                                    func=AF.Copy),
                      reads=[ts_], writes=[tw_])
            else:
                sc.op(cast_eng, lambda e: e.tensor_copy(out=wb_[:, 0:n], in_=st_[:, 0:n]),
                      reads=[ts_], writes=[tw_])
            return wb_[:, 0:n].rearrange("p (a b) -> p a b", a=a), tw_

        def fr0(ap):
            return ap.bitcast(mybir.dt.float32r) if USE_F32R else ap

        sc.op("pool", lambda e: e.memset(ones_f[:], 1.0), writes=[t_ones])
        sc.op("pool", lambda e: e.memset(ones_b[:], 1.0), writes=[t_ones])
        sc.op("pool", lambda e: e.affine_select(out=fr0(ident[:]), in_=ones_f[:], pattern=[[-1, 128]],
                                                compare_op=ALU.is_equal, fill=0.0, base=0,
                                                channel_multiplier=1),
              reads=[t_ones], writes=[t_ident])

        def load_x_loop(src_d, xin, t_xin, fence=()):
            for tt in range(S // 128):
                sl = tt % 2
                sc.op("sp", lambda e, tt=tt, sl=sl: e.dma_start(
                    out=xin[sl][:], in_=src_d[tt * 128:(tt + 1) * 128, :]),
                    writes=[t_xin[sl]], chan=ch_xin[sl], after=fence)
                for half in range(2):
                    b = next_ps()

                    def f(e, half=half, b=b, sl=sl):
                        for j in range(4):
                            c = half * 4 + j
                            r = e.transpose(out=ps[:, b, j * 128:(j + 1) * 128],
                                            in_=xin[sl][:, c * 128:(c + 1) * 128],
                                            identity=ident[:])
                        return r
                    sc.op("pe", f, reads=[t_xin[sl], t_ident], writes=[t_ps[b]])
                    eng = "dve" if half == 0 else "act"

                    def g(e, half=half, b=b, tt=tt, eng=eng):
                        o = xT[:, half * 4:half * 4 + 4, tt * 128:(tt + 1) * 128]
                        i = ps[:, b, :].rearrange("p (j r) -> p j r", j=4)
                        if eng == "dve":
                            return e.tensor_copy(out=o, in_=i)
                        return e.activation(out=o, in_=i, func=AF.Copy)
                    sc.op(eng, g, reads=[t_ps[b]],
                          writes=[t_xT[half * 4 + j][tt] for j in range(4)])


        with contextlib.ExitStack() as es1:
            xin = [sb("xin%d" % i, [128, D], F32, es1) for i in range(2)]
            t_xin = [Tok("xin0"), Tok("xin1")]
            sc.op("sp", lambda e: e.dma_start(out=xin[1][:], in_=vecs_d[:, :]), writes=[t_xin[1]],
                  chan=ch_xin[1])
            for half in range(2):
                b = next_ps()

                def f(e, half=half, b=b):
                    for j in range(4):
                        c = half * 4 + j
                        r = e.transpose(out=ps[:, b, j * 128:(j + 1) * 128],
                                        in_=xin[1][:, c * 128:(c + 1) * 128], identity=ident[:])
                    return r
                sc.op("pe", f, reads=[t_xin[1], t_ident], writes=[t_ps[b]])
                sc.op("dve", lambda e, half=half, b=b: e.tensor_copy(
                    out=vecs[:, half * 4:half * 4 + 4, :],
                    in_=ps[:, b, :].rearrange("p (j r) -> p j r", j=4)[:, :, 0:64]),
                    reads=[t_ps[b]], writes=[t_vecs])
            load_x_loop(x_d, xin, t_xin)
        sc.barrier()

        def vcol(c, r):
            return vecs[:, c, r:r + 1]

        def rmsnorm_fm(hT, t_hT, grow, sqtmp, t_sq):
            for t in range(NTT):
                tsl = slice(t * TT, (t + 1) * TT)
                sc.op("act", lambda e, tsl=tsl: e.activation(out=sqtmp[:], in_=xT[:, :, tsl],
                                                             func=AF.Square),
                      reads=[tk for c in range(NCH) for tk in xtoks(c, t)], writes=[t_sq])
                b = next_ps()

                def f(e, b=b):
                    for c in range(NCH):
                        r = e.matmul(ps[:, b, :], lhsT=ones_b[:], rhs=sqtmp[:, c, :],
                                     start=(c == 0), stop=(c == NCH - 1))
                    return r
                sc.op("pe", f, reads=[t_sq, t_ones], writes=[t_ps[b]])
                s0 = next_sm()
                sc.op("act", lambda e, b=b, s0=s0: e.activation(out=sm[s0][:], in_=ps[:, b, :],
                                                                func=AF.Sqrt, scale=1.0 / D,
                                                                bias=1e-6),
                      reads=[t_ps[b]], writes=[t_sm[s0]])
                s1 = next_sm()
                sc.op("dve", lambda e, s0=s0, s1=s1: e.reciprocal(out=sm[s1][:], in_=sm[s0][:]),
                      reads=[t_sm[s0]], writes=[t_sm[s1]])
                for c in range(NCH):
                    sc.op("dve", lambda e, c=c, tsl=tsl, s1=s1: e.scalar_tensor_tensor(
                        out=hT[:, c, tsl], in0=xT[:, c, tsl], scalar=vcol(c, grow), in1=sm[s1][:],
                        op0=ALU.mult, op1=ALU.mult),
                        reads=xtoks(c, t) + [t_vecs, t_sm[s1]], writes=[t_hT[c][t]])

        def dump(src_fn, toks_fn):
            for c in range(NCH):
                for t in range(NTT):
                    tsl = slice(t * TT, (t + 1) * TT)
                    sc.op("dve", lambda e, c=c, tsl=tsl: e.tensor_copy(out=xT[:, c, tsl],
                                                                       in_=src_fn(c, tsl)),
                          reads=toks_fn(c, t), writes=xtoks(c, t))

        def phase_conformer():
            with contextlib.ExitStack() as esp:
                set_pools(esp, 2, 6, WB)
                hT = sb("hT", [128, NCH, S], BF16, esp)
                t_hT = [[Tok("hT%d_%d" % (c, t)) for t in range(NTT)] for c in range(NCH)]
                sqtmp = sb("sqtmp", [128, NCH, TT], BF16, esp)
                t_sq = Tok("sqtmp")
                U = sb("ubuf", [128, NCH, S + 30], BF16, esp)
                t_U = [Tok("u%d" % c) for c in range(NCH)]
                diag = sb("diag", [128, 31, 128], BF16, esp)
                t_diag = Tok("diag")
                sig = [sb("sig%d" % i, [128, TT], F32, esp) for i in range(2)]
                t_sig = [Tok("sig0"), Tok("sig1")]
                lnst = [sb("lnst%d" % i, [128, TT], F32, esp) for i in range(3)]
                t_lnst = [Tok("lnst%d" % i) for i in range(3)]
                rmsnorm_fm(hT, t_hT, VR["mix0"], sqtmp, t_sq)
                if dbg == 1:
                    dump(lambda c, tsl: hT[:, c, tsl], lambda c, t: [t_hT[c][t]])
                    return
                sc.op("dve", lambda e: e.memset(U[:], 0.0), writes=t_U)
                sgn = 0
                for h in range(2):
                    wa = [load_block(w1_d[:, h * 512 + q * 256: h * 512 + (q + 1) * 256]
                                     .rearrange("(kc p) n -> p kc n", p=128), None) for q in range(2)]
                    wb_ = [load_block(w1_d[:, D + h * 512 + q * 256: D + h * 512 + (q + 1) * 256]
                                      .rearrange("(kc p) n -> p kc n", p=128), None) for q in range(2)]
                    for jj in range(4):
                        j = h * 4 + jj
                        wA, tA = wa[jj // 2]
                        wB, tB = wb_[jj // 2]
                        co = (jj % 2) * 128
                        for t in range(NTT):
                            tsl = slice(t * TT, (t + 1) * TT)
                            bA = next_ps()
                            bB = next_ps()

                            def f(e, wA=wA, wB=wB, co=co, bA=bA, bB=bB, tsl=tsl):
                                for kc in range(NCH):
                                    e.matmul(ps[:, bA, :], lhsT=wA[:, kc, co:co + 128],
                                             rhs=hT[:, kc, tsl], start=(kc == 0),
                                             stop=(kc == NCH - 1))
                                for kc in range(NCH):
                                    r = e.matmul(ps[:, bB, :], lhsT=wB[:, kc, co:co + 128],
                                                 rhs=hT[:, kc, tsl], start=(kc == 0),
                                                 stop=(kc == NCH - 1))
                                return r
                            sc.op("pe", f, reads=[tA, tB] + [t_hT[c][t] for c in range(NCH)],
                                  writes=[t_ps[bA], t_ps[bB]])
                            sg = sgn % 2
                            sgn += 1
                            sc.op("act", lambda e, bB=bB, sg=sg, j=j: e.activation(
                                out=sig[sg][:], in_=ps[:, bB, :], func=AF.Sigmoid,
                                bias=vcol(j, VR["pw1_bb"])),
                                reads=[t_ps[bB], t_vecs], writes=[t_sig[sg]])
                            sc.op("dve", lambda e, bA=bA, sg=sg, j=j, t=t: e.scalar_tensor_tensor(
                                out=U[:, j, 15 + t * TT:15 + (t + 1) * TT], in0=ps[:, bA, :],
                                scalar=vcol(j, VR["pw1_ba"]), in1=sig[sg][:],
                                op0=ALU.add, op1=ALU.mult),
                                reads=[t_ps[bA], t_sig[sg], t_vecs], writes=[t_U[j]])
                if dbg == 2:
                    dump(lambda c, tsl: U[:, c, 15 + tsl.start:15 + tsl.stop], lambda c, t: [t_U[c]])
                    return
                for j in range(NCH):
                    sc.op("dve", lambda e, j=j: e.tensor_tensor(
                        out=diag[:], in0=ident[:].unsqueeze(1).to_broadcast([128, 31, 128]),
                        in1=vecs[:, j, VR["dw_w"]:VR["dw_w"] + 31].unsqueeze(2)
                        .to_broadcast([128, 31, 128]), op=ALU.mult),
                        reads=[t_ident, t_vecs], writes=[t_diag])
                    for t in range(NTT):
                        b = next_ps()

                        def f(e, j=j, t=t, b=b):
                            for k in range(31):
                                r = e.matmul(ps[:, b, :], lhsT=diag[:, k, :],
                                             rhs=U[:, j, t * TT + k:t * TT + k + TT],
                                             start=(k == 0), stop=(k == 30))
                            return r
                        sc.op("pe", f, reads=[t_diag, t_U[j]], writes=[t_ps[b]])
                        sc.op("act", lambda e, j=j, t=t, b=b: e.activation(
                            out=hT[:, j, t * TT:(t + 1) * TT], in_=ps[:, b, :], func=AF.Identity,
                            bias=vcol(j, VR["dw_b"])),
                            reads=[t_ps[b], t_vecs], writes=[t_hT[j][t]])
                if dbg == 3:
                    dump(lambda c, tsl: hT[:, c, tsl], lambda c, t: [t_hT[c][t]])
                    return
                for t in range(NTT):
                    tsl = slice(t * TT, (t + 1) * TT)
                    sc.op("dve", lambda e, tsl=tsl: e.tensor_tensor(
                        out=sqtmp[:], in0=hT[:, :, tsl], in1=hT[:, :, tsl], op=ALU.mult),
                        reads=[t_hT[c][t] for c in range(NCH)], writes=[t_sq])
                    b1 = next_ps()
                    b2 = next_ps()

                    def f(e, b1=b1, b2=b2, tsl=tsl):
                        for c in range(NCH):
                            e.matmul(ps[:, b1, :], lhsT=ones_b[:], rhs=hT[:, c, tsl],
                                     start=(c == 0), stop=(c == NCH - 1))
                        for c in range(NCH):
                            r = e.matmul(ps[:, b2, :], lhsT=ones_b[:], rhs=sqtmp[:, c, :],
                                         start=(c == 0), stop=(c == NCH - 1))
                        return r
                    sc.op("pe", f, reads=[t_sq, t_ones] + [t_hT[c][t] for c in range(NCH)],
                          writes=[t_ps[b1], t_ps[b2]])
                    m = lnst[0]
                    q = lnst[1]
                    rs = lnst[2]
                    tm, tq, trs = t_lnst
                    sc.op("act", lambda e, m=m, b1=b1: e.activation(
                        out=m[:], in_=ps[:, b1, :], func=AF.Copy, scale=1.0 / D),
                        reads=[t_ps[b1]], writes=[tm])
                    sc.op("dve", lambda e, m=m, q=q: e.tensor_tensor(
                        out=q[:], in0=m[:], in1=m[:], op=ALU.mult),
                        reads=[tm], writes=[tq])
                    sc.op("dve", lambda e, q=q, b2=b2: e.scalar_tensor_tensor(
                        out=q[:], in0=ps[:, b2, :], scalar=1.0 / D, in1=q[:],
                        op0=ALU.mult, op1=ALU.subtract),
                        reads=[t_ps[b2], tq], writes=[tq])
                    sc.op("act", lambda e, q=q: e.activation(
                        out=q[:], in_=q[:], func=AF.Sqrt, bias=1e-5),
                        reads=[tq], writes=[tq])
                    sc.op("dve", lambda e, q=q, rs=rs: e.reciprocal(out=rs[:], in_=q[:]),
                          reads=[tq], writes=[trs])
                    sc.op("dve", lambda e, m=m, rs=rs: e.scalar_tensor_tensor(
                        out=m[:], in0=m[:], scalar=-1.0, in1=rs[:],
                        op0=ALU.mult, op1=ALU.mult),
                        reads=[tm, trs], writes=[tm])
                    for c in range(NCH):
                        w1s = next_sm()
                        sc.op("dve", lambda e, c=c, tsl=tsl, rs=rs, w1s=w1s: e.tensor_tensor(
                            out=sm[w1s][:], in0=hT[:, c, tsl], in1=rs[:], op=ALU.mult),
                            reads=[t_hT[c][t], trs], writes=[t_sm[w1s]])
                        sc.op("dve", lambda e, m=m, w1s=w1s: e.tensor_tensor(
                            out=sm[w1s][:], in0=sm[w1s][:], in1=m[:], op=ALU.add),
                            reads=[t_sm[w1s], tm], writes=[t_sm[w1s]])
                        sc.op("act", lambda e, c=c, tsl=tsl, w1s=w1s: e.activation(
                            out=hT[:, c, tsl], in_=sm[w1s][:], func=AF.Silu,
                            scale=vcol(c, VR["ln_g"]), bias=vcol(c, VR["ln_b"])),
                            reads=[t_sm[w1s], t_vecs], writes=[t_hT[c][t]])
                if dbg == 4:
                    dump(lambda c, tsl: hT[:, c, tsl], lambda c, t: [t_hT[c][t]])
                    return
                for h in range(2):
                    w2 = [load_block(w2_d[:, h * 512 + q * 256: h * 512 + (q + 1) * 256]
                                     .rearrange("(kc p) n -> p kc n", p=128), None) for q in range(2)]
                    for jj in range(4):
                        j = h * 4 + jj
                        wA, tA = w2[jj // 2]
                        co = (jj % 2) * 128
                        for t in range(NTT):
                            tsl = slice(t * TT, (t + 1) * TT)
                            b = next_ps()

                            def f(e, wA=wA, co=co, b=b, tsl=tsl):
                                for kc in range(NCH):
                                    r = e.matmul(ps[:, b, :], lhsT=wA[:, kc, co:co + 128],
                                                 rhs=hT[:, kc, tsl], start=(kc == 0),
                                                 stop=(kc == NCH - 1))
                                return r
                            sc.op("pe", f, reads=[tA] + [t_hT[c][t] for c in range(NCH)],
                                  writes=[t_ps[b]])
                            sc.op("dve", lambda e, b=b, j=j, tsl=tsl: e.scalar_tensor_tensor(
                                out=xT[:, j, tsl], in0=ps[:, b, :], scalar=vcol(j, VR["pw2_b"]),
                                in1=xT[:, j, tsl], op0=ALU.add, op1=ALU.add),
                                reads=[t_ps[b], t_vecs] + xtoks(j, t), writes=xtoks(j, t))

        def swiglu_stream(hT, t_hT, h1, t_h1, sg, t_sg, wg_d, wu_d, wd_d, dff,
                          gate_bc=None, t_gate=None):
            ngrp = (dff + 511) // 512
            sgn = 0
            for g in range(ngrp):
                c0 = g * 512
                ncol = min(512, dff - c0)
                nblk = ncol // 256
                nf = ncol // 128
                wg = [load_block(wg_d[:, c0 + q * 256:c0 + (q + 1) * 256]
                                 .rearrange("(kc p) n -> p kc n", p=128), None) for q in range(nblk)]
                wu = [load_block(wu_d[:, c0 + q * 256:c0 + (q + 1) * 256]
                                 .rearrange("(kc p) n -> p kc n", p=128), None) for q in range(nblk)]
                wd = [load_block(wd_d[c0 + q * 256:c0 + (q + 1) * 256, :]
                                 .rearrange("(fc p) n -> p fc n", p=128), None) for q in range(nblk)]
                for t in range(NTT):
                    tsl = slice(t * TT, (t + 1) * TT)
                    for f_ in range(nf):
                        wG, tG = wg[f_ // 2]
                        wU, tU = wu[f_ // 2]
                        co = (f_ % 2) * 128
                        bG = next_ps()
                        bU = next_ps()

                        def f(e, wG=wG, wU=wU, co=co, bG=bG, bU=bU, tsl=tsl):
                            for kc in range(NCH):
                                e.matmul(ps[:, bG, :], lhsT=wG[:, kc, co:co + 128],
                                         rhs=hT[:, kc, tsl], start=(kc == 0), stop=(kc == NCH - 1))
                            for kc in range(NCH):
                                r = e.matmul(ps[:, bU, :], lhsT=wU[:, kc, co:co + 128],
                                             rhs=hT[:, kc, tsl], start=(kc == 0),
                                             stop=(kc == NCH - 1))
                            return r
                        sc.op("pe", f, reads=[tG, tU] + [t_hT[c][t] for c in range(NCH)],
                              writes=[t_ps[bG], t_ps[bU]])
                        s_ = sgn % 2
                        sgn += 1
                        sc.op("act", lambda e, bG=bG, s_=s_: e.activation(
                            out=sg[s_][:], in_=ps[:, bG, :], func=AF.Silu),
                            reads=[t_ps[bG]], writes=[t_sg[s_]])
                        if gate_bc is None:
                            sc.op("dve", lambda e, bU=bU, s_=s_, f_=f_, tsl=tsl: e.tensor_tensor(
                                out=h1[:, f_, tsl], in0=ps[:, bU, :], in1=sg[s_][:], op=ALU.mult),
                                reads=[t_ps[bU], t_sg[s_]], writes=[t_h1[f_][t]])
                        else:
                            sc.op("dve", lambda e, s_=s_, tsl=tsl: e.tensor_tensor(
                                out=sg[s_][:], in0=sg[s_][:], in1=gate_bc[:, tsl], op=ALU.mult),
                                reads=[t_sg[s_], t_gate], writes=[t_sg[s_]])
                            sc.op("dve", lambda e, bU=bU, s_=s_, f_=f_, tsl=tsl: e.tensor_tensor(
                                out=h1[:, f_, tsl], in0=ps[:, bU, :], in1=sg[s_][:], op=ALU.mult),
                                reads=[t_ps[bU], t_sg[s_]], writes=[t_h1[f_][t]])
                for t in range(NTT):
                    tsl = slice(t * TT, (t + 1) * TT)
                    for j in range(NCH):
                        b = next_ps()

                        def f(e, b=b, j=j, tsl=tsl, wd=wd, nf=nf):
                            for f_ in range(nf):
                                wD, _ = wd[f_ // 2]
                                r = e.matmul(ps[:, b, :],
                                             lhsT=wD[:, f_ % 2, j * 128:(j + 1) * 128],
                                             rhs=h1[:, f_, tsl], start=(f_ == 0),
                                             stop=(f_ == nf - 1))
                            return r
                        sc.op("pe", f, reads=[w[1] for w in wd] + [t_h1[f_][t] for f_ in range(nf)],
                              writes=[t_ps[b]])
                        sc.op("dve", lambda e, b=b, j=j, tsl=tsl: e.tensor_tensor(
                            out=xT[:, j, tsl], in0=ps[:, b, :], in1=xT[:, j, tsl], op=ALU.add),
                            reads=[t_ps[b]] + xtoks(j, t), writes=xtoks(j, t))

        def phase_ffn():
            with contextlib.ExitStack() as esp:
                set_pools(esp, 2, 6, WB)
                hT = sb("hT", [128, NCH, S], BF16, esp)
                t_hT = [[Tok("hT%d_%d" % (c, t)) for t in range(NTT)] for c in range(NCH)]
                sqtmp = sb("sqtmp", [128, NCH, TT], BF16, esp)
                t_sq = Tok("sqtmp")
                h1 = sb("h1", [128, 4, S], BF16, esp)
                t_h1 = [[Tok("h1_%d_%d" % (f_, t)) for t in range(NTT)] for f_ in range(4)]
                sg = [sb("sg%d" % i, [128, TT], F32, esp) for i in range(2)]
                t_sg = [Tok("sg0"), Tok("sg1")]
                rmsnorm_fm(hT, t_hT, VR["ffn0"], sqtmp, t_sq)
                swiglu_stream(hT, t_hT, h1, t_h1, sg, t_sg, fg_d, fu_d, fd_d, DFF)


        F32R = mybir.dt.float32r

        def fr(ap):
            return ap.bitcast(F32R) if USE_F32R else ap

        def phase_gdn():
            NT16 = S // 128
            with contextlib.ExitStack() as esp:
                set_pools(esp, 2, 4, 1024)
                hT = sb("hT", [128, NCH, S], BF16, esp)
                t_hT = [[Tok("hT%d_%d" % (c, t)) for t in range(NTT)] for c in range(NCH)]
                with contextlib.ExitStack() as esq:
                    sqtmp = sb("sqtmp", [128, NCH, TT], BF16, esq)
                    t_sq = Tok("sqtmp")
                    rmsnorm_fm(hT, t_hT, VR["mix1"], sqtmp, t_sq)
                sc.barrier()
                if two_x:
                    with contextlib.ExitStack() as esx:
                        xin2 = [sb("xin2_%d" % i, [128, D], F32, esx) for i in range(2)]
                        t_xin2 = [Tok("xin2_0"), Tok("xin2_1")]
                        load_x_loop(xacc_d, xin2, t_xin2, fence=sc.last_ops())
                    sc.barrier()
                cst = sb("cst", [128, 2048], F32, esp)
                t_cst = Tok("cst")
                gsm = sb("gsm", [128, 160], F32, esp)
                t_gsm = Tok("gsm")
                graw = sb("graw", [128, NT16, 32], F32, esp)
                beta = sb("beta", [128, NT16, 16], F32, esp)
                la = sb("la", [128, NT16, 16], F32, esp)
                gcol = sb("gcol", [128, NT16, 16], F32, esp)
                glast = sb("glast", [128, NT16, 16], F32, esp)
                beg = sb("beg", [128, NT16, 16], F32, esp)
                kdc = graw[:, :, 16:32]
                egl = graw[:, :, 0:16]
                t_graw, t_beta, t_la, t_gcol, t_glast, t_beg = [
                    Tok(n) for n in ("graw", "beta", "la", "gcol", "glast", "beg")]
                t_kdc = t_egl = t_graw
                pre = sb("pre", [128, S + 4], BF16, esp)
                t_pre = Tok("pre")
                diag5 = sb("diag5", [128, 5, 128], BF16, esp)
                t_diag5 = Tok("diag5")
                QKV = [sb("qkv%d" % i, [128, S], F32, esp) for i in range(3)]
                t_QKV = [[Tok("qkv%d_%d" % (i, t)) for t in range(NTT)] for i in range(3)]
                Oacc = sb("Oacc", [128, NT16, 128], F32, esp)
                t_O = [Tok("O%d" % n) for n in range(NT16)]
                oT = pre[:, 2:2 + S]
                t_oT = [Tok("oT%d" % t) for t in range(NTT)]
                NW = 6
                names = ("bV", "Kt", "KD", "QDT", "AT", "PT", "Dm", "DTm", "X", "WK", "U")
                shp = dict(WK=[128, 256])
                TS = [{n: sb("%s_c%d" % (n, c_), shp.get(n, [128, 128]), F32, esp) for n in names}
                      for c_ in range(NW)]
                t_TS = [{n: Tok("%s_c%d" % (n, c_)) for n in names} for c_ in range(NW)]
                ETs = [sb("ETall%d" % c_, [128, 2, 128], F32, esp) for c_ in range(NW)]
                t_ETs = [[Tok("ET%d_0" % c_), Tok("ET%d_1" % c_)] for c_ in range(NW)]
                Ss = [sb("Sst%d" % c_, [128, 128], F32, esp) for c_ in range(2)]
                t_Ss = [Tok("S0"), Tok("S1")]
                T_ = {"sz": TS[0]["AT"], "yy": TS[1]["AT"]}
                t_T = {"sz": t_TS[0]["AT"], "yy": t_TS[1]["AT"]}
                def bank_pool(c_):
                    def nb_():
                        return c_
                    return nb_
                st16 = sb("st16", [128, 48], F32, esp)
                t_st16 = Tok("st16")
                ch_c = sc.new_chan()
                fence = sc.last_ops()
                sc.op("sp", lambda e: e.dma_start(out=cst[:], in_=cst_d[:, :]), writes=[t_cst],
                      chan=ch_c, after=fence)
                sc.op("sp", lambda e: e.dma_start(out=gsm[:],
                                                  in_=gsm_d[0:1, :].partition_broadcast(128)),
                      writes=[t_gsm], chan=ch_c, after=fence)
                cstr = sb("cstr", [128, 256], F32, esp)
                sc.op("act", lambda e: e.activation(out=fr(cstr[:]), in_=cst[:, 0:256], func=AF.Copy),
                      reads=[t_cst], writes=[t_cst])
                triX = [cstr[:, 0:128], cstr[:, 128:256]]
                MX = [cst[:, 256:256 + 896].rearrange("p (k i) -> p k i", k=7),
                      cst[:, 1152:1152 + 896].rearrange("p (k i) -> p k i", k=7)]
                sc.op("dve", lambda e: e.memset(pre[:], 0.0), writes=[t_pre])
                wgt, t_wgt = load_block(gin_d[:, 4096:4128].rearrange("(kc p) n -> p kc n", p=128),
                                        None)
                for n in range(NT16):
                    b = next_ps()
                    tsl = slice(n * 128, (n + 1) * 128)

                    def f(e, b=b, tsl=tsl):
                        for kc in range(NCH):
                            r = e.matmul(ps[:, b, 0:32], lhsT=hT[:, kc, tsl], rhs=wgt[:, kc, :],
                                         start=(kc == 0), stop=(kc == NCH - 1))
                        return r
                    sc.op("pe", f, reads=[t_wgt] + [t_hT[c][n // 4] for c in range(NCH)],
                          writes=[t_ps[b]])
                    sc.op("dve", lambda e, b=b, n=n: e.tensor_copy(out=graw[:, n, :],
                                                                   in_=ps[:, b, 0:32]),
                          reads=[t_ps[b]], writes=[t_graw])
                sc.op("act", lambda e: e.activation(out=beta[:], in_=graw[:, :, 0:16],
                                                    func=AF.Sigmoid),
                      reads=[t_graw], writes=[t_beta])
                sc.op("dve", lambda e: e.tensor_tensor(
                    out=la[:], in0=graw[:, :, 16:32],
                    in1=gsm[:, 16:32].unsqueeze(1).to_broadcast([128, NT16, 16]), op=ALU.add),
                    reads=[t_graw, t_gsm], writes=[t_la])
                sc.op("act", lambda e: e.activation(out=la[:], in_=la[:], func=AF.Exp),
                      reads=[t_la], writes=[t_la])
                sc.op("act", lambda e: e.activation(out=la[:], in_=la[:], func=AF.Ln, bias=1.0),
                      reads=[t_la], writes=[t_la])
                sc.op("act", lambda e: e.activation(out=gsm[:, 0:16], in_=gsm[:, 0:16],
                                                    func=AF.Exp),
                      reads=[t_gsm], writes=[t_gsm])
                sc.op("dve", lambda e: e.scalar_tensor_tensor(
                    out=la[:], in0=la[:], scalar=-1.0,
                    in1=gsm[:, 0:16].unsqueeze(1).to_broadcast([128, NT16, 16]),
                    op0=ALU.mult, op1=ALU.mult), reads=[t_la, t_gsm], writes=[t_la])
                for n in range(NT16):
                    b = next_ps()

                    def f(e, b=b, n=n):
                        e.matmul(ps[:, b, 0:8], lhsT=triX[0], rhs=la[:, n, 0:8], start=True,
                                 stop=True)
                        e.matmul(ps[:, b, 8:16], lhsT=triX[1], rhs=la[:, n, 8:16], start=True,
                                 stop=True)
                        return e.matmul(ps[:, b, 16:32], lhsT=ones_f[:], rhs=la[:, n, :],
                                        start=True, stop=True)
                    sc.op("pe", f, reads=[t_la, t_cst, t_ones], writes=[t_ps[b]])
                    sc.op("dve", lambda e, b=b, n=n: e.tensor_copy(out=gcol[:, n, :],
                                                                   in_=ps[:, b, 0:16]),
                          reads=[t_ps[b]], writes=[t_gcol])
                    sc.op("dve", lambda e, b=b, n=n: e.tensor_copy(out=glast[:, n, :],
                                                                   in_=ps[:, b, 16:32]),
                          reads=[t_ps[b]], writes=[t_glast])
                sc.op("act", lambda e: e.activation(out=beg[:], in_=gcol[:], func=AF.Exp),
                      reads=[t_gcol], writes=[t_beg])
                sc.op("dve", lambda e: e.tensor_tensor(out=beg[:], in0=beg[:], in1=beta[:],
                                                       op=ALU.mult),
                      reads=[t_beg, t_beta], writes=[t_beg])
                sc.op("dve", lambda e: e.tensor_tensor(out=kdc[:], in0=glast[:], in1=gcol[:],
                                                       op=ALU.subtract),
                      reads=[t_glast, t_gcol], writes=[t_kdc])
                sc.op("act", lambda e: e.activation(out=kdc[:], in_=kdc[:], func=AF.Exp),
                      reads=[t_kdc], writes=[t_kdc])
                sc.op("act", lambda e: e.activation(out=egl[:], in_=glast[:], func=AF.Exp),
                      reads=[t_glast], writes=[t_egl])

                def tt_(n, out, in0, in1, op, eng="dve", rd=(), wr=()):
                    sc.op(eng, lambda e: e.tensor_tensor(out=out, in0=in0, in1=in1, op=op),
                          reads=list(rd), writes=list(wr))

                if dbg == 11:
                    return
                for h in heads:
                    for part in range(3):
                        cidx = part * 8 + h
                        wv, t_wv = load_block(gin_d[:, cidx * 128:(cidx + 1) * 128]
                                              .rearrange("(kc p) n -> p kc n", p=128), None)
                        for k in range(5):
                            sc.op("dve", lambda e, k=k, part=part, h=h: e.tensor_scalar(
                                out=diag5[:, k, :], in0=ident[:],
                                scalar1=vcol(h, VR["gconv"] + k * 3 + part), scalar2=None,
                                op0=ALU.mult), reads=[t_ident, t_vecs], writes=[t_diag5])
                        for t in range(NTT):
                            tsl = slice(t * TT, (t + 1) * TT)
                            b = next_ps()

                            def f(e, b=b, tsl=tsl, wv=wv):
                                for kc in range(NCH):
                                    r = e.matmul(ps[:, b, :], lhsT=wv[:, kc, :], rhs=hT[:, kc, tsl],
                                                 start=(kc == 0), stop=(kc == NCH - 1))
                                return r
                            sc.op("pe", f, reads=[t_wv] + [t_hT[c][t] for c in range(NCH)],
                                  writes=[t_ps[b]])
                            sc.op("act", lambda e, b=b, t=t: e.activation(
                                out=pre[:, 2 + t * TT:2 + (t + 1) * TT], in_=ps[:, b, :],
                                func=AF.Copy), reads=[t_ps[b]], writes=[t_pre])
                        for t in range(NTT):
                            tsl = slice(t * TT, (t + 1) * TT)
                            b = next_ps()

                            def f(e, b=b, t=t):
                                for k in range(5):
                                    r = e.matmul(ps[:, b, :], lhsT=diag5[:, k, :],
                                                 rhs=pre[:, t * TT + k:t * TT + k + TT],
                                                 start=(k == 0), stop=(k == 4))
                                return r
                            sc.op("pe", f, reads=[t_diag5, t_pre], writes=[t_ps[b]])
                            sc.op("act", lambda e, b=b, tsl=tsl, part=part: e.activation(
                                out=fr(QKV[part][:, tsl]), in_=ps[:, b, :], func=AF.Silu),
                                reads=[t_ps[b]], writes=[t_QKV[part][t]])
                            if part < 2:
                                la_ = next_sm()
                                lb_ = next_sm()
                                sc.op("dve", lambda e, tsl=tsl, part=part, la_=la_: e.tensor_tensor(
                                    out=sm[la_][:], in0=QKV[part][:, tsl], in1=QKV[part][:, tsl],
                                    op=ALU.mult), reads=[t_QKV[part][t]], writes=[t_sm[la_]])
                                b2 = next_ps()
                                sc.op("pe", lambda e, b2=b2, la_=la_: e.matmul(
                                    ps[:, b2, :], lhsT=ones_f[:], rhs=sm[la_][:], start=True,
                                    stop=True), reads=[t_sm[la_], t_ones], writes=[t_ps[b2]])
                                sc.op("act", lambda e, b2=b2, lb_=lb_: e.activation(
                                    out=sm[lb_][:], in_=ps[:, b2, :], func=AF.Sqrt, bias=1e-6),
                                    reads=[t_ps[b2]], writes=[t_sm[lb_]])
                                sc.op("dve", lambda e, lb_=lb_: e.reciprocal(out=sm[lb_][:],
                                                                             in_=sm[lb_][:]),
                                      reads=[t_sm[lb_]], writes=[t_sm[lb_]])
                                scl = (128.0 ** -0.5) if part == 0 else 1.0
                                sc.op("dve", lambda e, tsl=tsl, part=part, scl=scl, lb_=lb_:
                                      e.scalar_tensor_tensor(
                                          out=fr(QKV[part][:, tsl]), in0=QKV[part][:, tsl], scalar=scl,
                                          in1=sm[lb_][:], op0=ALU.mult, op1=ALU.mult),
                                      reads=[t_QKV[part][t], t_sm[lb_]], writes=[t_QKV[part][t]])
                    if dbg == 12:
                        return
                    zwv, t_zwv = load_block(gin_d[:, 3072 + h * 128:3072 + (h + 1) * 128]
                                            .rearrange("(kc p) n -> p kc n", p=128), None)
                    QT, KT, VT = QKV
                    def chain(dr, T_, t_T, ETall, t_ET, Sst, t_S, next_ps, sc, chunks, do_memset):
                        if do_memset:
                            sc.op("act", lambda e: e.activation(out=fr(Sst[:]), in_=ones_f[:], func=AF.Copy,
                                                                scale=0.0),
                                  reads=[t_ones], writes=[t_S])
                        order = chunks
                        dh = dr * 8 + h
                        for n in order:
                            csl = slice(n * 128, (n + 1) * 128)
                            t4 = n // 4
                            rq = [t_QKV[0][t4]]
                            rk = [t_QKV[1][t4]]
                            rv = [t_QKV[2][t4]]
                            bt = next_ps()

                            def f(e, bt=bt, csl=csl):
                                e.transpose(out=ps[:, bt, 0:128], in_=KT[:, csl], identity=ident[:])
                                return e.transpose(out=ps[:, bt, 128:256], in_=VT[:, csl],
                                                   identity=ident[:])
                            sc.op("pe", f, reads=rk + rv + [t_ident], writes=[t_ps[bt]])
                            sc.op("act", lambda e, bt=bt, n=n, dh=dh: e.activation(
                                out=fr(T_["bV"][:]), in_=ps[:, bt, 128:256], func=AF.Copy,
                                scale=beta[:, n, dh:dh + 1]),
                                reads=[t_ps[bt], t_beta], writes=[t_T["bV"]])
                            sc.op("act", lambda e, bt=bt, n=n, dh=dh: e.activation(
                                out=fr(T_["Kt"][:]), in_=ps[:, bt, 0:128], func=AF.Copy,
                                scale=beg[:, n, dh:dh + 1]),
                                reads=[t_ps[bt], t_beg], writes=[t_T["Kt"]])
                            sc.op("act", lambda e, bt=bt, n=n, dh=dh: e.activation(
                                out=fr(T_["KD"][:]), in_=ps[:, bt, 0:128], func=AF.Copy,
                                scale=kdc[:, n, dh:dh + 1]),
                                reads=[t_ps[bt], t_kdc], writes=[t_T["KD"]])
                            sc.op("act", lambda e, n=n, dh=dh: e.activation(
                                out=fr(T_["X"][:]), in_=ones_f[:], func=AF.Copy,
                                scale=la[:, n, dh:dh + 1]),
                                reads=[t_la, t_ones], writes=[t_T["X"]])
                            sc.op("act", lambda e, n=n, dh=dh: e.activation(
                                out=fr(T_["U"][:]), in_=ones_f[:], func=AF.Copy,
                                scale=beta[:, n, dh:dh + 1]),
                                reads=[t_beta, t_ones], writes=[t_T["U"]])
                            bm = next_ps()

                            def f(e, bm=bm, csl=csl, dr=dr):
                                e.matmul(ps[:, bm, 0:128], lhsT=fr(T_["X"][:]), rhs=fr(triX[dr]),
                                         start=True, stop=True)
                                e.matmul(ps[:, bm, 128:256], lhsT=fr(T_["U"][:]), rhs=fr(ident[:]),
                                         start=True, stop=True)
                                e.matmul(ps[:, bm, 256:384], lhsT=fr(KT[:, csl]), rhs=fr(KT[:, csl]),
                                         start=True, stop=True)
                                return e.matmul(ps[:, bm, 384:512], lhsT=fr(KT[:, csl]),
                                                rhs=fr(QT[:, csl]), start=True, stop=True)
                            sc.op("pe", f, reads=[t_T["X"], t_T["U"], t_cst, t_ident] + rk + rq,
                                  writes=[t_ps[bm]])
                            sc.op("dve", lambda e, bm=bm, n=n, dh=dh: e.tensor_scalar(
                                out=fr(T_["X"][:]), in0=ps[:, bm, 0:128],
                                scalar1=gcol[:, n, dh:dh + 1], scalar2=0.0, op0=ALU.subtract,
                                op1=ALU.min), reads=[t_ps[bm], t_gcol], writes=[t_T["X"]])
                            sc.op("act", lambda e: e.activation(out=fr(T_["X"][:]), in_=T_["X"][:],
                                                                func=AF.Exp),
                                  reads=[t_T["X"]], writes=[t_T["X"]])
                            sc.op("act", lambda e, bm=bm: e.activation(
                                out=fr(T_["QDT"][:]), in_=ps[:, bm, 0:128], func=AF.Exp),
                                reads=[t_ps[bm]], writes=[t_T["QDT"]])
                            sc.op("dve", lambda e, csl=csl: e.tensor_tensor(
                                out=fr(T_["QDT"][:]), in0=T_["QDT"][:], in1=QT[:, csl], op=ALU.mult),
                                reads=[t_T["QDT"]] + rq, writes=[t_T["QDT"]])
                            sc.op("dve", lambda e, bm=bm: e.tensor_tensor(
                                out=T_["AT"][:], in0=ps[:, bm, 256:384], in1=T_["X"][:],
                                op=ALU.mult), reads=[t_ps[bm], t_T["X"]], writes=[t_T["AT"]])
                            sc.op("dve", lambda e, bm=bm: e.tensor_tensor(
                                out=T_["AT"][:], in0=ps[:, bm, 128:256], in1=T_["AT"][:],
                                op=ALU.mult), reads=[t_ps[bm], t_T["AT"]], writes=[t_T["AT"]])
                            sc.op("dve", lambda e, bm=bm: e.tensor_tensor(
                                out=fr(T_["PT"][:]), in0=ps[:, bm, 384:512], in1=T_["X"][:],
                                op=ALU.mult), reads=[t_ps[bm], t_T["X"]], writes=[t_T["PT"]])
                            sc.op("dve", lambda e, dr=dr: e.tensor_tensor(
                                out=fr(T_["PT"][:]), in0=T_["PT"][:], in1=triX[dr], op=ALU.mult),
                                reads=[t_T["PT"], t_cst], writes=[t_T["PT"]])
                            if dbg == 13:
                                return
                            for lv in range(7):
                                Dc = ident if lv == 0 else T_["Dm"]
                                DTc = ident if lv == 0 else T_["DTm"]
                                rD = [t_ident] if lv == 0 else [t_T["Dm"]]
                                rDT = [t_ident] if lv == 0 else [t_T["DTm"]]
                                bx = next_ps()
                                if lv == 0:
                                    sc.op("dve", lambda e, dr=dr: e.tensor_tensor(
                                        out=fr(ETall[:, 0, :]), in0=T_["AT"][:], in1=MX[dr][:, 0, :],
                                        op=ALU.mult), reads=[t_T["AT"], t_cst], writes=[t_ET[0]])
                                sc.op("pe", lambda e, bx=bx, lv=lv, Dc=Dc: e.matmul(
                                    ps[:, bx, 0:128], lhsT=fr(ETall[:, lv % 2, :]), rhs=fr(Dc[:]), start=True,
                                    stop=True), reads=[t_ET[lv % 2]] + rD, writes=[t_ps[bx]])
                                if lv < 6:
                                    sc.op("dve", lambda e, dr=dr, lv=lv: e.tensor_tensor(
                                        out=fr(ETall[:, (lv + 1) % 2, :]), in0=T_["AT"][:],
                                        in1=MX[dr][:, lv + 1, :], op=ALU.mult),
                                        reads=[t_T["AT"], t_cst], writes=[t_ET[(lv + 1) % 2]])
                                sc.op("act", lambda e, bx=bx: e.activation(
                                    out=fr(T_["X"][:]), in_=ps[:, bx, 0:128], func=AF.Copy),
                                    reads=[t_ps[bx]], writes=[t_T["X"]])
                                by = next_ps()

                                def f(e, by=by, Dc=Dc, DTc=DTc):
                                    e.matmul(ps[:, by, 0:128], lhsT=fr(DTc[:]), rhs=fr(T_["X"][:]),
                                             start=True, stop=True)
                                    return e.matmul(ps[:, by, 128:256], lhsT=fr(T_["X"][:]), rhs=fr(DTc[:]),
                                                    start=True, stop=True)
                                sc.op("pe", f, reads=[t_T["X"]] + rDT, writes=[t_ps[by]])
                                sc.op("dve", lambda e, by=by, Dc=Dc: e.tensor_tensor(
                                    out=fr(T_["Dm"][:]), in0=Dc[:], in1=ps[:, by, 0:128],
                                    op=ALU.subtract), reads=[t_ps[by]] + rD, writes=[t_T["Dm"]])
                                sc.op("dve", lambda e, by=by, DTc=DTc: e.tensor_tensor(
                                    out=fr(T_["DTm"][:]), in0=DTc[:], in1=ps[:, by, 128:256],
                                    op=ALU.subtract), reads=[t_ps[by]] + rDT, writes=[t_T["DTm"]])
                            if dbg == 14:
                                return
                            bw = next_ps()

                            def f(e, bw=bw):
                                e.matmul(ps[:, bw, 0:128], lhsT=fr(T_["DTm"][:]), rhs=fr(T_["bV"][:]),
                                         start=True, stop=True)
                                return e.matmul(ps[:, bw, 128:256], lhsT=fr(T_["Kt"][:]),
                                                rhs=fr(T_["DTm"][:]), start=True, stop=True)
                            sc.op("pe", f, reads=[t_T["DTm"], t_T["bV"], t_T["Kt"]],
                                  writes=[t_ps[bw]])
                            sc.op("act", lambda e, bw=bw: e.activation(
                                out=fr(T_["WK"][:]), in_=ps[:, bw, 0:256], func=AF.Copy),
                                reads=[t_ps[bw]], writes=[t_T["WK"]])
                            if dbg == 21:
                                return
                            bs = next_ps()
                            sc.op("pe", lambda e, bs=bs: e.matmul(
                                ps[:, bs, 0:128], lhsT=fr(T_["WK"][:, 128:256]), rhs=fr(Sst[:]), start=True,
                                stop=True), reads=[t_T["WK"], t_S], writes=[t_ps[bs]])
                            sc.op("dve", lambda e, bs=bs: e.tensor_tensor(
                                out=fr(T_["U"][:]), in0=T_["WK"][:, 0:128], in1=ps[:, bs, 0:128],
                                op=ALU.subtract), reads=[t_ps[bs], t_T["WK"]], writes=[t_T["U"]])
                            if dbg == 22:
                                return
                            bo = next_ps()

                            def f(e, bo=bo):
                                e.matmul(ps[:, bo, 0:128], lhsT=fr(T_["QDT"][:]), rhs=fr(Sst[:]), start=True,
                                         stop=False)
                                e.matmul(ps[:, bo, 0:128], lhsT=fr(T_["PT"][:]), rhs=fr(T_["U"][:]),
                                         start=False, stop=True)
                                return e.matmul(ps[:, bo, 128:256], lhsT=fr(T_["KD"][:]), rhs=fr(T_["U"][:]),
                                                start=True, stop=True)
                            sc.op("pe", f, reads=[t_T["QDT"], t_S, t_T["PT"], t_T["U"], t_T["KD"]],
                                  writes=[t_ps[bo]])
                            if dbg == 23:
                                return
                            sc.op("dve", lambda e, bo=bo, n=n: e.tensor_tensor(
                                out=Oacc[:, n, :], in0=ps[:, bo, 0:128], in1=Oacc[:, n, :],
                                op=ALU.add), reads=[t_ps[bo], t_O[n]], writes=[t_O[n]])
                            sc.op("act", lambda e, n=n, dh=dh: e.activation(
                                out=fr(Sst[:]), in_=Sst[:], func=AF.Copy, scale=egl[:, n, dh:dh + 1]),
                                reads=[t_S, t_egl], writes=[t_S])
                            sc.op("dve", lambda e, bo=bo: e.tensor_tensor(
                                out=fr(Sst[:]), in0=ps[:, bo, 128:256], in1=Sst[:], op=ALU.add),
                                reads=[t_ps[bo], t_S], writes=[t_S])
                    sc.op("dve", lambda e: e.memset(Oacc[:], 0.0), writes=t_O)
                    recs = [Rec() for _ in range(NW)]
                    NPD = NW // 2
                    for w_ in range(NW):
                        dr_, par_ = w_ % 2, w_ // 2
                        full = list(range(NT16)) if dr_ == 0 else list(range(NT16 - 1, -1, -1))
                        chain(dr_, TS[w_], t_TS[w_], ETs[w_], t_ETs[w_], Ss[dr_], t_Ss[dr_],
                              bank_pool(w_), recs[w_], full[par_::NPD], par_ == 0)
                    per_inst = len(recs[NW - 1].items) // len(range(NT16)[NPD - 1::NPD])
                    offs = [(w_ // 2) * (per_inst // NPD + 1) for w_ in range(NW)]
                    tot = max(len(r.items) + o_ for r, o_ in zip(recs, offs))
                    for i_ in range(tot):
                        for r, o_ in zip(recs, offs):
                            j_ = i_ - o_
                            if 0 <= j_ < len(r.items):
                                a_, k_ = r.items[j_]
                                sc.op(*a_, **k_)
                    for n in range(NT16):
                        sc.op("act", lambda e, n=n: e.activation(
                            out=T_["yy"][:], in_=Oacc[:, n, :], func=AF.Square,
                            accum_out=st16[:, n:n + 1]), reads=[t_O[n]],
                            writes=[t_T["yy"], t_st16])
                    sc.op("act", lambda e: e.activation(
                        out=st16[:, 16:32], in_=st16[:, 0:16], func=AF.Sqrt, scale=1.0 / 128,
                        bias=1e-6), reads=[t_st16], writes=[t_st16])
                    sc.op("dve", lambda e: e.reciprocal(out=st16[:, 32:48], in_=st16[:, 16:32]),
                          reads=[t_st16], writes=[t_st16])
                    for q in range(NTT):
                        oq = Oacc[:, q * 4:(q + 1) * 4, :]
                        tq = [t_O[q * 4 + i] for i in range(4)]
                        sc.op("dve", lambda e, q=q, oq=oq: e.tensor_tensor(
                            out=oq, in0=oq,
                            in1=st16[:, 32 + q * 4:36 + q * 4].unsqueeze(2).to_broadcast([128, 4, 128]),
                            op=ALU.mult), reads=tq + [t_st16], writes=tq)
                        sc.op("dve", lambda e, oq=oq: e.tensor_tensor(
                            out=oq, in0=oq,
                            in1=gsm[:, 32:160].unsqueeze(1).to_broadcast([128, 4, 128]),
                            op=ALU.mult), reads=tq + [t_gsm], writes=tq)
                        bz = next_ps()

                        def f(e, bz=bz, q=q, zwv=zwv):
                            for i4 in range(4):
                                n = q * 4 + i4
                                for kc in range(NCH):
                                    r = e.matmul(ps[:, bz, i4 * 128:(i4 + 1) * 128],
                                                 lhsT=hT[:, kc, n * 128:(n + 1) * 128],
                                                 rhs=zwv[:, kc, :], start=(kc == 0),
                                                 stop=(kc == NCH - 1))
                            return r
                        sc.op("pe", f, reads=[t_zwv] + [t_hT[c][q] for c in range(NCH)],
                              writes=[t_ps[bz]])
                        s_ = next_sm()
                        sc.op("act", lambda e, bz=bz, s_=s_: e.activation(
                            out=sm[s_][:], in_=ps[:, bz, :], func=AF.Silu),
                            reads=[t_ps[bz]], writes=[t_sm[s_]])
                        sc.op("dve", lambda e, oq=oq, s_=s_: e.tensor_tensor(
                            out=oq, in0=oq, in1=sm[s_][:].rearrange("p (a b) -> p a b", a=4),
                            op=ALU.mult), reads=tq + [t_sm[s_]], writes=tq)
                        bT = next_ps()

                        def f2(e, bT=bT, q=q):
                            for i4 in range(4):
                                r = e.transpose(out=ps[:, bT, i4 * 128:(i4 + 1) * 128],
                                                in_=Oacc[:, q * 4 + i4, :], identity=ident[:])
                            return r
                        sc.op("pe", f2, reads=tq + [t_ident], writes=[t_ps[bT]])
                        sc.op("act", lambda e, bT=bT, q=q: e.activation(
                            out=oT[:, q * TT:(q + 1) * TT], in_=ps[:, bT, :], func=AF.Copy),
                            reads=[t_ps[bT]], writes=[t_oT[q], t_pre])
                    if dbg == 17:
                        return
                    wo, t_wo = load_block(gout_d[h * 128:(h + 1) * 128, :]
                                          .rearrange("p (a n) -> p a n", a=1), None)
                    for t in range(NTT):
                        tsl = slice(t * TT, (t + 1) * TT)
                        for j in range(NCH):
                            b = next_ps()
                            sc.op("pe", lambda e, b=b, j=j, tsl=tsl, wo=wo: e.matmul(
                                ps[:, b, :], lhsT=wo[:, 0, j * 128:(j + 1) * 128], rhs=oT[:, tsl],
                                start=True, stop=True), reads=[t_wo, t_oT[t], t_pre], writes=[t_ps[b]])
                            sc.op("dve", lambda e, b=b, j=j, tsl=tsl: e.tensor_tensor(
                                out=xT[:, j, tsl], in0=ps[:, b, :], in1=xT[:, j, tsl], op=ALU.add),
                                reads=[t_ps[b]] + xtoks(j, t), writes=xtoks(j, t))
                    if dbg == 30 + h:
                        return


        def phase_moe():
            NT16 = S // 128
            with contextlib.ExitStack() as esp:
                set_pools(esp, 2, 6, WB)
                hT = sb("hT", [128, NCH, S], BF16, esp)
                t_hT = [[Tok("hT%d_%d" % (c, t)) for t in range(NTT)] for c in range(NCH)]
                sqtmp = sb("sqtmp", [128, NCH, TT], BF16, esp)
                t_sq = Tok("sqtmp")
                h1 = sb("h1", [128, 4, S], BF16, esp)
                t_h1 = [[Tok("h1_%d_%d" % (f_, t)) for t in range(NTT)] for f_ in range(4)]
                sg = [sb("sg%d" % i, [128, TT], F32, esp) for i in range(2)]
                t_sg = [Tok("sg0"), Tok("sg1")]
                G = [sb("G%d" % i, [128, S], F32, esp) for i in range(2)]
                t_G = [Tok("G0"), Tok("G1")]
                wr = sb("wr", [128, NCH, 8], F32, esp)
                t_wr = Tok("wr")
                sq32 = [sb("sq32_%d" % i, [128, NCH, 128], F32, esp) for i in range(2)]
                t_sq32 = [Tok("sq32_0"), Tok("sq32_1")]
                rst = sb("rst", [128, NT16, 9], F32, esp)
                t_rst = Tok("rst")
                L = sb("L", [128, NT16, 8], F32, esp)
                v8 = sb("v8", [128, NT16, 8], F32, esp)
                gate = sb("gate", [128, NT16, 8], F32, esp)
                tmpr = sb("tmpr", [128, NT16, 8], F32, esp)
                rs16 = sb("rs16", [128, NT16, 4], F32, esp)
                dg = [sb("dg%d" % i, [128, 128], F32, esp) for i in range(2)]
                t_dg = [Tok("dg0"), Tok("dg1")]
                t_L, t_v8, t_gate, t_tmpr, t_rs16 = (Tok("L"), Tok("v8"), Tok("gate"),
                                                     Tok("tmpr"), Tok("rs16"))
                ch_wr = sc.new_chan()
                rmsnorm_fm(hT, t_hT, VR["ffn1"], sqtmp, t_sq)
                sc.op("sp", lambda e: e.dma_start(
                    out=wr[:], in_=rt_d.rearrange("(c p) e -> p c e", p=128)),
                    writes=[t_wr], chan=ch_wr, after=sc.last_ops())
                for c in range(NCH):
                    sc.op("dve", lambda e, c=c: e.tensor_scalar(
                        out=wr[:, c, :], in0=wr[:, c, :], scalar1=vcol(c, VR["ffn1"]),
                        scalar2=None, op0=ALU.mult), reads=[t_wr, t_vecs], writes=[t_wr])
                for tt in range(NT16):
                    s_ = tt % 2
                    tsl = slice(tt * 128, (tt + 1) * 128)
                    sc.op("act", lambda e, s_=s_, tsl=tsl: e.activation(
                        out=sq32[s_][:], in_=xT[:, :, tsl], func=AF.Square),
                        reads=[t_xT[c][tt] for c in range(NCH)], writes=[t_sq32[s_]])
                    b = next_ps()

                    def f(e, b=b, tsl=tsl, s_=s_):
                        for c in range(NCH):
                            e.matmul(ps[:, b, 0:8], lhsT=xT[:, c, tsl], rhs=wr[:, c, :],
                                     start=(c == 0), stop=(c == NCH - 1))
                        for c in range(NCH):
                            r = e.matmul(ps[:, b, 8:9], lhsT=sq32[s_][:, c, :],
                                         rhs=ones_f[:, 0:1], start=(c == 0), stop=(c == NCH - 1))
                        return r
                    sc.op("pe", f, reads=[t_wr, t_sq32[s_], t_ones] +
                          [t_xT[c][tt] for c in range(NCH)], writes=[t_ps[b]])
                    sc.op("dve", lambda e, b=b, tt=tt: e.tensor_copy(
                        out=rst[:, tt, :], in_=ps[:, b, 0:9]), reads=[t_ps[b]], writes=[t_rst])
                sc.op("act", lambda e: e.activation(
                    out=rs16[:, :, 0:1], in_=rst[:, :, 8:9], func=AF.Sqrt, scale=1.0 / D,
                    bias=1e-6), reads=[t_rst], writes=[t_rs16])
                sc.op("dve", lambda e: e.reciprocal(out=rs16[:, :, 1:2], in_=rs16[:, :, 0:1]),
                      reads=[t_rs16], writes=[t_rs16])
                sc.op("dve", lambda e: e.tensor_tensor(
                    out=L[:], in0=rst[:, :, 0:8],
                    in1=rs16[:, :, 1:2].to_broadcast([128, NT16, 8]), op=ALU.mult),
                    reads=[t_rst, t_rs16], writes=[t_L])
                for tt in range(NT16):
                    sc.op("dve", lambda e, tt=tt: e.max(out=v8[:, tt, :], in_=L[:, tt, :]),
                          reads=[t_L], writes=[t_v8])
                sc.op("dve", lambda e: e.tensor_tensor(
                    out=gate[:], in0=L[:], in1=v8[:, :, 1:2].to_broadcast([128, NT16, 8]),
                    op=ALU.is_ge), reads=[t_L, t_v8], writes=[t_gate])
                sc.op("dve", lambda e: e.tensor_tensor(
                    out=tmpr[:], in0=L[:], in1=v8[:, :, 0:1].to_broadcast([128, NT16, 8]),
                    op=ALU.subtract), reads=[t_L, t_v8], writes=[t_tmpr])
                sc.op("act", lambda e: e.activation(out=tmpr[:], in_=tmpr[:], func=AF.Exp),
                      reads=[t_tmpr], writes=[t_tmpr])
                sc.op("dve", lambda e: e.tensor_tensor(
                    out=rs16[:, :, 2:3], in0=v8[:, :, 1:2], in1=v8[:, :, 0:1], op=ALU.subtract),
                    reads=[t_v8], writes=[t_rs16])
                sc.op("act", lambda e: e.activation(out=rs16[:, :, 2:3], in_=rs16[:, :, 2:3],
                                                    func=AF.Exp),
                      reads=[t_rs16], writes=[t_rs16])
                sc.op("dve", lambda e: e.tensor_scalar(
                    out=rs16[:, :, 2:3], in0=rs16[:, :, 2:3], scalar1=1.0, scalar2=None,
                    op0=ALU.add), reads=[t_rs16], writes=[t_rs16])
                sc.op("dve", lambda e: e.reciprocal(out=rs16[:, :, 3:4], in_=rs16[:, :, 2:3]),
                      reads=[t_rs16], writes=[t_rs16])
                sc.op("dve", lambda e: e.tensor_tensor(
                    out=gate[:], in0=gate[:], in1=tmpr[:], op=ALU.mult),
                    reads=[t_gate, t_tmpr], writes=[t_gate])
                sc.op("dve", lambda e: e.tensor_tensor(
                    out=gate[:], in0=gate[:], in1=rs16[:, :, 3:4].to_broadcast([128, NT16, 8]),
                    op=ALU.mult), reads=[t_gate, t_rs16], writes=[t_gate])
                dgn = 0
                for ex in range(8):
                    gi = ex % 2
                    for q in range(NTT):
                        b = next_ps()
                        for i4 in range(4):
                            tt = q * 4 + i4
                            di = dgn % 2
                            dgn += 1
                            sc.op("dve", lambda e, di=di, tt=tt, ex=ex: e.tensor_tensor(
                                out=dg[di][:], in0=ident[:],
                                in1=gate[:, tt, ex:ex + 1].to_broadcast([128, 128]), op=ALU.mult),
                                reads=[t_ident, t_gate], writes=[t_dg[di]])
                            sc.op("pe", lambda e, b=b, i4=i4, di=di: e.matmul(
                                ps[:, b, i4 * 128:(i4 + 1) * 128], lhsT=ones_f[:], rhs=dg[di][:],
                                start=True, stop=True),
                                reads=[t_dg[di], t_ones], writes=[t_ps[b]])
                        sc.op("act", lambda e, b=b, gi=gi, q=q: e.activation(
                            out=G[gi][:, q * TT:(q + 1) * TT], in_=ps[:, b, :], func=AF.Copy),
                            reads=[t_ps[b]], writes=[t_G[gi]])
                    swiglu_stream(hT, t_hT, h1, t_h1, sg, t_sg, mg_d[ex], mu_d[ex], md_d[ex], DFFE,
                                  gate_bc=G[gi], t_gate=t_G[gi])

        if "c0" in phases:
            phase_conformer()
            sc.barrier()
        if "f0" in phases:
            phase_ffn()
            sc.barrier()
        if "g1" in phases:
            phase_gdn()
            sc.barrier()
        if "m1" in phases:
            phase_moe()
            sc.barrier()

        do_norm = "final" in phases
        with contextlib.ExitStack() as es2:
            xo = [sb("xo%d" % i, [128, D], F32, es2) for i in range(2)]
            fng = sb("fng_sb", [128, D], F32, es2)
            sc.op("sp", lambda e: e.dma_start(out=fng[:],
                                              in_=fng_d[0:1, :].partition_broadcast(128)),
                  writes=[t_fng], chan=ch_misc, after=sc.last_ops())
            yo = [sb("yo%d" % i, [128, D], F32, es2) for i in range(2)]
            sq = sb("sqj", [128, D], F32, es2)
            st = [sb("st%d" % i, [128, 4], F32, es2) for i in range(2)]
            t_xo = [Tok("xo0"), Tok("xo1")]
            t_yo = [Tok("yo0"), Tok("yo1")]
            t_sq2 = Tok("sq")
            t_st = [Tok("st0"), Tok("st1")]
            out_ops = []
            for tt in range(S // 128):
                sl = tt % 2
                for half in range(2):
                    b = next_ps()

                    def f(e, half=half, b=b, tt=tt):
                        for j in range(4):
                            c = half * 4 + j
                            r = e.transpose(out=ps[:, b, j * 128:(j + 1) * 128],
                                            in_=xT[:, c, tt * 128:(tt + 1) * 128],
                                            identity=ident[:])
                        return r
                    sc.op("pe", f, reads=[t_ident] + [t_xT[half * 4 + j][tt] for j in range(4)],
                          writes=[t_ps[b]])
                    dst = xo if do_norm else yo
                    t_dst = t_xo if do_norm else t_yo
                    sc.op("act", lambda e, half=half, b=b, sl=sl, dst=dst: e.activation(
                        out=dst[sl][:, half * 512:(half + 1) * 512], in_=ps[:, b, :], func=AF.Copy),
                        reads=[t_ps[b]], writes=[t_dst[sl]])
                if do_norm:
                    sc.op("act", lambda e, sl=sl: e.activation(
                        out=sq[:], in_=xo[sl][:], func=AF.Square, accum_out=st[sl][:, 0:1]),
                        reads=[t_xo[sl]], writes=[t_sq2, t_st[sl]])
                    sc.op("act", lambda e, sl=sl: e.activation(
                        out=st[sl][:, 1:2], in_=st[sl][:, 0:1], func=AF.Sqrt, scale=1.0 / D,
                        bias=1e-6), reads=[t_st[sl]], writes=[t_st[sl]])
                    sc.op("dve", lambda e, sl=sl: e.reciprocal(out=st[sl][:, 2:3],
                                                               in_=st[sl][:, 1:2]),
                          reads=[t_st[sl]], writes=[t_st[sl]])
                    sc.op("dve", lambda e, sl=sl: e.scalar_tensor_tensor(
                        out=yo[sl][:], in0=xo[sl][:], scalar=st[sl][:, 2:3], in1=fng[:],
                        op0=ALU.mult, op1=ALU.mult),
                        reads=[t_xo[sl], t_st[sl], t_fng], writes=[t_yo[sl]])
                o = sc.op("sp", lambda e, sl=sl, tt=tt: e.dma_start(
                    out=out_d[tt * 128:(tt + 1) * 128, :], in_=yo[sl][:]),
                    reads=[t_yo[sl]], chan=ch_out[sl])
                out_ops.append(o)
            fin = sc.op("sp", lambda e: e.nop())
            fin.is_nop = True
            fin.deps.extend(out_ops[-2:])
            sc.emit(nc)
    return nc


def pack_vecs(inp):
    rows = np.zeros((128, D), np.float32)
    rows[VR["mix0"]] = inp["mix_norm"][0]
    rows[VR["mix1"]] = inp["mix_norm"][1]
    rows[VR["ffn0"]] = inp["ffn_norm"][0]
    rows[VR["ffn1"]] = inp["ffn_norm"][1]
    rows[VR["pw1_ba"]] = inp["cf_pw1_b"][0, :D]
    rows[VR["pw1_bb"]] = inp["cf_pw1_b"][0, D:]
    rows[VR["dw_b"]] = inp["cf_dw_b"][0]
    rows[VR["ln_g"]] = inp["cf_ln_g"][0]
    rows[VR["ln_b"]] = inp["cf_ln_b"][0]
    rows[VR["pw2_b"]] = inp["cf_pw2_b"][0]
    rows[VR["dw_w"]:VR["dw_w"] + 31] = inp["cf_dw_w"][0]
    gc = inp["gdn_conv_w"][0]
    for k in range(5):
        for part in range(3):
            rows[VR["gconv"] + k * 3 + part] = gc[k, part * D:(part + 1) * D]
    return rows


def gdn_consts():
    i = np.arange(128)
    c = np.zeros((128, 2048), np.float32)
    c[:, 0:128] = (i[:, None] <= i[None, :])
    c[:, 128:256] = (i[:, None] >= i[None, :])
    for k in range(7):
        bsz = 1 << k
        I, J = i[:, None], i[None, :]
        m = ((I // (2 * bsz)) == (J // (2 * bsz))) & (((I // bsz) % 2) == 1) & (((J // bsz) % 2) == 0)
        c[:, 256 + k * 128:256 + (k + 1) * 128] = m.T
        c[:, 1152 + k * 128:1152 + (k + 1) * 128] = m
    return c


def make_in_maps(inp, phases, nb):
    x = np.ascontiguousarray(inp["x"], dtype=np.float32)
    vecs = pack_vecs(inp)
    fng = np.ascontiguousarray(inp["final_norm"], dtype=np.float32).reshape(1, D)
    base = {"vecs": vecs, "fng": fng}
    if "c0" in phases:
        base["cf_pw1_w"] = np.ascontiguousarray(inp["cf_pw1_w"][0])
        base["cf_pw2_w"] = np.ascontiguousarray(inp["cf_pw2_w"][0])
    if "f0" in phases:
        base["ffn_w_gate"] = np.ascontiguousarray(inp["ffn_w_gate"][0])
        base["ffn_w_up"] = np.ascontiguousarray(inp["ffn_w_up"][0])
        base["ffn_w_down"] = np.ascontiguousarray(inp["ffn_w_down"][0])
    if "g1" in phases:
        base["gdn_w_in"] = np.ascontiguousarray(inp["gdn_w_in"][0])
        base["gdn_w_out"] = np.ascontiguousarray(inp["gdn_w_out"][0])
        base["gsmall"] = np.concatenate([inp["gdn_a_log"][0].reshape(-1),
                                         inp["gdn_dt_bias"][0].reshape(-1),
                                         inp["gdn_o_norm"][0].reshape(-1)]).astype(np.float32)[None]
        base["gconst"] = gdn_consts()
    if "m1" in phases:
        base["moe_router"] = np.ascontiguousarray(inp["moe_router"][0])
        base["moe_w_gate"] = np.ascontiguousarray(inp["moe_w_gate"][0])
        base["moe_w_up"] = np.ascontiguousarray(inp["moe_w_up"][0])
        base["moe_w_down"] = np.ascontiguousarray(inp["moe_w_down"][0])
    return [dict(base, x=x[b]) for b in range(nb)]


def kernel(**inp):
    phases = ("c0", "f0", "g1", "m1", "final")
    nb = inp["x"].shape[0]
    nc = build_program(phases)
    in_maps = make_in_maps(inp, phases, nb)
    res = run_bass_kernel_spmd(nc, in_maps, core_ids=list(range(nb)))
    return np.stack([r["out"] for r in res.results], axis=0)
```

```python
import contextlib
import numpy as np
import concourse.bass as bass
import concourse.mybir as mybir
from concourse.bass_utils import run_bass_kernel_spmd

F32 = mybir.dt.float32
BF16 = mybir.dt.bfloat16
I32 = mybir.dt.int32
AF = mybir.ActivationFunctionType
ALU = mybir.AluOpType

D = 1024
S = 2048
NCH = D // 128
TT = 512
NTT = S // TT
ENGS = ("sp", "pe", "act", "dve", "pool")


class Tok:
    __slots__ = ("name", "w", "readers")

    def __init__(self, name):
        self.name = name
        self.w = None
        self.readers = []


class Op:
    __slots__ = ("eng", "fn", "deps", "chan", "has_dep", "sig", "name", "is_nop")

    def __init__(self, eng, fn, chan, name):
        self.eng = eng
        self.fn = fn
        self.deps = []
        self.chan = chan
        self.has_dep = False
        self.sig = None
        self.name = name
        self.is_nop = False


class Rec:
    def __init__(self):
        self.items = []

    def op(self, *a, **k):
        self.items.append((a, k))


class Sched:
    def __init__(self):
        self.ops = {e: [] for e in ENGS}
        self.nchan = 0

    def new_chan(self):
        self.nchan += 1
        return self.nchan - 1

    def last_real(self, e):
        for o in reversed(self.ops[e]):
            if not o.is_nop:
                return o
        return None

    def last_ops(self, engs=("pe", "act", "dve", "pool")):
        return [o for o in (self.last_real(e) for e in engs) if o is not None]

    def op(self, eng, fn, reads=(), writes=(), chan=None, name="", after=()):
        o = Op(eng, fn, chan, name)
        for d in after:
            o.deps.append(d)
            if d.chan is None:
                d.has_dep = True
        cand = []
        for t in reads:
            if t.w is not None:
                cand.append((t.w, "raw"))
        for t in writes:
            if t.w is not None:
                cand.append((t.w, "waw"))
            for r in t.readers:
                cand.append((r, "war"))
        seen = set()
        for d, kind in cand:
            if d is o or id(d) in seen:
                continue
            same = (d.eng == eng and d.chan is None and chan is None)
            if same and (eng == "pe" or kind == "war"):
                continue
            seen.add(id(d))
            o.deps.append(d)
            d.has_dep = True
        for t in reads:
            t.readers.append(o)
        for t in writes:
            t.w = o
            t.readers = []
        self.ops[eng].append(o)
        return o

    def barrier(self, engs=("pe", "act", "dve", "pool")):
        last = {e: self.last_real(e) for e in engs}
        for e in engs:
            o = Op(e, lambda eng: eng.nop(), None, "barrier")
            o.is_nop = True
            for e2, l in last.items():
                if e2 != e and l is not None:
                    o.deps.append(l)
                    if l.chan is None:
                        l.has_dep = True
            self.ops[e].append(o)

    def emit(self, nc, final_wait_ops=()):
        with contextlib.ExitStack() as es:
            EPOCH = 1000
            nsig = {e: sum(1 for o in self.ops[e] if o.chan is None and o.has_dep) for e in ENGS}
            esem = {e: [es.enter_context(nc.semaphore("s_%s%d" % (e, i)))
                        for i in range(nsig[e] // EPOCH + 1)] for e in ENGS}
            for e in ENGS:
                cnt = 0
                for o in self.ops[e]:
                    if o.chan is None and o.has_dep:
                        o.sig = (esem[e][cnt // EPOCH], cnt % EPOCH + 1, 1)
                        cnt += 1
            CEP = 100
            ccnt = [0] * self.nchan
            ntot = [0] * self.nchan
            for e in ENGS:
                for o in self.ops[e]:
                    if o.chan is not None:
                        ntot[o.chan] += 1
            csem = [[es.enter_context(nc.semaphore("c_%d_%d" % (i, k)))
                     for k in range(ntot[i] // CEP + 1)] for i in range(self.nchan)]
            for e in ENGS:
                for o in self.ops[e]:
                    if o.chan is not None:
                        k = ccnt[o.chan]
                        o.sig = (csem[o.chan][k // CEP], (k % CEP + 1) * 16, 16)
                        ccnt[o.chan] += 1
            block = es.enter_context(nc.Block())
            ops = self.ops

            def run(engname, eobj):
                waited = {}
                for o in ops[engname]:
                    need = {}
                    for d in o.deps:
                        sem, val, _ = d.sig
                        k = id(sem)
                        if val > waited.get(k, 0) and val > need.get(k, (None, 0))[1]:
                            need[k] = (sem, val)
                    for k, (sem, val) in need.items():
                        eobj.wait_ge(sem, val)
                        waited[k] = val
                    inst = o.fn(eobj)
                    if o.sig is not None:
                        inst.then_inc(o.sig[0], o.sig[2])

            @block.sync
            def _(e):
                run("sp", e)

            @block.tensor
            def _(e):
                run("pe", e)

            @block.scalar
            def _(e):
                run("act", e)

            @block.vector
            def _(e):
                run("dve", e)

            @block.gpsimd
            def _(e):
                run("pool", e)


VR = dict(mix0=0, mix1=1, ffn0=2, ffn1=3, pw1_ba=4, pw1_bb=5, dw_b=6, ln_g=7, ln_b=8, pw2_b=9,
          dw_w=10, gconv=41)
DFF = 2816
DFFE = 3584
USE_F32R = True
WB = 2048


def build_program(phases=("c0", "f0", "g1", "m1"), dbg=0, heads=tuple(range(8)), two_x=False):
    nc = bass.Bass("TRN2", target_bir_lowering=False, dynamic_dma_scratch_size=2048)

    def din(name, shape):
        return nc.dram_tensor(name, shape, F32, kind="ExternalInput").ap()

    x_d = din("x", [S, D])
    vecs_d = din("vecs", [128, D])
    fng_d = din("fng", [1, D])
    out_d = nc.dram_tensor("out", [S, D], F32, kind="ExternalOutput").ap()
    xacc_d = din("xacc", [S, D]) if two_x else None
    if "c0" in phases:
        w1_d = din("cf_pw1_w", [D, 2 * D])
        w2_d = din("cf_pw2_w", [D, D])
    if "f0" in phases:
        fg_d = din("ffn_w_gate", [D, DFF])
        fu_d = din("ffn_w_up", [D, DFF])
        fd_d = din("ffn_w_down", [DFF, D])

    if "g1" in phases:
        gin_d = din("gdn_w_in", [D, 4128])
        gout_d = din("gdn_w_out", [D, D])
        gsm_d = din("gsmall", [1, 160])
    if "m1" in phases:
        rt_d = din("moe_router", [D, 8])
        mg_d = din("moe_w_gate", [8, D, DFFE])
        mu_d = din("moe_w_up", [8, D, DFFE])
        md_d = din("moe_w_down", [8, DFFE, D])

    sc = Sched()
    with contextlib.ExitStack() as es:
        uniq = [0]

        def sb(name, shape, dt, stack=es):
            uniq[0] += 1
            return stack.enter_context(nc.sbuf_tensor("%s_%d" % (name, uniq[0]), shape, dt))

        xT = sb("xT", [128, NCH, S], F32)
        ident = sb("ident", [128, 128], F32)
        ones_f = sb("ones_f", [128, 128], F32)
        ones_b = sb("ones_b", [128, 128], BF16)
        vecs = sb("vecs_sb", [128, NCH, 64], F32)
        ps = es.enter_context(nc.psum_tensor("ps", [128, 8, 512], F32))
        stage, wbf, t_stage, t_wbf = [], [], [], []
        pool_fence = [[], 0]

        def set_pools(stack, nst, nwb, elems):
            stage[:] = [sb("stage%d" % i, [128, elems], F32, stack) for i in range(nst)]
            wbf[:] = [sb("wbf%d" % i, [128, elems], BF16, stack) for i in range(nwb)]
            t_stage[:] = [Tok("stage%d" % i) for i in range(nst)]
            t_wbf[:] = [Tok("wbf%d" % i) for i in range(nwb)]
            pool_fence[0] = sc.last_ops()
            pool_fence[1] = nst
        cst_d = din("gconst", [128, 2048]) if "g1" in phases else None
        sm = [sb("sm%d" % i, [128, TT], F32) for i in range(4)]
        t_sm = [Tok("sm%d" % i) for i in range(4)]
        smn = [0]

        def next_sm():
            i = smn[0] % 4
            smn[0] += 1
            return i

        t_xT = [[Tok("xT%d_%d" % (c, t)) for t in range(S // 128)] for c in range(NCH)]

        def xtoks(c, t512):
            return [t_xT[c][t512 * 4 + i] for i in range(4)]
        t_ident = Tok("ident")
        t_ones = Tok("ones")
        t_vecs = Tok("vecs")
        t_fng = Tok("fng")
        t_ps = [Tok("ps%d" % i) for i in range(8)]
        ch_stage = [sc.new_chan() for _ in range(2)]
        ch_xin = [sc.new_chan(), sc.new_chan()]
        ch_misc = sc.new_chan()
        ch_out = [sc.new_chan(), sc.new_chan()]
        psn = [0]
        stn = [0]
        wbn = [0]

        def next_ps():
            i = psn[0] % 8
            psn[0] += 1
            return i

        cast_cfg = ["act"]

        def load_block(src, view, cast_eng=None):
            cast_eng = cast_eng or cast_cfg[0]
            a, b = src.shape[1], src.shape[2]
            n = a * b
            s = stn[0] % len(stage)
            stn[0] += 1
            k = wbn[0] % len(wbf)
            wbn[0] += 1
            aft = ()
            if pool_fence[1] > 0:
                aft = pool_fence[0]
                pool_fence[1] -= 1
            st_, wb_, ts_, tw_ = stage[s], wbf[k], t_stage[s], t_wbf[k]
            sc.op("sp", lambda e: e.dma_start(
                out=st_[:, 0:n].rearrange("p (a b) -> p a b", a=a), in_=src),
                writes=[ts_], chan=ch_stage[s], after=aft)
            if cast_eng == "act":
                sc.op("act", lambda e: e.activation(out=wb_[:, 0:n], in_=st_[:, 0:n],
                                                    func=AF.Copy),
                      reads=[ts_], writes=[tw_])
            else:
                sc.op(cast_eng, lambda e: e.tensor_copy(out=wb_[:, 0:n], in_=st_[:, 0:n]),
                      reads=[ts_], writes=[tw_])
            return wb_[:, 0:n].rearrange("p (a b) -> p a b", a=a), tw_

        def fr0(ap):
            return ap.bitcast(mybir.dt.float32r) if USE_F32R else ap

        sc.op("pool", lambda e: e.memset(ones_f[:], 1.0), writes=[t_ones])
        sc.op("pool", lambda e: e.memset(ones_b[:], 1.0), writes=[t_ones])
        sc.op("pool", lambda e: e.affine_select(out=fr0(ident[:]), in_=ones_f[:], pattern=[[-1, 128]],
                                                compare_op=ALU.is_equal, fill=0.0, base=0,
                                                channel_multiplier=1),
              reads=[t_ones], writes=[t_ident])

        def load_x_loop(src_d, xin, t_xin, fence=()):
            for tt in range(S // 128):
                sl = tt % 2
                sc.op("sp", lambda e, tt=tt, sl=sl: e.dma_start(
                    out=xin[sl][:], in_=src_d[tt * 128:(tt + 1) * 128, :]),
                    writes=[t_xin[sl]], chan=ch_xin[sl], after=fence)
                for half in range(2):
                    b = next_ps()

                    def f(e, half=half, b=b, sl=sl):
                        for j in range(4):
                            c = half * 4 + j
                            r = e.transpose(out=ps[:, b, j * 128:(j + 1) * 128],
                                            in_=xin[sl][:, c * 128:(c + 1) * 128],
                                            identity=ident[:])
                        return r
                    sc.op("pe", f, reads=[t_xin[sl], t_ident], writes=[t_ps[b]])
                    eng = "dve" if half == 0 else "act"

                    def g(e, half=half, b=b, tt=tt, eng=eng):
                        o = xT[:, half * 4:half * 4 + 4, tt * 128:(tt + 1) * 128]
                        i = ps[:, b, :].rearrange("p (j r) -> p j r", j=4)
                        if eng == "dve":
                            return e.tensor_copy(out=o, in_=i)
                        return e.activation(out=o, in_=i, func=AF.Copy)
                    sc.op(eng, g, reads=[t_ps[b]],
                          writes=[t_xT[half * 4 + j][tt] for j in range(4)])


        with contextlib.ExitStack() as es1:
            xin = [sb("xin%d" % i, [128, D], F32, es1) for i in range(2)]
            t_xin = [Tok("xin0"), Tok("xin1")]
            sc.op("sp", lambda e: e.dma_start(out=xin[1][:], in_=vecs_d[:, :]), writes=[t_xin[1]],
                  chan=ch_xin[1])
            for half in range(2):
                b = next_ps()

                def f(e, half=half, b=b):
                    for j in range(4):
                        c = half * 4 + j
                        r = e.transpose(out=ps[:, b, j * 128:(j + 1) * 128],
                                        in_=xin[1][:, c * 128:(c + 1) * 128], identity=ident[:])
                    return r
                sc.op("pe", f, reads=[t_xin[1], t_ident], writes=[t_ps[b]])
                sc.op("dve", lambda e, half=half, b=b: e.tensor_copy(
                    out=vecs[:, half * 4:half * 4 + 4, :],
                    in_=ps[:, b, :].rearrange("p (j r) -> p j r", j=4)[:, :, 0:64]),
                    reads=[t_ps[b]], writes=[t_vecs])
            load_x_loop(x_d, xin, t_xin)
        sc.barrier()

        def vcol(c, r):
            return vecs[:, c, r:r + 1]

        def rmsnorm_fm(hT, t_hT, grow, sqtmp, t_sq):
            for t in range(NTT):
                tsl = slice(t * TT, (t + 1) * TT)
                sc.op("act", lambda e, tsl=tsl: e.activation(out=sqtmp[:], in_=xT[:, :, tsl],
                                                             func=AF.Square),
                      reads=[tk for c in range(NCH) for tk in xtoks(c, t)], writes=[t_sq])
                b = next_ps()

                def f(e, b=b):
                    for c in range(NCH):
                        r = e.matmul(ps[:, b, :], lhsT=ones_b[:], rhs=sqtmp[:, c, :],
                                     start=(c == 0), stop=(c == NCH - 1))
                    return r
                sc.op("pe", f, reads=[t_sq, t_ones], writes=[t_ps[b]])
                s0 = next_sm()
                sc.op("act", lambda e, b=b, s0=s0: e.activation(out=sm[s0][:], in_=ps[:, b, :],
                                                                func=AF.Sqrt, scale=1.0 / D,
                                                                bias=1e-6),
                      reads=[t_ps[b]], writes=[t_sm[s0]])
                s1 = next_sm()
                sc.op("dve", lambda e, s0=s0, s1=s1: e.reciprocal(out=sm[s1][:], in_=sm[s0][:]),
                      reads=[t_sm[s0]], writes=[t_sm[s1]])
                for c in range(NCH):
                    sc.op("dve", lambda e, c=c, tsl=tsl, s1=s1: e.scalar_tensor_tensor(
                        out=hT[:, c, tsl], in0=xT[:, c, tsl], scalar=vcol(c, grow), in1=sm[s1][:],
                        op0=ALU.mult, op1=ALU.mult),
                        reads=xtoks(c, t) + [t_vecs, t_sm[s1]], writes=[t_hT[c][t]])

        def dump(src_fn, toks_fn):
            for c in range(NCH):
                for t in range(NTT):
                    tsl = slice(t * TT, (t + 1) * TT)
                    sc.op("dve", lambda e, c=c, tsl=tsl: e.tensor_copy(out=xT[:, c, tsl],
                                                                       in_=src_fn(c, tsl)),
                          reads=toks_fn(c, t), writes=xtoks(c, t))

        def phase_conformer():
            with contextlib.ExitStack() as esp:
                set_pools(esp, 2, 6, WB)
                hT = sb("hT", [128, NCH, S], BF16, esp)
                t_hT = [[Tok("hT%d_%d" % (c, t)) for t in range(NTT)] for c in range(NCH)]
                sqtmp = sb("sqtmp", [128, NCH, TT], BF16, esp)
                t_sq = Tok("sqtmp")
                U = sb("ubuf", [128, NCH, S + 30], BF16, esp)
                t_U = [Tok("u%d" % c) for c in range(NCH)]
                diag = sb("diag", [128, 31, 128], BF16, esp)
                t_diag = Tok("diag")
                sig = [sb("sig%d" % i, [128, TT], F32, esp) for i in range(2)]
                t_sig = [Tok("sig0"), Tok("sig1")]
                lnst = [sb("lnst%d" % i, [128, TT], F32, esp) for i in range(3)]
                t_lnst = [Tok("lnst%d" % i) for i in range(3)]
                rmsnorm_fm(hT, t_hT, VR["mix0"], sqtmp, t_sq)
                if dbg == 1:
                    dump(lambda c, tsl: hT[:, c, tsl], lambda c, t: [t_hT[c][t]])
                    return
                sc.op("dve", lambda e: e.memset(U[:], 0.0), writes=t_U)
                sgn = 0
                for h in range(2):
                    wa = [load_block(w1_d[:, h * 512 + q * 256: h * 512 + (q + 1) * 256]
                                     .rearrange("(kc p) n -> p kc n", p=128), None) for q in range(2)]
                    wb_ = [load_block(w1_d[:, D + h * 512 + q * 256: D + h * 512 + (q + 1) * 256]
                                      .rearrange("(kc p) n -> p kc n", p=128), None) for q in range(2)]
                    for jj in range(4):
                        j = h * 4 + jj
                        wA, tA = wa[jj // 2]
                        wB, tB = wb_[jj // 2]
                        co = (jj % 2) * 128
                        for t in range(NTT):
                            tsl = slice(t * TT, (t + 1) * TT)
                            bA = next_ps()
                            bB = next_ps()

                            def f(e, wA=wA, wB=wB, co=co, bA=bA, bB=bB, tsl=tsl):
                                for kc in range(NCH):
                                    e.matmul(ps[:, bA, :], lhsT=wA[:, kc, co:co + 128],
                                             rhs=hT[:, kc, tsl], start=(kc == 0),
                                             stop=(kc == NCH - 1))
                                for kc in range(NCH):
                                    r = e.matmul(ps[:, bB, :], lhsT=wB[:, kc, co:co + 128],
                                                 rhs=hT[:, kc, tsl], start=(kc == 0),
                                                 stop=(kc == NCH - 1))
                                return r
                            sc.op("pe", f, reads=[tA, tB] + [t_hT[c][t] for c in range(NCH)],
                                  writes=[t_ps[bA], t_ps[bB]])
                            sg = sgn % 2
                            sgn += 1
                            sc.op("act", lambda e, bB=bB, sg=sg, j=j: e.activation(
                                out=sig[sg][:], in_=ps[:, bB, :], func=AF.Sigmoid,
                                bias=vcol(j, VR["pw1_bb"])),
                                reads=[t_ps[bB], t_vecs], writes=[t_sig[sg]])
                            sc.op("dve", lambda e, bA=bA, sg=sg, j=j, t=t: e.scalar_tensor_tensor(
                                out=U[:, j, 15 + t * TT:15 + (t + 1) * TT], in0=ps[:, bA, :],
                                scalar=vcol(j, VR["pw1_ba"]), in1=sig[sg][:],
                                op0=ALU.add, op1=ALU.mult),
                                reads=[t_ps[bA], t_sig[sg], t_vecs], writes=[t_U[j]])
                if dbg == 2:
                    dump(lambda c, tsl: U[:, c, 15 + tsl.start:15 + tsl.stop], lambda c, t: [t_U[c]])
                    return
                for j in range(NCH):
                    sc.op("dve", lambda e, j=j: e.tensor_tensor(
                        out=diag[:], in0=ident[:].unsqueeze(1).to_broadcast([128, 31, 128]),
                        in1=vecs[:, j, VR["dw_w"]:VR["dw_w"] + 31].unsqueeze(2)
                        .to_broadcast([128, 31, 128]), op=ALU.mult),
                        reads=[t_ident, t_vecs], writes=[t_diag])
                    for t in range(NTT):
                        b = next_ps()

                        def f(e, j=j, t=t, b=b):
                            for k in range(31):
                                r = e.matmul(ps[:, b, :], lhsT=diag[:, k, :],
                                             rhs=U[:, j, t * TT + k:t * TT + k + TT],
                                             start=(k == 0), stop=(k == 30))
                            return r
                        sc.op("pe", f, reads=[t_diag, t_U[j]], writes=[t_ps[b]])
                        sc.op("act", lambda e, j=j, t=t, b=b: e.activation(
                            out=hT[:, j, t * TT:(t + 1) * TT], in_=ps[:, b, :], func=AF.Identity,
                            bias=vcol(j, VR["dw_b"])),
                            reads=[t_ps[b], t_vecs], writes=[t_hT[j][t]])
                if dbg == 3:
                    dump(lambda c, tsl: hT[:, c, tsl], lambda c, t: [t_hT[c][t]])
                    return
                for t in range(NTT):
                    tsl = slice(t * TT, (t + 1) * TT)
                    sc.op("dve", lambda e, tsl=tsl: e.tensor_tensor(
                        out=sqtmp[:], in0=hT[:, :, tsl], in1=hT[:, :, tsl], op=ALU.mult),
                        reads=[t_hT[c][t] for c in range(NCH)], writes=[t_sq])
                    b1 = next_ps()
                    b2 = next_ps()

                    def f(e, b1=b1, b2=b2, tsl=tsl):
                        for c in range(NCH):
                            e.matmul(ps[:, b1, :], lhsT=ones_b[:], rhs=hT[:, c, tsl],
                                     start=(c == 0), stop=(c == NCH - 1))
                        for c in range(NCH):
                            r = e.matmul(ps[:, b2, :], lhsT=ones_b[:], rhs=sqtmp[:, c, :],
                                         start=(c == 0), stop=(c == NCH - 1))
                        return r
                    sc.op("pe", f, reads=[t_sq, t_ones] + [t_hT[c][t] for c in range(NCH)],
                          writes=[t_ps[b1], t_ps[b2]])
                    m = lnst[0]
                    q = lnst[1]
                    rs = lnst[2]
                    tm, tq, trs = t_lnst
                    sc.op("act", lambda e, m=m, b1=b1: e.activation(
                        out=m[:], in_=ps[:, b1, :], func=AF.Copy, scale=1.0 / D),
                        reads=[t_ps[b1]], writes=[tm])
                    sc.op("dve", lambda e, m=m, q=q: e.tensor_tensor(
                        out=q[:], in0=m[:], in1=m[:], op=ALU.mult),
                        reads=[tm], writes=[tq])
                    sc.op("dve", lambda e, q=q, b2=b2: e.scalar_tensor_tensor(
                        out=q[:], in0=ps[:, b2, :], scalar=1.0 / D, in1=q[:],
                        op0=ALU.mult, op1=ALU.subtract),
                        reads=[t_ps[b2], tq], writes=[tq])
                    sc.op("act", lambda e, q=q: e.activation(
                        out=q[:], in_=q[:], func=AF.Sqrt, bias=1e-5),
                        reads=[tq], writes=[tq])
                    sc.op("dve", lambda e, q=q, rs=rs: e.reciprocal(out=rs[:], in_=q[:]),
                          reads=[tq], writes=[trs])
                    sc.op("dve", lambda e, m=m, rs=rs: e.scalar_tensor_tensor(
                        out=m[:], in0=m[:], scalar=-1.0, in1=rs[:],
                        op0=ALU.mult, op1=ALU.mult),
                        reads=[tm, trs], writes=[tm])
                    for c in range(NCH):
                        w1s = next_sm()
                        sc.op("dve", lambda e, c=c, tsl=tsl, rs=rs, w1s=w1s: e.tensor_tensor(
                            out=sm[w1s][:], in0=hT[:, c, tsl], in1=rs[:], op=ALU.mult),
                            reads=[t_hT[c][t], trs], writes=[t_sm[w1s]])
                        sc.op("dve", lambda e, m=m, w1s=w1s: e.tensor_tensor(
                            out=sm[w1s][:], in0=sm[w1s][:], in1=m[:], op=ALU.add),
                            reads=[t_sm[w1s], tm], writes=[t_sm[w1s]])
                        sc.op("act", lambda e, c=c, tsl=tsl, w1s=w1s: e.activation(
                            out=hT[:, c, tsl], in_=sm[w1s][:], func=AF.Silu,
                            scale=vcol(c, VR["ln_g"]), bias=vcol(c, VR["ln_b"])),
                            reads=[t_sm[w1s], t_vecs], writes=[t_hT[c][t]])
                if dbg == 4:
                    dump(lambda c, tsl: hT[:, c, tsl], lambda c, t: [t_hT[c][t]])
                    return
                for h in range(2):
                    w2 = [load_block(w2_d[:, h * 512 + q * 256: h * 512 + (q + 1) * 256]
                                     .rearrange("(kc p) n -> p kc n", p=128), None) for q in range(2)]
                    for jj in range(4):
                        j = h * 4 + jj
                        wA, tA = w2[jj // 2]
                        co = (jj % 2) * 128
                        for t in range(NTT):
                            tsl = slice(t * TT, (t + 1) * TT)
                            b = next_ps()

                            def f(e, wA=wA, co=co, b=b, tsl=tsl):
                                for kc in range(NCH):
                                    r = e.matmul(ps[:, b, :], lhsT=wA[:, kc, co:co + 128],
                                                 rhs=hT[:, kc, tsl], start=(kc == 0),
                                                 stop=(kc == NCH - 1))
                                return r
                            sc.op("pe", f, reads=[tA] + [t_hT[c][t] for c in range(NCH)],
                                  writes=[t_ps[b]])
                            sc.op("dve", lambda e, b=b, j=j, tsl=tsl: e.scalar_tensor_tensor(
                                out=xT[:, j, tsl], in0=ps[:, b, :], scalar=vcol(j, VR["pw2_b"]),
                                in1=xT[:, j, tsl], op0=ALU.add, op1=ALU.add),
                                reads=[t_ps[b], t_vecs] + xtoks(j, t), writes=xtoks(j, t))

        def swiglu_stream(hT, t_hT, h1, t_h1, sg, t_sg, wg_d, wu_d, wd_d, dff,
                          gate_bc=None, t_gate=None):
            ngrp = (dff + 511) // 512
            sgn = 0
            for g in range(ngrp):
                c0 = g * 512
                ncol = min(512, dff - c0)
                nblk = ncol // 256
                nf = ncol // 128
                wg = [load_block(wg_d[:, c0 + q * 256:c0 + (q + 1) * 256]
                                 .rearrange("(kc p) n -> p kc n", p=128), None) for q in range(nblk)]
                wu = [load_block(wu_d[:, c0 + q * 256:c0 + (q + 1) * 256]
                                 .rearrange("(kc p) n -> p kc n", p=128), None) for q in range(nblk)]
                wd = [load_block(wd_d[c0 + q * 256:c0 + (q + 1) * 256, :]
                                 .rearrange("(fc p) n -> p fc n", p=128), None) for q in range(nblk)]
                for t in range(NTT):
                    tsl = slice(t * TT, (t + 1) * TT)
                    for f_ in range(nf):
                        wG, tG = wg[f_ // 2]
                        wU, tU = wu[f_ // 2]
                        co = (f_ % 2) * 128
                        bG = next_ps()
                        bU = next_ps()

                        def f(e, wG=wG, wU=wU, co=co, bG=bG, bU=bU, tsl=tsl):
                            for kc in range(NCH):
                                e.matmul(ps[:, bG, :], lhsT=wG[:, kc, co:co + 128],
                                         rhs=hT[:, kc, tsl], start=(kc == 0), stop=(kc == NCH - 1))
                            for kc in range(NCH):
                                r = e.matmul(ps[:, bU, :], lhsT=wU[:, kc, co:co + 128],
                                             rhs=hT[:, kc, tsl], start=(kc == 0),
                                             stop=(kc == NCH - 1))
                            return r
                        sc.op("pe", f, reads=[tG, tU] + [t_hT[c][t] for c in range(NCH)],
                              writes=[t_ps[bG], t_ps[bU]])
                        s_ = sgn % 2
                        sgn += 1
                        sc.op("act", lambda e, bG=bG, s_=s_: e.activation(
                            out=sg[s_][:], in_=ps[:, bG, :], func=AF.Silu),
                            reads=[t_ps[bG]], writes=[t_sg[s_]])
                        if gate_bc is None:
                            sc.op("dve", lambda e, bU=bU, s_=s_, f_=f_, tsl=tsl: e.tensor_tensor(
                                out=h1[:, f_, tsl], in0=ps[:, bU, :], in1=sg[s_][:], op=ALU.mult),
                                reads=[t_ps[bU], t_sg[s_]], writes=[t_h1[f_][t]])
                        else:
                            sc.op("dve", lambda e, s_=s_, tsl=tsl: e.tensor_tensor(
                                out=sg[s_][:], in0=sg[s_][:], in1=gate_bc[:, tsl], op=ALU.mult),
                                reads=[t_sg[s_], t_gate], writes=[t_sg[s_]])
                            sc.op("dve", lambda e, bU=bU, s_=s_, f_=f_, tsl=tsl: e.tensor_tensor(
                                out=h1[:, f_, tsl], in0=ps[:, bU, :], in1=sg[s_][:], op=ALU.mult),
                                reads=[t_ps[bU], t_sg[s_]], writes=[t_h1[f_][t]])
                for t in range(NTT):
                    tsl = slice(t * TT, (t + 1) * TT)
                    for j in range(NCH):
                        b = next_ps()

                        def f(e, b=b, j=j, tsl=tsl, wd=wd, nf=nf):
                            for f_ in range(nf):
                                wD, _ = wd[f_ // 2]
                                r = e.matmul(ps[:, b, :],
                                             lhsT=wD[:, f_ % 2, j * 128:(j + 1) * 128],
                                             rhs=h1[:, f_, tsl], start=(f_ == 0),
                                             stop=(f_ == nf - 1))
                            return r
                        sc.op("pe", f, reads=[w[1] for w in wd] + [t_h1[f_][t] for f_ in range(nf)],
                              writes=[t_ps[b]])
                        sc.op("dve", lambda e, b=b, j=j, tsl=tsl: e.tensor_tensor(
                            out=xT[:, j, tsl], in0=ps[:, b, :], in1=xT[:, j, tsl], op=ALU.add),
                            reads=[t_ps[b]] + xtoks(j, t), writes=xtoks(j, t))

        def phase_ffn():
            with contextlib.ExitStack() as esp:
                set_pools(esp, 2, 6, WB)
                hT = sb("hT", [128, NCH, S], BF16, esp)
                t_hT = [[Tok("hT%d_%d" % (c, t)) for t in range(NTT)] for c in range(NCH)]
                sqtmp = sb("sqtmp", [128, NCH, TT], BF16, esp)
                t_sq = Tok("sqtmp")
                h1 = sb("h1", [128, 4, S], BF16, esp)
                t_h1 = [[Tok("h1_%d_%d" % (f_, t)) for t in range(NTT)] for f_ in range(4)]
                sg = [sb("sg%d" % i, [128, TT], F32, esp) for i in range(2)]
                t_sg = [Tok("sg0"), Tok("sg1")]
                rmsnorm_fm(hT, t_hT, VR["ffn0"], sqtmp, t_sq)
                swiglu_stream(hT, t_hT, h1, t_h1, sg, t_sg, fg_d, fu_d, fd_d, DFF)


        F32R = mybir.dt.float32r

        def fr(ap):
            return ap.bitcast(F32R) if USE_F32R else ap

        def phase_gdn():
            NT16 = S // 128
            with contextlib.ExitStack() as esp:
                set_pools(esp, 2, 4, 1024)
                hT = sb("hT", [128, NCH, S], BF16, esp)
                t_hT = [[Tok("hT%d_%d" % (c, t)) for t in range(NTT)] for c in range(NCH)]
                with contextlib.ExitStack() as esq:
                    sqtmp = sb("sqtmp", [128, NCH, TT], BF16, esq)
                    t_sq = Tok("sqtmp")
                    rmsnorm_fm(hT, t_hT, VR["mix1"], sqtmp, t_sq)
                sc.barrier()
                if two_x:
                    with contextlib.ExitStack() as esx:
                        xin2 = [sb("xin2_%d" % i, [128, D], F32, esx) for i in range(2)]
                        t_xin2 = [Tok("xin2_0"), Tok("xin2_1")]
                        load_x_loop(xacc_d, xin2, t_xin2, fence=sc.last_ops())
                    sc.barrier()
                cst = sb("cst", [128, 2048], F32, esp)
                t_cst = Tok("cst")
                gsm = sb("gsm", [128, 160], F32, esp)
                t_gsm = Tok("gsm")
                graw = sb("graw", [128, NT16, 32], F32, esp)
                beta = sb("beta", [128, NT16, 16], F32, esp)
                la = sb("la", [128, NT16, 16], F32, esp)
                gcol = sb("gcol", [128, NT16, 16], F32, esp)
                glast = sb("glast", [128, NT16, 16], F32, esp)
                beg = sb("beg", [128, NT16, 16], F32, esp)
                kdc = graw[:, :, 16:32]
                egl = graw[:, :, 0:16]
                t_graw, t_beta, t_la, t_gcol, t_glast, t_beg = [
                    Tok(n) for n in ("graw", "beta", "la", "gcol", "glast", "beg")]
                t_kdc = t_egl = t_graw
                pre = sb("pre", [128, S + 4], BF16, esp)
                t_pre = Tok("pre")
                diag5 = sb("diag5", [128, 5, 128], BF16, esp)
                t_diag5 = Tok("diag5")
                QKV = [sb("qkv%d" % i, [128, S], F32, esp) for i in range(3)]
                t_QKV = [[Tok("qkv%d_%d" % (i, t)) for t in range(NTT)] for i in range(3)]
                Oacc = sb("Oacc", [128, NT16, 128], F32, esp)
                t_O = [Tok("O%d" % n) for n in range(NT16)]
                oT = pre[:, 2:2 + S]
                t_oT = [Tok("oT%d" % t) for t in range(NTT)]
                NW = 6
                names = ("bV", "Kt", "KD", "QDT", "AT", "PT", "Dm", "DTm", "X", "WK", "U")
                shp = dict(WK=[128, 256])
                TS = [{n: sb("%s_c%d" % (n, c_), shp.get(n, [128, 128]), F32, esp) for n in names}
                      for c_ in range(NW)]
                t_TS = [{n: Tok("%s_c%d" % (n, c_)) for n in names} for c_ in range(NW)]
                ETs = [sb("ETall%d" % c_, [128, 2, 128], F32, esp) for c_ in range(NW)]
                t_ETs = [[Tok("ET%d_0" % c_), Tok("ET%d_1" % c_)] for c_ in range(NW)]
                Ss = [sb("Sst%d" % c_, [128, 128], F32, esp) for c_ in range(2)]
                t_Ss = [Tok("S0"), Tok("S1")]
                T_ = {"sz": TS[0]["AT"], "yy": TS[1]["AT"]}
                t_T = {"sz": t_TS[0]["AT"], "yy": t_TS[1]["AT"]}
                def bank_pool(c_):
                    def nb_():
                        return c_
                    return nb_
                st16 = sb("st16", [128, 48], F32, esp)
                t_st16 = Tok("st16")
                ch_c = sc.new_chan()
                fence = sc.last_ops()
                sc.op("sp", lambda e: e.dma_start(out=cst[:], in_=cst_d[:, :]), writes=[t_cst],
                      chan=ch_c, after=fence)
                ch_c2 = sc.new_chan()
                sc.op("sp", lambda e: e.dma_start(out=gsm[:],
                                                  in_=gsm_d[0:1, :].partition_broadcast(128)),
                      writes=[t_gsm], chan=ch_c2, after=fence)
                cstr = sb("cstr", [128, 256], F32, esp)
                sc.op("act", lambda e: e.activation(out=fr(cstr[:]), in_=cst[:, 0:256], func=AF.Copy),
                      reads=[t_cst], writes=[t_cst])
                triX = [cstr[:, 0:128], cstr[:, 128:256]]
                MX = [cst[:, 256:256 + 896].rearrange("p (k i) -> p k i", k=7),
                      cst[:, 1152:1152 + 896].rearrange("p (k i) -> p k i", k=7)]
                sc.op("dve", lambda e: e.memset(pre[:], 0.0), writes=[t_pre])
                wgt, t_wgt = load_block(gin_d[:, 4096:4128].rearrange("(kc p) n -> p kc n", p=128),
                                        None)
                for n in range(NT16):
                    b = next_ps()
                    tsl = slice(n * 128, (n + 1) * 128)

                    def f(e, b=b, tsl=tsl):
                        for kc in range(NCH):
                            r = e.matmul(ps[:, b, 0:32], lhsT=hT[:, kc, tsl], rhs=wgt[:, kc, :],
                                         start=(kc == 0), stop=(kc == NCH - 1))
                        return r
                    sc.op("pe", f, reads=[t_wgt] + [t_hT[c][n // 4] for c in range(NCH)],
                          writes=[t_ps[b]])
                    sc.op("dve", lambda e, b=b, n=n: e.tensor_copy(out=graw[:, n, :],
                                                                   in_=ps[:, b, 0:32]),
                          reads=[t_ps[b]], writes=[t_graw])
                sc.op("act", lambda e: e.activation(out=beta[:], in_=graw[:, :, 0:16],
                                                    func=AF.Sigmoid),
                      reads=[t_graw], writes=[t_beta])
                sc.op("dve", lambda e: e.tensor_tensor(
                    out=la[:], in0=graw[:, :, 16:32],
                    in1=gsm[:, 16:32].unsqueeze(1).to_broadcast([128, NT16, 16]), op=ALU.add),
                    reads=[t_graw, t_gsm], writes=[t_la])
                sc.op("act", lambda e: e.activation(out=la[:], in_=la[:], func=AF.Exp),
                      reads=[t_la], writes=[t_la])
                sc.op("act", lambda e: e.activation(out=la[:], in_=la[:], func=AF.Ln, bias=1.0),
                      reads=[t_la], writes=[t_la])
                sc.op("act", lambda e: e.activation(out=gsm[:, 0:16], in_=gsm[:, 0:16],
                                                    func=AF.Exp),
                      reads=[t_gsm], writes=[t_gsm])
                sc.op("dve", lambda e: e.scalar_tensor_tensor(
                    out=la[:], in0=la[:], scalar=-1.0,
                    in1=gsm[:, 0:16].unsqueeze(1).to_broadcast([128, NT16, 16]),
                    op0=ALU.mult, op1=ALU.mult), reads=[t_la, t_gsm], writes=[t_la])
                for n in range(NT16):
                    b = next_ps()

                    def f(e, b=b, n=n):
                        e.matmul(ps[:, b, 0:8], lhsT=triX[0], rhs=la[:, n, 0:8], start=True,
                                 stop=True)
                        e.matmul(ps[:, b, 8:16], lhsT=triX[1], rhs=la[:, n, 8:16], start=True,
                                 stop=True)
                        return e.matmul(ps[:, b, 16:32], lhsT=ones_f[:], rhs=la[:, n, :],
                                        start=True, stop=True)
                    sc.op("pe", f, reads=[t_la, t_cst, t_ones], writes=[t_ps[b]])
                    sc.op("dve", lambda e, b=b, n=n: e.tensor_copy(out=gcol[:, n, :],
                                                                   in_=ps[:, b, 0:16]),
                          reads=[t_ps[b]], writes=[t_gcol])
                    sc.op("dve", lambda e, b=b, n=n: e.tensor_copy(out=glast[:, n, :],
                                                                   in_=ps[:, b, 16:32]),
                          reads=[t_ps[b]], writes=[t_glast])
                sc.op("act", lambda e: e.activation(out=beg[:], in_=gcol[:], func=AF.Exp),
                      reads=[t_gcol], writes=[t_beg])
                sc.op("dve", lambda e: e.tensor_tensor(out=beg[:], in0=beg[:], in1=beta[:],
                                                       op=ALU.mult),
                      reads=[t_beg, t_beta], writes=[t_beg])
                sc.op("dve", lambda e: e.tensor_tensor(out=kdc[:], in0=glast[:], in1=gcol[:],
                                                       op=ALU.subtract),
                      reads=[t_glast, t_gcol], writes=[t_kdc])
                sc.op("act", lambda e: e.activation(out=kdc[:], in_=kdc[:], func=AF.Exp),
                      reads=[t_kdc], writes=[t_kdc])
                sc.op("act", lambda e: e.activation(out=egl[:], in_=glast[:], func=AF.Exp),
                      reads=[t_glast], writes=[t_egl])

                def tt_(n, out, in0, in1, op, eng="dve", rd=(), wr=()):
                    sc.op(eng, lambda e: e.tensor_tensor(out=out, in0=in0, in1=in1, op=op),
                          reads=list(rd), writes=list(wr))

                if dbg == 11:
                    return
                for h in heads:
                    for part in range(3):
                        cidx = part * 8 + h
                        wv, t_wv = load_block(gin_d[:, cidx * 128:(cidx + 1) * 128]
                                              .rearrange("(kc p) n -> p kc n", p=128), None)
                        for k in range(5):
                            sc.op("dve", lambda e, k=k, part=part, h=h: e.tensor_scalar(
                                out=diag5[:, k, :], in0=ident[:],
                                scalar1=vcol(h, VR["gconv"] + k * 3 + part), scalar2=None,
                                op0=ALU.mult), reads=[t_ident, t_vecs], writes=[t_diag5])
                        for t in range(NTT):
                            tsl = slice(t * TT, (t + 1) * TT)
                            b = next_ps()

                            def f(e, b=b, tsl=tsl, wv=wv):
                                for kc in range(NCH):
                                    r = e.matmul(ps[:, b, :], lhsT=wv[:, kc, :], rhs=hT[:, kc, tsl],
                                                 start=(kc == 0), stop=(kc == NCH - 1))
                                return r
                            sc.op("pe", f, reads=[t_wv] + [t_hT[c][t] for c in range(NCH)],
                                  writes=[t_ps[b]])
                            sc.op("act", lambda e, b=b, t=t: e.activation(
                                out=pre[:, 2 + t * TT:2 + (t + 1) * TT], in_=ps[:, b, :],
                                func=AF.Copy), reads=[t_ps[b]], writes=[t_pre])
                        for t in range(NTT):
                            tsl = slice(t * TT, (t + 1) * TT)
                            b = next_ps()

                            def f(e, b=b, t=t):
                                for k in range(5):
                                    r = e.matmul(ps[:, b, :], lhsT=diag5[:, k, :],
                                                 rhs=pre[:, t * TT + k:t * TT + k + TT],
                                                 start=(k == 0), stop=(k == 4))
                                return r
                            sc.op("pe", f, reads=[t_diag5, t_pre], writes=[t_ps[b]])
                            sc.op("act", lambda e, b=b, tsl=tsl, part=part: e.activation(
                                out=fr(QKV[part][:, tsl]), in_=ps[:, b, :], func=AF.Silu),
                                reads=[t_ps[b]], writes=[t_QKV[part][t]])
                        if part < 2:
                            for t in range(NTT):
                                tsl = slice(t * TT, (t + 1) * TT)
                                la_ = next_sm()
                                lb_ = next_sm()
                                sc.op("dve", lambda e, tsl=tsl, part=part, la_=la_: e.tensor_tensor(
                                    out=sm[la_][:], in0=QKV[part][:, tsl], in1=QKV[part][:, tsl],
                                    op=ALU.mult), reads=[t_QKV[part][t]], writes=[t_sm[la_]])
                                b2 = next_ps()
                                sc.op("pe", lambda e, b2=b2, la_=la_: e.matmul(
                                    ps[:, b2, :], lhsT=ones_f[:], rhs=sm[la_][:], start=True,
                                    stop=True), reads=[t_sm[la_], t_ones], writes=[t_ps[b2]])
                                sc.op("act", lambda e, b2=b2, lb_=lb_: e.activation(
                                    out=sm[lb_][:], in_=ps[:, b2, :], func=AF.Sqrt, bias=1e-6),
                                    reads=[t_ps[b2]], writes=[t_sm[lb_]])
                                sc.op("dve", lambda e, lb_=lb_: e.reciprocal(out=sm[lb_][:],
                                                                             in_=sm[lb_][:]),
                                      reads=[t_sm[lb_]], writes=[t_sm[lb_]])
                                scl = (128.0 ** -0.5) if part == 0 else 1.0
                                sc.op("dve", lambda e, tsl=tsl, part=part, scl=scl, lb_=lb_:
                                      e.scalar_tensor_tensor(
                                          out=fr(QKV[part][:, tsl]), in0=QKV[part][:, tsl], scalar=scl,
                                          in1=sm[lb_][:], op0=ALU.mult, op1=ALU.mult),
                                      reads=[t_QKV[part][t], t_sm[lb_]], writes=[t_QKV[part][t]])
                    if dbg == 12:
                        return
                    zwv, t_zwv = load_block(gin_d[:, 3072 + h * 128:3072 + (h + 1) * 128]
                                            .rearrange("(kc p) n -> p kc n", p=128), None)
                    QT, KT, VT = QKV
                    def chain(dr, T_, t_T, ETall, t_ET, Sst, t_S, next_ps, sc, chunks, do_memset):
                        if do_memset:
                            sc.op("act", lambda e: e.activation(out=fr(Sst[:]), in_=ones_f[:], func=AF.Copy,
                                                                scale=0.0),
                                  reads=[t_ones], writes=[t_S])
                        order = chunks
                        dh = dr * 8 + h
                        for n in order:
                            csl = slice(n * 128, (n + 1) * 128)
                            t4 = n // 4
                            rq = [t_QKV[0][t4]]
                            rk = [t_QKV[1][t4]]
                            rv = [t_QKV[2][t4]]
                            bt = next_ps()

                            def f(e, bt=bt, csl=csl):
                                e.transpose(out=ps[:, bt, 0:128], in_=KT[:, csl], identity=ident[:])
                                return e.transpose(out=ps[:, bt, 128:256], in_=VT[:, csl],
                                                   identity=ident[:])
                            sc.op("pe", f, reads=rk + rv + [t_ident], writes=[t_ps[bt]])
                            sc.op("act", lambda e, bt=bt, n=n, dh=dh: e.activation(
                                out=fr(T_["bV"][:]), in_=ps[:, bt, 128:256], func=AF.Copy,
                                scale=beta[:, n, dh:dh + 1]),
                                reads=[t_ps[bt], t_beta], writes=[t_T["bV"]])
                            sc.op("act", lambda e, bt=bt, n=n, dh=dh: e.activation(
                                out=fr(T_["Kt"][:]), in_=ps[:, bt, 0:128], func=AF.Copy,
                                scale=beg[:, n, dh:dh + 1]),
                                reads=[t_ps[bt], t_beg], writes=[t_T["Kt"]])
                            sc.op("act", lambda e, bt=bt, n=n, dh=dh: e.activation(
                                out=fr(T_["KD"][:]), in_=ps[:, bt, 0:128], func=AF.Copy,
                                scale=kdc[:, n, dh:dh + 1]),
                                reads=[t_ps[bt], t_kdc], writes=[t_T["KD"]])
                            sc.op("act", lambda e, n=n, dh=dh: e.activation(
                                out=fr(T_["X"][:]), in_=ones_f[:], func=AF.Copy,
                                scale=la[:, n, dh:dh + 1]),
                                reads=[t_la, t_ones], writes=[t_T["X"]])
                            sc.op("act", lambda e, n=n, dh=dh: e.activation(
                                out=fr(T_["U"][:]), in_=ones_f[:], func=AF.Copy,
                                scale=beta[:, n, dh:dh + 1]),
                                reads=[t_beta, t_ones], writes=[t_T["U"]])
                            bm = next_ps()

                            def f(e, bm=bm, csl=csl, dr=dr):
                                e.matmul(ps[:, bm, 0:128], lhsT=fr(T_["X"][:]), rhs=fr(triX[dr]),
                                         start=True, stop=True)
                                e.matmul(ps[:, bm, 128:256], lhsT=fr(T_["U"][:]), rhs=fr(ident[:]),
                                         start=True, stop=True)
                                e.matmul(ps[:, bm, 256:384], lhsT=fr(KT[:, csl]), rhs=fr(KT[:, csl]),
                                         start=True, stop=True)
                                return e.matmul(ps[:, bm, 384:512], lhsT=fr(KT[:, csl]),
                                                rhs=fr(QT[:, csl]), start=True, stop=True)
                            sc.op("pe", f, reads=[t_T["X"], t_T["U"], t_cst, t_ident] + rk + rq,
                                  writes=[t_ps[bm]])
                            sc.op("dve", lambda e, bm=bm, n=n, dh=dh: e.tensor_scalar(
                                out=fr(T_["X"][:]), in0=ps[:, bm, 0:128],
                                scalar1=gcol[:, n, dh:dh + 1], scalar2=0.0, op0=ALU.subtract,
                                op1=ALU.min), reads=[t_ps[bm], t_gcol], writes=[t_T["X"]])
                            sc.op("act", lambda e: e.activation(out=fr(T_["X"][:]), in_=T_["X"][:],
                                                                func=AF.Exp),
                                  reads=[t_T["X"]], writes=[t_T["X"]])
                            sc.op("act", lambda e, bm=bm: e.activation(
                                out=fr(T_["QDT"][:]), in_=ps[:, bm, 0:128], func=AF.Exp),
                                reads=[t_ps[bm]], writes=[t_T["QDT"]])
                            sc.op("dve", lambda e, csl=csl: e.tensor_tensor(
                                out=fr(T_["QDT"][:]), in0=T_["QDT"][:], in1=QT[:, csl], op=ALU.mult),
                                reads=[t_T["QDT"]] + rq, writes=[t_T["QDT"]])
                            sc.op("dve", lambda e, bm=bm: e.tensor_tensor(
                                out=T_["AT"][:], in0=ps[:, bm, 256:384], in1=T_["X"][:],
                                op=ALU.mult), reads=[t_ps[bm], t_T["X"]], writes=[t_T["AT"]])
                            sc.op("dve", lambda e, bm=bm: e.tensor_tensor(
                                out=T_["AT"][:], in0=ps[:, bm, 128:256], in1=T_["AT"][:],
                                op=ALU.mult), reads=[t_ps[bm], t_T["AT"]], writes=[t_T["AT"]])
                            sc.op("dve", lambda e, bm=bm: e.tensor_tensor(
                                out=fr(T_["PT"][:]), in0=ps[:, bm, 384:512], in1=T_["X"][:],
                                op=ALU.mult), reads=[t_ps[bm], t_T["X"]], writes=[t_T["PT"]])
                            sc.op("dve", lambda e, dr=dr: e.tensor_tensor(
                                out=fr(T_["PT"][:]), in0=T_["PT"][:], in1=triX[dr], op=ALU.mult),
                                reads=[t_T["PT"], t_cst], writes=[t_T["PT"]])
                            if dbg == 13:
                                return
                            for lv in range(7):
                                Dc = ident if lv == 0 else T_["Dm"]
                                DTc = ident if lv == 0 else T_["DTm"]
                                rD = [t_ident] if lv == 0 else [t_T["Dm"]]
                                rDT = [t_ident] if lv == 0 else [t_T["DTm"]]
                                bx = next_ps()
                                if lv == 0:
                                    sc.op("dve", lambda e, dr=dr: e.tensor_tensor(
                                        out=fr(ETall[:, 0, :]), in0=T_["AT"][:], in1=MX[dr][:, 0, :],
                                        op=ALU.mult), reads=[t_T["AT"], t_cst], writes=[t_ET[0]])
                                sc.op("pe", lambda e, bx=bx, lv=lv, Dc=Dc: e.matmul(
                                    ps[:, bx, 0:128], lhsT=fr(ETall[:, lv % 2, :]), rhs=fr(Dc[:]), start=True,
                                    stop=True), reads=[t_ET[lv % 2]] + rD, writes=[t_ps[bx]])
                                if lv < 6:
                                    sc.op("dve", lambda e, dr=dr, lv=lv: e.tensor_tensor(
                                        out=fr(ETall[:, (lv + 1) % 2, :]), in0=T_["AT"][:],
                                        in1=MX[dr][:, lv + 1, :], op=ALU.mult),
                                        reads=[t_T["AT"], t_cst], writes=[t_ET[(lv + 1) % 2]])
                                sc.op("act", lambda e, bx=bx: e.activation(
                                    out=fr(T_["X"][:]), in_=ps[:, bx, 0:128], func=AF.Copy),
                                    reads=[t_ps[bx]], writes=[t_T["X"]])
                                by = next_ps()

                                def f(e, by=by, Dc=Dc, DTc=DTc):
                                    e.matmul(ps[:, by, 0:128], lhsT=fr(DTc[:]), rhs=fr(T_["X"][:]),
                                             start=True, stop=True)
                                    return e.matmul(ps[:, by, 128:256], lhsT=fr(T_["X"][:]), rhs=fr(DTc[:]),
                                                    start=True, stop=True)
                                sc.op("pe", f, reads=[t_T["X"]] + rDT, writes=[t_ps[by]])
                                sc.op("dve", lambda e, by=by, Dc=Dc: e.tensor_tensor(
                                    out=fr(T_["Dm"][:]), in0=Dc[:], in1=ps[:, by, 0:128],
                                    op=ALU.subtract), reads=[t_ps[by]] + rD, writes=[t_T["Dm"]])
                                sc.op("dve", lambda e, by=by, DTc=DTc: e.tensor_tensor(
                                    out=fr(T_["DTm"][:]), in0=DTc[:], in1=ps[:, by, 128:256],
                                    op=ALU.subtract), reads=[t_ps[by]] + rDT, writes=[t_T["DTm"]])
                            if dbg == 14:
                                return
                            bw = next_ps()

                            def f(e, bw=bw):
                                e.matmul(ps[:, bw, 0:128], lhsT=fr(T_["DTm"][:]), rhs=fr(T_["bV"][:]),
                                         start=True, stop=True)
                                return e.matmul(ps[:, bw, 128:256], lhsT=fr(T_["Kt"][:]),
                                                rhs=fr(T_["DTm"][:]), start=True, stop=True)
                            sc.op("pe", f, reads=[t_T["DTm"], t_T["bV"], t_T["Kt"]],
                                  writes=[t_ps[bw]])
                            sc.op("act", lambda e, bw=bw: e.activation(
                                out=fr(T_["WK"][:]), in_=ps[:, bw, 0:256], func=AF.Copy),
                                reads=[t_ps[bw]], writes=[t_T["WK"]])
                            if dbg == 21:
                                return
                            bs = next_ps()
                            sc.op("pe", lambda e, bs=bs: e.matmul(
                                ps[:, bs, 0:128], lhsT=fr(T_["WK"][:, 128:256]), rhs=fr(Sst[:]), start=True,
                                stop=True), reads=[t_T["WK"], t_S], writes=[t_ps[bs]])
                            sc.op("dve", lambda e, bs=bs: e.tensor_tensor(
                                out=fr(T_["U"][:]), in0=T_["WK"][:, 0:128], in1=ps[:, bs, 0:128],
                                op=ALU.subtract), reads=[t_ps[bs], t_T["WK"]], writes=[t_T["U"]])
                            if dbg == 22:
                                return
                            bo = next_ps()

                            def f(e, bo=bo):
                                e.matmul(ps[:, bo, 0:128], lhsT=fr(T_["QDT"][:]), rhs=fr(Sst[:]), start=True,
                                         stop=False)
                                e.matmul(ps[:, bo, 0:128], lhsT=fr(T_["PT"][:]), rhs=fr(T_["U"][:]),
                                         start=False, stop=True)
                                return e.matmul(ps[:, bo, 128:256], lhsT=fr(T_["KD"][:]), rhs=fr(T_["U"][:]),
                                                start=True, stop=True)
                            sc.op("pe", f, reads=[t_T["QDT"], t_S, t_T["PT"], t_T["U"], t_T["KD"]],
                                  writes=[t_ps[bo]])
                            if dbg == 23:
                                return
                            sc.op("dve", lambda e, bo=bo, n=n: e.tensor_tensor(
                                out=Oacc[:, n, :], in0=ps[:, bo, 0:128], in1=Oacc[:, n, :],
                                op=ALU.add), reads=[t_ps[bo], t_O[n]], writes=[t_O[n]])
                            sc.op("act", lambda e, n=n, dh=dh: e.activation(
                                out=fr(Sst[:]), in_=Sst[:], func=AF.Copy, scale=egl[:, n, dh:dh + 1]),
                                reads=[t_S, t_egl], writes=[t_S])
                            sc.op("dve", lambda e, bo=bo: e.tensor_tensor(
                                out=fr(Sst[:]), in0=ps[:, bo, 128:256], in1=Sst[:], op=ALU.add),
                                reads=[t_ps[bo], t_S], writes=[t_S])
                    sc.op("dve", lambda e: e.memset(Oacc[:], 0.0), writes=t_O)
                    recs = [Rec() for _ in range(NW)]
                    NPD = NW // 2
                    for w_ in range(NW):
                        dr_, par_ = w_ % 2, w_ // 2
                        full = list(range(NT16)) if dr_ == 0 else list(range(NT16 - 1, -1, -1))
                        chain(dr_, TS[w_], t_TS[w_], ETs[w_], t_ETs[w_], Ss[dr_], t_Ss[dr_],
                              bank_pool(w_), recs[w_], full[par_::NPD], par_ == 0)
                    per_inst = len(recs[NW - 1].items) // len(range(NT16)[NPD - 1::NPD])
                    offs = [(w_ // 2) * (per_inst // NPD + 1) for w_ in range(NW)]
                    tot = max(len(r.items) + o_ for r, o_ in zip(recs, offs))
                    for i_ in range(tot):
                        for r, o_ in zip(recs, offs):
                            j_ = i_ - o_
                            if 0 <= j_ < len(r.items):
                                a_, k_ = r.items[j_]
                                sc.op(*a_, **k_)
                    for n in range(NT16):
                        sc.op("act", lambda e, n=n: e.activation(
                            out=T_["yy"][:], in_=Oacc[:, n, :], func=AF.Square,
                            accum_out=st16[:, n:n + 1]), reads=[t_O[n]],
                            writes=[t_T["yy"], t_st16])
                    sc.op("act", lambda e: e.activation(
                        out=st16[:, 16:32], in_=st16[:, 0:16], func=AF.Sqrt, scale=1.0 / 128,
                        bias=1e-6), reads=[t_st16], writes=[t_st16])
                    sc.op("dve", lambda e: e.reciprocal(out=st16[:, 32:48], in_=st16[:, 16:32]),
                          reads=[t_st16], writes=[t_st16])
                    for q in range(NTT):
                        oq = Oacc[:, q * 4:(q + 1) * 4, :]
                        tq = [t_O[q * 4 + i] for i in range(4)]
                        sc.op("dve", lambda e, q=q, oq=oq: e.tensor_tensor(
                            out=oq, in0=oq,
                            in1=st16[:, 32 + q * 4:36 + q * 4].unsqueeze(2).to_broadcast([128, 4, 128]),
                            op=ALU.mult), reads=tq + [t_st16], writes=tq)
                        sc.op("dve", lambda e, oq=oq: e.tensor_tensor(
                            out=oq, in0=oq,
                            in1=gsm[:, 32:160].unsqueeze(1).to_broadcast([128, 4, 128]),
                            op=ALU.mult), reads=tq + [t_gsm], writes=tq)
                        bz = next_ps()

                        def f(e, bz=bz, q=q, zwv=zwv):
                            for i4 in range(4):
                                n = q * 4 + i4
                                for kc in range(NCH):
                                    r = e.matmul(ps[:, bz, i4 * 128:(i4 + 1) * 128],
                                                 lhsT=hT[:, kc, n * 128:(n + 1) * 128],
                                                 rhs=zwv[:, kc, :], start=(kc == 0),
                                                 stop=(kc == NCH - 1))
                            return r
                        sc.op("pe", f, reads=[t_zwv] + [t_hT[c][q] for c in range(NCH)],
                              writes=[t_ps[bz]])
                        s_ = next_sm()
                        sc.op("act", lambda e, bz=bz, s_=s_: e.activation(
                            out=sm[s_][:], in_=ps[:, bz, :], func=AF.Silu),
                            reads=[t_ps[bz]], writes=[t_sm[s_]])
                        sc.op("dve", lambda e, oq=oq, s_=s_: e.tensor_tensor(
                            out=oq, in0=oq, in1=sm[s_][:].rearrange("p (a b) -> p a b", a=4),
                            op=ALU.mult), reads=tq + [t_sm[s_]], writes=tq)
                        bT = next_ps()

                        def f2(e, bT=bT, q=q):
                            for i4 in range(4):
                                r = e.transpose(out=ps[:, bT, i4 * 128:(i4 + 1) * 128],
                                                in_=Oacc[:, q * 4 + i4, :], identity=ident[:])
                            return r
                        sc.op("pe", f2, reads=tq + [t_ident], writes=[t_ps[bT]])
                        sc.op("act", lambda e, bT=bT, q=q: e.activation(
                            out=oT[:, q * TT:(q + 1) * TT], in_=ps[:, bT, :], func=AF.Copy),
                            reads=[t_ps[bT]], writes=[t_oT[q], t_pre])
                    if dbg == 17:
                        return
                    wo, t_wo = load_block(gout_d[h * 128:(h + 1) * 128, :]
                                          .rearrange("p (a n) -> p a n", a=1), None)
                    for t in range(NTT):
                        tsl = slice(t * TT, (t + 1) * TT)
                        for j in range(NCH):
                            b = next_ps()
                            sc.op("pe", lambda e, b=b, j=j, tsl=tsl, wo=wo: e.matmul(
                                ps[:, b, :], lhsT=wo[:, 0, j * 128:(j + 1) * 128], rhs=oT[:, tsl],
                                start=True, stop=True), reads=[t_wo, t_oT[t], t_pre], writes=[t_ps[b]])
                            sc.op("dve", lambda e, b=b, j=j, tsl=tsl: e.tensor_tensor(
                                out=xT[:, j, tsl], in0=ps[:, b, :], in1=xT[:, j, tsl], op=ALU.add),
                                reads=[t_ps[b]] + xtoks(j, t), writes=xtoks(j, t))
                    if dbg == 30 + h:
                        return


        def phase_moe():
            NT16 = S // 128
            with contextlib.ExitStack() as esp:
                set_pools(esp, 2, 6, WB)
                hT = sb("hT", [128, NCH, S], BF16, esp)
                t_hT = [[Tok("hT%d_%d" % (c, t)) for t in range(NTT)] for c in range(NCH)]
                sqtmp = sb("sqtmp", [128, NCH, TT], BF16, esp)
                t_sq = Tok("sqtmp")
                h1 = sb("h1", [128, 4, S], BF16, esp)
                t_h1 = [[Tok("h1_%d_%d" % (f_, t)) for t in range(NTT)] for f_ in range(4)]
                sg = [sb("sg%d" % i, [128, TT], F32, esp) for i in range(2)]
                t_sg = [Tok("sg0"), Tok("sg1")]
                G = [sb("G%d" % i, [128, S], F32, esp) for i in range(2)]
                t_G = [Tok("G0"), Tok("G1")]
                wr = sb("wr", [128, NCH, 8], F32, esp)
                t_wr = Tok("wr")
                sq32 = [sb("sq32_%d" % i, [128, NCH, 128], F32, esp) for i in range(2)]
                t_sq32 = [Tok("sq32_0"), Tok("sq32_1")]
                rst = sb("rst", [128, NT16, 9], F32, esp)
                t_rst = Tok("rst")
                L = sb("L", [128, NT16, 8], F32, esp)
                v8 = sb("v8", [128, NT16, 8], F32, esp)
                gate = sb("gate", [128, NT16, 8], F32, esp)
                tmpr = sb("tmpr", [128, NT16, 8], F32, esp)
                rs16 = sb("rs16", [128, NT16, 4], F32, esp)
                dg = [sb("dg%d" % i, [128, 128], F32, esp) for i in range(2)]
                t_dg = [Tok("dg0"), Tok("dg1")]
                t_L, t_v8, t_gate, t_tmpr, t_rs16 = (Tok("L"), Tok("v8"), Tok("gate"),
                                                     Tok("tmpr"), Tok("rs16"))
                ch_wr = sc.new_chan()
                rmsnorm_fm(hT, t_hT, VR["ffn1"], sqtmp, t_sq)
                sc.op("sp", lambda e: e.dma_start(
                    out=wr[:], in_=rt_d.rearrange("(c p) e -> p c e", p=128)),
                    writes=[t_wr], chan=ch_wr, after=sc.last_ops())
                for c in range(NCH):
                    sc.op("dve", lambda e, c=c: e.tensor_scalar(
                        out=wr[:, c, :], in0=wr[:, c, :], scalar1=vcol(c, VR["ffn1"]),
                        scalar2=None, op0=ALU.mult), reads=[t_wr, t_vecs], writes=[t_wr])
                for tt in range(NT16):
                    s_ = tt % 2
                    tsl = slice(tt * 128, (tt + 1) * 128)
                    sc.op("act", lambda e, s_=s_, tsl=tsl: e.activation(
                        out=sq32[s_][:], in_=xT[:, :, tsl], func=AF.Square),
                        reads=[t_xT[c][tt] for c in range(NCH)], writes=[t_sq32[s_]])
                    b = next_ps()

                    def f(e, b=b, tsl=tsl, s_=s_):
                        for c in range(NCH):
                            e.matmul(ps[:, b, 0:8], lhsT=xT[:, c, tsl], rhs=wr[:, c, :],
                                     start=(c == 0), stop=(c == NCH - 1))
                        for c in range(NCH):
                            r = e.matmul(ps[:, b, 8:9], lhsT=sq32[s_][:, c, :],
                                         rhs=ones_f[:, 0:1], start=(c == 0), stop=(c == NCH - 1))
                        return r
                    sc.op("pe", f, reads=[t_wr, t_sq32[s_], t_ones] +
                          [t_xT[c][tt] for c in range(NCH)], writes=[t_ps[b]])
                    sc.op("dve", lambda e, b=b, tt=tt: e.tensor_copy(
                        out=rst[:, tt, :], in_=ps[:, b, 0:9]), reads=[t_ps[b]], writes=[t_rst])
                sc.op("act", lambda e: e.activation(
                    out=rs16[:, :, 0:1], in_=rst[:, :, 8:9], func=AF.Sqrt, scale=1.0 / D,
                    bias=1e-6), reads=[t_rst], writes=[t_rs16])
                sc.op("dve", lambda e: e.reciprocal(out=rs16[:, :, 1:2], in_=rs16[:, :, 0:1]),
                      reads=[t_rs16], writes=[t_rs16])
                sc.op("dve", lambda e: e.tensor_tensor(
                    out=L[:], in0=rst[:, :, 0:8],
                    in1=rs16[:, :, 1:2].to_broadcast([128, NT16, 8]), op=ALU.mult),
                    reads=[t_rst, t_rs16], writes=[t_L])
                for tt in range(NT16):
                    sc.op("dve", lambda e, tt=tt: e.max(out=v8[:, tt, :], in_=L[:, tt, :]),
                          reads=[t_L], writes=[t_v8])
                sc.op("dve", lambda e: e.tensor_tensor(
                    out=gate[:], in0=L[:], in1=v8[:, :, 1:2].to_broadcast([128, NT16, 8]),
                    op=ALU.is_ge), reads=[t_L, t_v8], writes=[t_gate])
                sc.op("dve", lambda e: e.tensor_tensor(
                    out=tmpr[:], in0=L[:], in1=v8[:, :, 0:1].to_broadcast([128, NT16, 8]),
                    op=ALU.subtract), reads=[t_L, t_v8], writes=[t_tmpr])
                sc.op("act", lambda e: e.activation(out=tmpr[:], in_=tmpr[:], func=AF.Exp),
                      reads=[t_tmpr], writes=[t_tmpr])
                sc.op("dve", lambda e: e.tensor_tensor(
                    out=rs16[:, :, 2:3], in0=v8[:, :, 1:2], in1=v8[:, :, 0:1], op=ALU.subtract),
                    reads=[t_v8], writes=[t_rs16])
                sc.op("act", lambda e: e.activation(out=rs16[:, :, 2:3], in_=rs16[:, :, 2:3],
                                                    func=AF.Exp),
                      reads=[t_rs16], writes=[t_rs16])
                sc.op("dve", lambda e: e.tensor_scalar(
                    out=rs16[:, :, 2:3], in0=rs16[:, :, 2:3], scalar1=1.0, scalar2=None,
                    op0=ALU.add), reads=[t_rs16], writes=[t_rs16])
                sc.op("dve", lambda e: e.reciprocal(out=rs16[:, :, 3:4], in_=rs16[:, :, 2:3]),
                      reads=[t_rs16], writes=[t_rs16])
                sc.op("dve", lambda e: e.tensor_tensor(
                    out=gate[:], in0=gate[:], in1=tmpr[:], op=ALU.mult),
                    reads=[t_gate, t_tmpr], writes=[t_gate])
                sc.op("dve", lambda e: e.tensor_tensor(
                    out=gate[:], in0=gate[:], in1=rs16[:, :, 3:4].to_broadcast([128, NT16, 8]),
                    op=ALU.mult), reads=[t_gate, t_rs16], writes=[t_gate])
                dgn = 0
                for ex in range(8):
                    gi = ex % 2
                    for q in range(NTT):
                        b = next_ps()
                        for i4 in range(4):
                            tt = q * 4 + i4
                            di = dgn % 2
                            dgn += 1
                            sc.op("dve", lambda e, di=di, tt=tt, ex=ex: e.tensor_tensor(
                                out=dg[di][:], in0=ident[:],
                                in1=gate[:, tt, ex:ex + 1].to_broadcast([128, 128]), op=ALU.mult),
                                reads=[t_ident, t_gate], writes=[t_dg[di]])
                            sc.op("pe", lambda e, b=b, i4=i4, di=di: e.matmul(
                                ps[:, b, i4 * 128:(i4 + 1) * 128], lhsT=ones_f[:], rhs=dg[di][:],
                                start=True, stop=True),
                                reads=[t_dg[di], t_ones], writes=[t_ps[b]])
                        sc.op("act", lambda e, b=b, gi=gi, q=q: e.activation(
                            out=G[gi][:, q * TT:(q + 1) * TT], in_=ps[:, b, :], func=AF.Copy),
                            reads=[t_ps[b]], writes=[t_G[gi]])
                    swiglu_stream(hT, t_hT, h1, t_h1, sg, t_sg, mg_d[ex], mu_d[ex], md_d[ex], DFFE,
                                  gate_bc=G[gi], t_gate=t_G[gi])

        if "c0" in phases:
            phase_conformer()
            sc.barrier()
        if "f0" in phases:
            phase_ffn()
            sc.barrier()
        if "g1" in phases:
            phase_gdn()
            sc.barrier()
        if "m1" in phases:
            phase_moe()
            sc.barrier()

        do_norm = "final" in phases
        with contextlib.ExitStack() as es2:
            xo = [sb("xo%d" % i, [128, D], F32, es2) for i in range(2)]
            fng = sb("fng_sb", [128, D], F32, es2)
            sc.op("sp", lambda e: e.dma_start(out=fng[:],
                                              in_=fng_d[0:1, :].partition_broadcast(128)),
                  writes=[t_fng], chan=ch_misc, after=sc.last_ops())
            yo = [sb("yo%d" % i, [128, D], F32, es2) for i in range(2)]
            sq = sb("sqj", [128, D], F32, es2)
            st = [sb("st%d" % i, [128, 4], F32, es2) for i in range(2)]
            t_xo = [Tok("xo0"), Tok("xo1")]
            t_yo = [Tok("yo0"), Tok("yo1")]
            t_sq2 = Tok("sq")
            t_st = [Tok("st0"), Tok("st1")]
            out_ops = []
            for tt in range(S // 128):
                sl = tt % 2
                for half in range(2):
                    b = next_ps()

                    def f(e, half=half, b=b, tt=tt):
                        for j in range(4):
                            c = half * 4 + j
                            r = e.transpose(out=ps[:, b, j * 128:(j + 1) * 128],
                                            in_=xT[:, c, tt * 128:(tt + 1) * 128],
                                            identity=ident[:])
                        return r
                    sc.op("pe", f, reads=[t_ident] + [t_xT[half * 4 + j][tt] for j in range(4)],
                          writes=[t_ps[b]])
                    dst = xo if do_norm else yo
                    t_dst = t_xo if do_norm else t_yo
                    sc.op("act", lambda e, half=half, b=b, sl=sl, dst=dst: e.activation(
                        out=dst[sl][:, half * 512:(half + 1) * 512], in_=ps[:, b, :], func=AF.Copy),
                        reads=[t_ps[b]], writes=[t_dst[sl]])
                if do_norm:
                    sc.op("act", lambda e, sl=sl: e.activation(
                        out=sq[:], in_=xo[sl][:], func=AF.Square, accum_out=st[sl][:, 0:1]),
                        reads=[t_xo[sl]], writes=[t_sq2, t_st[sl]])
                    sc.op("act", lambda e, sl=sl: e.activation(
                        out=st[sl][:, 1:2], in_=st[sl][:, 0:1], func=AF.Sqrt, scale=1.0 / D,
                        bias=1e-6), reads=[t_st[sl]], writes=[t_st[sl]])
                    sc.op("dve", lambda e, sl=sl: e.reciprocal(out=st[sl][:, 2:3],
                                                               in_=st[sl][:, 1:2]),
                          reads=[t_st[sl]], writes=[t_st[sl]])
                    sc.op("dve", lambda e, sl=sl: e.scalar_tensor_tensor(
                        out=yo[sl][:], in0=xo[sl][:], scalar=st[sl][:, 2:3], in1=fng[:],
                        op0=ALU.mult, op1=ALU.mult),
                        reads=[t_xo[sl], t_st[sl], t_fng], writes=[t_yo[sl]])
                o = sc.op("sp", lambda e, sl=sl, tt=tt: e.dma_start(
                    out=out_d[tt * 128:(tt + 1) * 128, :], in_=yo[sl][:]),
                    reads=[t_yo[sl]], chan=ch_out[sl])
                out_ops.append(o)
            fin = sc.op("sp", lambda e: e.nop())
            fin.is_nop = True
            fin.deps.extend(out_ops[-2:])
            sc.emit(nc)
    return nc


def pack_vecs(inp):
    rows = np.zeros((128, D), np.float32)
    rows[VR["mix0"]] = inp["mix_norm"][0]
    rows[VR["mix1"]] = inp["mix_norm"][1]
    rows[VR["ffn0"]] = inp["ffn_norm"][0]
    rows[VR["ffn1"]] = inp["ffn_norm"][1]
    rows[VR["pw1_ba"]] = inp["cf_pw1_b"][0, :D]
    rows[VR["pw1_bb"]] = inp["cf_pw1_b"][0, D:]
    rows[VR["dw_b"]] = inp["cf_dw_b"][0]
    rows[VR["ln_g"]] = inp["cf_ln_g"][0]
    rows[VR["ln_b"]] = inp["cf_ln_b"][0]
    rows[VR["pw2_b"]] = inp["cf_pw2_b"][0]
    rows[VR["dw_w"]:VR["dw_w"] + 31] = inp["cf_dw_w"][0]
    gc = inp["gdn_conv_w"][0]
    for k in range(5):
        for part in range(3):
            rows[VR["gconv"] + k * 3 + part] = gc[k, part * D:(part + 1) * D]
    return rows


def gdn_consts():
    i = np.arange(128)
    c = np.zeros((128, 2048), np.float32)
    c[:, 0:128] = (i[:, None] <= i[None, :])
    c[:, 128:256] = (i[:, None] >= i[None, :])
    for k in range(7):
        bsz = 1 << k
        I, J = i[:, None], i[None, :]
        m = ((I // (2 * bsz)) == (J // (2 * bsz))) & (((I // bsz) % 2) == 1) & (((J // bsz) % 2) == 0)
        c[:, 256 + k * 128:256 + (k + 1) * 128] = m.T
        c[:, 1152 + k * 128:1152 + (k + 1) * 128] = m
    return c


def make_in_maps(inp, phases, nb):
    x = np.ascontiguousarray(inp["x"], dtype=np.float32)
    vecs = pack_vecs(inp)
    fng = np.ascontiguousarray(inp["final_norm"], dtype=np.float32).reshape(1, D)
    base = {"vecs": vecs, "fng": fng}
    if "c0" in phases:
        base["cf_pw1_w"] = np.ascontiguousarray(inp["cf_pw1_w"][0])
        base["cf_pw2_w"] = np.ascontiguousarray(inp["cf_pw2_w"][0])
    if "f0" in phases:
        base["ffn_w_gate"] = np.ascontiguousarray(inp["ffn_w_gate"][0])
        base["ffn_w_up"] = np.ascontiguousarray(inp["ffn_w_up"][0])
        base["ffn_w_down"] = np.ascontiguousarray(inp["ffn_w_down"][0])
    if "g1" in phases:
        base["gdn_w_in"] = np.ascontiguousarray(inp["gdn_w_in"][0])
        base["gdn_w_out"] = np.ascontiguousarray(inp["gdn_w_out"][0])
        base["gsmall"] = np.concatenate([inp["gdn_a_log"][0].reshape(-1),
                                         inp["gdn_dt_bias"][0].reshape(-1),
                                         inp["gdn_o_norm"][0].reshape(-1)]).astype(np.float32)[None]
        base["gconst"] = gdn_consts()
    if "m1" in phases:
        base["moe_router"] = np.ascontiguousarray(inp["moe_router"][0])
        base["moe_w_gate"] = np.ascontiguousarray(inp["moe_w_gate"][0])
        base["moe_w_up"] = np.ascontiguousarray(inp["moe_w_up"][0])
        base["moe_w_down"] = np.ascontiguousarray(inp["moe_w_down"][0])
    return [dict(base, x=x[b]) for b in range(nb)]


def kernel(**inp):
    phases = ("c0", "f0", "g1", "m1", "final")
    nb = inp["x"].shape[0]
    nc = build_program(phases)
    in_maps = make_in_maps(inp, phases, nb)
    res = run_bass_kernel_spmd(nc, in_maps, core_ids=list(range(nb)))
    return np.stack([r["out"] for r in res.results], axis=0)
```
